# Optimizing a Trainium2 kernel written in Bass

```python
import math
import jax, jax.numpy as jnp
from jax import lax
import numpy as np

D_MODEL = 1024
BATCH = 16
SEQ = 2048
DEPTH = 2

GRID_W = 64
CTX_LEN = 256

FOURIER_WIDTH = 256
FOURIER_GROUPS = 4
FOURIER_GROUP_DIM = FOURIER_WIDTH // FOURIER_GROUPS
ATTN_WIDTH = D_MODEL - FOURIER_WIDTH
DIFF_HEAD_DIM = 64
DIFF_HEADS = ATTN_WIDTH // (2 * DIFF_HEAD_DIM)
DIFF_SUBHEADS = 2 * DIFF_HEADS
DIFF_V_DIM = 2 * DIFF_HEAD_DIM
MIX_WIDTH = FOURIER_WIDTH + ATTN_WIDTH
IN_WIDTH = FOURIER_WIDTH + 3 * ATTN_WIDTH
Q_OFF = FOURIER_WIDTH
K_OFF = FOURIER_WIDTH + ATTN_WIDTH
V_OFF = FOURIER_WIDTH + 2 * ATTN_WIDTH
Q_BLOCK = 128
ROPE_BASE = 10000.0
ROPE_AXIS_DIM = DIFF_HEAD_DIM // 2

N_EXPERTS = 16
N_GROUPS = 4
EXPERTS_PER_GROUP = N_EXPERTS // N_GROUPS
TOP_K = 2
D_EXPERT = D_MODEL // 2

ALPHA = (2 * DEPTH) ** 0.25
BETA = (8 * DEPTH) ** -0.25
LN_EPS = 1e-5

kernel_name = "hybrid_fourier_diffattn_grouped_moe_dit"


def layer_norm(x, g=None, b=None):
    xf = x.astype(jnp.float32)
    mu = jnp.mean(xf, axis=-1, keepdims=True)
    var = jnp.mean(jnp.square(xf - mu), axis=-1, keepdims=True)
    y = (xf - mu) * lax.rsqrt(var + LN_EPS)
    if g is not None:
        y = y * g.astype(jnp.float32) + b.astype(jnp.float32)
    return y.astype(x.dtype)


def modulate(x, shift, scale):
    return x * (1 + scale) + shift


def fourier_mix(u, w_f):
    B, T, _ = u.shape
    ug = u.reshape(B, T, FOURIER_GROUPS, FOURIER_GROUP_DIM).astype(jnp.float32)
    f = jnp.fft.fft2(ug, axes=(1, 3)).real * (1.0 / math.sqrt(T * FOURIER_GROUP_DIM))
    f = f.astype(u.dtype)
    return jnp.einsum('btgc,gcd->btgd', f, w_f).reshape(B, T, FOURIER_WIDTH)


def rope1d(x, ang):
    half = x.shape[-1] // 2
    cos = jnp.cos(ang)[:, None, :].astype(x.dtype)
    sin = jnp.sin(ang)[:, None, :].astype(x.dtype)
    x1, x2 = x[..., :half], x[..., half:]
    return jnp.concatenate([x1 * cos - x2 * sin, x2 * cos + x1 * sin], axis=-1)


def rope2d(x, ang_row, ang_col):
    return jnp.concatenate([rope1d(x[..., :ROPE_AXIS_DIM], ang_row),
                            rope1d(x[..., ROPE_AXIS_DIM:], ang_col)], axis=-1)


def diff_weights(s, lam):
    p = jax.nn.softmax(s, axis=-1)
    p = p.reshape(p.shape[0], DIFF_HEADS, 2, p.shape[2], p.shape[3])
    return p[:, :, 0] - lam * p[:, :, 1]


def diff_attn_latent(q, k, v, kc, vc, lam):
    B, S = q.shape[0], q.shape[1]
    keys = jnp.concatenate([kc, k], axis=1).transpose(0, 2, 1, 3)
    vals = jnp.concatenate([vc, v], axis=1).transpose(0, 2, 1, 3)
    n_blk = S // Q_BLOCK
    qb = q.reshape(B, n_blk, Q_BLOCK, DIFF_SUBHEADS, DIFF_HEAD_DIM).transpose(1, 0, 3, 2, 4)
    scale = DIFF_HEAD_DIM ** -0.5

    def attend(qblk):
        s = jnp.einsum('bhqd,bhkd->bhqk', qblk, keys).astype(jnp.float32) * scale
        a = diff_weights(s, lam)
        return jnp.einsum('bhqk,bhkv->bhqv', a.astype(vals.dtype), vals)

    o = lax.map(attend, qb)
    return o.transpose(1, 0, 3, 2, 4).reshape(B, S, DIFF_HEADS, DIFF_V_DIM)


def diff_attn_context(qc, kc, vc, lam):
    s = jnp.einsum('bqhd,bkhd->bhqk', qc, kc).astype(jnp.float32) * (DIFF_HEAD_DIM ** -0.5)
    a = diff_weights(s, lam)
    return jnp.einsum('bhqk,bkhv->bqhv', a.astype(vc.dtype), vc)


def head_norm(o, g, lam_init):
    of = o.astype(jnp.float32)
    of = of * lax.rsqrt(jnp.mean(jnp.square(of), axis=-1, keepdims=True) + LN_EPS)
    of = of * g.astype(jnp.float32) * (1.0 - lam_init)
    return of.astype(o.dtype).reshape(o.shape[0], o.shape[1], ATTN_WIDTH)


def moe_ffn(h, w_router, router_bias, w_gate, w_up, w_down):
    scores = jax.nn.sigmoid((h @ w_router).astype(jnp.float32))
    sel = scores + router_bias.astype(jnp.float32)
    grp_sel = sel.reshape(sel.shape[:-1] + (N_GROUPS, EXPERTS_PER_GROUP))
    grp_score = lax.top_k(grp_sel, TOP_K)[0].sum(-1)
    best_grp = jnp.argmax(grp_score, axis=-1)
    in_grp = (jnp.arange(N_EXPERTS) // EXPERTS_PER_GROUP) == best_grp[..., None]
    masked = jnp.where(in_grp, sel, -jnp.inf)
    _, idx = lax.top_k(masked, TOP_K)
    w = jnp.take_along_axis(scores, idx, axis=-1)
    w = w / jnp.sum(w, axis=-1, keepdims=True)
    combine = jnp.sum(jax.nn.one_hot(idx, N_EXPERTS, dtype=jnp.float32) * w[..., None], axis=-2)
    combine = combine.astype(h.dtype)
    y = jnp.zeros_like(h)
    for e in range(N_EXPERTS):
        a = jax.nn.silu(h @ w_gate[e]) * (h @ w_up[e])
        y = y + combine[..., e:e + 1] * (a @ w_down[e])
    return y


def setup_inputs(seed: int = 0) -> dict:
    key = jax.random.key(seed)
    ks = jax.random.split(key, 20)

    def n(k, shape, s):
        return jax.random.normal(k, shape, jnp.float32) * s

    C = FOURIER_GROUP_DIM
    return {
        "x": n(ks[0], (BATCH, SEQ, D_MODEL), 1.0),
        "c": n(ks[1], (BATCH, D_MODEL), 1.0),
        "ctx": n(ks[2], (BATCH, CTX_LEN, D_MODEL), 1.0),
        "c_ctx": n(ks[3], (D_MODEL,), 1.0),
        "w_mod": n(ks[4], (DEPTH, D_MODEL, 6 * D_MODEL), D_MODEL ** -0.5),
        "b_mod": n(ks[5], (DEPTH, 6 * D_MODEL), 0.02),
        "w_in": n(ks[6], (DEPTH, D_MODEL, IN_WIDTH), D_MODEL ** -0.5),
        "w_fourier": n(ks[7], (DEPTH, FOURIER_GROUPS, C, C), C ** -0.5),
        "lam_qk": n(ks[8], (DEPTH, 4, DIFF_HEAD_DIM), 0.1),
        "subln_g": 1.0 + n(ks[9], (DEPTH, DIFF_V_DIM), 0.02),
        "w_out": n(ks[10], (DEPTH, MIX_WIDTH, D_MODEL), MIX_WIDTH ** -0.5 * BETA),
        "ln_attn_g": 1.0 + n(ks[11], (DEPTH, D_MODEL), 0.02),
        "ln_attn_b": n(ks[12], (DEPTH, D_MODEL), 0.02),
        "ln_ffn_g": 1.0 + n(ks[13], (DEPTH, D_MODEL), 0.02),
        "ln_ffn_b": n(ks[14], (DEPTH, D_MODEL), 0.02),
        "w_router": n(ks[15], (D_MODEL, N_EXPERTS), D_MODEL ** -0.5),
        "router_bias": n(ks[16], (N_EXPERTS,), 0.01),
        "w_gate": n(ks[17], (DEPTH, N_EXPERTS, D_MODEL, D_EXPERT), D_MODEL ** -0.5),
        "w_up": n(ks[18], (DEPTH, N_EXPERTS, D_MODEL, D_EXPERT), D_MODEL ** -0.5),
        "w_down": n(ks[19], (DEPTH, N_EXPERTS, D_EXPERT, D_MODEL), D_EXPERT ** -0.5 * BETA),
    }


def reference(x, c, ctx, c_ctx, w_mod, b_mod, w_in, w_fourier, lam_qk, subln_g, w_out,
              ln_attn_g, ln_attn_b, ln_ffn_g, ln_ffn_b, w_router, router_bias,
              w_gate, w_up, w_down):
    B, S, _ = x.shape
    L = ctx.shape[1]
    ROWS = S // GRID_W
    row = jnp.repeat(jnp.arange(ROWS), GRID_W).astype(jnp.float32)
    col = jnp.tile(jnp.arange(GRID_W), ROWS).astype(jnp.float32)
    freqs = ROPE_BASE ** (-jnp.arange(0, ROPE_AXIS_DIM, 2, dtype=jnp.float32) / ROPE_AXIS_DIM)
    ang_row = row[:, None] * freqs
    ang_col = col[:, None] * freqs

    silu_c = jax.nn.silu(c)
    silu_cc = jax.nn.silu(c_ctx)
    xc = ctx

    for l in range(DEPTH):
        last = l == DEPTH - 1
        lam_init = 0.8 - 0.6 * math.exp(-0.3 * l)
        lq = lam_qk[l].astype(jnp.float32)
        lam = jnp.exp(jnp.sum(lq[0] * lq[1])) - jnp.exp(jnp.sum(lq[2] * lq[3])) + lam_init

        mod = silu_c @ w_mod[l] + b_mod[l]
        sh_a, sc_a, g_a, sh_f, sc_f, g_f = [m[:, None, :] for m in jnp.split(mod, 6, axis=-1)]
        mod_c = silu_cc @ w_mod[l] + b_mod[l]
        csh_a, csc_a, cg_a, csh_f, csc_f, cg_f = jnp.split(mod_c, 6)

        h = modulate(layer_norm(x), sh_a, sc_a)
        hc = modulate(layer_norm(xc), csh_a, csc_a)
        p = h @ w_in[l]
        u = p[..., :Q_OFF]
        q = rope2d(p[..., Q_OFF:K_OFF].reshape(B, S, DIFF_SUBHEADS, DIFF_HEAD_DIM), ang_row, ang_col)
        k = rope2d(p[..., K_OFF:V_OFF].reshape(B, S, DIFF_SUBHEADS, DIFF_HEAD_DIM), ang_row, ang_col)
        v = p[..., V_OFF:].reshape(B, S, DIFF_HEADS, DIFF_V_DIM)
        if last:
            pkv = hc @ w_in[l][:, K_OFF:]
            kc = pkv[..., :ATTN_WIDTH].reshape(B, L, DIFF_SUBHEADS, DIFF_HEAD_DIM)
            vc = pkv[..., ATTN_WIDTH:].reshape(B, L, DIFF_HEADS, DIFF_V_DIM)
        else:
            pc = hc @ w_in[l]
            uc = pc[..., :Q_OFF]
            qc = pc[..., Q_OFF:K_OFF].reshape(B, L, DIFF_SUBHEADS, DIFF_HEAD_DIM)
            kc = pc[..., K_OFF:V_OFF].reshape(B, L, DIFF_SUBHEADS, DIFF_HEAD_DIM)
            vc = pc[..., V_OFF:].reshape(B, L, DIFF_HEADS, DIFF_V_DIM)

        o_attn = head_norm(diff_attn_latent(q, k, v, kc, vc, lam), subln_g[l], lam_init)
        o = jnp.concatenate([fourier_mix(u, w_fourier[l]), o_attn], axis=-1) @ w_out[l]
        x_new = layer_norm(ALPHA * x + g_a * o, ln_attn_g[l], ln_attn_b[l])

        if not last:
            oc_attn = head_norm(diff_attn_context(qc, kc, vc, lam), subln_g[l], lam_init)
            oc = jnp.concatenate([fourier_mix(uc, w_fourier[l]), oc_attn], axis=-1) @ w_out[l]
            xc = layer_norm(ALPHA * xc + cg_a * oc, ln_attn_g[l], ln_attn_b[l])
        x = x_new

        h = modulate(layer_norm(x), sh_f, sc_f)
        if last:
            y = moe_ffn(h, w_router, router_bias, w_gate[l], w_up[l], w_down[l])
        else:
            hc = modulate(layer_norm(xc), csh_f, csc_f)
            y_all = moe_ffn(jnp.concatenate([hc, h], axis=1), w_router, router_bias,
                            w_gate[l], w_up[l], w_down[l])
            yc, y = y_all[:, :L], y_all[:, L:]
            xc = layer_norm(ALPHA * xc + cg_f * yc, ln_ffn_g[l], ln_ffn_b[l])
        x = layer_norm(ALPHA * x + g_f * y, ln_ffn_g[l], ln_ffn_b[l])

    return x
```

```python
import math
from contextlib import ExitStack

import numpy as np
import ml_dtypes
import concourse.bass as bass
import concourse.mybir as mybir
from concourse.bass_utils import run_bass_kernel_spmd

F32 = mybir.dt.float32
BF16 = mybir.dt.bfloat16
ALU = mybir.AluOpType
AF = mybir.ActivationFunctionType
AX = mybir.AxisListType

N_CORES = 8
NB = 2
S = 2048
LC = 256
TT = S + LC
NT = TT // 128
D = 1024
KC = 8
DEPTH = 2
WCOLS = 2816
ALPHA = (2 * DEPTH) ** 0.25
LN_EPS = 1e-5
NE = 16
VST = 132

ENGS = ("pe", "act", "dve", "pool", "sp")
DMAQ = ("sp", "pool", "act")


class Res:
    __slots__ = ("name", "w", "r")

    def __init__(self, name=""):
        self.name = name
        self.w = None
        self.r = []


class Prog:
    def __init__(self, nc, nq=16):
        self.nc = nc
        self.NQ = nq
        self.ops = {e: [] for e in ENGS}
        self.waited = {e: {} for e in ENGS}
        self.dma_n = {q: 0 for q in DMAQ}
        self.res = []

    def R(self, name=""):
        r = Res(name)
        self.res.append(r)
        return r

    def _add_wait(self, eng, o, d, raw):
        if d[0] == "c":
            _, e2, idx = d
            if e2 == eng and (eng == "pe" or not raw):
                return
            key = ("c", e2)
            if self.waited[eng].get(key, -1) >= idx:
                return
            self.waited[eng][key] = idx
            self.ops[e2][idx]["signal"] = True
            o["waits"].append(d)
        else:
            _, q, slot, cnt = d
            key = ("d", q, slot)
            if self.waited[eng].get(key, 0) >= cnt:
                return
            self.waited[eng][key] = cnt
            o["waits"].append(d)

    def op(self, eng, fn, reads=(), writes=(), dma=False):
        ops = self.ops[eng]
        idx = len(ops)
        o = dict(fn=fn, waits=[], signal=False, dma=None)
        raw_deps = []
        oth_deps = []
        for r in reads:
            if r.w is not None:
                raw_deps.append(r.w)
        for r in writes:
            if r.w is not None:
                oth_deps.append(r.w)
            oth_deps.extend(r.r)
        if dma:
            n = self.dma_n[eng]
            slot = n % self.NQ
            cnt = 16 * (n // self.NQ + 1)
            self.dma_n[eng] += 1
            if n >= self.NQ:
                oth_deps.append(("d", eng, slot, cnt - 16))
            ev = ("d", eng, slot, cnt)
            o["dma"] = (slot, cnt)
        else:
            ev = ("c", eng, idx)
        for d in raw_deps:
            self._add_wait(eng, o, d, True)
        for d in oth_deps:
            self._add_wait(eng, o, d, d[0] == "d")
        ops.append(o)
        for r in reads:
            r.r.append(ev)
        for r in writes:
            r.w = ev
            r.r = []
        return ev

    def barrier(self):
        evs = []
        for e in ENGS:
            for idx in range(len(self.ops[e]) - 1, -1, -1):
                o = self.ops[e][idx]
                if o["fn"] is not None and o["dma"] is None:
                    evs.append(("c", e, idx))
                    break
        for q in DMAQ:
            n = self.dma_n[q]
            for slot in range(min(n, self.NQ)):
                last_n = ((n - 1 - slot) // self.NQ) * self.NQ + slot
                evs.append(("d", q, slot, 16 * (last_n // self.NQ + 1)))
        for e in ENGS:
            o = dict(fn=None, waits=[], signal=False, dma=None)
            for d in evs:
                if d[0] == "c" and d[1] == e:
                    continue
                self._add_wait(e, o, d, True)
            self.ops[e].append(o)
        for r in self.res:
            r.w = None
            r.r = []

    def emit(self):
        nc = self.nc
        ranks = {}
        for e in ENGS:
            c = 0
            rk = []
            for o in self.ops[e]:
                if o["signal"]:
                    c += 1
                rk.append(c)
            ranks[e] = rk
        self.n_signal = {e: (ranks[e][-1] if ranks[e] else 0) for e in ENGS}
        self.n_ops = {e: len(self.ops[e]) for e in ENGS}
        with ExitStack() as st:
            csem = {e: st.enter_context(nc.semaphore("c_" + e)) for e in ENGS}
            dsem = {q: [st.enter_context(nc.semaphore("d_%s_%d" % (q, i))) for i in range(self.NQ)]
                    for q in DMAQ}
            block = st.enter_context(nc.Block())

            def run(e):
                def body(eng):
                    for o in self.ops[e]:
                        for d in o["waits"]:
                            if d[0] == "c":
                                eng.wait_ge(csem[d[1]], ranks[d[1]][d[2]])
                            else:
                                eng.wait_ge(dsem[d[1]][d[2]], d[3])
                        if o["fn"] is None:
                            continue
                        ins = o["fn"](eng)
                        if o["dma"] is not None:
                            ins.then_inc(dsem[e][o["dma"][0]], 16)
                        elif o["signal"]:
                            ins.then_inc(csem[e], 1)
                return body

            block.tensor(run("pe"))
            block.scalar(run("act"))
            block.vector(run("dve"))
            block.gpsimd(run("pool"))
            block.sync(run("sp"))


class Buf:
    __slots__ = ("ap", "res")

    def __init__(self, ap, res):
        self.ap = ap
        self.res = res


class Ring:
    def __init__(self, bufs):
        self.bufs = bufs
        self.i = 0

    def next(self):
        b = self.bufs[self.i % len(self.bufs)]
        self.i += 1
        return b


def mm(out, lhsT, rhs, start, stop):
    return lambda e: e.matmul(out, lhsT=lhsT, rhs=rhs, start=start, stop=stop)


def tr(out, in_, ident):
    return lambda e: e.transpose(out=out, in_=in_, identity=ident)


def dma(out, in_):
    return lambda e: e.dma_start(out=out, in_=in_)


def act(out, in_, func, bias=0.0, scale=1.0):
    return lambda e: e.activation(out=out, in_=in_, func=func, bias=bias, scale=scale)


def tcopy(out, in_):
    return lambda e: e.tensor_copy(out=out, in_=in_)


def tt(out, in0, in1, op):
    return lambda e: e.tensor_tensor(out=out, in0=in0, in1=in1, op=op)


def ts(out, in0, s1, op0, s2=None, op1=None):
    if op1 is None:
        return lambda e: e.tensor_scalar(out=out, in0=in0, scalar1=s1, scalar2=None, op0=op0)
    return lambda e: e.tensor_scalar(out=out, in0=in0, scalar1=s1, scalar2=s2, op0=op0, op1=op1)


def stt(out, in0, scalar, in1, op0, op1, accum_out=None):
    if accum_out is None:
        return lambda e: e.scalar_tensor_tensor(out=out, in0=in0, scalar=scalar, in1=in1, op0=op0, op1=op1)
    return lambda e: e.scalar_tensor_tensor(out=out, in0=in0, scalar=scalar, in1=in1, op0=op0, op1=op1,
                                            accum_out=accum_out)


def memset(ap, v):
    return lambda e: e.memset(ap, v)


ARENA_BYTES = 206 * 1024


class Arena:
    def __init__(self, ap_bf16):
        self.base = ap_bf16
        self.off = 0
        self.floor = 0

    def alloc(self, shape, dtype):
        n = 1
        for s in shape:
            n *= s
        nbytes = n * (4 if dtype == F32 else 2)
        nbytes_al = (nbytes + 63) // 64 * 64
        assert self.off + nbytes_al <= ARENA_BYTES, ("SBUF arena overflow", self.off, nbytes_al)
        v = self.base[:, self.off // 2:(self.off + nbytes) // 2]
        self.off += nbytes_al
        if dtype == F32:
            v = v.bitcast(F32)
        if len(shape) == 2:
            v = v.rearrange("p (a b) -> p a b", b=shape[1])
        elif len(shape) == 3:
            v = v.rearrange("p (a b c) -> p a b c", b=shape[1], c=shape[2])
        return v

    def mark(self):
        return self.off

    def reset(self, to):
        self.off = to


def build_program(debug=False, stop_after=None):
    nc = bass.Bass("TRN2", target_bir_lowering=False)
    kin = "ExternalInput"
    dt_ = lambda name, shape, dtype, kind: nc.dram_tensor(name, shape, dtype, kind=kind).ap()
    x_in = dt_("x", [NB, S, D], F32, kin)
    ctx_in = dt_("ctx", [NB, LC, D], F32, kin)
    c_fm = dt_("c_fm", [128, KC, 3], F32, kin)
    w_mod = dt_("w_mod", [DEPTH, D, 6 * D], F32, kin)
    b_mod_fm = dt_("b_mod_fm", [DEPTH, 128, 48], F32, kin)
    w_in = dt_("w_in", [DEPTH, D, 2560], F32, kin)
    w_uT = dt_("w_uT", [DEPTH, 256, D], F32, kin)
    w_f = dt_("w_f", [DEPTH, 256, 64], F32, kin)
    lam_qk = dt_("lam_qk", [DEPTH, 1, 256], F32, kin)
    subln_g = dt_("subln_g", [DEPTH, 1, 128], F32, kin)
    w_out = dt_("w_out", [DEPTH, D, D], F32, kin)
    ln_vecs = dt_("ln_vecs", [DEPTH, 4, 1, D], F32, kin)
    w_router = dt_("w_router", [D, NE], F32, kin)
    router_bias = dt_("router_bias", [1, NE], F32, kin)
    w_gate = dt_("w_gate", [DEPTH, NE, D, 512], F32, kin)
    w_up = dt_("w_up", [DEPTH, NE, D, 512], F32, kin)
    w_down = dt_("w_down", [DEPTH, NE, 512, D], F32, kin)
    dftc = dt_("dftc", [S, S], BF16, kin)
    dftns = dt_("dftns", [S, S], BF16, kin)
    dft256 = dt_("dft256", [2, LC, LC], BF16, kin)
    c64bd = dt_("c64bd", [2, 128, 128], F32, kin)
    rope_cs = dt_("rope_cs", [2, 128, S], F32, kin)
    rmat_d = dt_("rmat", [128, 128], BF16, kin)
    ident_d = dt_("ident", [128, 128], BF16, kin)
    identf_d = dt_("identf", [128, 128], F32, kin)
    out_d = dt_("out", [NB, S, D], F32, "ExternalOutput")
    skind = "ExternalOutput" if debug else "Internal"
    QT_s = dt_("QT_s", [NB, 128, 6, TT], BF16, skind)
    KT_s = dt_("KT_s", [NB, 128, 6, TT], BF16, skind)
    VAB_s = dt_("VAB_s", [NB, TT, 1280], BF16, skind)
    X1_s = dt_("X1_s", [NB, TT, D], F32, skind)
    X2_s = dt_("X2_s", [NB, TT, D], F32, skind)
    MIX_s = dt_("MIX_s", [NB, 128, 8, TT], BF16, skind) if debug else None
    DBG_s = dt_("DBG_s", [128, 4096], F32, skind) if debug else None

    st = ExitStack()
    arena_t = st.enter_context(nc.sbuf_tensor("arena", [128, ARENA_BYTES // 2], BF16))
    psum_t = st.enter_context(nc.psum_tensor("psum", [128, 4096], F32))
    P = Prog(nc)
    AR = Arena(arena_t[:, :])

    def bank(b, n=1):
        return psum_t[:, b * 512:(b + n) * 512]

    rp = [P.R("bank%d" % i) for i in range(8)]

    ident = AR.alloc([128], BF16)
    identf = AR.alloc([128], F32)
    onesf = AR.alloc([128], F32)
    rmat = AR.alloc([128], BF16)
    modT = AR.alloc([48, 3], F32)
    s1p_a = AR.alloc([KC, 3], F32)
    s1p_f = AR.alloc([KC, 3], F32)
    nlam = AR.alloc([1], F32)
    gsub = AR.alloc([128], F32)
    rbias = AR.alloc([NE], F32)
    wr_sb = AR.alloc([KC, NE], BF16)
    r_const = P.R("const")
    r_mod = P.R("mod")
    P.op("sp", dma(ident, ident_d), writes=[r_const], dma=True)
    P.op("sp", dma(identf, identf_d), writes=[r_const], dma=True)
    P.op("sp", dma(rmat, rmat_d), writes=[r_const], dma=True)
    P.op("sp", dma(rbias, router_bias.partition_broadcast(128)), writes=[r_const], dma=True)
    P.op("pool", dma(wr_sb, w_router.rearrange("(kc p) e -> p kc e", p=128)), writes=[r_const], dma=True)
    P.op("dve", memset(onesf, 1.0), writes=[r_const])
    P.barrier()
    PERSIST = AR.mark()

    def ln_stats(xt_ap, r_x, sm, r_sm):
        P.op("dve", lambda e: e.bn_stats(out=sm[:, 0:6], in_=xt_ap[:, 0:512]), reads=[r_x], writes=[r_sm])
        P.op("dve", lambda e: e.bn_stats(out=sm[:, 6:12], in_=xt_ap[:, 512:1024]), reads=[r_x], writes=[r_sm])
        P.op("dve", lambda e: e.bn_aggr(out=sm[:, 12:14], in_=sm[:, 0:12].rearrange("p (a b) -> p a b", b=6)),
             reads=[r_sm], writes=[r_sm])
        P.op("act", act(sm[:, 14:15], sm[:, 13:14], AF.Ln, bias=LN_EPS, scale=1.0), reads=[r_sm], writes=[r_sm])
        P.op("act", act(sm[:, 15:16], sm[:, 14:15], AF.Exp, scale=-0.5), reads=[r_sm], writes=[r_sm])
        P.op("dve", stt(sm[:, 16:17], sm[:, 12:13], -1.0, sm[:, 15:16], ALU.mult, ALU.mult),
             reads=[r_sm], writes=[r_sm])
        return sm[:, 15:16], sm[:, 16:17]

    def make_bcast(dst, r_dst, src_fm, diag, r_diag, pbank):
        for c in range(KC):
            P.op("dve", ts(diag, identf, src_fm[:, c:c + 1], ALU.mult), reads=[r_const, r_mod], writes=[r_diag])
            half, cc = c // 4, c % 4
            P.op("pe", mm(bank(pbank + half)[:, cc * 128:(cc + 1) * 128], onesf, diag, True, True),
                 reads=[r_const, r_diag], writes=[rp[pbank + half]])
        P.op("dve", tcopy(dst, bank(pbank, 2)), reads=[rp[pbank], rp[pbank + 1]], writes=[r_dst])

    def tile_src(l, b, t):
        if l == 0:
            if t < 2:
                return ctx_in[b, t * 128:(t + 1) * 128, :]
            return x_in[b, (t - 2) * 128:(t - 1) * 128, :]
        return X2_s[b, t * 128:(t + 1) * 128, :]

    for l in range(DEPTH):
        last = (l == DEPTH - 1)
        lam_init = 0.8 - 0.6 * math.exp(-0.3 * l)
        tiles_q = list(range(2, NT)) if last else list(range(NT))
        AR.reset(PERSIST)
        cfm = AR.alloc([KC, 3], F32)
        silu_c = AR.alloc([KC, 3], F32)
        bmod = AR.alloc([48], F32)
        lqb = AR.alloc([256], F32)
        junk = AR.alloc([64], F32)
        s12 = AR.alloc([4], F32)
        wm_ring = Ring([Buf(AR.alloc([KC, 1024], F32), P.R()) for _ in range(2)])
        r_c, r_s, r_b, r_l, r_j = P.R(), P.R(), P.R(), P.R(), P.R()
        P.op("sp", dma(cfm, c_fm), writes=[r_c], dma=True)
        P.op("sp", dma(bmod, b_mod_fm[l]), writes=[r_b], dma=True)
        P.op("sp", dma(lqb, lam_qk[l].partition_broadcast(128)), writes=[r_l], dma=True)
        P.op("sp", dma(gsub, subln_g[l].partition_broadcast(128)), writes=[r_mod], dma=True)
        P.op("act", act(silu_c, cfm, AF.Silu), reads=[r_c], writes=[r_s])
        psm = bank(0)[:, 0:144].rearrange("p (a b) -> p a b", b=3)
        for cg in range(6):
            wb = wm_ring.next()
            for kc in range(KC):
                P.op("sp", dma(wb.ap[:, kc, :], w_mod[l, kc * 128:(kc + 1) * 128, cg * 1024:(cg + 1) * 1024]),
                     writes=[wb.res], dma=True)
            for j in range(8):
                for kc in range(KC):
                    P.op("pe", mm(psm[:, cg * 8 + j, :], wb.ap[:, kc, j * 128:(j + 1) * 128], silu_c[:, kc, :],
                                  kc == 0, kc == KC - 1), reads=[wb.res, r_s], writes=[rp[0]])
        for j in range(3):
            P.op("dve", tt(modT[:, :, j], psm[:, :, j], bmod, ALU.add), reads=[rp[0], r_b], writes=[r_mod])
        P.op("dve", ts(s1p_a, modT[:, 8:16, :], 1.0, ALU.add), reads=[r_mod], writes=[r_mod])
        P.op("dve", ts(s1p_f, modT[:, 32:40, :], 1.0, ALU.add), reads=[r_mod], writes=[r_mod])
        P.op("dve", memset(s12, 0.0), writes=[r_j])
        P.op("dve", stt(junk, lqb[:, 0:64], 1.0, lqb[:, 64:128], ALU.mult, ALU.mult, accum_out=s12[:, 0:1]),
             reads=[r_l], writes=[r_j])
        P.op("dve", stt(junk, lqb[:, 128:192], 1.0, lqb[:, 192:256], ALU.mult, ALU.mult, accum_out=s12[:, 1:2]),
             reads=[r_l], writes=[r_j])
        P.op("act", act(s12[:, 2:4], s12[:, 0:2], AF.Exp), reads=[r_j], writes=[r_j])
        P.op("dve", tt(nlam, s12[:, 3:4], s12[:, 2:3], ALU.subtract), reads=[r_j], writes=[r_mod])
        P.op("dve", ts(nlam, nlam, -lam_init, ALU.add), reads=[r_mod], writes=[r_mod])
        P.op("dve", ts(gsub, gsub, 1.0 - lam_init, ALU.mult), reads=[r_mod], writes=[r_mod])
        P.barrier()
        sh_a = modT[:, 0:8, :]
        g_a = modT[:, 16:24, :]
        sh_f = modT[:, 24:32, :]
        g_f = modT[:, 40:48, :]

        AR.reset(PERSIST)
        w_sb = AR.alloc([KC, WCOLS], BF16)
        r_w = P.R("w_in")
        cs_sb = AR.alloc([2, S], F32)
        r_cs = P.R()
        xt_ring = Ring([Buf(AR.alloc([D], F32), P.R()) for _ in range(2)])
        xn_ring = Ring([Buf(AR.alloc([D], BF16), P.R()) for _ in range(2)])
        sm_ring = Ring([Buf(AR.alloc([24], F32), P.R()) for _ in range(2)])
        hT_ring = Ring([Buf(AR.alloc([KC, 512], BF16), P.R()) for _ in range(2)])
        pl_ring = Ring([Buf(AR.alloc([512], BF16), P.R()) for _ in range(2)])
        t1_ring = Ring([Buf(AR.alloc([512], F32), P.R()) for _ in range(2)])
        t2_ring = Ring([Buf(AR.alloc([512], F32), P.R()) for _ in range(2)])
        qk_ring = Ring([Buf(AR.alloc([12, 512], BF16), P.R()) for _ in range(2)])
        vab_ring = Ring([Buf(AR.alloc([1280], BF16), P.R()) for _ in range(2)])
        c64 = AR.alloc([2, 128], F32)
        wf_sb = AR.alloc([2, 64], F32)
        bd = AR.alloc([4, 128], BF16)
        wuT = AR.alloc([2, D], BF16)
        r_a, r_bd = P.R(), P.R()
        P.op("sp", dma(c64, c64bd.rearrange("a p n -> p a n")), writes=[r_a], dma=True)
        P.op("sp", dma(wf_sb, w_f[l].rearrange("(j p) d -> p j d", p=128)), writes=[r_a], dma=True)
        P.op("pool", dma(wuT, w_uT[l].rearrange("(j p) k -> p j k", p=128)), writes=[r_a], dma=True)
        P.op("sp", dma(cs_sb, rope_cs.rearrange("a p n -> p a n")), writes=[r_cs], dma=True)
        for kc in range(KC):
            P.op("pool", dma(w_sb[:, kc, 512:WCOLS], w_in[l, kc * 128:(kc + 1) * 128, 256:2560]),
                 writes=[r_w], dma=True)
        P.op("dve", memset(bd, 0.0), writes=[r_bd])
        for cs in range(2):
            for j in range(2):
                idx = cs * 2 + j
                P.op("pe", mm(bank(0)[:, idx * 64:(idx + 1) * 64], c64[:, cs, :], wf_sb[:, j, :], True, True),
                     reads=[r_a], writes=[rp[0]])
        for idx in range(4):
            P.op("dve", tcopy(bd[0:64, idx, 0:64], bank(0)[0:64, idx * 64:(idx + 1) * 64]),
                 reads=[rp[0]], writes=[r_bd])
            P.op("dve", tcopy(bd[64:128, idx, 64:128], bank(0)[64:128, idx * 64:(idx + 1) * 64]),
                 reads=[rp[0]], writes=[r_bd])
        for kc in range(KC):
            pb = 2 + (kc % 2)
            for cs in range(2):
                for j in range(2):
                    idx = cs * 2 + j
                    P.op("pe", mm(bank(pb)[:, idx * 128:(idx + 1) * 128], wuT[:, j, kc * 128:(kc + 1) * 128],
                                  bd[:, idx, :], True, True), reads=[r_a, r_bd], writes=[rp[pb]])
            P.op("dve", tcopy(w_sb[:, kc, 0:512], bank(pb)), reads=[rp[pb]], writes=[r_w])

        blocks = []
        for b in range(NB):
            blocks.append((b, 0, 2))
            for i in range(4):
                blocks.append((b, 2 + 4 * i, 4))
        psT_i = [0]
        fm_i = [0]
        tm_i = [0]
        rot_i = [0]

        def p1_A(blk):
            b, t0, ntl = blk
            hb = hT_ring.next()
            for ti in range(ntl):
                t = t0 + ti
                j = 2 if t < 2 else b
                xb_, xnb, smb = xt_ring.next(), xn_ring.next(), sm_ring.next()
                P.op("sp", dma(xb_.ap, tile_src(l, b, t)), writes=[xb_.res], dma=True)
                rstd, nmr = ln_stats(xb_.ap, xb_.res, smb.ap, smb.res)
                P.op("act", act(xnb.ap, xb_.ap, AF.Identity, bias=nmr, scale=rstd),
                     reads=[xb_.res, smb.res], writes=[xnb.res])
                pb = psT_i[0] % 2
                psT_i[0] += 1
                psT = bank(pb).bitcast(BF16).rearrange("p (a b) -> p a b", b=128)
                for kc in range(KC):
                    P.op("pe", tr(psT[:, kc, :], xnb.ap[:, kc * 128:(kc + 1) * 128], ident),
                         reads=[xnb.res, r_const], writes=[rp[pb]])
                for kc in range(KC):
                    P.op("dve", ts(hb.ap[:, kc, ti * 128:(ti + 1) * 128], psT[:, kc, :], s1p_a[:, kc, j:j + 1],
                                   ALU.mult, sh_a[:, kc, j:j + 1], ALU.add),
                         reads=[rp[pb], r_mod], writes=[hb.res])
            return hb

        def p1_B(blk, hb):
            b, t0, ntl = blk
            n = ntl * 128
            is_ctx = t0 < 2
            tok0 = t0 * 128
            qb = qk_ring.next()
            for c in range(12):
                pb = 2 + fm_i[0] % 2
                fm_i[0] += 1
                col = 512 + c * 128
                for kc in range(KC):
                    P.op("pe", mm(bank(pb)[:, 0:n], w_sb[:, kc, col:col + 128], hb.ap[:, kc, 0:n],
                                  kc == 0, kc == KC - 1), reads=[r_w, hb.res], writes=[rp[pb]])
                if is_ctx:
                    P.op("act", tcopy_act(qb.ap[:, c, 0:n], bank(pb)[:, 0:n]), reads=[rp[pb]], writes=[qb.res])
                else:
                    pl, t1, t2 = pl_ring.next(), t1_ring.next(), t2_ring.next()
                    P.op("act", tcopy_act(pl.ap[:, 0:n], bank(pb)[:, 0:n]), reads=[rp[pb]], writes=[pl.res])
                    prb = 4 + rot_i[0] % 2
                    rot_i[0] += 1
                    P.op("pe", mm(bank(prb)[:, 0:n], rmat, pl.ap[:, 0:n], True, True),
                         reads=[pl.res, r_const], writes=[rp[prb]])
                    s0 = tok0 - LC
                    P.op("dve", tt(t1.ap[:, 0:n], pl.ap[:, 0:n], cs_sb[:, 0, s0:s0 + n], ALU.mult),
                         reads=[pl.res, r_cs], writes=[t1.res])
                    P.op("dve", tt(t2.ap[:, 0:n], bank(prb)[:, 0:n], cs_sb[:, 1, s0:s0 + n], ALU.mult),
                         reads=[rp[prb], r_cs], writes=[t2.res])
                    P.op("dve", tt(qb.ap[:, c, 0:n], t1.ap[:, 0:n], t2.ap[:, 0:n], ALU.add),
                         reads=[t1.res, t2.res], writes=[qb.res])
            P.op("pool", dma(QT_s[b, :, :, tok0:tok0 + n], qb.ap[:, 0:6, 0:n]), reads=[qb.res], dma=True)
            P.op("pool", dma(KT_s[b, :, :, tok0:tok0 + n], qb.ap[:, 6:12, 0:n]), reads=[qb.res], dma=True)
            for ti in range(ntl):
                vb = vab_ring.next()
                for (c0, c1, wc0) in ((0, 512, 0), (512, 1024, 2048), (1024, 1280, 2560)):
                    pb = 6 + tm_i[0] % 2
                    tm_i[0] += 1
                    w_ = c1 - c0
                    for kc in range(KC):
                        P.op("pe", mm(bank(pb)[:, 0:w_], hb.ap[:, kc, ti * 128:(ti + 1) * 128],
                                      w_sb[:, kc, wc0:wc0 + w_], kc == 0, kc == KC - 1),
                             reads=[r_w, hb.res], writes=[rp[pb]])
                    P.op("act", tcopy_act(vb.ap[:, c0:c1], bank(pb)[:, 0:w_]), reads=[rp[pb]], writes=[vb.res])
                r0 = tok0 + ti * 128
                P.op("pool", dma(VAB_s[b, r0:r0 + 128, :], vb.ap), reads=[vb.res], dma=True)

        def tcopy_act(out, in_):
            return lambda e: e.copy(out=out, in_=in_)

        prev = None
        for i in range(len(blocks) + 1):
            cur = None
            if i < len(blocks):
                cur = (blocks[i], p1_A(blocks[i]))
            if prev is not None:
                p1_B(*prev)
            prev = cur
        P.barrier()
        if stop_after == ("P1", l):
            break

        for b in range(NB):
            AR.reset(PERSIST)
            fT = AR.alloc([2, TT], BF16)
            mixA = AR.alloc([6, TT], BF16)
            r_fT, r_mix = P.R(), P.R()
            SUB = AR.mark()
            ab_sb = AR.alloc([NT, 512], BF16)
            d256 = AR.alloc([2, 2, LC], BF16)
            tb_ring = Ring([Buf(AR.alloc([16, 512], BF16), P.R()) for _ in range(2)])
            r_ab, r_d = P.R(), P.R()
            P.op("sp", dma(ab_sb, VAB_s[b, :, 0:512].rearrange("(t p) n -> p t n", p=128)), writes=[r_ab], dma=True)
            if not last:
                for cs in range(2):
                    P.op("sp", dma(d256[:, :, cs, :], dft256[cs].rearrange("(tc p) n -> p tc n", p=128)),
                         writes=[r_d], dma=True)
                for j in range(2):
                    k = 0
                    for cs in range(2):
                        for tc in range(2):
                            P.op("pe", mm(bank(j)[:, 0:LC], ab_sb[:, tc, cs * 256 + j * 128: cs * 256 + (j + 1) * 128],
                                          d256[:, tc, cs, :], k == 0, k == 3), reads=[r_ab, r_d], writes=[rp[j]])
                            k += 1
                    P.op("act", tcopy_act(fT[:, j, 0:LC], bank(j)[:, 0:LC]), reads=[rp[j]], writes=[r_fT])
            for tb in range(4):
                for cs in range(2):
                    tbuf = tb_ring.next()
                    src = (dftc if cs == 0 else dftns)[:, tb * 512:(tb + 1) * 512].rearrange("(tc p) n -> p tc n", p=128)
                    for hh in range(2):
                        P.op("sp", dma(tbuf.ap[:, hh * 8:(hh + 1) * 8, :], src[:, hh * 8:(hh + 1) * 8, :]),
                             writes=[tbuf.res], dma=True)
                    for j in range(2):
                        pb = 2 + (tb % 2) * 2 + j
                        for tc in range(16):
                            P.op("pe", mm(bank(pb), ab_sb[:, 2 + tc, cs * 256 + j * 128: cs * 256 + (j + 1) * 128],
                                          tbuf.ap[:, tc, :], cs == 0 and tc == 0, cs == 1 and tc == 15),
                                 reads=[r_ab, tbuf.res], writes=[rp[pb]])
                for j in range(2):
                    pb = 2 + (tb % 2) * 2 + j
                    P.op("act", tcopy_act(fT[:, j, LC + tb * 512: LC + (tb + 1) * 512], bank(pb)),
                         reads=[rp[pb]], writes=[r_fT])
            if debug:
                P.op("sp", dma(MIX_s[b, :, 0:2, :], fT), reads=[r_fT], dma=True)
            P.barrier()

            AR.reset(SUB)
            qT = AR.alloc([6, TT], BF16)
            kT = AR.alloc([6, TT], BF16)
            vaug = AR.alloc([NT, 6, VST], BF16)
            r_q, r_k, r_v = P.R(), P.R(), P.R()
            PT_ring = Ring([Buf(AR.alloc([NT, 512], BF16), P.R()) for _ in range(2)])
            tq_ring = Ring([Buf(AR.alloc([4, 128], F32), P.R()) for _ in range(2)])
            o_ring = Ring([Buf(AR.alloc([4, 128], F32), P.R()) for _ in range(2)])
            on_ring = Ring([Buf(AR.alloc([4, 128], BF16), P.R()) for _ in range(2)])
            rs_ring = Ring([Buf(AR.alloc([16], F32), P.R()) for _ in range(4)])
            junk2 = AR.alloc([128], F32)
            r_j2 = P.R()
            P.op("sp", dma(qT, QT_s[b]), writes=[r_q], dma=True)
            P.op("sp", dma(kT, KT_s[b]), writes=[r_k], dma=True)
            P.op("dve", memset(vaug[:, :, :, 128:VST], 1.0), writes=[r_v])
            for h in range(6):
                P.op("sp", dma(vaug[:, :, h, 0:128],
                               VAB_s[b, :, 512 + h * 128: 512 + (h + 1) * 128].rearrange("(t p) d -> p t d", p=128)),
                     writes=[r_v], dma=True)
            units = []
            if not last:
                for h in range(6):
                    for sub in range(2):
                        units.append((0, 2, [0, 1], h, sub))
            for qb_ in range(4):
                for h in range(6):
                    for sub in range(2):
                        units.append((LC + qb_ * 512, 4, list(range(NT)), h, sub))
            sg_i = [0]

            def att_S(u, ui):
                q0, nqt, kts, h, sub = u
                n = nqt * 128
                pt = PT_ring.next()
                p0, p1 = sub * 64, (sub + 1) * 64
                for g in range(0, len(kts), 2):
                    pb = (sg_i[0] % 2) * 2
                    sg_i[0] += 1
                    grp = kts[g:g + 2]
                    for gi, kt in enumerate(grp):
                        P.op("pe", mm(bank(pb + gi)[:, 0:n], kT[p0:p1, h, kt * 128:(kt + 1) * 128],
                                      qT[p0:p1, h, q0:q0 + n], True, True),
                             reads=[r_q, r_k], writes=[rp[pb + gi]])
                    src = bank(pb, 2).rearrange("p (a b) -> p a b", b=512)[:, 0:len(grp), 0:n]
                    P.op("act", act(pt.ap[:, g:g + len(grp), 0:n], src, AF.Exp, scale=0.125),
                         reads=[rp[pb], rp[pb + 1]], writes=[pt.res])
                return pt

            def acc_ap(par, qt):
                if qt < 3:
                    return bank(4 + 2 * par)[:, qt * 160: qt * 160 + 129]
                return bank(5 + 2 * par)[:, 0:129]

            def att_AV(u, ui, pt, state):
                q0, nqt, kts, h, sub = u
                par = ui % 2
                for qt in range(nqt):
                    a = acc_ap(par, qt)
                    for ki, kt in enumerate(kts):
                        P.op("pe", mm(a, pt.ap[:, ki, qt * 128:(qt + 1) * 128], vaug[:, kt, h, 0:129],
                                      ki == 0, ki == len(kts) - 1),
                             reads=[pt.res, r_v], writes=[rp[4 + 2 * par], rp[5 + 2 * par]])
                accr = [rp[4 + 2 * par], rp[5 + 2 * par]]
                rs = rs_ring.next()
                if sub == 0:
                    tq = tq_ring.next()
                    state["tq"] = tq
                    for qt in range(nqt):
                        a = acc_ap(par, qt)
                        P.op("dve", lambda e, o=rs.ap[:, qt:qt + 1], i=a[:, 128:129]: e.reciprocal(out=o, in_=i),
                             reads=accr, writes=[rs.res])
                        P.op("dve", ts(tq.ap[:, qt, :], a[:, 0:128], rs.ap[:, qt:qt + 1], ALU.mult),
                             reads=accr + [rs.res], writes=[tq.res])
                else:
                    tq = state["tq"]
                    ob, onb = o_ring.next(), on_ring.next()
                    P.op("dve", memset(rs.ap[:, 8:12], 0.0), writes=[rs.res])
                    for qt in range(nqt):
                        a = acc_ap(par, qt)
                        P.op("dve", lambda e, o=rs.ap[:, qt:qt + 1], i=a[:, 128:129]: e.reciprocal(out=o, in_=i),
                             reads=accr, writes=[rs.res])
                        P.op("dve", ts(rs.ap[:, 4 + qt:5 + qt], rs.ap[:, qt:qt + 1], nlam[:, 0:1], ALU.mult),
                             reads=[rs.res, r_mod], writes=[rs.res])
                        P.op("dve", stt(ob.ap[:, qt, :], a[:, 0:128], rs.ap[:, 4 + qt:5 + qt], tq.ap[:, qt, :],
                                        ALU.mult, ALU.add), reads=accr + [rs.res, tq.res], writes=[ob.res])
                        P.op("dve", stt(junk2, ob.ap[:, qt, :], 1.0, ob.ap[:, qt, :], ALU.mult, ALU.mult,
                                        accum_out=rs.ap[:, 8 + qt:9 + qt]), reads=[ob.res], writes=[rs.res, r_j2])
                    P.op("act", act(rs.ap[:, 12:12 + nqt], rs.ap[:, 8:8 + nqt], AF.Ln, bias=LN_EPS, scale=1.0 / 128),
                         reads=[rs.res], writes=[rs.res])
                    P.op("act", act(rs.ap[:, 12:12 + nqt], rs.ap[:, 12:12 + nqt], AF.Exp, scale=-0.5),
                         reads=[rs.res], writes=[rs.res])
                    for qt in range(nqt):
                        P.op("dve", stt(onb.ap[:, qt, :], ob.ap[:, qt, :], rs.ap[:, 12 + qt:13 + qt], gsub,
                                        ALU.mult, ALU.mult), reads=[ob.res, rs.res, r_mod], writes=[onb.res])
                    psT = bank(5 + 2 * par)[:, 256:512].bitcast(BF16).rearrange("p (a b) -> p a b", b=128)
                    for qt in range(nqt):
                        P.op("pe", tr(psT[:, qt, :], onb.ap[:, qt, :], ident), reads=[onb.res, r_const],
                             writes=[rp[5 + 2 * par]])
                    P.op("dve", tcopy(mixA[:, h, q0:q0 + nqt * 128].rearrange("p (a b) -> p a b", b=128), psT[:, 0:nqt, :]),
                         reads=[rp[5 + 2 * par]], writes=[r_mix])

            state = {}
            prevu = None
            for ui in range(len(units) + 1):
                curu = None
                if ui < len(units):
                    curu = (units[ui], ui, att_S(units[ui], ui))
                if prevu is not None:
                    att_AV(prevu[0], prevu[1], prevu[2], state)
                prevu = curu
            if debug:
                P.op("sp", dma(MIX_s[b, :, 2:8, :], mixA), reads=[r_mix], dma=True)
            P.barrier()

            AR.reset(SUB)
            wo_sb = AR.alloc([KC, D], BF16)
            gbc = [AR.alloc([D], F32) for _ in range(2)]
            lng = AR.alloc([D], F32)
            lnb = AR.alloc([D], F32)
            diag = AR.alloc([128], F32)
            r_wo, r_g, r_ln, r_dg = P.R(), P.R(), P.R(), P.R()
            xt_ring = Ring([Buf(AR.alloc([D], F32), P.R()) for _ in range(2)])
            z_ring = Ring([Buf(AR.alloc([D], F32), P.R()) for _ in range(2)])
            sm_ring = Ring([Buf(AR.alloc([24], F32), P.R()) for _ in range(2)])
            for kc in range(KC):
                P.op("pool", dma(wo_sb[:, kc, :], w_out[l, kc * 128:(kc + 1) * 128, :]), writes=[r_wo], dma=True)
            P.op("sp", dma(lng, ln_vecs[l, 0].partition_broadcast(128)), writes=[r_ln], dma=True)
            P.op("sp", dma(lnb, ln_vecs[l, 1].partition_broadcast(128)), writes=[r_ln], dma=True)
            make_bcast(gbc[0], r_g, g_a[:, :, b], diag, r_dg, 0)
            if not last:
                make_bcast(gbc[1], r_g, g_a[:, :, 2], diag, r_dg, 0)
            for ti, t in enumerate(tiles_q):
                xb_, zb, smb = xt_ring.next(), z_ring.next(), sm_ring.next()
                P.op("sp", dma(xb_.ap, tile_src(l, b, t)), writes=[xb_.res], dma=True)
                pb = 2 + (ti % 3) * 2
                for half in range(2):
                    for kc in range(KC):
                        lhs = fT[:, kc, t * 128:(t + 1) * 128] if kc < 2 else mixA[:, kc - 2, t * 128:(t + 1) * 128]
                        P.op("pe", mm(bank(pb + half), lhs, wo_sb[:, kc, half * 512:(half + 1) * 512],
                                      kc == 0, kc == KC - 1), reads=[r_fT, r_mix, r_wo], writes=[rp[pb + half]])
                gsel = gbc[1] if t < 2 else gbc[0]
                P.op("dve", tt(zb.ap, bank(pb, 2), gsel, ALU.mult), reads=[rp[pb], rp[pb + 1], r_g], writes=[zb.res])
                P.op("dve", stt(zb.ap, xb_.ap, ALPHA, zb.ap, ALU.mult, ALU.add), reads=[xb_.res, zb.res],
                     writes=[zb.res])
                rstd, nmr = ln_stats(zb.ap, zb.res, smb.ap, smb.res)
                P.op("act", act(xb_.ap, zb.ap, AF.Identity, bias=nmr, scale=rstd), reads=[zb.res, smb.res],
                     writes=[xb_.res])
                P.op("dve", tt(xb_.ap, xb_.ap, lng, ALU.mult), reads=[xb_.res, r_ln], writes=[xb_.res])
                P.op("dve", tt(xb_.ap, xb_.ap, lnb, ALU.add), reads=[xb_.res, r_ln], writes=[xb_.res])
                P.op("pool", dma(X1_s[b, t * 128:(t + 1) * 128, :], xb_.ap), reads=[xb_.res], dma=True)
            P.barrier()
        if stop_after == ("MIX", l):
            break

        for b in range(NB):
            AR.reset(PERSIST)
            ntm = len(tiles_q)
            t_off = tiles_q[0]
            h2T = AR.alloc([KC, ntm * 128], BF16)
            comb = AR.alloc([ntm, NE], F32)
            yacc = AR.alloc([ntm, D], F32)
            r_h2, r_cb, r_y = P.R(), P.R(), P.R()
            SUB = AR.mark()
            xt_ring = Ring([Buf(AR.alloc([D], F32), P.R()) for _ in range(2)])
            xn_ring = Ring([Buf(AR.alloc([D], BF16), P.R()) for _ in range(2)])
            sm_ring = Ring([Buf(AR.alloc([24], F32), P.R()) for _ in range(2)])
            rt_ring = Ring([Buf(AR.alloc([160], F32), P.R()) for _ in range(2)])
            P.op("pool", memset(yacc, 0.0), writes=[r_y])
            for ti, t in enumerate(tiles_q):
                j = 2 if t < 2 else b
                xb_, xnb, smb, rt = xt_ring.next(), xn_ring.next(), sm_ring.next(), rt_ring.next()
                P.op("sp", dma(xb_.ap, X1_s[b, t * 128:(t + 1) * 128, :]), writes=[xb_.res], dma=True)
                rstd, nmr = ln_stats(xb_.ap, xb_.res, smb.ap, smb.res)
                P.op("act", act(xnb.ap, xb_.ap, AF.Identity, bias=nmr, scale=rstd),
                     reads=[xb_.res, smb.res], writes=[xnb.res])
                pb = ti % 2
                psT = bank(pb).bitcast(BF16).rearrange("p (a b) -> p a b", b=128)
                for kc in range(KC):
                    P.op("pe", tr(psT[:, kc, :], xnb.ap[:, kc * 128:(kc + 1) * 128], ident),
                         reads=[xnb.res, r_const], writes=[rp[pb]])
                for kc in range(KC):
                    P.op("dve", ts(h2T[:, kc, ti * 128:(ti + 1) * 128], psT[:, kc, :], s1p_f[:, kc, j:j + 1],
                                   ALU.mult, sh_f[:, kc, j:j + 1], ALU.add),
                         reads=[rp[pb], r_mod], writes=[r_h2])
                prb = 2 + ti % 2
                for kc in range(KC):
                    P.op("pe", mm(bank(prb)[:, 0:NE], h2T[:, kc, ti * 128:(ti + 1) * 128], wr_sb[:, kc, :],
                                  kc == 0, kc == KC - 1), reads=[r_h2, r_const], writes=[rp[prb]])
                A = rt.ap
                sc, sel, w4, gs, m2, gm, msk, ww, tmp = (A[:, 0:16], A[:, 16:32], A[:, 32:64], A[:, 64:68],
                                                           A[:, 68:72], A[:, 72:76], A[:, 80:96], A[:, 96:112],
                                                           A[:, 112:116])
                P.op("act", act(sc, bank(prb)[:, 0:NE], AF.Exp, scale=-1.0), reads=[rp[prb]], writes=[rt.res])
                rr = [rt.res]
                P.op("dve", ts(sc, sc, 1.0, ALU.add), reads=rr, writes=rr)
                P.op("dve", lambda e, o=sc: e.reciprocal(out=o, in_=o), reads=rr, writes=rr)
                P.op("dve", tt(sel, sc, rbias, ALU.add), reads=rr + [r_const], writes=rr)
                sv = sel.rearrange("p (g e) -> p g e", e=4)
                hi01, lo01, hi23, lo23 = w4[:, 0:4], w4[:, 4:8], w4[:, 8:12], w4[:, 12:16]
                m1, mid, lom = w4[:, 16:20], w4[:, 20:24], w4[:, 24:28]
                P.op("dve", tt(hi01, sv[:, :, 0], sv[:, :, 1], ALU.max), reads=rr, writes=rr)
                P.op("dve", tt(lo01, sv[:, :, 0], sv[:, :, 1], ALU.min), reads=rr, writes=rr)
                P.op("dve", tt(hi23, sv[:, :, 2], sv[:, :, 3], ALU.max), reads=rr, writes=rr)
                P.op("dve", tt(lo23, sv[:, :, 2], sv[:, :, 3], ALU.min), reads=rr, writes=rr)
                P.op("dve", tt(m1, hi01, hi23, ALU.max), reads=rr, writes=rr)
                P.op("dve", tt(mid, hi01, hi23, ALU.min), reads=rr, writes=rr)
                P.op("dve", tt(lom, lo01, lo23, ALU.max), reads=rr, writes=rr)
                P.op("dve", tt(m2, mid, lom, ALU.max), reads=rr, writes=rr)
                P.op("dve", tt(gs, m1, m2, ALU.add), reads=rr, writes=rr)
                P.op("dve", lambda e, o=tmp[:, 0:1], i=gs: e.tensor_reduce(out=o, in_=i, axis=AX.X, op=ALU.max),
                     reads=rr, writes=rr)
                P.op("dve", ts(gm, gs, tmp[:, 0:1], ALU.is_ge), reads=rr, writes=rr)
                mv_ = msk.rearrange("p (g e) -> p g e", e=4)
                for ee in range(4):
                    P.op("dve", tt(mv_[:, :, ee], sv[:, :, ee], m2, ALU.is_ge), reads=rr, writes=rr)
                    P.op("dve", tt(mv_[:, :, ee], mv_[:, :, ee], gm, ALU.mult), reads=rr, writes=rr)
                P.op("dve", tt(ww, sc, msk, ALU.mult), reads=rr, writes=rr)
                P.op("dve", lambda e, o=tmp[:, 1:2], i=ww: e.tensor_reduce(out=o, in_=i, axis=AX.X, op=ALU.add),
                     reads=rr, writes=rr)
                P.op("dve", lambda e, o=tmp[:, 2:3], i=tmp[:, 1:2]: e.reciprocal(out=o, in_=i), reads=rr, writes=rr)
                P.op("dve", ts(comb[:, ti, :], ww, tmp[:, 2:3], ALU.mult), reads=rr, writes=[r_cb])
            if debug and l == 0 and b == 0:
                P.op("sp", dma(DBG_s[:, 0:ntm * NE], comb.rearrange("p a b -> p (a b)")), reads=[r_cb], dma=True)
            P.barrier()

            AR.reset(SUB)
            w_ring = Ring([Buf((AR.alloc([KC, 512], BF16), AR.alloc([KC, 512], BF16), AR.alloc([4, D], BF16)), P.R())
                           for _ in range(2)])
            sg_ring = Ring([Buf(AR.alloc([512], F32), P.R()) for _ in range(2)])
            a_ring = Ring([Buf(AR.alloc([512], BF16), P.R()) for _ in range(2)])
            aT_ring = Ring([Buf(AR.alloc([4, 128], BF16), P.R()) for _ in range(2)])
            gu_i = [0]

            def moe_GU(e_, ti, wbuf):
                wg, wu, wd = wbuf.ap
                pb = (gu_i[0] % 2) * 2
                gu_i[0] += 1
                for which, wmat in ((0, wg), (1, wu)):
                    for kc in range(KC):
                        P.op("pe", mm(bank(pb + which), h2T[:, kc, ti * 128:(ti + 1) * 128], wmat[:, kc, :],
                                      kc == 0, kc == KC - 1), reads=[r_h2, wbuf.res], writes=[rp[pb + which]])
                sg, ab_ = sg_ring.next(), a_ring.next()
                P.op("act", act(sg.ap, bank(pb), AF.Silu), reads=[rp[pb]], writes=[sg.res])
                P.op("dve", stt(ab_.ap, bank(pb + 1), comb[:, ti, e_:e_ + 1], sg.ap, ALU.mult, ALU.mult),
                     reads=[rp[pb + 1], sg.res, r_cb], writes=[ab_.res])
                return ab_

            def moe_D(e_, ti, wbuf, ab_):
                wg, wu, wd = wbuf.ap
                par = ti % 2
                psT = bank(4)[:, par * 256:(par + 1) * 256].bitcast(BF16).rearrange("p (a b) -> p a b", b=128)
                aT = aT_ring.next()
                for f in range(4):
                    P.op("pe", tr(psT[:, f, :], ab_.ap[:, f * 128:(f + 1) * 128], ident), reads=[ab_.res, r_const],
                         writes=[rp[4]])
                P.op("dve", tcopy(aT.ap, psT), reads=[rp[4]], writes=[aT.res])
                for half in range(2):
                    for f in range(4):
                        P.op("pe", mm(bank(6 + half), aT.ap[:, f, :], wd[:, f, half * 512:(half + 1) * 512],
                                      f == 0, f == 3), reads=[aT.res, wbuf.res], writes=[rp[6 + half]])
                P.op("dve", tt(yacc[:, ti, :], yacc[:, ti, :], bank(6, 2), ALU.add),
                     reads=[rp[6], rp[7], r_y], writes=[r_y])

            for e_ in range(NE):
                wbuf = w_ring.next()
                wg, wu, wd = wbuf.ap
                for hh in range(2):
                    P.op("pool", dma(wg[:, hh * 4:(hh + 1) * 4, :],
                                     w_gate[l, e_, hh * 512:(hh + 1) * 512, :].rearrange("(kc p) f -> p kc f", p=128)),
                         writes=[wbuf.res], dma=True)
                    P.op("pool", dma(wu[:, hh * 4:(hh + 1) * 4, :],
                                     w_up[l, e_, hh * 512:(hh + 1) * 512, :].rearrange("(kc p) f -> p kc f", p=128)),
                         writes=[wbuf.res], dma=True)
                    P.op("pool", dma(wd[:, hh * 2:(hh + 1) * 2, :],
                                     w_down[l, e_, hh * 256:(hh + 1) * 256, :].rearrange("(kc p) f -> p kc f", p=128)),
                         writes=[wbuf.res], dma=True)
                prevt = None
                for ti in range(ntm + 1):
                    curt = None
                    if ti < ntm:
                        curt = (ti, moe_GU(e_, ti, wbuf))
                    if prevt is not None:
                        moe_D(e_, prevt[0], wbuf, prevt[1])
                    prevt = curt
            P.barrier()

            AR.reset(SUB)
            gbc = [AR.alloc([D], F32) for _ in range(2)]
            lng = AR.alloc([D], F32)
            lnb = AR.alloc([D], F32)
            diag = AR.alloc([128], F32)
            r_g, r_ln, r_dg = P.R(), P.R(), P.R()
            xt_ring = Ring([Buf(AR.alloc([D], F32), P.R()) for _ in range(2)])
            z_ring = Ring([Buf(AR.alloc([D], F32), P.R()) for _ in range(2)])
            sm_ring = Ring([Buf(AR.alloc([24], F32), P.R()) for _ in range(2)])
            P.op("sp", dma(lng, ln_vecs[l, 2].partition_broadcast(128)), writes=[r_ln], dma=True)
            P.op("sp", dma(lnb, ln_vecs[l, 3].partition_broadcast(128)), writes=[r_ln], dma=True)
            make_bcast(gbc[0], r_g, g_f[:, :, b], diag, r_dg, 0)
            if not last:
                make_bcast(gbc[1], r_g, g_f[:, :, 2], diag, r_dg, 0)
            for ti, t in enumerate(tiles_q):
                xb_, zb, smb = xt_ring.next(), z_ring.next(), sm_ring.next()
                P.op("sp", dma(xb_.ap, X1_s[b, t * 128:(t + 1) * 128, :]), writes=[xb_.res], dma=True)
                gsel = gbc[1] if t < 2 else gbc[0]
                P.op("dve", tt(zb.ap, yacc[:, ti, :], gsel, ALU.mult), reads=[r_y, r_g], writes=[zb.res])
                P.op("dve", stt(zb.ap, xb_.ap, ALPHA, zb.ap, ALU.mult, ALU.add), reads=[xb_.res, zb.res],
                     writes=[zb.res])
                rstd, nmr = ln_stats(zb.ap, zb.res, smb.ap, smb.res)
                P.op("act", act(xb_.ap, zb.ap, AF.Identity, bias=nmr, scale=rstd), reads=[zb.res, smb.res],
                     writes=[xb_.res])
                P.op("dve", tt(xb_.ap, xb_.ap, lng, ALU.mult), reads=[xb_.res, r_ln], writes=[xb_.res])
                P.op("dve", tt(xb_.ap, xb_.ap, lnb, ALU.add), reads=[xb_.res, r_ln], writes=[xb_.res])
                dst = out_d[b, (t - 2) * 128:(t - 1) * 128, :] if last else X2_s[b, t * 128:(t + 1) * 128, :]
                P.op("pool", dma(dst, xb_.ap), reads=[xb_.res], dma=True)
            P.barrier()

    P.barrier()
    P.emit()
    st.close()
    return nc, P


def _consts():
    bf = ml_dtypes.bfloat16
    t = np.arange(S, dtype=np.float64)
    ang = 2.0 * np.pi * ((np.outer(t, t)) % S) / S
    dftc = (np.cos(ang) / math.sqrt(S)).astype(np.float32).astype(bf)
    dftns = (-np.sin(ang) / math.sqrt(S)).astype(np.float32).astype(bf)
    t2 = np.arange(LC, dtype=np.float64)
    a2 = 2.0 * np.pi * ((np.outer(t2, t2)) % LC) / LC
    dft256 = np.stack([np.cos(a2) / math.sqrt(LC), -np.sin(a2) / math.sqrt(LC)]).astype(np.float32).astype(bf)
    c = np.arange(64, dtype=np.float64)
    a3 = 2.0 * np.pi * ((np.outer(c, c)) % 64) / 64
    c64 = np.cos(a3) / 8.0
    s64 = np.sin(a3) / 8.0
    c64bd = np.zeros((2, 128, 128), np.float32)
    for i, m in enumerate((c64, s64)):
        c64bd[i, 0:64, 0:64] = m
        c64bd[i, 64:128, 64:128] = m
    freqs = (10000.0 ** (-np.arange(0, 32, 2, dtype=np.float32) / 32)).astype(np.float32)
    pos = np.arange(S)
    row = (pos // 64).astype(np.float32)
    col = (pos % 64).astype(np.float32)
    ang_row = row[:, None] * freqs
    ang_col = col[:, None] * freqs
    rope = np.zeros((2, 128, S), np.float32)
    for p in range(128):
        d = p % 64
        a = ang_row[:, d % 16] if d < 32 else ang_col[:, d % 16]
        rope[0, p] = np.cos(a)
        rope[1, p] = np.sin(a)
    R = np.zeros((128, 128), np.float32)
    for m in range(128):
        d = m % 64
        if (d % 32) < 16:
            R[m + 16, m] = -1.0
        else:
            R[m - 16, m] = 1.0
    return dict(dftc=dftc, dftns=dftns, dft256=dft256, c64bd=c64bd, rope_cs=rope,
                rmat=R.astype(bf), ident=np.eye(128, dtype=np.float32).astype(bf),
                identf=np.eye(128, dtype=np.float32))


def make_in_maps(inputs, cores=range(N_CORES)):
    f = lambda a: np.ascontiguousarray(np.asarray(a, dtype=np.float32))
    x, c, ctx, c_ctx = f(inputs["x"]), f(inputs["c"]), f(inputs["ctx"]), f(inputs["c_ctx"])
    shared = dict(
        w_mod=f(inputs["w_mod"]),
        b_mod_fm=np.ascontiguousarray(f(inputs["b_mod"]).reshape(DEPTH, 48, 128).transpose(0, 2, 1)),
        w_in=f(inputs["w_in"]),
        w_uT=np.ascontiguousarray(f(inputs["w_in"])[:, :, :256].transpose(0, 2, 1)),
        w_f=f(inputs["w_fourier"]).reshape(DEPTH, 256, 64),
        lam_qk=f(inputs["lam_qk"]).reshape(DEPTH, 1, 256),
        subln_g=f(inputs["subln_g"]).reshape(DEPTH, 1, 128),
        w_out=f(inputs["w_out"]),
        ln_vecs=np.ascontiguousarray(np.stack([f(inputs["ln_attn_g"]), f(inputs["ln_attn_b"]),
                                               f(inputs["ln_ffn_g"]), f(inputs["ln_ffn_b"])], axis=1)
                                     .reshape(DEPTH, 4, 1, D)),
        w_router=f(inputs["w_router"]),
        router_bias=f(inputs["router_bias"]).reshape(1, NE),
        w_gate=f(inputs["w_gate"]), w_up=f(inputs["w_up"]), w_down=f(inputs["w_down"]),
    )
    shared.update(_consts())
    maps = []
    for ci in cores:
        b0 = ci * NB
        cc = np.stack([c[b0], c[b0 + 1], c_ctx], axis=-1)
        c_fm = np.ascontiguousarray(cc.reshape(KC, 128, 3).transpose(1, 0, 2))
        m = dict(shared)
        m["x"] = np.ascontiguousarray(x[b0:b0 + NB])
        m["ctx"] = np.ascontiguousarray(ctx[b0:b0 + NB])
        m["c_fm"] = c_fm
        maps.append(m)
    return maps


_CACHE = {}


def kernel(**inputs):
    if "nc" not in _CACHE:
        _CACHE["nc"] = build_program()[0]
    nc = _CACHE["nc"]
    in_maps = make_in_maps(inputs)
    res = run_bass_kernel_spmd(nc, in_maps, core_ids=list(range(N_CORES)))
    out = np.concatenate([np.asarray(r["out"], dtype=np.float32) for r in res.results], axis=0)
    return out
```

```python
import math
from contextlib import ExitStack

import numpy as np
import ml_dtypes
import concourse.bass as bass
import concourse.mybir as mybir
from concourse.bass_utils import run_bass_kernel_spmd

F32 = mybir.dt.float32
BF16 = mybir.dt.bfloat16
ALU = mybir.AluOpType
AF = mybir.ActivationFunctionType
AX = mybir.AxisListType

N_CORES = 8
NB = 2
S = 2048
LC = 256
TT = S + LC
NT = TT // 128
D = 1024
KC = 8
DEPTH = 2
WCOLS = 2816
ALPHA = (2 * DEPTH) ** 0.25
LN_EPS = 1e-5
NE = 16
VST = 132
CAP = NB * TT + 128
NSLOT = NE * CAP
I32 = mybir.dt.int32

ENGS = ("pe", "act", "dve", "pool", "sp")
DMAQ = ("sp", "pool", "act")


class Res:
    __slots__ = ("name", "w", "r")

    def __init__(self, name=""):
        self.name = name
        self.w = None
        self.r = []


class Prog:
    def __init__(self, nc, nq=16):
        self.nc = nc
        self.NQ = nq
        self.ops = {e: [] for e in ENGS}
        self.waited = {e: {} for e in ENGS}
        self.dma_n = {q: 0 for q in DMAQ}
        self.res = []
        self.cur_cond = None
        self.vload_specs = {}

    def cond(self, key):
        prog = self

        class _C:
            def __enter__(s_):
                assert prog.cur_cond is None
                prog.cur_cond = key

            def __exit__(s_, *a):
                prog.cur_cond = None
                return False
        return _C()

    def vload(self, name, ap, res, max_val):
        self.vload_specs[name] = (ap, max_val)
        for e in ENGS:
            self.op(e, ("vload", name), reads=[res])

    def R(self, name=""):
        r = Res(name)
        self.res.append(r)
        return r

    def _add_wait(self, eng, o, d, raw):
        if d[0] == "c":
            _, e2, idx = d
            if e2 == eng and (eng == "pe" or not raw):
                return
            key = ("c", e2)
            if self.waited[eng].get(key, -1) >= idx:
                return
            self.waited[eng][key] = idx
            self.ops[e2][idx]["signal"] = True
            o["waits"].append(d)
        else:
            _, q, slot, cnt = d
            key = ("d", q, slot)
            if self.waited[eng].get(key, 0) >= cnt:
                return
            self.waited[eng][key] = cnt
            o["waits"].append(d)

    def op(self, eng, fn, reads=(), writes=(), dma=False):
        ops = self.ops[eng]
        idx = len(ops)
        o = dict(fn=fn, waits=[], signal=False, dma=None, cond=self.cur_cond)
        raw_deps = []
        oth_deps = []
        for r in reads:
            if r.w is not None:
                raw_deps.append(r.w)
        for r in writes:
            if r.w is not None:
                oth_deps.append(r.w)
            oth_deps.extend(r.r)
        if dma:
            n = self.dma_n[eng]
            slot = n % self.NQ
            cnt = 16 * (n // self.NQ + 1)
            self.dma_n[eng] += 1
            if n >= self.NQ:
                oth_deps.append(("d", eng, slot, cnt - 16))
            ev = ("d", eng, slot, cnt)
            o["dma"] = (slot, cnt)
        else:
            ev = ("c", eng, idx)
        best = {}
        for lst, raw in ((raw_deps, True), (oth_deps, False)):
            for d in lst:
                if d[0] == "c":
                    if d[1] == eng and (eng == "pe" or not raw):
                        continue
                    k = ("c", d[1])
                    if k not in best or best[k][2] < d[2]:
                        best[k] = d
                else:
                    k = ("d", d[1], d[2])
                    if k not in best or best[k][3] < d[3]:
                        best[k] = d
        for d in best.values():
            self._add_wait(eng, o, d, True)
        ops.append(o)
        for r in reads:
            r.r.append(ev)
        for r in writes:
            r.w = ev
            r.r = []
        return ev

    def barrier(self):
        evs = []
        for e in ENGS:
            for idx in range(len(self.ops[e]) - 1, -1, -1):
                o = self.ops[e][idx]
                if o["fn"] is not None and o["dma"] is None:
                    evs.append(("c", e, idx))
                    break
        for q in DMAQ:
            n = self.dma_n[q]
            for slot in range(min(n, self.NQ)):
                last_n = ((n - 1 - slot) // self.NQ) * self.NQ + slot
                evs.append(("d", q, slot, 16 * (last_n // self.NQ + 1)))
        for e in ENGS:
            assert self.cur_cond is None
            o = dict(fn=None, waits=[], signal=False, dma=None, cond=None)
            for d in evs:
                if d[0] == "c" and d[1] == e:
                    continue
                self._add_wait(e, o, d, True)
            self.ops[e].append(o)
        for r in self.res:
            r.w = None
            r.r = []

    def emit(self):
        nc = self.nc
        ranks = {}
        for e in ENGS:
            c = 0
            rk = []
            for o in self.ops[e]:
                if o["signal"]:
                    c += 1
                rk.append(c)
            ranks[e] = rk
        self.n_signal = {e: (ranks[e][-1] if ranks[e] else 0) for e in ENGS}
        self.n_ops = {e: len(self.ops[e]) for e in ENGS}
        with ExitStack() as st:
            csem = {e: st.enter_context(nc.semaphore("c_" + e)) for e in ENGS}
            dsem = {q: [st.enter_context(nc.semaphore("d_%s_%d" % (q, i))) for i in range(self.NQ)]
                    for q in DMAQ}
            block = st.enter_context(nc.Block())

            def run(e):
                def emit_waits(eng, o):
                    for d in o["waits"]:
                        if d[0] == "c":
                            eng.wait_ge(csem[d[1]], ranks[d[1]][d[2]])
                        else:
                            eng.wait_ge(dsem[d[1]][d[2]], d[3])

                def emit_op(eng, o, vals):
                    emit_waits(eng, o)
                    fn = o["fn"]
                    if fn is None:
                        return
                    if isinstance(fn, tuple) and fn[0] == "vload":
                        ap, mx = self.vload_specs[fn[1]]
                        vals[fn[1]] = eng.value_load(ap)
                        if o["signal"]:
                            eng.sem_inc(csem[e], 1)
                        return
                    ins = fn(eng)
                    if o["dma"] is not None:
                        ins.then_inc(dsem[e][o["dma"][0]], 16)
                    elif o["signal"]:
                        ins.then_inc(csem[e], 1)

                import os as _os2
                _cond_eng = _os2.environ.get("COND_ENG", "pe,act,dve,pool,sp").split(",")

                def body(eng):
                    vals = {}
                    ops = self.ops[e]
                    i = 0
                    n = len(ops)
                    while i < n:
                        o = ops[i]
                        if o["cond"] is None or e not in _cond_eng:
                            emit_op(eng, o, vals)
                            i += 1
                            continue
                        key = o["cond"]
                        j = i
                        while j < n and ops[j]["cond"] == key:
                            j += 1
                        grp = ops[i:j]
                        rank_before = ranks[e][i - 1] if i > 0 else 0
                        nsig = sum(1 for g in grp if g["signal"])
                        dmas = [g["dma"] for g in grp if g["dma"] is not None]
                        with eng.If(vals[key[0]] > key[1]):
                            for g in grp:
                                emit_op(eng, g, vals)
                        nw = sum(len(g["waits"]) for g in grp)
                        if nsig or dmas or nw:
                            with eng.Else():
                                drained = False
                                for g in grp:
                                    emit_waits(eng, g)
                                    if g["dma"] is not None:
                                        slot, cnt = g["dma"]
                                        if cnt > 16:
                                            eng.wait_ge(dsem[e][slot], cnt - 16)
                                        eng.sem_inc(dsem[e][slot], 16)
                                    elif g["signal"]:
                                        if not drained and rank_before > 0:
                                            eng.wait_ge(csem[e], rank_before)
                                        drained = True
                                        eng.sem_inc(csem[e], 1)
                        i = j
                return body

            block.tensor(run("pe"))
            block.scalar(run("act"))
            block.vector(run("dve"))
            block.gpsimd(run("pool"))
            block.sync(run("sp"))


class Buf:
    __slots__ = ("ap", "res")

    def __init__(self, ap, res):
        self.ap = ap
        self.res = res


class Ring:
    def __init__(self, bufs):
        self.bufs = bufs
        self.i = 0

    def next(self):
        b = self.bufs[self.i % len(self.bufs)]
        self.i += 1
        return b


def mm(out, lhsT, rhs, start, stop):
    return lambda e: e.matmul(out, lhsT=lhsT, rhs=rhs, start=start, stop=stop)


def tr(out, in_, ident):
    return lambda e: e.transpose(out=out, in_=in_, identity=ident)


def dma(out, in_):
    return lambda e: e.dma_start(out=out, in_=in_)


def act(out, in_, func, bias=0.0, scale=1.0):
    return lambda e: e.activation(out=out, in_=in_, func=func, bias=bias, scale=scale)


def tcopy(out, in_):
    return lambda e: e.tensor_copy(out=out, in_=in_)


def tt(out, in0, in1, op):
    return lambda e: e.tensor_tensor(out=out, in0=in0, in1=in1, op=op)


def ts(out, in0, s1, op0, s2=None, op1=None):
    if op1 is None:
        return lambda e: e.tensor_scalar(out=out, in0=in0, scalar1=s1, scalar2=None, op0=op0)
    return lambda e: e.tensor_scalar(out=out, in0=in0, scalar1=s1, scalar2=s2, op0=op0, op1=op1)


def stt(out, in0, scalar, in1, op0, op1, accum_out=None):
    if accum_out is None:
        return lambda e: e.scalar_tensor_tensor(out=out, in0=in0, scalar=scalar, in1=in1, op0=op0, op1=op1)
    return lambda e: e.scalar_tensor_tensor(out=out, in0=in0, scalar=scalar, in1=in1, op0=op0, op1=op1,
                                            accum_out=accum_out)


def memset(ap, v):
    return lambda e: e.memset(ap, v)


ARENA_BYTES = 206 * 1024


class Arena:
    def __init__(self, ap_bf16):
        self.base = ap_bf16
        self.off = 0
        self.floor = 0

    def alloc(self, shape, dtype):
        n = 1
        for s in shape:
            n *= s
        nbytes = n * (2 if dtype == BF16 else 4)
        nbytes_al = (nbytes + 63) // 64 * 64
        assert self.off + nbytes_al <= ARENA_BYTES, ("SBUF arena overflow", self.off, nbytes_al)
        v = self.base[:, self.off // 2:(self.off + nbytes) // 2]
        self.off += nbytes_al
        if dtype != BF16:
            v = v.bitcast(dtype)
        if len(shape) == 2:
            v = v.rearrange("p (a b) -> p a b", b=shape[1])
        elif len(shape) == 3:
            v = v.rearrange("p (a b c) -> p a b c", b=shape[1], c=shape[2])
        return v

    def mark(self):
        return self.off

    def reset(self, to):
        self.off = to


def build_program(debug=False, stop_after=None):
    nc = bass.Bass("TRN2", target_bir_lowering=False)
    kin = "ExternalInput"
    dt_ = lambda name, shape, dtype, kind: nc.dram_tensor(name, shape, dtype, kind=kind).ap()
    x_in = dt_("x", [NB, S, D], F32, kin)
    ctx_in = dt_("ctx", [NB, LC, D], F32, kin)
    c_fm = dt_("c_fm", [128, KC, 3], F32, kin)
    w_mod = dt_("w_mod", [DEPTH, D, 6 * D], F32, kin)
    b_mod_fm = dt_("b_mod_fm", [DEPTH, 128, 48], F32, kin)
    w_in = dt_("w_in", [DEPTH, D, 2560], F32, kin)
    w_uT = dt_("w_uT", [DEPTH, 256, D], F32, kin)
    w_f = dt_("w_f", [DEPTH, 256, 64], F32, kin)
    lam_qk = dt_("lam_qk", [DEPTH, 1, 256], F32, kin)
    subln_g = dt_("subln_g", [DEPTH, 1, 128], F32, kin)
    w_out = dt_("w_out", [DEPTH, D, D], F32, kin)
    ln_vecs = dt_("ln_vecs", [DEPTH, 4, 1, D], F32, kin)
    w_router = dt_("w_router", [D, NE], F32, kin)
    router_bias = dt_("router_bias", [1, NE], F32, kin)
    w_gate = dt_("w_gate", [DEPTH, NE, D, 512], F32, kin)
    w_up = dt_("w_up", [DEPTH, NE, D, 512], F32, kin)
    w_down = dt_("w_down", [DEPTH, NE, 512, D], F32, kin)
    dftc = dt_("dftc", [S, S], BF16, kin)
    dftns = dt_("dftns", [S, S], BF16, kin)
    dft256 = dt_("dft256", [2, LC, LC], BF16, kin)
    c64bd = dt_("c64bd", [2, 128, 128], F32, kin)
    rope_cs = dt_("rope_cs", [2, 128, S], F32, kin)
    rmat_d = dt_("rmat", [128, 128], BF16, kin)
    ident_d = dt_("ident", [128, 128], BF16, kin)
    identf_d = dt_("identf", [128, 128], F32, kin)
    uts_d = dt_("uts", [128, 128], F32, kin)
    ebase_d = dt_("ebase", [1, NE], F32, kin)
    iota_d = dt_("iota_p", [128, NE], F32, kin)
    out_d = dt_("out", [NB, S, D], F32, "ExternalOutput")
    skind = "ExternalOutput" if debug else "Internal"
    QT_s = dt_("QT_s", [NB, 128, 6, TT], BF16, skind)
    KT_s = dt_("KT_s", [NB, 128, 6, TT], BF16, skind)
    VAB_s = dt_("VAB_s", [NB, TT, 1280], BF16, skind)
    X1_s = dt_("X1_s", [NB, TT, D], F32, skind)
    X2_s = dt_("X2_s", [NB, TT, D], F32, skind)
    HG_s = dt_("HG_s", [NSLOT, D], BF16, "Internal")
    Y_s = [dt_("Y_s%d" % i, [NSLOT, 512], F32, "Internal") for i in range(2)]
    MIX_s = dt_("MIX_s", [NB, 128, 8, TT], BF16, skind) if debug else None
    DBG_s = dt_("DBG_s", [128, 4096], F32, skind) if debug else None
    DBG2_s = dt_("DBG2_s", [3, 128, 512], F32, skind) if debug else None

    st = ExitStack()
    arena_t = st.enter_context(nc.sbuf_tensor("arena", [128, ARENA_BYTES // 2], BF16))
    psum_t = st.enter_context(nc.psum_tensor("psum", [128, 4096], F32))
    P = Prog(nc)
    AR = Arena(arena_t[:, :])

    def bank(b, n=1):
        return psum_t[:, b * 512:(b + n) * 512]

    rp = [P.R("bank%d" % i) for i in range(8)]

    ident = AR.alloc([128], BF16)
    identf = AR.alloc([128], F32)
    onesf = AR.alloc([128], F32)
    rmat = AR.alloc([128], BF16)
    modT = AR.alloc([48, 3], F32)
    s1p_a = AR.alloc([KC, 3], F32)
    s1p_f = AR.alloc([KC, 3], F32)
    nlam = AR.alloc([1], F32)
    gsub = AR.alloc([128], F32)
    rbias = AR.alloc([NE], F32)
    wr_sb = AR.alloc([KC, NE], BF16)
    r_const = P.R("const")
    r_mod = P.R("mod")
    P.op("sp", dma(ident, ident_d), writes=[r_const], dma=True)
    P.op("sp", dma(identf, identf_d), writes=[r_const], dma=True)
    P.op("sp", dma(rmat, rmat_d), writes=[r_const], dma=True)
    P.op("sp", dma(rbias, router_bias.partition_broadcast(128)), writes=[r_const], dma=True)
    P.op("pool", dma(wr_sb, w_router.rearrange("(kc p) e -> p kc e", p=128)), writes=[r_const], dma=True)
    P.op("dve", memset(onesf, 1.0), writes=[r_const])
    P.barrier()
    PERSIST = AR.mark()

    def ln_stats(xt_ap, r_x, sm, r_sm):
        P.op("dve", lambda e: e.bn_stats(out=sm[:, 0:6], in_=xt_ap[:, 0:512]), reads=[r_x], writes=[r_sm])
        P.op("dve", lambda e: e.bn_stats(out=sm[:, 6:12], in_=xt_ap[:, 512:1024]), reads=[r_x], writes=[r_sm])
        P.op("dve", lambda e: e.bn_aggr(out=sm[:, 12:14], in_=sm[:, 0:12].rearrange("p (a b) -> p a b", b=6)),
             reads=[r_sm], writes=[r_sm])
        P.op("act", act(sm[:, 14:15], sm[:, 13:14], AF.Ln, bias=LN_EPS, scale=1.0), reads=[r_sm], writes=[r_sm])
        P.op("act", act(sm[:, 15:16], sm[:, 14:15], AF.Exp, scale=-0.5), reads=[r_sm], writes=[r_sm])
        P.op("dve", stt(sm[:, 16:17], sm[:, 12:13], -1.0, sm[:, 15:16], ALU.mult, ALU.mult),
             reads=[r_sm], writes=[r_sm])
        return sm[:, 15:16], sm[:, 16:17]

    def make_bcast(dst, r_dst, src_fm, diag, r_diag, pbank):
        for c in range(KC):
            P.op("dve", ts(diag, identf, src_fm[:, c:c + 1], ALU.mult), reads=[r_const, r_mod], writes=[r_diag])
            half, cc = c // 4, c % 4
            P.op("pe", mm(bank(pbank + half)[:, cc * 128:(cc + 1) * 128], onesf, diag, True, True),
                 reads=[r_const, r_diag], writes=[rp[pbank + half]])
        P.op("dve", tcopy(dst, bank(pbank, 2)), reads=[rp[pbank], rp[pbank + 1]], writes=[r_dst])

    _breg = {}

    def bound_reg(eng):
        if "r" not in _breg:
            _breg["r"] = eng.alloc_register("slot_bound")
            eng.reg_mov(_breg["r"], NSLOT - 1)
        return _breg["r"]

    def tile_src(l, b, t):
        if l == 0:
            if t < 2:
                return ctx_in[b, t * 128:(t + 1) * 128, :]
            return x_in[b, (t - 2) * 128:(t - 1) * 128, :]
        return X2_s[b, t * 128:(t + 1) * 128, :]

    for l in range(DEPTH):
        last = (l == DEPTH - 1)
        lam_init = 0.8 - 0.6 * math.exp(-0.3 * l)
        tiles_q = list(range(2, NT)) if last else list(range(NT))
        AR.reset(PERSIST)
        cfm = AR.alloc([KC, 3], F32)
        silu_c = AR.alloc([KC, 3], F32)
        bmod = AR.alloc([48], F32)
        lqb = AR.alloc([256], F32)
        junk = AR.alloc([64], F32)
        s12 = AR.alloc([4], F32)
        wm_ring = Ring([Buf(AR.alloc([KC, 1024], F32), P.R()) for _ in range(2)])
        r_c, r_s, r_b, r_l, r_j = P.R(), P.R(), P.R(), P.R(), P.R()
        P.op("sp", dma(cfm, c_fm), writes=[r_c], dma=True)
        P.op("sp", dma(bmod, b_mod_fm[l]), writes=[r_b], dma=True)
        P.op("sp", dma(lqb, lam_qk[l].partition_broadcast(128)), writes=[r_l], dma=True)
        P.op("sp", dma(gsub, subln_g[l].partition_broadcast(128)), writes=[r_mod], dma=True)
        P.op("act", act(silu_c, cfm, AF.Silu), reads=[r_c], writes=[r_s])
        psm = bank(0)[:, 0:144].rearrange("p (a b) -> p a b", b=3)
        for cg in range(6):
            wb = wm_ring.next()
            for kc in range(KC):
                P.op("sp", dma(wb.ap[:, kc, :], w_mod[l, kc * 128:(kc + 1) * 128, cg * 1024:(cg + 1) * 1024]),
                     writes=[wb.res], dma=True)
            for j in range(8):
                for kc in range(KC):
                    P.op("pe", mm(psm[:, cg * 8 + j, :], wb.ap[:, kc, j * 128:(j + 1) * 128], silu_c[:, kc, :],
                                  kc == 0, kc == KC - 1), reads=[wb.res, r_s], writes=[rp[0]])
        for j in range(3):
            P.op("dve", tt(modT[:, :, j], psm[:, :, j], bmod, ALU.add), reads=[rp[0], r_b], writes=[r_mod])
        P.op("dve", ts(s1p_a, modT[:, 8:16, :], 1.0, ALU.add), reads=[r_mod], writes=[r_mod])
        P.op("dve", ts(s1p_f, modT[:, 32:40, :], 1.0, ALU.add), reads=[r_mod], writes=[r_mod])
        P.op("dve", memset(s12, 0.0), writes=[r_j])
        P.op("dve", stt(junk, lqb[:, 0:64], 1.0, lqb[:, 64:128], ALU.mult, ALU.mult, accum_out=s12[:, 0:1]),
             reads=[r_l], writes=[r_j])
        P.op("dve", stt(junk, lqb[:, 128:192], 1.0, lqb[:, 192:256], ALU.mult, ALU.mult, accum_out=s12[:, 1:2]),
             reads=[r_l], writes=[r_j])
        P.op("act", act(s12[:, 2:4], s12[:, 0:2], AF.Exp), reads=[r_j], writes=[r_j])
        P.op("dve", tt(nlam, s12[:, 3:4], s12[:, 2:3], ALU.subtract), reads=[r_j], writes=[r_mod])
        P.op("dve", ts(nlam, nlam, -lam_init, ALU.add), reads=[r_mod], writes=[r_mod])
        P.op("dve", ts(gsub, gsub, 1.0 - lam_init, ALU.mult), reads=[r_mod], writes=[r_mod])
        P.barrier()
        sh_a = modT[:, 0:8, :]
        g_a = modT[:, 16:24, :]
        sh_f = modT[:, 24:32, :]
        g_f = modT[:, 40:48, :]

        import os as _os
        _skipmix = _os.environ.get("SKIP_MIX") is not None
        AR.reset(PERSIST)
        w_sb = AR.alloc([KC, WCOLS], BF16)
        r_w = P.R("w_in")
        cs_sb = AR.alloc([2, S], F32)
        r_cs = P.R()
        xt_ring = Ring([Buf(AR.alloc([D], F32), P.R()) for _ in range(2)])
        xn_ring = Ring([Buf(AR.alloc([D], BF16), P.R()) for _ in range(2)])
        sm_ring = Ring([Buf(AR.alloc([24], F32), P.R()) for _ in range(2)])
        hT_ring = Ring([Buf(AR.alloc([KC, 512], BF16), P.R()) for _ in range(2)])
        pl_ring = Ring([Buf(AR.alloc([512], BF16), P.R()) for _ in range(2)])
        t1_ring = Ring([Buf(AR.alloc([512], F32), P.R()) for _ in range(2)])
        t2_ring = Ring([Buf(AR.alloc([512], F32), P.R()) for _ in range(2)])
        qk_ring = Ring([Buf(AR.alloc([12, 512], BF16), P.R()) for _ in range(2)])
        vab_ring = Ring([Buf(AR.alloc([1280], BF16), P.R()) for _ in range(2)])
        c64 = AR.alloc([2, 128], F32)
        wf_sb = AR.alloc([2, 64], F32)
        bd = AR.alloc([4, 128], BF16)
        wuT = AR.alloc([2, D], BF16)
        r_a, r_bd = P.R(), P.R()
        P.op("sp", dma(c64, c64bd.rearrange("a p n -> p a n")), writes=[r_a], dma=True)
        P.op("sp", dma(wf_sb, w_f[l].rearrange("(j p) d -> p j d", p=128)), writes=[r_a], dma=True)
        P.op("pool", dma(wuT, w_uT[l].rearrange("(j p) k -> p j k", p=128)), writes=[r_a], dma=True)
        P.op("sp", dma(cs_sb, rope_cs.rearrange("a p n -> p a n")), writes=[r_cs], dma=True)
        for kc in range(KC):
            P.op("pool", dma(w_sb[:, kc, 512:WCOLS], w_in[l, kc * 128:(kc + 1) * 128, 256:2560]),
                 writes=[r_w], dma=True)
        P.op("dve", memset(bd, 0.0), writes=[r_bd])
        for cs in range(2):
            for j in range(2):
                idx = cs * 2 + j
                P.op("pe", mm(bank(0)[:, idx * 64:(idx + 1) * 64], c64[:, cs, :], wf_sb[:, j, :], True, True),
                     reads=[r_a], writes=[rp[0]])
        for idx in range(4):
            P.op("dve", tcopy(bd[0:64, idx, 0:64], bank(0)[0:64, idx * 64:(idx + 1) * 64]),
                 reads=[rp[0]], writes=[r_bd])
            P.op("dve", tcopy(bd[64:128, idx, 64:128], bank(0)[64:128, idx * 64:(idx + 1) * 64]),
                 reads=[rp[0]], writes=[r_bd])
        for kc in range(KC):
            pb = 2 + (kc % 2)
            for cs in range(2):
                for j in range(2):
                    idx = cs * 2 + j
                    P.op("pe", mm(bank(pb)[:, idx * 128:(idx + 1) * 128], wuT[:, j, kc * 128:(kc + 1) * 128],
                                  bd[:, idx, :], True, True), reads=[r_a, r_bd], writes=[rp[pb]])
            P.op("dve", tcopy(w_sb[:, kc, 0:512], bank(pb)), reads=[rp[pb]], writes=[r_w])

        blocks = []
        for b in range(NB):
            blocks.append((b, 0, 2))
            for i in range(4):
                blocks.append((b, 2 + 4 * i, 4))
        psT_i = [0]
        fm_i = [0]
        tm_i = [0]
        rot_i = [0]

        def p1_A(blk):
            b, t0, ntl = blk
            hb = hT_ring.next()
            for ti in range(ntl):
                t = t0 + ti
                j = 2 if t < 2 else b
                xb_, xnb, smb = xt_ring.next(), xn_ring.next(), sm_ring.next()
                P.op("sp", dma(xb_.ap, tile_src(l, b, t)), writes=[xb_.res], dma=True)
                rstd, nmr = ln_stats(xb_.ap, xb_.res, smb.ap, smb.res)
                P.op("act", act(xnb.ap, xb_.ap, AF.Identity, bias=nmr, scale=rstd),
                     reads=[xb_.res, smb.res], writes=[xnb.res])
                pb = psT_i[0] % 2
                psT_i[0] += 1
                psT = bank(pb).bitcast(BF16).rearrange("p (a b) -> p a b", b=128)
                for kc in range(KC):
                    P.op("pe", tr(psT[:, kc, :], xnb.ap[:, kc * 128:(kc + 1) * 128], ident),
                         reads=[xnb.res, r_const], writes=[rp[pb]])
                for kc in range(KC):
                    P.op("dve", ts(hb.ap[:, kc, ti * 128:(ti + 1) * 128], psT[:, kc, :], s1p_a[:, kc, j:j + 1],
                                   ALU.mult, sh_a[:, kc, j:j + 1], ALU.add),
                         reads=[rp[pb], r_mod], writes=[hb.res])
            return hb

        def p1_B(blk, hb):
            b, t0, ntl = blk
            n = ntl * 128
            is_ctx = t0 < 2
            tok0 = t0 * 128
            qb = qk_ring.next()
            for c in range(12):
                pb = 2 + fm_i[0] % 2
                fm_i[0] += 1
                col = 512 + c * 128
                for kc in range(KC):
                    P.op("pe", mm(bank(pb)[:, 0:n], w_sb[:, kc, col:col + 128], hb.ap[:, kc, 0:n],
                                  kc == 0, kc == KC - 1), reads=[r_w, hb.res], writes=[rp[pb]])
                if is_ctx:
                    P.op("act", tcopy_act(qb.ap[:, c, 0:n], bank(pb)[:, 0:n]), reads=[rp[pb]], writes=[qb.res])
                else:
                    pl, t1, t2 = pl_ring.next(), t1_ring.next(), t2_ring.next()
                    P.op("act", tcopy_act(pl.ap[:, 0:n], bank(pb)[:, 0:n]), reads=[rp[pb]], writes=[pl.res])
                    prb = 4 + rot_i[0] % 2
                    rot_i[0] += 1
                    P.op("pe", mm(bank(prb)[:, 0:n], rmat, pl.ap[:, 0:n], True, True),
                         reads=[pl.res, r_const], writes=[rp[prb]])
                    s0 = tok0 - LC
                    P.op("dve", tt(t1.ap[:, 0:n], pl.ap[:, 0:n], cs_sb[:, 0, s0:s0 + n], ALU.mult),
                         reads=[pl.res, r_cs], writes=[t1.res])
                    P.op("dve", tt(t2.ap[:, 0:n], bank(prb)[:, 0:n], cs_sb[:, 1, s0:s0 + n], ALU.mult),
                         reads=[rp[prb], r_cs], writes=[t2.res])
                    P.op("dve", tt(qb.ap[:, c, 0:n], t1.ap[:, 0:n], t2.ap[:, 0:n], ALU.add),
                         reads=[t1.res, t2.res], writes=[qb.res])
            P.op("pool", dma(QT_s[b, :, :, tok0:tok0 + n], qb.ap[:, 0:6, 0:n]), reads=[qb.res], dma=True)
            P.op("pool", dma(KT_s[b, :, :, tok0:tok0 + n], qb.ap[:, 6:12, 0:n]), reads=[qb.res], dma=True)
            for ti in range(ntl):
                vb = vab_ring.next()
                for (c0, c1, wc0) in ((0, 512, 0), (512, 1024, 2048), (1024, 1280, 2560)):
                    pb = 6 + tm_i[0] % 2
                    tm_i[0] += 1
                    w_ = c1 - c0
                    for kc in range(KC):
                        P.op("pe", mm(bank(pb)[:, 0:w_], hb.ap[:, kc, ti * 128:(ti + 1) * 128],
                                      w_sb[:, kc, wc0:wc0 + w_], kc == 0, kc == KC - 1),
                             reads=[r_w, hb.res], writes=[rp[pb]])
                    P.op("act", tcopy_act(vb.ap[:, c0:c1], bank(pb)[:, 0:w_]), reads=[rp[pb]], writes=[vb.res])
                r0 = tok0 + ti * 128
                P.op("pool", dma(VAB_s[b, r0:r0 + 128, :], vb.ap), reads=[vb.res], dma=True)

        def tcopy_act(out, in_):
            return lambda e: e.copy(out=out, in_=in_)

        prev = None
        for i in range(0 if _skipmix else len(blocks) + 1):
            cur = None
            if i < len(blocks):
                cur = (blocks[i], p1_A(blocks[i]))
            if prev is not None:
                p1_B(*prev)
            prev = cur
        P.barrier()
        if stop_after == ("P1", l):
            break

        for b in range(0 if _skipmix else NB):
            AR.reset(PERSIST)
            fT = AR.alloc([2, TT], BF16)
            mixA = AR.alloc([6, TT], BF16)
            r_fT, r_mix = P.R(), P.R()
            SUB = AR.mark()
            ab_sb = AR.alloc([NT, 512], BF16)
            d256 = AR.alloc([2, 2, LC], BF16)
            tb_ring = Ring([Buf(AR.alloc([16, 512], BF16), P.R()) for _ in range(2)])
            r_ab, r_d = P.R(), P.R()
            P.op("sp", dma(ab_sb, VAB_s[b, :, 0:512].rearrange("(t p) n -> p t n", p=128)), writes=[r_ab], dma=True)
            if not last:
                for cs in range(2):
                    P.op("sp", dma(d256[:, :, cs, :], dft256[cs].rearrange("(tc p) n -> p tc n", p=128)),
                         writes=[r_d], dma=True)
                for j in range(2):
                    k = 0
                    for cs in range(2):
                        for tc in range(2):
                            P.op("pe", mm(bank(j)[:, 0:LC], ab_sb[:, tc, cs * 256 + j * 128: cs * 256 + (j + 1) * 128],
                                          d256[:, tc, cs, :], k == 0, k == 3), reads=[r_ab, r_d], writes=[rp[j]])
                            k += 1
                    P.op("act", tcopy_act(fT[:, j, 0:LC], bank(j)[:, 0:LC]), reads=[rp[j]], writes=[r_fT])
            for tb in range(4):
                for cs in range(2):
                    tbuf = tb_ring.next()
                    src = (dftc if cs == 0 else dftns)[:, tb * 512:(tb + 1) * 512].rearrange("(tc p) n -> p tc n", p=128)
                    for hh in range(2):
                        P.op("sp", dma(tbuf.ap[:, hh * 8:(hh + 1) * 8, :], src[:, hh * 8:(hh + 1) * 8, :]),
                             writes=[tbuf.res], dma=True)
                    for j in range(2):
                        pb = 2 + (tb % 2) * 2 + j
                        for tc in range(16):
                            P.op("pe", mm(bank(pb), ab_sb[:, 2 + tc, cs * 256 + j * 128: cs * 256 + (j + 1) * 128],
                                          tbuf.ap[:, tc, :], cs == 0 and tc == 0, cs == 1 and tc == 15),
                                 reads=[r_ab, tbuf.res], writes=[rp[pb]])
                for j in range(2):
                    pb = 2 + (tb % 2) * 2 + j
                    P.op("act", tcopy_act(fT[:, j, LC + tb * 512: LC + (tb + 1) * 512], bank(pb)),
                         reads=[rp[pb]], writes=[r_fT])
            if debug:
                P.op("sp", dma(MIX_s[b, :, 0:2, :], fT), reads=[r_fT], dma=True)
            P.barrier()

            AR.reset(SUB)
            qT = AR.alloc([6, TT], BF16)
            kT = AR.alloc([6, TT], BF16)
            vaug = AR.alloc([NT, 6, VST], BF16)
            r_q, r_k, r_v = P.R(), P.R(), P.R()
            PT_ring = Ring([Buf(AR.alloc([NT, 512], BF16), P.R()) for _ in range(2)])
            tq_ring = Ring([Buf(AR.alloc([4, 128], F32), P.R()) for _ in range(2)])
            o_ring = Ring([Buf(AR.alloc([4, 128], F32), P.R()) for _ in range(2)])
            on_ring = Ring([Buf(AR.alloc([4, 128], BF16), P.R()) for _ in range(2)])
            rs_ring = Ring([Buf(AR.alloc([16], F32), P.R()) for _ in range(4)])
            junk2 = AR.alloc([128], F32)
            r_j2 = P.R()
            P.op("sp", dma(qT, QT_s[b]), writes=[r_q], dma=True)
            P.op("sp", dma(kT, KT_s[b]), writes=[r_k], dma=True)
            P.op("dve", memset(vaug[:, :, :, 128:VST], 1.0), writes=[r_v])
            for h in range(6):
                P.op("sp", dma(vaug[:, :, h, 0:128],
                               VAB_s[b, :, 512 + h * 128: 512 + (h + 1) * 128].rearrange("(t p) d -> p t d", p=128)),
                     writes=[r_v], dma=True)
            units = []
            if not last:
                for h in range(6):
                    for sub in range(2):
                        units.append((0, 2, [0, 1], h, sub))
            for qb_ in range(4):
                for h in range(6):
                    for sub in range(2):
                        units.append((LC + qb_ * 512, 4, list(range(NT)), h, sub))
            sg_i = [0]

            def att_S(u, ui):
                q0, nqt, kts, h, sub = u
                n = nqt * 128
                pt = PT_ring.next()
                p0, p1 = sub * 64, (sub + 1) * 64
                for g in range(0, len(kts), 2):
                    pb = (sg_i[0] % 2) * 2
                    sg_i[0] += 1
                    grp = kts[g:g + 2]
                    for gi, kt in enumerate(grp):
                        P.op("pe", mm(bank(pb + gi)[:, 0:n], kT[p0:p1, h, kt * 128:(kt + 1) * 128],
                                      qT[p0:p1, h, q0:q0 + n], True, True),
                             reads=[r_q, r_k], writes=[rp[pb + gi]])
                    src = bank(pb, 2).rearrange("p (a b) -> p a b", b=512)[:, 0:len(grp), 0:n]
                    P.op("act", act(pt.ap[:, g:g + len(grp), 0:n], src, AF.Exp, scale=0.125),
                         reads=[rp[pb], rp[pb + 1]], writes=[pt.res])
                return pt

            def acc_ap(par, qt):
                if qt < 3:
                    return bank(4 + 2 * par)[:, qt * 160: qt * 160 + 129]
                return bank(5 + 2 * par)[:, 0:129]

            def att_AV(u, ui, pt, state):
                q0, nqt, kts, h, sub = u
                par = ui % 2
                for qt in range(nqt):
                    a = acc_ap(par, qt)
                    for ki, kt in enumerate(kts):
                        P.op("pe", mm(a, pt.ap[:, ki, qt * 128:(qt + 1) * 128], vaug[:, kt, h, 0:129],
                                      ki == 0, ki == len(kts) - 1),
                             reads=[pt.res, r_v], writes=[rp[4 + 2 * par], rp[5 + 2 * par]])
                accr = [rp[4 + 2 * par], rp[5 + 2 * par]]
                rs = rs_ring.next()
                if sub == 0:
                    tq = tq_ring.next()
                    state["tq"] = tq
                    for qt in range(nqt):
                        a = acc_ap(par, qt)
                        P.op("dve", lambda e, o=rs.ap[:, qt:qt + 1], i=a[:, 128:129]: e.reciprocal(out=o, in_=i),
                             reads=accr, writes=[rs.res])
                        P.op("dve", ts(tq.ap[:, qt, :], a[:, 0:128], rs.ap[:, qt:qt + 1], ALU.mult),
                             reads=accr + [rs.res], writes=[tq.res])
                else:
                    tq = state["tq"]
                    ob, onb = o_ring.next(), on_ring.next()
                    P.op("dve", memset(rs.ap[:, 8:12], 0.0), writes=[rs.res])
                    for qt in range(nqt):
                        a = acc_ap(par, qt)
                        P.op("dve", lambda e, o=rs.ap[:, qt:qt + 1], i=a[:, 128:129]: e.reciprocal(out=o, in_=i),
                             reads=accr, writes=[rs.res])
                        P.op("dve", ts(rs.ap[:, 4 + qt:5 + qt], rs.ap[:, qt:qt + 1], nlam[:, 0:1], ALU.mult),
                             reads=[rs.res, r_mod], writes=[rs.res])
                        P.op("dve", stt(ob.ap[:, qt, :], a[:, 0:128], rs.ap[:, 4 + qt:5 + qt], tq.ap[:, qt, :],
                                        ALU.mult, ALU.add), reads=accr + [rs.res, tq.res], writes=[ob.res])
                        P.op("dve", stt(junk2, ob.ap[:, qt, :], 1.0, ob.ap[:, qt, :], ALU.mult, ALU.mult,
                                        accum_out=rs.ap[:, 8 + qt:9 + qt]), reads=[ob.res], writes=[rs.res, r_j2])
                    P.op("act", act(rs.ap[:, 12:12 + nqt], rs.ap[:, 8:8 + nqt], AF.Ln, bias=LN_EPS, scale=1.0 / 128),
                         reads=[rs.res], writes=[rs.res])
                    P.op("act", act(rs.ap[:, 12:12 + nqt], rs.ap[:, 12:12 + nqt], AF.Exp, scale=-0.5),
                         reads=[rs.res], writes=[rs.res])
                    for qt in range(nqt):
                        P.op("dve", stt(onb.ap[:, qt, :], ob.ap[:, qt, :], rs.ap[:, 12 + qt:13 + qt], gsub,
                                        ALU.mult, ALU.mult), reads=[ob.res, rs.res, r_mod], writes=[onb.res])
                    psT = bank(5 + 2 * par)[:, 256:512].bitcast(BF16).rearrange("p (a b) -> p a b", b=128)
                    for qt in range(nqt):
                        P.op("pe", tr(psT[:, qt, :], onb.ap[:, qt, :], ident), reads=[onb.res, r_const],
                             writes=[rp[5 + 2 * par]])
                    P.op("dve", tcopy(mixA[:, h, q0:q0 + nqt * 128].rearrange("p (a b) -> p a b", b=128), psT[:, 0:nqt, :]),
                         reads=[rp[5 + 2 * par]], writes=[r_mix])

            state = {}
            prevu = None
            for ui in range(len(units) + 1):
                curu = None
                if ui < len(units):
                    curu = (units[ui], ui, att_S(units[ui], ui))
                if prevu is not None:
                    att_AV(prevu[0], prevu[1], prevu[2], state)
                prevu = curu
            if debug:
                P.op("sp", dma(MIX_s[b, :, 2:8, :], mixA), reads=[r_mix], dma=True)
            P.barrier()

            AR.reset(SUB)
            wo_sb = AR.alloc([KC, D], BF16)
            gbc = [AR.alloc([D], F32) for _ in range(2)]
            lng = AR.alloc([D], F32)
            lnb = AR.alloc([D], F32)
            diag = AR.alloc([128], F32)
            r_wo, r_g, r_ln, r_dg = P.R(), P.R(), P.R(), P.R()
            xt_ring = Ring([Buf(AR.alloc([D], F32), P.R()) for _ in range(2)])
            z_ring = Ring([Buf(AR.alloc([D], F32), P.R()) for _ in range(2)])
            sm_ring = Ring([Buf(AR.alloc([24], F32), P.R()) for _ in range(2)])
            for kc in range(KC):
                P.op("pool", dma(wo_sb[:, kc, :], w_out[l, kc * 128:(kc + 1) * 128, :]), writes=[r_wo], dma=True)
            P.op("sp", dma(lng, ln_vecs[l, 0].partition_broadcast(128)), writes=[r_ln], dma=True)
            P.op("sp", dma(lnb, ln_vecs[l, 1].partition_broadcast(128)), writes=[r_ln], dma=True)
            make_bcast(gbc[0], r_g, g_a[:, :, b], diag, r_dg, 0)
            if not last:
                make_bcast(gbc[1], r_g, g_a[:, :, 2], diag, r_dg, 0)
            for ti, t in enumerate(tiles_q):
                xb_, zb, smb = xt_ring.next(), z_ring.next(), sm_ring.next()
                P.op("sp", dma(xb_.ap, tile_src(l, b, t)), writes=[xb_.res], dma=True)
                pb = 2 + (ti % 3) * 2
                for half in range(2):
                    for kc in range(KC):
                        lhs = fT[:, kc, t * 128:(t + 1) * 128] if kc < 2 else mixA[:, kc - 2, t * 128:(t + 1) * 128]
                        P.op("pe", mm(bank(pb + half), lhs, wo_sb[:, kc, half * 512:(half + 1) * 512],
                                      kc == 0, kc == KC - 1), reads=[r_fT, r_mix, r_wo], writes=[rp[pb + half]])
                gsel = gbc[1] if t < 2 else gbc[0]
                P.op("dve", tt(zb.ap, bank(pb, 2), gsel, ALU.mult), reads=[rp[pb], rp[pb + 1], r_g], writes=[zb.res])
                P.op("dve", stt(zb.ap, xb_.ap, ALPHA, zb.ap, ALU.mult, ALU.add), reads=[xb_.res, zb.res],
                     writes=[zb.res])
                rstd, nmr = ln_stats(zb.ap, zb.res, smb.ap, smb.res)
                P.op("act", act(xb_.ap, zb.ap, AF.Identity, bias=nmr, scale=rstd), reads=[zb.res, smb.res],
                     writes=[xb_.res])
                P.op("dve", tt(xb_.ap, xb_.ap, lng, ALU.mult), reads=[xb_.res, r_ln], writes=[xb_.res])
                P.op("dve", tt(xb_.ap, xb_.ap, lnb, ALU.add), reads=[xb_.res, r_ln], writes=[xb_.res])
                P.op("pool", dma(X1_s[b, t * 128:(t + 1) * 128, :], xb_.ap), reads=[xb_.res], dma=True)
            P.barrier()
        if stop_after == ("MIX", l):
            break

        tl = [(b, t) for b in range(NB) for t in tiles_q]
        NG = len(tl)
        AR.reset(PERSIST)
        slots_i = AR.alloc([NG, 2], I32)
        wts = AR.alloc([NG, 2], F32)
        cnt_i = AR.alloc([NE], I32)
        r_sl, r_cnt = P.R(), P.R()
        SUB = AR.mark()
        jl = [0, 1] if last else [0, 1, 2]
        s_bc = {j: AR.alloc([D], F32) for j in jl}
        h_bc = {j: AR.alloc([D], F32) for j in jl}
        diag = AR.alloc([128], F32)
        uts = AR.alloc([128], F32)
        ebase = AR.alloc([NE], F32)
        runb = AR.alloc([NE], F32)
        r_bc, r_dg, r_ut, r_run = P.R(), P.R(), P.R(), P.R()
        xt_ring = Ring([Buf(AR.alloc([D], F32), P.R()) for _ in range(2)])
        xn_ring = Ring([Buf(AR.alloc([D], BF16), P.R()) for _ in range(2)])
        hf_ring = Ring([Buf(AR.alloc([D], F32), P.R()) for _ in range(2)])
        h2_ring = Ring([Buf(AR.alloc([D], BF16), P.R()) for _ in range(3)])
        h2T_ring = Ring([Buf(AR.alloc([KC, 128], BF16), P.R()) for _ in range(2)])
        sm_ring = Ring([Buf(AR.alloc([24], F32), P.R()) for _ in range(2)])
        rt_ring = Ring([Buf(AR.alloc([256], F32), P.R()) for _ in range(2)])
        P.op("sp", dma(uts, uts_d), writes=[r_ut], dma=True)
        P.op("sp", dma(ebase, ebase_d.partition_broadcast(128)), writes=[r_ut], dma=True)
        P.op("sp", dma(runb, ebase_d.partition_broadcast(128)), writes=[r_run], dma=True)
        for j in jl:
            make_bcast(s_bc[j], r_bc, s1p_f[:, :, j], diag, r_dg, 6)
            make_bcast(h_bc[j], r_bc, sh_f[:, :, j], diag, r_dg, 6)
        for gt, (b, t) in enumerate(tl):
            j = 2 if t < 2 else b
            xb_, xnb, smb, rt = xt_ring.next(), xn_ring.next(), sm_ring.next(), rt_ring.next()
            hf, h2, h2T = hf_ring.next(), h2_ring.next(), h2T_ring.next()
            P.op("sp", dma(xb_.ap, tile_src(0, b, t) if _skipmix else X1_s[b, t * 128:(t + 1) * 128, :]),
                 writes=[xb_.res], dma=True)
            rstd, nmr = ln_stats(xb_.ap, xb_.res, smb.ap, smb.res)
            P.op("act", act(xnb.ap, xb_.ap, AF.Identity, bias=nmr, scale=rstd),
                 reads=[xb_.res, smb.res], writes=[xnb.res])
            P.op("dve", tt(hf.ap, xnb.ap, s_bc[j], ALU.mult), reads=[xnb.res, r_bc], writes=[hf.res])
            P.op("dve", tt(h2.ap, hf.ap, h_bc[j], ALU.add), reads=[hf.res, r_bc], writes=[h2.res])
            pb = gt % 2
            psT = bank(pb).bitcast(BF16).rearrange("p (a b) -> p a b", b=128)
            for kc in range(KC):
                P.op("pe", tr(psT[:, kc, :], h2.ap[:, kc * 128:(kc + 1) * 128], ident),
                     reads=[h2.res, r_const], writes=[rp[pb]])
            P.op("act", tcopy_act(h2T.ap, psT), reads=[rp[pb]], writes=[h2T.res])
            prb = 2 + gt % 2
            for kc in range(KC):
                P.op("pe", mm(bank(prb)[:, 0:NE], h2T.ap[:, kc, :], wr_sb[:, kc, :], kc == 0, kc == KC - 1),
                     reads=[h2T.res, r_const], writes=[rp[prb]])
            A = rt.ap
            sc, sel, w4, gs, m2, gm, msk, ww, tmp = (A[:, 0:16], A[:, 16:32], A[:, 32:64], A[:, 64:68],
                                                       A[:, 68:72], A[:, 72:76], A[:, 80:96], A[:, 96:112],
                                                       A[:, 112:120])
            cmb, slv, msl, indb, sf = A[:, 128:144], A[:, 144:160], A[:, 160:176], A[:, 176:192], A[:, 192:196]
            P.op("act", act(sc, bank(prb)[:, 0:NE], AF.Exp, scale=-1.0), reads=[rp[prb]], writes=[rt.res])
            rr = [rt.res]
            P.op("dve", ts(sc, sc, 1.0, ALU.add), reads=rr, writes=rr)
            P.op("dve", lambda e, o=sc: e.reciprocal(out=o, in_=o), reads=rr, writes=rr)
            P.op("dve", tt(sel, sc, rbias, ALU.add), reads=rr + [r_const], writes=rr)
            sv = sel.rearrange("p (g e) -> p g e", e=4)
            hi01, lo01, hi23, lo23 = w4[:, 0:4], w4[:, 4:8], w4[:, 8:12], w4[:, 12:16]
            m1, mid, lom = w4[:, 16:20], w4[:, 20:24], w4[:, 24:28]
            P.op("dve", tt(hi01, sv[:, :, 0], sv[:, :, 1], ALU.max), reads=rr, writes=rr)
            P.op("dve", tt(lo01, sv[:, :, 0], sv[:, :, 1], ALU.min), reads=rr, writes=rr)
            P.op("dve", tt(hi23, sv[:, :, 2], sv[:, :, 3], ALU.max), reads=rr, writes=rr)
            P.op("dve", tt(lo23, sv[:, :, 2], sv[:, :, 3], ALU.min), reads=rr, writes=rr)
            P.op("dve", tt(m1, hi01, hi23, ALU.max), reads=rr, writes=rr)
            P.op("dve", tt(mid, hi01, hi23, ALU.min), reads=rr, writes=rr)
            P.op("dve", tt(lom, lo01, lo23, ALU.max), reads=rr, writes=rr)
            P.op("dve", tt(m2, mid, lom, ALU.max), reads=rr, writes=rr)
            P.op("dve", tt(gs, m1, m2, ALU.add), reads=rr, writes=rr)
            P.op("dve", lambda e, o=tmp[:, 0:1], i=gs: e.tensor_reduce(out=o, in_=i, axis=AX.X, op=ALU.max),
                 reads=rr, writes=rr)
            P.op("dve", ts(gm, gs, tmp[:, 0:1], ALU.is_ge), reads=rr, writes=rr)
            mv_ = msk.rearrange("p (g e) -> p g e", e=4)
            for ee in range(4):
                P.op("dve", tt(mv_[:, :, ee], sv[:, :, ee], m2, ALU.is_ge), reads=rr, writes=rr)
                P.op("dve", tt(mv_[:, :, ee], mv_[:, :, ee], gm, ALU.mult), reads=rr, writes=rr)
            P.op("dve", tt(ww, sc, msk, ALU.mult), reads=rr, writes=rr)
            P.op("dve", lambda e, o=tmp[:, 1:2], i=ww: e.tensor_reduce(out=o, in_=i, axis=AX.X, op=ALU.add),
                 reads=rr, writes=rr)
            P.op("dve", lambda e, o=tmp[:, 2:3], i=tmp[:, 1:2]: e.reciprocal(out=o, in_=i), reads=rr, writes=rr)
            P.op("dve", ts(cmb, ww, tmp[:, 2:3], ALU.mult), reads=rr, writes=rr)
            ppb = 4 + gt % 2
            P.op("pe", mm(bank(ppb)[:, 0:NE], uts, msk, True, True), reads=[r_ut, rt.res], writes=[rp[ppb]])
            P.op("pe", mm(bank(ppb)[:, 32:32 + NE], onesf, msk, True, True), reads=[r_const, rt.res],
                 writes=[rp[ppb]])
            P.op("dve", tt(slv, bank(ppb)[:, 0:NE], runb, ALU.add), reads=[rp[ppb], r_run] + rr, writes=rr)
            P.op("dve", tt(runb, runb, bank(ppb)[:, 32:32 + NE], ALU.add), reads=[rp[ppb], r_run], writes=[r_run])
            P.op("dve", tt(msl, slv, msk, ALU.mult), reads=rr, writes=rr)
            P.op("dve", lambda e, o=sf[:, 1:2], i=msl: e.tensor_reduce(out=o, in_=i, axis=AX.X, op=ALU.max),
                 reads=rr, writes=rr)
            P.op("dve", lambda e, o=tmp[:, 3:4], i=msl: e.tensor_reduce(out=o, in_=i, axis=AX.X, op=ALU.add),
                 reads=rr, writes=rr)
            P.op("dve", tt(sf[:, 0:1], tmp[:, 3:4], sf[:, 1:2], ALU.subtract), reads=rr, writes=rr)
            P.op("dve", ts(indb, msl, sf[:, 1:2], ALU.is_equal), reads=rr, writes=rr)
            P.op("dve", tt(indb, indb, cmb, ALU.mult), reads=rr, writes=rr)
            P.op("dve", lambda e, o=wts[:, gt, 1:2], i=indb: e.tensor_reduce(out=o, in_=i, axis=AX.X, op=ALU.add),
                 reads=rr, writes=[r_sl])
            P.op("dve", ts(wts[:, gt, 0:1], wts[:, gt, 1:2], -1.0, ALU.mult, 1.0, ALU.add), reads=[r_sl],
                 writes=[r_sl])
            P.op("dve", tcopy(slots_i[:, gt, :], sf[:, 0:2]), reads=rr, writes=[r_sl])
            for k in range(2):
                P.op("pool", lambda e, off=slots_i[:, gt, k:k + 1], src=h2.ap: e.indirect_dma_start(
                    out=HG_s[:, :], out_offset=bass.IndirectOffsetOnAxis(ap=off, axis=0), in_=src, in_offset=None,
                    bounds_check=bound_reg(e), oob_is_err=False), reads=[r_sl, h2.res], dma=True)
        zt = hf_ring.bufs[0]
        padf = rt_ring.bufs[0]
        P.op("sp", dma(padf.ap[:, 0:NE], iota_d), writes=[padf.res], dma=True)
        P.op("dve", memset(zt.ap.bitcast(BF16)[:, 0:D], 0.0), writes=[zt.res])
        P.op("dve", tt(padf.ap[:, 0:NE], padf.ap[:, 0:NE], runb, ALU.add), reads=[padf.res, r_run], writes=[padf.res])
        pad_i = padf.ap[:, 32:32 + NE].bitcast(I32)
        P.op("dve", tcopy(pad_i, padf.ap[:, 0:NE]), reads=[padf.res], writes=[padf.res])
        for e_ in range(NE):
            P.op("pool", lambda e, off=pad_i[:, e_:e_ + 1], src=zt.ap.bitcast(BF16)[:, 0:D]: e.indirect_dma_start(
                out=HG_s[:, :], out_offset=bass.IndirectOffsetOnAxis(ap=off, axis=0), in_=src, in_offset=None,
                bounds_check=bound_reg(e), oob_is_err=False), reads=[padf.res, zt.res], dma=True)
        P.op("dve", tt(runb, runb, ebase, ALU.subtract), reads=[r_run, r_ut, padf.res], writes=[r_run])
        P.op("dve", tcopy(cnt_i, runb), reads=[r_run], writes=[r_cnt])
        if debug and l == 0:
            P.op("sp", dma(DBG_s[:, 0:NG * 2], wts.rearrange("p a b -> p (a b)")), reads=[r_sl], dma=True)
            P.op("sp", dma(DBG_s[:, 256:256 + NE], runb), reads=[r_run], dma=True)
            P.op("dve", tcopy(xt_ring.bufs[0].ap[:, 0:NG * 2], slots_i.rearrange("p a b -> p (a b)")), reads=[r_sl],
                 writes=[xt_ring.bufs[0].res])
            P.op("sp", dma(DBG_s[:, 512:512 + NG * 2], xt_ring.bufs[0].ap[:, 0:NG * 2]),
                 reads=[xt_ring.bufs[0].res], dma=True)
        P.barrier()
        if stop_after == ("ROUTE", l):
            break
        for e_ in range(NE):
            P.vload("cnt%d_%d" % (l, e_), cnt_i[0:1, e_:e_ + 1], r_cnt, NB * TT)

        AR.reset(SUB)
        w_ring = Ring([Buf((AR.alloc([KC, 512], BF16), AR.alloc([KC, 512], BF16), AR.alloc([4, D], BF16)), P.R())
                       for _ in range(2)])
        hg_ring = Ring([Buf(AR.alloc([D], BF16), P.R()) for _ in range(3)])
        hgT_ring = Ring([Buf(AR.alloc([KC, 128], BF16), P.R()) for _ in range(2)])
        sg_ring = Ring([Buf(AR.alloc([512], F32), P.R()) for _ in range(2)])
        a_ring = Ring([Buf(AR.alloc([512], BF16), P.R()) for _ in range(2)])
        aT_ring = Ring([Buf(AR.alloc([4, 128], BF16), P.R()) for _ in range(2)])
        y_ring = Ring([Buf(AR.alloc([D], F32), P.R()) for _ in range(2)])
        gu_i = [0]

        def moe_GU(e_, jt, wbuf):
            wg, wu, wd = wbuf.ap
            hg, hgT = hg_ring.next(), hgT_ring.next()
            r0 = e_ * CAP + jt * 128
            P.op("sp", dma(hg.ap, HG_s[r0:r0 + 128, :]), writes=[hg.res], dma=True)
            psT = bank(5).bitcast(BF16).rearrange("p (a b) -> p a b", b=128)
            for kc in range(KC):
                P.op("pe", tr(psT[:, kc, :], hg.ap[:, kc * 128:(kc + 1) * 128], ident),
                     reads=[hg.res, r_const], writes=[rp[5]])
            P.op("act", tcopy_act(hgT.ap, psT), reads=[rp[5]], writes=[hgT.res])
            pb = (gu_i[0] % 2) * 2
            gu_i[0] += 1
            for which, wmat in ((0, wg), (1, wu)):
                for kc in range(KC):
                    P.op("pe", mm(bank(pb + which), hgT.ap[:, kc, :], wmat[:, kc, :], kc == 0, kc == KC - 1),
                         reads=[hgT.res, wbuf.res], writes=[rp[pb + which]])
            sg, ab_ = sg_ring.next(), a_ring.next()
            P.op("act", act(sg.ap, bank(pb), AF.Silu), reads=[rp[pb]], writes=[sg.res])
            P.op("dve", tt(ab_.ap, bank(pb + 1), sg.ap, ALU.mult), reads=[rp[pb + 1], sg.res], writes=[ab_.res])
            return ab_

        def moe_D(e_, jt, wbuf, ab_):
            wg, wu, wd = wbuf.ap
            par = jt % 2
            psT = bank(4)[:, par * 256:(par + 1) * 256].bitcast(BF16).rearrange("p (a b) -> p a b", b=128)
            aT, yb = aT_ring.next(), y_ring.next()
            for f in range(4):
                P.op("pe", tr(psT[:, f, :], ab_.ap[:, f * 128:(f + 1) * 128], ident), reads=[ab_.res, r_const],
                     writes=[rp[4]])
            P.op("dve", tcopy(aT.ap, psT), reads=[rp[4]], writes=[aT.res])
            for half in range(2):
                for f in range(4):
                    P.op("pe", mm(bank(6 + half), aT.ap[:, f, :], wd[:, f, half * 512:(half + 1) * 512],
                                  f == 0, f == 3), reads=[aT.res, wbuf.res], writes=[rp[6 + half]])
            P.op("act", tcopy_act(yb.ap, bank(6, 2)), reads=[rp[6], rp[7]], writes=[yb.res])
            r0 = e_ * CAP + jt * 128
            for hf_ in range(2):
                P.op("sp", dma(Y_s[hf_][r0:r0 + 128, :], yb.ap[:, hf_ * 512:(hf_ + 1) * 512]), reads=[yb.res],
                     dma=True)

        import os as _os
        _ne = int(_os.environ.get("MOE_NE", NE))
        _nt = int(_os.environ.get("MOE_NT", NG))
        _nocond = _os.environ.get("MOE_NOCOND") is not None

        class _NoCond:
            def __enter__(s_):
                return None

            def __exit__(s_, *a):
                return False
        for e_ in range(_ne):
            wbuf = w_ring.next()
            wg, wu, wd = wbuf.ap
            for hh in range(2):
                P.op("pool", dma(wg[:, hh * 4:(hh + 1) * 4, :],
                                 w_gate[l, e_, hh * 512:(hh + 1) * 512, :].rearrange("(kc p) f -> p kc f", p=128)),
                     writes=[wbuf.res], dma=True)
                P.op("pool", dma(wu[:, hh * 4:(hh + 1) * 4, :],
                                 w_up[l, e_, hh * 512:(hh + 1) * 512, :].rearrange("(kc p) f -> p kc f", p=128)),
                     writes=[wbuf.res], dma=True)
                P.op("pool", dma(wd[:, hh * 2:(hh + 1) * 2, :],
                                 w_down[l, e_, hh * 256:(hh + 1) * 256, :].rearrange("(kc p) f -> p kc f", p=128)),
                     writes=[wbuf.res], dma=True)
            key = "cnt%d_%d" % (l, e_)
            prevt = None
            for jt in range(_nt + 1):
                curt = None
                if jt < _nt:
                    with (_NoCond() if _nocond else P.cond((key, jt * 128))):
                        curt = (jt, moe_GU(e_, jt, wbuf))
                if prevt is not None:
                    with (_NoCond() if _nocond else P.cond((key, prevt[0] * 128))):
                        moe_D(e_, prevt[0], wbuf, prevt[1])
                prevt = curt
        P.barrier()

        if stop_after == ("EXP", l):
            if debug:
                for i_, jt_ in enumerate((0, 6, 7)):
                    P.op("sp", dma(DBG2_s[i_], Y_s[0][jt_ * 128:(jt_ + 1) * 128, :]), dma=True)
                P.barrier()
            break
        AR.reset(SUB)
        gbc = {j: AR.alloc([D], F32) for j in jl}
        lng = AR.alloc([D], F32)
        lnb = AR.alloc([D], F32)
        diag = AR.alloc([128], F32)
        r_g, r_ln, r_dg = P.R(), P.R(), P.R()
        xt_ring = Ring([Buf(AR.alloc([D], F32), P.R()) for _ in range(2)])
        z_ring = Ring([Buf(AR.alloc([D], F32), P.R()) for _ in range(2)])
        ya_ring = Ring([Buf(AR.alloc([D], F32), P.R()) for _ in range(2)])
        yb_ring = Ring([Buf(AR.alloc([D], F32), P.R()) for _ in range(2)])
        sm_ring = Ring([Buf(AR.alloc([24], F32), P.R()) for _ in range(2)])
        P.op("sp", dma(lng, ln_vecs[l, 2].partition_broadcast(128)), writes=[r_ln], dma=True)
        P.op("sp", dma(lnb, ln_vecs[l, 3].partition_broadcast(128)), writes=[r_ln], dma=True)
        for j in jl:
            make_bcast(gbc[j], r_g, g_f[:, :, j], diag, r_dg, 0)
        for gt, (b, t) in enumerate(tl):
            j = 2 if t < 2 else b
            xb_, zb, smb, ya, yb = xt_ring.next(), z_ring.next(), sm_ring.next(), ya_ring.next(), yb_ring.next()
            P.op("sp", dma(xb_.ap, X1_s[b, t * 128:(t + 1) * 128, :]), writes=[xb_.res], dma=True)
            for k, yy in ((0, ya), (1, yb)):
                for hf_ in range(2):
                    P.op("pool", lambda e, off=slots_i[:, gt, k:k + 1], dst=yy.ap[:, hf_ * 512:(hf_ + 1) * 512],
                         src=Y_s[hf_]: e.indirect_dma_start(
                        out=dst, out_offset=None, in_=src[:, :], in_offset=bass.IndirectOffsetOnAxis(ap=off, axis=0),
                        bounds_check=bound_reg(e), oob_is_err=False), reads=[r_sl], writes=[yy.res], dma=True)
            P.op("dve", ts(zb.ap, ya.ap, wts[:, gt, 0:1], ALU.mult), reads=[ya.res, r_sl], writes=[zb.res])
            P.op("dve", stt(zb.ap, yb.ap, wts[:, gt, 1:2], zb.ap, ALU.mult, ALU.add), reads=[yb.res, r_sl, zb.res],
                 writes=[zb.res])
            P.op("dve", tt(zb.ap, zb.ap, gbc[j], ALU.mult), reads=[zb.res, r_g], writes=[zb.res])
            P.op("dve", stt(zb.ap, xb_.ap, ALPHA, zb.ap, ALU.mult, ALU.add), reads=[xb_.res, zb.res],
                 writes=[zb.res])
            rstd, nmr = ln_stats(zb.ap, zb.res, smb.ap, smb.res)
            P.op("act", act(xb_.ap, zb.ap, AF.Identity, bias=nmr, scale=rstd), reads=[zb.res, smb.res],
                 writes=[xb_.res])
            P.op("dve", tt(xb_.ap, xb_.ap, lng, ALU.mult), reads=[xb_.res, r_ln], writes=[xb_.res])
            P.op("dve", tt(xb_.ap, xb_.ap, lnb, ALU.add), reads=[xb_.res, r_ln], writes=[xb_.res])
            dst = out_d[b, (t - 2) * 128:(t - 1) * 128, :] if last else X2_s[b, t * 128:(t + 1) * 128, :]
            P.op("sp", dma(dst, xb_.ap), reads=[xb_.res], dma=True)
        P.barrier()

    P.barrier()
    P.emit()
    st.close()
    return nc, P


def _consts():
    bf = ml_dtypes.bfloat16
    t = np.arange(S, dtype=np.float64)
    ang = 2.0 * np.pi * ((np.outer(t, t)) % S) / S
    dftc = (np.cos(ang) / math.sqrt(S)).astype(np.float32).astype(bf)
    dftns = (-np.sin(ang) / math.sqrt(S)).astype(np.float32).astype(bf)
    t2 = np.arange(LC, dtype=np.float64)
    a2 = 2.0 * np.pi * ((np.outer(t2, t2)) % LC) / LC
    dft256 = np.stack([np.cos(a2) / math.sqrt(LC), -np.sin(a2) / math.sqrt(LC)]).astype(np.float32).astype(bf)
    c = np.arange(64, dtype=np.float64)
    a3 = 2.0 * np.pi * ((np.outer(c, c)) % 64) / 64
    c64 = np.cos(a3) / 8.0
    s64 = np.sin(a3) / 8.0
    c64bd = np.zeros((2, 128, 128), np.float32)
    for i, m in enumerate((c64, s64)):
        c64bd[i, 0:64, 0:64] = m
        c64bd[i, 64:128, 64:128] = m
    freqs = (10000.0 ** (-np.arange(0, 32, 2, dtype=np.float32) / 32)).astype(np.float32)
    pos = np.arange(S)
    row = (pos // 64).astype(np.float32)
    col = (pos % 64).astype(np.float32)
    ang_row = row[:, None] * freqs
    ang_col = col[:, None] * freqs
    rope = np.zeros((2, 128, S), np.float32)
    for p in range(128):
        d = p % 64
        a = ang_row[:, d % 16] if d < 32 else ang_col[:, d % 16]
        rope[0, p] = np.cos(a)
        rope[1, p] = np.sin(a)
    R = np.zeros((128, 128), np.float32)
    for m in range(128):
        d = m % 64
        if (d % 32) < 16:
            R[m + 16, m] = -1.0
        else:
            R[m - 16, m] = 1.0
    return dict(dftc=dftc, dftns=dftns, dft256=dft256, c64bd=c64bd, rope_cs=rope,
                rmat=R.astype(bf), ident=np.eye(128, dtype=np.float32).astype(bf),
                uts=np.triu(np.ones((128, 128), np.float32), 1),
                ebase=(np.arange(NE, dtype=np.float32) * CAP).reshape(1, NE),
                iota_p=np.repeat(np.arange(128, dtype=np.float32)[:, None], NE, axis=1),
                identf=np.eye(128, dtype=np.float32))


def make_in_maps(inputs, cores=range(N_CORES)):
    f = lambda a: np.ascontiguousarray(np.asarray(a, dtype=np.float32))
    x, c, ctx, c_ctx = f(inputs["x"]), f(inputs["c"]), f(inputs["ctx"]), f(inputs["c_ctx"])
    shared = dict(
        w_mod=f(inputs["w_mod"]),
        b_mod_fm=np.ascontiguousarray(f(inputs["b_mod"]).reshape(DEPTH, 48, 128).transpose(0, 2, 1)),
        w_in=f(inputs["w_in"]),
        w_uT=np.ascontiguousarray(f(inputs["w_in"])[:, :, :256].transpose(0, 2, 1)),
        w_f=f(inputs["w_fourier"]).reshape(DEPTH, 256, 64),
        lam_qk=f(inputs["lam_qk"]).reshape(DEPTH, 1, 256),
        subln_g=f(inputs["subln_g"]).reshape(DEPTH, 1, 128),
        w_out=f(inputs["w_out"]),
        ln_vecs=np.ascontiguousarray(np.stack([f(inputs["ln_attn_g"]), f(inputs["ln_attn_b"]),
                                               f(inputs["ln_ffn_g"]), f(inputs["ln_ffn_b"])], axis=1)
                                     .reshape(DEPTH, 4, 1, D)),
        w_router=f(inputs["w_router"]),
        router_bias=f(inputs["router_bias"]).reshape(1, NE),
        w_gate=f(inputs["w_gate"]), w_up=f(inputs["w_up"]), w_down=f(inputs["w_down"]),
    )
    shared.update(_consts())
    maps = []
    for ci in cores:
        b0 = ci * NB
        cc = np.stack([c[b0], c[b0 + 1], c_ctx], axis=-1)
        c_fm = np.ascontiguousarray(cc.reshape(KC, 128, 3).transpose(1, 0, 2))
        m = dict(shared)
        m["x"] = np.ascontiguousarray(x[b0:b0 + NB])
        m["ctx"] = np.ascontiguousarray(ctx[b0:b0 + NB])
        m["c_fm"] = c_fm
        maps.append(m)
    return maps


_CACHE = {}


def kernel(**inputs):
    if "nc" not in _CACHE:
        _CACHE["nc"] = build_program()[0]
    nc = _CACHE["nc"]
    in_maps = make_in_maps(inputs)
    res = run_bass_kernel_spmd(nc, in_maps, core_ids=list(range(N_CORES)))
    out = np.concatenate([np.asarray(r["out"], dtype=np.float32) for r in res.results], axis=0)
    return out
```

```python
import math
from contextlib import ExitStack

import numpy as np
import ml_dtypes
import concourse.bass as bass
import concourse.mybir as mybir
from concourse.bass_utils import run_bass_kernel_spmd

F32 = mybir.dt.float32
BF16 = mybir.dt.bfloat16
ALU = mybir.AluOpType
AF = mybir.ActivationFunctionType
AX = mybir.AxisListType

N_CORES = 8
NB = 2
S = 2048
LC = 256
TT = S + LC
NT = TT // 128
D = 1024
KC = 8
DEPTH = 2
WCOLS = 2816
ALPHA = (2 * DEPTH) ** 0.25
LN_EPS = 1e-5
NE = 16
VST = 132
CAP = NB * TT + 128
NSLOT = NE * CAP
I32 = mybir.dt.int32

ENGS = ("pe", "act", "dve", "pool", "sp")
DMAQ = ("sp", "pool", "act")


class Res:
    __slots__ = ("name", "w", "r")

    def __init__(self, name=""):
        self.name = name
        self.w = None
        self.r = []


class Prog:
    def __init__(self, nc, nq=16):
        self.nc = nc
        self.NQ = nq
        self.ops = {e: [] for e in ENGS}
        self.waited = {e: {} for e in ENGS}
        self.dma_n = {q: 0 for q in DMAQ}
        self.res = []
        self.cur_cond = None
        self.vload_specs = {}

    def cond(self, key):
        prog = self

        class _C:
            def __enter__(s_):
                assert prog.cur_cond is None
                prog.cur_cond = key
                prog._saved_waited = {e: dict(prog.waited[e]) for e in ENGS}

            def __exit__(s_, *a):
                prog.cur_cond = None
                prog.waited = prog._saved_waited
                return False
        return _C()

    def vload(self, name, ap, res, max_val):
        self.vload_specs[name] = (ap, max_val)
        for e in ENGS:
            self.op(e, ("vload", name), reads=[res])

    def R(self, name=""):
        r = Res(name)
        self.res.append(r)
        return r

    def _add_wait(self, eng, o, d, raw):
        if d[0] == "c":
            _, e2, idx = d
            if e2 == eng and (eng == "pe" or not raw):
                return
            key = ("c", e2)
            if self.waited[eng].get(key, -1) >= idx:
                return
            self.waited[eng][key] = idx
            self.ops[e2][idx]["signal"] = True
            o["waits"].append(d)
        else:
            _, q, slot, cnt = d
            key = ("d", q, slot)
            if self.waited[eng].get(key, 0) >= cnt:
                return
            self.waited[eng][key] = cnt
            o["waits"].append(d)

    def op(self, eng, fn, reads=(), writes=(), dma=False):
        ops = self.ops[eng]
        idx = len(ops)
        o = dict(fn=fn, waits=[], signal=False, dma=None, cond=self.cur_cond)
        raw_deps = []
        oth_deps = []
        for r in reads:
            if r.w is not None:
                raw_deps.append(r.w)
        for r in writes:
            if r.w is not None:
                oth_deps.append(r.w)
            oth_deps.extend(r.r)
        if dma:
            n = self.dma_n[eng]
            slot = n % self.NQ
            cnt = 16 * (n // self.NQ + 1)
            self.dma_n[eng] += 1
            if n >= self.NQ:
                oth_deps.append(("d", eng, slot, cnt - 16))
            ev = ("d", eng, slot, cnt)
            o["dma"] = (slot, cnt)
        else:
            ev = ("c", eng, idx)
        best = {}
        for lst, raw in ((raw_deps, True), (oth_deps, False)):
            for d in lst:
                if d[0] == "c":
                    if d[1] == eng and (eng == "pe" or not raw):
                        continue
                    k = ("c", d[1])
                    if k not in best or best[k][2] < d[2]:
                        best[k] = d
                else:
                    k = ("d", d[1], d[2])
                    if k not in best or best[k][3] < d[3]:
                        best[k] = d
        for d in best.values():
            self._add_wait(eng, o, d, True)
        ops.append(o)
        for r in reads:
            r.r.append(ev)
        for r in writes:
            r.w = ev
            r.r = []
        return ev

    def barrier(self):
        evs = []
        for e in ENGS:
            for idx in range(len(self.ops[e]) - 1, -1, -1):
                o = self.ops[e][idx]
                if o["fn"] is not None and o["dma"] is None:
                    evs.append(("c", e, idx))
                    break
        for q in DMAQ:
            n = self.dma_n[q]
            for slot in range(min(n, self.NQ)):
                last_n = ((n - 1 - slot) // self.NQ) * self.NQ + slot
                evs.append(("d", q, slot, 16 * (last_n // self.NQ + 1)))
        for e in ENGS:
            assert self.cur_cond is None
            o = dict(fn=None, waits=[], signal=False, dma=None, cond=None)
            for d in evs:
                if d[0] == "c" and d[1] == e:
                    continue
                self._add_wait(e, o, d, True)
            self.ops[e].append(o)
        for r in self.res:
            r.w = None
            r.r = []

    def emit(self):
        nc = self.nc
        ranks = {}
        for e in ENGS:
            c = 0
            rk = []
            for o in self.ops[e]:
                if o["signal"]:
                    c += 1
                rk.append(c)
            ranks[e] = rk
        self.n_signal = {e: (ranks[e][-1] if ranks[e] else 0) for e in ENGS}
        self.n_ops = {e: len(self.ops[e]) for e in ENGS}
        with ExitStack() as st:
            csem = {e: st.enter_context(nc.semaphore("c_" + e)) for e in ENGS}
            dsem = {q: [st.enter_context(nc.semaphore("d_%s_%d" % (q, i))) for i in range(self.NQ)]
                    for q in DMAQ}
            block = st.enter_context(nc.Block())

            def run(e):
                def emit_waits(eng, o):
                    for d in o["waits"]:
                        if d[0] == "c":
                            eng.wait_ge(csem[d[1]], ranks[d[1]][d[2]])
                        else:
                            eng.wait_ge(dsem[d[1]][d[2]], d[3])

                def emit_op(eng, o, vals):
                    emit_waits(eng, o)
                    fn = o["fn"]
                    if fn is None:
                        return
                    if isinstance(fn, tuple) and fn[0] == "vload":
                        ap, mx = self.vload_specs[fn[1]]
                        vals[fn[1]] = eng.value_load(ap)
                        if o["signal"]:
                            eng.sem_inc(csem[e], 1)
                        return
                    ins = fn(eng)
                    if o["dma"] is not None:
                        ins.then_inc(dsem[e][o["dma"][0]], 16)
                    elif o["signal"]:
                        ins.then_inc(csem[e], 1)

                import os as _os2
                _cond_eng = _os2.environ.get("COND_ENG", "pe,act,dve,pool,sp").split(",")

                dma_before = []
                _cur = {}
                for o_ in self.ops[e]:
                    dma_before.append(dict(_cur))
                    if o_["dma"] is not None:
                        _cur[o_["dma"][0]] = o_["dma"][1]

                def body(eng):
                    vals = {}
                    ops = self.ops[e]
                    i = 0
                    n = len(ops)
                    while i < n:
                        o = ops[i]
                        if o["cond"] is None or e not in _cond_eng:
                            emit_op(eng, o, vals)
                            i += 1
                            continue
                        key = o["cond"]
                        j = i
                        while j < n and ops[j]["cond"] == key:
                            j += 1
                        grp = ops[i:j]
                        rank_before = ranks[e][i - 1] if i > 0 else 0
                        nsig = sum(1 for g in grp if g["signal"])
                        dmas = [g["dma"] for g in grp if g["dma"] is not None]
                        with eng.If(vals[key[0]] > key[1]):
                            for g in grp:
                                emit_op(eng, g, vals)
                        if nsig or dmas:
                            with eng.Else():
                                if nsig:
                                    if rank_before > 0:
                                        eng.wait_ge(csem[e], rank_before)
                                    for _ in range(nsig):
                                        eng.sem_inc(csem[e], 1)
                                if dmas:
                                    for slot in range(self.NQ):
                                        c0 = dma_before[i].get(slot, 0)
                                        if c0 > 0:
                                            eng.wait_ge(dsem[e][slot], c0)
                                    for (slot, cnt) in dmas:
                                        eng.sem_inc(dsem[e][slot], 16)
                        i = j
                return body

            block.tensor(run("pe"))
            block.scalar(run("act"))
            block.vector(run("dve"))
            block.gpsimd(run("pool"))
            block.sync(run("sp"))


class Buf:
    __slots__ = ("ap", "res")

    def __init__(self, ap, res):
        self.ap = ap
        self.res = res


class Ring:
    def __init__(self, bufs):
        self.bufs = bufs
        self.i = 0

    def next(self):
        b = self.bufs[self.i % len(self.bufs)]
        self.i += 1
        return b


def mm(out, lhsT, rhs, start, stop):
    return lambda e: e.matmul(out, lhsT=lhsT, rhs=rhs, start=start, stop=stop)


def tr(out, in_, ident):
    return lambda e: e.transpose(out=out, in_=in_, identity=ident)


def dma(out, in_):
    return lambda e: e.dma_start(out=out, in_=in_)


def act(out, in_, func, bias=0.0, scale=1.0):
    return lambda e: e.activation(out=out, in_=in_, func=func, bias=bias, scale=scale)


def tcopy(out, in_):
    return lambda e: e.tensor_copy(out=out, in_=in_)


def tt(out, in0, in1, op):
    return lambda e: e.tensor_tensor(out=out, in0=in0, in1=in1, op=op)


def ts(out, in0, s1, op0, s2=None, op1=None):
    if op1 is None:
        return lambda e: e.tensor_scalar(out=out, in0=in0, scalar1=s1, scalar2=None, op0=op0)
    return lambda e: e.tensor_scalar(out=out, in0=in0, scalar1=s1, scalar2=s2, op0=op0, op1=op1)


def stt(out, in0, scalar, in1, op0, op1, accum_out=None):
    if accum_out is None:
        return lambda e: e.scalar_tensor_tensor(out=out, in0=in0, scalar=scalar, in1=in1, op0=op0, op1=op1)
    return lambda e: e.scalar_tensor_tensor(out=out, in0=in0, scalar=scalar, in1=in1, op0=op0, op1=op1,
                                            accum_out=accum_out)


def memset(ap, v):
    return lambda e: e.memset(ap, v)


ARENA_BYTES = 206 * 1024


class Arena:
    def __init__(self, ap_bf16):
        self.base = ap_bf16
        self.off = 0
        self.floor = 0

    def alloc(self, shape, dtype):
        n = 1
        for s in shape:
            n *= s
        nbytes = n * (2 if dtype == BF16 else 4)
        nbytes_al = (nbytes + 63) // 64 * 64
        assert self.off + nbytes_al <= ARENA_BYTES, ("SBUF arena overflow", self.off, nbytes_al)
        v = self.base[:, self.off // 2:(self.off + nbytes) // 2]
        self.off += nbytes_al
        if dtype != BF16:
            v = v.bitcast(dtype)
        if len(shape) == 2:
            v = v.rearrange("p (a b) -> p a b", b=shape[1])
        elif len(shape) == 3:
            v = v.rearrange("p (a b c) -> p a b c", b=shape[1], c=shape[2])
        return v

    def mark(self):
        return self.off

    def reset(self, to):
        self.off = to


def build_program(debug=False, stop_after=None):
    nc = bass.Bass("TRN2", target_bir_lowering=False)
    kin = "ExternalInput"
    dt_ = lambda name, shape, dtype, kind: nc.dram_tensor(name, shape, dtype, kind=kind).ap()
    x_in = dt_("x", [NB, S, D], F32, kin)
    ctx_in = dt_("ctx", [NB, LC, D], F32, kin)
    c_fm = dt_("c_fm", [128, KC, 3], F32, kin)
    w_mod = dt_("w_mod", [DEPTH, D, 6 * D], F32, kin)
    b_mod_fm = dt_("b_mod_fm", [DEPTH, 128, 48], F32, kin)
    w_in = dt_("w_in", [DEPTH, D, 2560], F32, kin)
    w_uT = dt_("w_uT", [DEPTH, 256, D], F32, kin)
    w_f = dt_("w_f", [DEPTH, 256, 64], F32, kin)
    lam_qk = dt_("lam_qk", [DEPTH, 1, 256], F32, kin)
    subln_g = dt_("subln_g", [DEPTH, 1, 128], F32, kin)
    w_out = dt_("w_out", [DEPTH, D, D], F32, kin)
    ln_vecs = dt_("ln_vecs", [DEPTH, 4, 1, D], F32, kin)
    w_router = dt_("w_router", [D, NE], F32, kin)
    router_bias = dt_("router_bias", [1, NE], F32, kin)
    w_gate = dt_("w_gate", [DEPTH, NE, D, 512], F32, kin)
    w_up = dt_("w_up", [DEPTH, NE, D, 512], F32, kin)
    w_down = dt_("w_down", [DEPTH, NE, 512, D], F32, kin)
    dftc = dt_("dftc", [S, S], BF16, kin)
    dftns = dt_("dftns", [S, S], BF16, kin)
    dft256 = dt_("dft256", [2, LC, LC], BF16, kin)
    c64bd = dt_("c64bd", [2, 128, 128], F32, kin)
    rope_cs = dt_("rope_cs", [2, 128, S], F32, kin)
    rmat_d = dt_("rmat", [128, 128], BF16, kin)
    ident_d = dt_("ident", [128, 128], BF16, kin)
    identf_d = dt_("identf", [128, 128], F32, kin)
    uts_d = dt_("uts", [128, 128], F32, kin)
    ebase_d = dt_("ebase", [1, NE], F32, kin)
    iota_d = dt_("iota_p", [128, NE], F32, kin)
    out_d = dt_("out", [NB, S, D], F32, "ExternalOutput")
    skind = "ExternalOutput" if debug else "Internal"
    QT_s = dt_("QT_s", [NB, 128, 6, TT], BF16, skind)
    KT_s = dt_("KT_s", [NB, 128, 6, TT], BF16, skind)
    VAB_s = dt_("VAB_s", [NB, TT, 1280], BF16, skind)
    X1_s = dt_("X1_s", [NB, TT, D], F32, skind)
    X2_s = dt_("X2_s", [NB, TT, D], F32, skind)
    HG_s = dt_("HG_s", [NSLOT, D], BF16, "Internal")
    Y_s = [dt_("Y_s%d" % i, [NSLOT, 512], F32, "Internal") for i in range(2)]
    MIX_s = dt_("MIX_s", [NB, 128, 8, TT], BF16, skind) if debug else None
    DBG_s = dt_("DBG_s", [128, 4096], F32, skind) if debug else None
    DBG2_s = dt_("DBG2_s", [3, 128, 512], F32, skind) if debug else None

    st = ExitStack()
    arena_t = st.enter_context(nc.sbuf_tensor("arena", [128, ARENA_BYTES // 2], BF16))
    psum_t = st.enter_context(nc.psum_tensor("psum", [128, 4096], F32))
    P = Prog(nc)
    AR = Arena(arena_t[:, :])

    def bank(b, n=1):
        return psum_t[:, b * 512:(b + n) * 512]

    rp = [P.R("bank%d" % i) for i in range(8)]

    ident = AR.alloc([128], BF16)
    identf = AR.alloc([128], F32)
    onesf = AR.alloc([128], F32)
    rmat = AR.alloc([128], BF16)
    modT = AR.alloc([48, 3], F32)
    s1p_a = AR.alloc([KC, 3], F32)
    s1p_f = AR.alloc([KC, 3], F32)
    nlam = AR.alloc([1], F32)
    gsub = AR.alloc([128], F32)
    rbias = AR.alloc([NE], F32)
    wr_sb = AR.alloc([KC, NE], BF16)
    r_const = P.R("const")
    r_mod = P.R("mod")
    P.op("sp", dma(ident, ident_d), writes=[r_const], dma=True)
    P.op("sp", dma(identf, identf_d), writes=[r_const], dma=True)
    P.op("sp", dma(rmat, rmat_d), writes=[r_const], dma=True)
    P.op("sp", dma(rbias, router_bias.partition_broadcast(128)), writes=[r_const], dma=True)
    P.op("pool", dma(wr_sb, w_router.rearrange("(kc p) e -> p kc e", p=128)), writes=[r_const], dma=True)
    P.op("dve", memset(onesf, 1.0), writes=[r_const])
    P.barrier()
    PERSIST = AR.mark()

    def ln_stats(xt_ap, r_x, sm, r_sm):
        P.op("dve", lambda e: e.bn_stats(out=sm[:, 0:6], in_=xt_ap[:, 0:512]), reads=[r_x], writes=[r_sm])
        P.op("dve", lambda e: e.bn_stats(out=sm[:, 6:12], in_=xt_ap[:, 512:1024]), reads=[r_x], writes=[r_sm])
        P.op("dve", lambda e: e.bn_aggr(out=sm[:, 12:14], in_=sm[:, 0:12].rearrange("p (a b) -> p a b", b=6)),
             reads=[r_sm], writes=[r_sm])
        P.op("act", act(sm[:, 14:15], sm[:, 13:14], AF.Ln, bias=LN_EPS, scale=1.0), reads=[r_sm], writes=[r_sm])
        P.op("act", act(sm[:, 15:16], sm[:, 14:15], AF.Exp, scale=-0.5), reads=[r_sm], writes=[r_sm])
        P.op("dve", stt(sm[:, 16:17], sm[:, 12:13], -1.0, sm[:, 15:16], ALU.mult, ALU.mult),
             reads=[r_sm], writes=[r_sm])
        return sm[:, 15:16], sm[:, 16:17]

    def make_bcast(dst, r_dst, src_fm, diag, r_diag, pbank):
        for c in range(KC):
            P.op("dve", ts(diag, identf, src_fm[:, c:c + 1], ALU.mult), reads=[r_const, r_mod], writes=[r_diag])
            half, cc = c // 4, c % 4
            P.op("pe", mm(bank(pbank + half)[:, cc * 128:(cc + 1) * 128], onesf, diag, True, True),
                 reads=[r_const, r_diag], writes=[rp[pbank + half]])
        P.op("dve", tcopy(dst, bank(pbank, 2)), reads=[rp[pbank], rp[pbank + 1]], writes=[r_dst])

    _breg = {}

    def bound_reg(eng):
        if "r" not in _breg:
            _breg["r"] = eng.alloc_register("slot_bound")
            eng.reg_mov(_breg["r"], NSLOT - 1)
        return _breg["r"]

    def tile_src(l, b, t):
        if l == 0:
            if t < 2:
                return ctx_in[b, t * 128:(t + 1) * 128, :]
            return x_in[b, (t - 2) * 128:(t - 1) * 128, :]
        return X2_s[b, t * 128:(t + 1) * 128, :]

    for l in range(DEPTH):
        last = (l == DEPTH - 1)
        lam_init = 0.8 - 0.6 * math.exp(-0.3 * l)
        tiles_q = list(range(2, NT)) if last else list(range(NT))
        AR.reset(PERSIST)
        cfm = AR.alloc([KC, 3], F32)
        silu_c = AR.alloc([KC, 3], F32)
        bmod = AR.alloc([48], F32)
        lqb = AR.alloc([256], F32)
        junk = AR.alloc([64], F32)
        s12 = AR.alloc([4], F32)
        wm_ring = Ring([Buf(AR.alloc([KC, 1024], F32), P.R()) for _ in range(2)])
        r_c, r_s, r_b, r_l, r_j = P.R(), P.R(), P.R(), P.R(), P.R()
        P.op("sp", dma(cfm, c_fm), writes=[r_c], dma=True)
        P.op("sp", dma(bmod, b_mod_fm[l]), writes=[r_b], dma=True)
        P.op("sp", dma(lqb, lam_qk[l].partition_broadcast(128)), writes=[r_l], dma=True)
        P.op("sp", dma(gsub, subln_g[l].partition_broadcast(128)), writes=[r_mod], dma=True)
        P.op("act", act(silu_c, cfm, AF.Silu), reads=[r_c], writes=[r_s])
        psm = bank(0)[:, 0:144].rearrange("p (a b) -> p a b", b=3)
        for cg in range(6):
            wb = wm_ring.next()
            for kc in range(KC):
                P.op("sp", dma(wb.ap[:, kc, :], w_mod[l, kc * 128:(kc + 1) * 128, cg * 1024:(cg + 1) * 1024]),
                     writes=[wb.res], dma=True)
            for j in range(8):
                for kc in range(KC):
                    P.op("pe", mm(psm[:, cg * 8 + j, :], wb.ap[:, kc, j * 128:(j + 1) * 128], silu_c[:, kc, :],
                                  kc == 0, kc == KC - 1), reads=[wb.res, r_s], writes=[rp[0]])
        for j in range(3):
            P.op("dve", tt(modT[:, :, j], psm[:, :, j], bmod, ALU.add), reads=[rp[0], r_b], writes=[r_mod])
        P.op("dve", ts(s1p_a, modT[:, 8:16, :], 1.0, ALU.add), reads=[r_mod], writes=[r_mod])
        P.op("dve", ts(s1p_f, modT[:, 32:40, :], 1.0, ALU.add), reads=[r_mod], writes=[r_mod])
        P.op("dve", memset(s12, 0.0), writes=[r_j])
        P.op("dve", stt(junk, lqb[:, 0:64], 1.0, lqb[:, 64:128], ALU.mult, ALU.mult, accum_out=s12[:, 0:1]),
             reads=[r_l], writes=[r_j])
        P.op("dve", stt(junk, lqb[:, 128:192], 1.0, lqb[:, 192:256], ALU.mult, ALU.mult, accum_out=s12[:, 1:2]),
             reads=[r_l], writes=[r_j])
        P.op("act", act(s12[:, 2:4], s12[:, 0:2], AF.Exp), reads=[r_j], writes=[r_j])
        P.op("dve", tt(nlam, s12[:, 3:4], s12[:, 2:3], ALU.subtract), reads=[r_j], writes=[r_mod])
        P.op("dve", ts(nlam, nlam, -lam_init, ALU.add), reads=[r_mod], writes=[r_mod])
        P.op("dve", ts(gsub, gsub, 1.0 - lam_init, ALU.mult), reads=[r_mod], writes=[r_mod])
        P.barrier()
        sh_a = modT[:, 0:8, :]
        g_a = modT[:, 16:24, :]
        sh_f = modT[:, 24:32, :]
        g_f = modT[:, 40:48, :]

        import os as _os
        _skipmix = _os.environ.get("SKIP_MIX") is not None
        AR.reset(PERSIST)
        w_sb = AR.alloc([KC, WCOLS], BF16)
        r_w = P.R("w_in")
        cs_sb = AR.alloc([2, S], F32)
        r_cs = P.R()
        xt_ring = Ring([Buf(AR.alloc([D], F32), P.R()) for _ in range(2)])
        xn_ring = Ring([Buf(AR.alloc([D], BF16), P.R()) for _ in range(2)])
        sm_ring = Ring([Buf(AR.alloc([24], F32), P.R()) for _ in range(2)])
        hT_ring = Ring([Buf(AR.alloc([KC, 512], BF16), P.R()) for _ in range(2)])
        pl_ring = Ring([Buf(AR.alloc([512], BF16), P.R()) for _ in range(2)])
        t1_ring = Ring([Buf(AR.alloc([512], F32), P.R()) for _ in range(2)])
        t2_ring = Ring([Buf(AR.alloc([512], F32), P.R()) for _ in range(2)])
        qk_ring = Ring([Buf(AR.alloc([12, 512], BF16), P.R()) for _ in range(2)])
        vab_ring = Ring([Buf(AR.alloc([1280], BF16), P.R()) for _ in range(2)])
        c64 = AR.alloc([2, 128], F32)
        wf_sb = AR.alloc([2, 64], F32)
        bd = AR.alloc([4, 128], BF16)
        wuT = AR.alloc([2, D], BF16)
        r_a, r_bd = P.R(), P.R()
        P.op("sp", dma(c64, c64bd.rearrange("a p n -> p a n")), writes=[r_a], dma=True)
        P.op("sp", dma(wf_sb, w_f[l].rearrange("(j p) d -> p j d", p=128)), writes=[r_a], dma=True)
        P.op("pool", dma(wuT, w_uT[l].rearrange("(j p) k -> p j k", p=128)), writes=[r_a], dma=True)
        P.op("sp", dma(cs_sb, rope_cs.rearrange("a p n -> p a n")), writes=[r_cs], dma=True)
        for kc in range(KC):
            P.op("pool", dma(w_sb[:, kc, 512:WCOLS], w_in[l, kc * 128:(kc + 1) * 128, 256:2560]),
                 writes=[r_w], dma=True)
        P.op("dve", memset(bd, 0.0), writes=[r_bd])
        for cs in range(2):
            for j in range(2):
                idx = cs * 2 + j
                P.op("pe", mm(bank(0)[:, idx * 64:(idx + 1) * 64], c64[:, cs, :], wf_sb[:, j, :], True, True),
                     reads=[r_a], writes=[rp[0]])
        for idx in range(4):
            P.op("dve", tcopy(bd[0:64, idx, 0:64], bank(0)[0:64, idx * 64:(idx + 1) * 64]),
                 reads=[rp[0]], writes=[r_bd])
            P.op("dve", tcopy(bd[64:128, idx, 64:128], bank(0)[64:128, idx * 64:(idx + 1) * 64]),
                 reads=[rp[0]], writes=[r_bd])
        for kc in range(KC):
            pb = 2 + (kc % 2)
            for cs in range(2):
                for j in range(2):
                    idx = cs * 2 + j
                    P.op("pe", mm(bank(pb)[:, idx * 128:(idx + 1) * 128], wuT[:, j, kc * 128:(kc + 1) * 128],
                                  bd[:, idx, :], True, True), reads=[r_a, r_bd], writes=[rp[pb]])
            P.op("dve", tcopy(w_sb[:, kc, 0:512], bank(pb)), reads=[rp[pb]], writes=[r_w])

        blocks = []
        for b in range(NB):
            blocks.append((b, 0, 2))
            for i in range(4):
                blocks.append((b, 2 + 4 * i, 4))
        psT_i = [0]
        fm_i = [0]
        tm_i = [0]
        rot_i = [0]

        def p1_A(blk):
            b, t0, ntl = blk
            hb = hT_ring.next()
            for ti in range(ntl):
                t = t0 + ti
                j = 2 if t < 2 else b
                xb_, xnb, smb = xt_ring.next(), xn_ring.next(), sm_ring.next()
                P.op("sp", dma(xb_.ap, tile_src(l, b, t)), writes=[xb_.res], dma=True)
                rstd, nmr = ln_stats(xb_.ap, xb_.res, smb.ap, smb.res)
                P.op("act", act(xnb.ap, xb_.ap, AF.Identity, bias=nmr, scale=rstd),
                     reads=[xb_.res, smb.res], writes=[xnb.res])
                pb = psT_i[0] % 2
                psT_i[0] += 1
                psT = bank(pb).bitcast(BF16).rearrange("p (a b) -> p a b", b=128)
                for kc in range(KC):
                    P.op("pe", tr(psT[:, kc, :], xnb.ap[:, kc * 128:(kc + 1) * 128], ident),
                         reads=[xnb.res, r_const], writes=[rp[pb]])
                for kc in range(KC):
                    P.op("dve", ts(hb.ap[:, kc, ti * 128:(ti + 1) * 128], psT[:, kc, :], s1p_a[:, kc, j:j + 1],
                                   ALU.mult, sh_a[:, kc, j:j + 1], ALU.add),
                         reads=[rp[pb], r_mod], writes=[hb.res])
            return hb

        def p1_B(blk, hb):
            b, t0, ntl = blk
            n = ntl * 128
            is_ctx = t0 < 2
            tok0 = t0 * 128
            qb = qk_ring.next()
            for c in range(12):
                pb = 2 + fm_i[0] % 2
                fm_i[0] += 1
                col = 512 + c * 128
                for kc in range(KC):
                    P.op("pe", mm(bank(pb)[:, 0:n], w_sb[:, kc, col:col + 128], hb.ap[:, kc, 0:n],
                                  kc == 0, kc == KC - 1), reads=[r_w, hb.res], writes=[rp[pb]])
                if is_ctx:
                    P.op("act", tcopy_act(qb.ap[:, c, 0:n], bank(pb)[:, 0:n]), reads=[rp[pb]], writes=[qb.res])
                else:
                    pl, t1, t2 = pl_ring.next(), t1_ring.next(), t2_ring.next()
                    P.op("act", tcopy_act(pl.ap[:, 0:n], bank(pb)[:, 0:n]), reads=[rp[pb]], writes=[pl.res])
                    prb = 4 + rot_i[0] % 2
                    rot_i[0] += 1
                    P.op("pe", mm(bank(prb)[:, 0:n], rmat, pl.ap[:, 0:n], True, True),
                         reads=[pl.res, r_const], writes=[rp[prb]])
                    s0 = tok0 - LC
                    P.op("dve", tt(t1.ap[:, 0:n], pl.ap[:, 0:n], cs_sb[:, 0, s0:s0 + n], ALU.mult),
                         reads=[pl.res, r_cs], writes=[t1.res])
                    P.op("dve", tt(t2.ap[:, 0:n], bank(prb)[:, 0:n], cs_sb[:, 1, s0:s0 + n], ALU.mult),
                         reads=[rp[prb], r_cs], writes=[t2.res])
                    P.op("dve", tt(qb.ap[:, c, 0:n], t1.ap[:, 0:n], t2.ap[:, 0:n], ALU.add),
                         reads=[t1.res, t2.res], writes=[qb.res])
            P.op("pool", dma(QT_s[b, :, :, tok0:tok0 + n], qb.ap[:, 0:6, 0:n]), reads=[qb.res], dma=True)
            P.op("pool", dma(KT_s[b, :, :, tok0:tok0 + n], qb.ap[:, 6:12, 0:n]), reads=[qb.res], dma=True)
            for ti in range(ntl):
                vb = vab_ring.next()
                for (c0, c1, wc0) in ((0, 512, 0), (512, 1024, 2048), (1024, 1280, 2560)):
                    pb = 6 + tm_i[0] % 2
                    tm_i[0] += 1
                    w_ = c1 - c0
                    for kc in range(KC):
                        P.op("pe", mm(bank(pb)[:, 0:w_], hb.ap[:, kc, ti * 128:(ti + 1) * 128],
                                      w_sb[:, kc, wc0:wc0 + w_], kc == 0, kc == KC - 1),
                             reads=[r_w, hb.res], writes=[rp[pb]])
                    P.op("act", tcopy_act(vb.ap[:, c0:c1], bank(pb)[:, 0:w_]), reads=[rp[pb]], writes=[vb.res])
                r0 = tok0 + ti * 128
                P.op("pool", dma(VAB_s[b, r0:r0 + 128, :], vb.ap), reads=[vb.res], dma=True)

        def tcopy_act(out, in_):
            return lambda e: e.copy(out=out, in_=in_)

        prev = None
        for i in range(0 if _skipmix else len(blocks) + 1):
            cur = None
            if i < len(blocks):
                cur = (blocks[i], p1_A(blocks[i]))
            if prev is not None:
                p1_B(*prev)
            prev = cur
        P.barrier()
        if stop_after == ("P1", l):
            break

        for b in range(0 if _skipmix else NB):
            AR.reset(PERSIST)
            fT = AR.alloc([2, TT], BF16)
            mixA = AR.alloc([6, TT], BF16)
            r_fT, r_mix = P.R(), P.R()
            SUB = AR.mark()
            ab_sb = AR.alloc([NT, 512], BF16)
            d256 = AR.alloc([2, 2, LC], BF16)
            tb_ring = Ring([Buf(AR.alloc([16, 512], BF16), P.R()) for _ in range(2)])
            r_ab, r_d = P.R(), P.R()
            P.op("sp", dma(ab_sb, VAB_s[b, :, 0:512].rearrange("(t p) n -> p t n", p=128)), writes=[r_ab], dma=True)
            if not last:
                for cs in range(2):
                    P.op("sp", dma(d256[:, :, cs, :], dft256[cs].rearrange("(tc p) n -> p tc n", p=128)),
                         writes=[r_d], dma=True)
                for j in range(2):
                    k = 0
                    for cs in range(2):
                        for tc in range(2):
                            P.op("pe", mm(bank(j)[:, 0:LC], ab_sb[:, tc, cs * 256 + j * 128: cs * 256 + (j + 1) * 128],
                                          d256[:, tc, cs, :], k == 0, k == 3), reads=[r_ab, r_d], writes=[rp[j]])
                            k += 1
                    P.op("act", tcopy_act(fT[:, j, 0:LC], bank(j)[:, 0:LC]), reads=[rp[j]], writes=[r_fT])
            for tb in range(4):
                for cs in range(2):
                    tbuf = tb_ring.next()
                    src = (dftc if cs == 0 else dftns)[:, tb * 512:(tb + 1) * 512].rearrange("(tc p) n -> p tc n", p=128)
                    for hh in range(2):
                        P.op("sp", dma(tbuf.ap[:, hh * 8:(hh + 1) * 8, :], src[:, hh * 8:(hh + 1) * 8, :]),
                             writes=[tbuf.res], dma=True)
                    for j in range(2):
                        pb = 2 + (tb % 2) * 2 + j
                        for tc in range(16):
                            P.op("pe", mm(bank(pb), ab_sb[:, 2 + tc, cs * 256 + j * 128: cs * 256 + (j + 1) * 128],
                                          tbuf.ap[:, tc, :], cs == 0 and tc == 0, cs == 1 and tc == 15),
                                 reads=[r_ab, tbuf.res], writes=[rp[pb]])
                for j in range(2):
                    pb = 2 + (tb % 2) * 2 + j
                    P.op("act", tcopy_act(fT[:, j, LC + tb * 512: LC + (tb + 1) * 512], bank(pb)),
                         reads=[rp[pb]], writes=[r_fT])
            if debug:
                P.op("sp", dma(MIX_s[b, :, 0:2, :], fT), reads=[r_fT], dma=True)
            P.barrier()

            AR.reset(SUB)
            qT = AR.alloc([6, TT], BF16)
            kT = AR.alloc([6, TT], BF16)
            vaug = AR.alloc([NT, 6, VST], BF16)
            r_q, r_k, r_v = P.R(), P.R(), P.R()
            PT_ring = Ring([Buf(AR.alloc([NT, 512], BF16), P.R()) for _ in range(2)])
            tq_ring = Ring([Buf(AR.alloc([4, 128], F32), P.R()) for _ in range(2)])
            o_ring = Ring([Buf(AR.alloc([4, 128], F32), P.R()) for _ in range(2)])
            on_ring = Ring([Buf(AR.alloc([4, 128], BF16), P.R()) for _ in range(2)])
            rs_ring = Ring([Buf(AR.alloc([16], F32), P.R()) for _ in range(4)])
            junk2 = AR.alloc([128], F32)
            r_j2 = P.R()
            P.op("sp", dma(qT, QT_s[b]), writes=[r_q], dma=True)
            P.op("sp", dma(kT, KT_s[b]), writes=[r_k], dma=True)
            P.op("dve", memset(vaug[:, :, :, 128:VST], 1.0), writes=[r_v])
            for h in range(6):
                P.op("sp", dma(vaug[:, :, h, 0:128],
                               VAB_s[b, :, 512 + h * 128: 512 + (h + 1) * 128].rearrange("(t p) d -> p t d", p=128)),
                     writes=[r_v], dma=True)
            units = []
            if not last:
                for h in range(6):
                    for sub in range(2):
                        units.append((0, 2, [0, 1], h, sub))
            for qb_ in range(4):
                for h in range(6):
                    for sub in range(2):
                        units.append((LC + qb_ * 512, 4, list(range(NT)), h, sub))
            sg_i = [0]

            def att_S(u, ui):
                q0, nqt, kts, h, sub = u
                n = nqt * 128
                pt = PT_ring.next()
                p0, p1 = sub * 64, (sub + 1) * 64
                for g in range(0, len(kts), 2):
                    pb = (sg_i[0] % 2) * 2
                    sg_i[0] += 1
                    grp = kts[g:g + 2]
                    for gi, kt in enumerate(grp):
                        P.op("pe", mm(bank(pb + gi)[:, 0:n], kT[p0:p1, h, kt * 128:(kt + 1) * 128],
                                      qT[p0:p1, h, q0:q0 + n], True, True),
                             reads=[r_q, r_k], writes=[rp[pb + gi]])
                    src = bank(pb, 2).rearrange("p (a b) -> p a b", b=512)[:, 0:len(grp), 0:n]
                    P.op("act", act(pt.ap[:, g:g + len(grp), 0:n], src, AF.Exp, scale=0.125),
                         reads=[rp[pb], rp[pb + 1]], writes=[pt.res])
                return pt

            def acc_ap(par, qt):
                if qt < 3:
                    return bank(4 + 2 * par)[:, qt * 160: qt * 160 + 129]
                return bank(5 + 2 * par)[:, 0:129]

            def att_AV(u, ui, pt, state):
                q0, nqt, kts, h, sub = u
                par = ui % 2
                for qt in range(nqt):
                    a = acc_ap(par, qt)
                    for ki, kt in enumerate(kts):
                        P.op("pe", mm(a, pt.ap[:, ki, qt * 128:(qt + 1) * 128], vaug[:, kt, h, 0:129],
                                      ki == 0, ki == len(kts) - 1),
                             reads=[pt.res, r_v], writes=[rp[4 + 2 * par], rp[5 + 2 * par]])
                accr = [rp[4 + 2 * par], rp[5 + 2 * par]]
                rs = rs_ring.next()
                if sub == 0:
                    tq = tq_ring.next()
                    state["tq"] = tq
                    for qt in range(nqt):
                        a = acc_ap(par, qt)
                        P.op("dve", lambda e, o=rs.ap[:, qt:qt + 1], i=a[:, 128:129]: e.reciprocal(out=o, in_=i),
                             reads=accr, writes=[rs.res])
                        P.op("dve", ts(tq.ap[:, qt, :], a[:, 0:128], rs.ap[:, qt:qt + 1], ALU.mult),
                             reads=accr + [rs.res], writes=[tq.res])
                else:
                    tq = state["tq"]
                    ob, onb = o_ring.next(), on_ring.next()
                    P.op("dve", memset(rs.ap[:, 8:12], 0.0), writes=[rs.res])
                    for qt in range(nqt):
                        a = acc_ap(par, qt)
                        P.op("dve", lambda e, o=rs.ap[:, qt:qt + 1], i=a[:, 128:129]: e.reciprocal(out=o, in_=i),
                             reads=accr, writes=[rs.res])
                        P.op("dve", ts(rs.ap[:, 4 + qt:5 + qt], rs.ap[:, qt:qt + 1], nlam[:, 0:1], ALU.mult),
                             reads=[rs.res, r_mod], writes=[rs.res])
                        P.op("dve", stt(ob.ap[:, qt, :], a[:, 0:128], rs.ap[:, 4 + qt:5 + qt], tq.ap[:, qt, :],
                                        ALU.mult, ALU.add), reads=accr + [rs.res, tq.res], writes=[ob.res])
                        P.op("dve", stt(junk2, ob.ap[:, qt, :], 1.0, ob.ap[:, qt, :], ALU.mult, ALU.mult,
                                        accum_out=rs.ap[:, 8 + qt:9 + qt]), reads=[ob.res], writes=[rs.res, r_j2])
                    P.op("act", act(rs.ap[:, 12:12 + nqt], rs.ap[:, 8:8 + nqt], AF.Ln, bias=LN_EPS, scale=1.0 / 128),
                         reads=[rs.res], writes=[rs.res])
                    P.op("act", act(rs.ap[:, 12:12 + nqt], rs.ap[:, 12:12 + nqt], AF.Exp, scale=-0.5),
                         reads=[rs.res], writes=[rs.res])
                    for qt in range(nqt):
                        P.op("dve", stt(onb.ap[:, qt, :], ob.ap[:, qt, :], rs.ap[:, 12 + qt:13 + qt], gsub,
                                        ALU.mult, ALU.mult), reads=[ob.res, rs.res, r_mod], writes=[onb.res])
                    psT = bank(5 + 2 * par)[:, 256:512].bitcast(BF16).rearrange("p (a b) -> p a b", b=128)
                    for qt in range(nqt):
                        P.op("pe", tr(psT[:, qt, :], onb.ap[:, qt, :], ident), reads=[onb.res, r_const],
                             writes=[rp[5 + 2 * par]])
                    P.op("dve", tcopy(mixA[:, h, q0:q0 + nqt * 128].rearrange("p (a b) -> p a b", b=128), psT[:, 0:nqt, :]),
                         reads=[rp[5 + 2 * par]], writes=[r_mix])

            state = {}
            prevu = None
            for ui in range(len(units) + 1):
                curu = None
                if ui < len(units):
                    curu = (units[ui], ui, att_S(units[ui], ui))
                if prevu is not None:
                    att_AV(prevu[0], prevu[1], prevu[2], state)
                prevu = curu
            if debug:
                P.op("sp", dma(MIX_s[b, :, 2:8, :], mixA), reads=[r_mix], dma=True)
            P.barrier()

            AR.reset(SUB)
            wo_sb = AR.alloc([KC, D], BF16)
            gbc = [AR.alloc([D], F32) for _ in range(2)]
            lng = AR.alloc([D], F32)
            lnb = AR.alloc([D], F32)
            diag = AR.alloc([128], F32)
            r_wo, r_g, r_ln, r_dg = P.R(), P.R(), P.R(), P.R()
            xt_ring = Ring([Buf(AR.alloc([D], F32), P.R()) for _ in range(2)])
            z_ring = Ring([Buf(AR.alloc([D], F32), P.R()) for _ in range(2)])
            sm_ring = Ring([Buf(AR.alloc([24], F32), P.R()) for _ in range(2)])
            for kc in range(KC):
                P.op("pool", dma(wo_sb[:, kc, :], w_out[l, kc * 128:(kc + 1) * 128, :]), writes=[r_wo], dma=True)
            P.op("sp", dma(lng, ln_vecs[l, 0].partition_broadcast(128)), writes=[r_ln], dma=True)
            P.op("sp", dma(lnb, ln_vecs[l, 1].partition_broadcast(128)), writes=[r_ln], dma=True)
            make_bcast(gbc[0], r_g, g_a[:, :, b], diag, r_dg, 0)
            if not last:
                make_bcast(gbc[1], r_g, g_a[:, :, 2], diag, r_dg, 0)
            for ti, t in enumerate(tiles_q):
                xb_, zb, smb = xt_ring.next(), z_ring.next(), sm_ring.next()
                P.op("sp", dma(xb_.ap, tile_src(l, b, t)), writes=[xb_.res], dma=True)
                pb = 2 + (ti % 3) * 2
                for half in range(2):
                    for kc in range(KC):
                        lhs = fT[:, kc, t * 128:(t + 1) * 128] if kc < 2 else mixA[:, kc - 2, t * 128:(t + 1) * 128]
                        P.op("pe", mm(bank(pb + half), lhs, wo_sb[:, kc, half * 512:(half + 1) * 512],
                                      kc == 0, kc == KC - 1), reads=[r_fT, r_mix, r_wo], writes=[rp[pb + half]])
                gsel = gbc[1] if t < 2 else gbc[0]
                P.op("dve", tt(zb.ap, bank(pb, 2), gsel, ALU.mult), reads=[rp[pb], rp[pb + 1], r_g], writes=[zb.res])
                P.op("dve", stt(zb.ap, xb_.ap, ALPHA, zb.ap, ALU.mult, ALU.add), reads=[xb_.res, zb.res],
                     writes=[zb.res])
                rstd, nmr = ln_stats(zb.ap, zb.res, smb.ap, smb.res)
                P.op("act", act(xb_.ap, zb.ap, AF.Identity, bias=nmr, scale=rstd), reads=[zb.res, smb.res],
                     writes=[xb_.res])
                P.op("dve", tt(xb_.ap, xb_.ap, lng, ALU.mult), reads=[xb_.res, r_ln], writes=[xb_.res])
                P.op("dve", tt(xb_.ap, xb_.ap, lnb, ALU.add), reads=[xb_.res, r_ln], writes=[xb_.res])
                P.op("pool", dma(X1_s[b, t * 128:(t + 1) * 128, :], xb_.ap), reads=[xb_.res], dma=True)
            P.barrier()
        if stop_after == ("MIX", l):
            break

        tl = [(b, t) for b in range(NB) for t in tiles_q]
        NG = len(tl)
        AR.reset(PERSIST)
        slots_i = AR.alloc([NG, 2], I32)
        wts = AR.alloc([NG, 2], F32)
        cnt_i = AR.alloc([NE], I32)
        r_sl, r_cnt = P.R(), P.R()
        SUB = AR.mark()
        jl = [0, 1] if last else [0, 1, 2]
        s_bc = {j: AR.alloc([D], F32) for j in jl}
        h_bc = {j: AR.alloc([D], F32) for j in jl}
        diag = AR.alloc([128], F32)
        uts = AR.alloc([128], F32)
        ebase = AR.alloc([NE], F32)
        runb = AR.alloc([NE], F32)
        r_bc, r_dg, r_ut, r_run = P.R(), P.R(), P.R(), P.R()
        xt_ring = Ring([Buf(AR.alloc([D], F32), P.R()) for _ in range(2)])
        xn_ring = Ring([Buf(AR.alloc([D], BF16), P.R()) for _ in range(2)])
        hf_ring = Ring([Buf(AR.alloc([D], F32), P.R()) for _ in range(2)])
        h2_ring = Ring([Buf(AR.alloc([D], BF16), P.R()) for _ in range(3)])
        h2T_ring = Ring([Buf(AR.alloc([KC, 128], BF16), P.R()) for _ in range(2)])
        sm_ring = Ring([Buf(AR.alloc([24], F32), P.R()) for _ in range(2)])
        rt_ring = Ring([Buf(AR.alloc([256], F32), P.R()) for _ in range(2)])
        P.op("sp", dma(uts, uts_d), writes=[r_ut], dma=True)
        P.op("sp", dma(ebase, ebase_d.partition_broadcast(128)), writes=[r_ut], dma=True)
        P.op("sp", dma(runb, ebase_d.partition_broadcast(128)), writes=[r_run], dma=True)
        for j in jl:
            make_bcast(s_bc[j], r_bc, s1p_f[:, :, j], diag, r_dg, 6)
            make_bcast(h_bc[j], r_bc, sh_f[:, :, j], diag, r_dg, 6)
        for gt, (b, t) in enumerate(tl):
            j = 2 if t < 2 else b
            xb_, xnb, smb, rt = xt_ring.next(), xn_ring.next(), sm_ring.next(), rt_ring.next()
            hf, h2, h2T = hf_ring.next(), h2_ring.next(), h2T_ring.next()
            P.op("sp", dma(xb_.ap, tile_src(0, b, t) if _skipmix else X1_s[b, t * 128:(t + 1) * 128, :]),
                 writes=[xb_.res], dma=True)
            rstd, nmr = ln_stats(xb_.ap, xb_.res, smb.ap, smb.res)
            P.op("act", act(xnb.ap, xb_.ap, AF.Identity, bias=nmr, scale=rstd),
                 reads=[xb_.res, smb.res], writes=[xnb.res])
            P.op("dve", tt(hf.ap, xnb.ap, s_bc[j], ALU.mult), reads=[xnb.res, r_bc], writes=[hf.res])
            P.op("dve", tt(h2.ap, hf.ap, h_bc[j], ALU.add), reads=[hf.res, r_bc], writes=[h2.res])
            pb = gt % 2
            psT = bank(pb).bitcast(BF16).rearrange("p (a b) -> p a b", b=128)
            for kc in range(KC):
                P.op("pe", tr(psT[:, kc, :], h2.ap[:, kc * 128:(kc + 1) * 128], ident),
                     reads=[h2.res, r_const], writes=[rp[pb]])
            P.op("act", tcopy_act(h2T.ap, psT), reads=[rp[pb]], writes=[h2T.res])
            prb = 2 + gt % 2
            for kc in range(KC):
                P.op("pe", mm(bank(prb)[:, 0:NE], h2T.ap[:, kc, :], wr_sb[:, kc, :], kc == 0, kc == KC - 1),
                     reads=[h2T.res, r_const], writes=[rp[prb]])
            A = rt.ap
            sc, sel, w4, gs, m2, gm, msk, ww, tmp = (A[:, 0:16], A[:, 16:32], A[:, 32:64], A[:, 64:68],
                                                       A[:, 68:72], A[:, 72:76], A[:, 80:96], A[:, 96:112],
                                                       A[:, 112:120])
            cmb, slv, msl, indb, sf = A[:, 128:144], A[:, 144:160], A[:, 160:176], A[:, 176:192], A[:, 192:196]
            P.op("act", act(sc, bank(prb)[:, 0:NE], AF.Exp, scale=-1.0), reads=[rp[prb]], writes=[rt.res])
            rr = [rt.res]
            P.op("dve", ts(sc, sc, 1.0, ALU.add), reads=rr, writes=rr)
            P.op("dve", lambda e, o=sc: e.reciprocal(out=o, in_=o), reads=rr, writes=rr)
            P.op("dve", tt(sel, sc, rbias, ALU.add), reads=rr + [r_const], writes=rr)
            sv = sel.rearrange("p (g e) -> p g e", e=4)
            hi01, lo01, hi23, lo23 = w4[:, 0:4], w4[:, 4:8], w4[:, 8:12], w4[:, 12:16]
            m1, mid, lom = w4[:, 16:20], w4[:, 20:24], w4[:, 24:28]
            P.op("dve", tt(hi01, sv[:, :, 0], sv[:, :, 1], ALU.max), reads=rr, writes=rr)
            P.op("dve", tt(lo01, sv[:, :, 0], sv[:, :, 1], ALU.min), reads=rr, writes=rr)
            P.op("dve", tt(hi23, sv[:, :, 2], sv[:, :, 3], ALU.max), reads=rr, writes=rr)
            P.op("dve", tt(lo23, sv[:, :, 2], sv[:, :, 3], ALU.min), reads=rr, writes=rr)
            P.op("dve", tt(m1, hi01, hi23, ALU.max), reads=rr, writes=rr)
            P.op("dve", tt(mid, hi01, hi23, ALU.min), reads=rr, writes=rr)
            P.op("dve", tt(lom, lo01, lo23, ALU.max), reads=rr, writes=rr)
            P.op("dve", tt(m2, mid, lom, ALU.max), reads=rr, writes=rr)
            P.op("dve", tt(gs, m1, m2, ALU.add), reads=rr, writes=rr)
            P.op("dve", lambda e, o=tmp[:, 0:1], i=gs: e.tensor_reduce(out=o, in_=i, axis=AX.X, op=ALU.max),
                 reads=rr, writes=rr)
            P.op("dve", ts(gm, gs, tmp[:, 0:1], ALU.is_ge), reads=rr, writes=rr)
            mv_ = msk.rearrange("p (g e) -> p g e", e=4)
            for ee in range(4):
                P.op("dve", tt(mv_[:, :, ee], sv[:, :, ee], m2, ALU.is_ge), reads=rr, writes=rr)
                P.op("dve", tt(mv_[:, :, ee], mv_[:, :, ee], gm, ALU.mult), reads=rr, writes=rr)
            P.op("dve", tt(ww, sc, msk, ALU.mult), reads=rr, writes=rr)
            P.op("dve", lambda e, o=tmp[:, 1:2], i=ww: e.tensor_reduce(out=o, in_=i, axis=AX.X, op=ALU.add),
                 reads=rr, writes=rr)
            P.op("dve", lambda e, o=tmp[:, 2:3], i=tmp[:, 1:2]: e.reciprocal(out=o, in_=i), reads=rr, writes=rr)
            P.op("dve", ts(cmb, ww, tmp[:, 2:3], ALU.mult), reads=rr, writes=rr)
            ppb = 4 + gt % 2
            P.op("pe", mm(bank(ppb)[:, 0:NE], uts, msk, True, True), reads=[r_ut, rt.res], writes=[rp[ppb]])
            P.op("pe", mm(bank(ppb)[:, 32:32 + NE], onesf, msk, True, True), reads=[r_const, rt.res],
                 writes=[rp[ppb]])
            P.op("dve", tt(slv, bank(ppb)[:, 0:NE], runb, ALU.add), reads=[rp[ppb], r_run] + rr, writes=rr)
            P.op("dve", tt(runb, runb, bank(ppb)[:, 32:32 + NE], ALU.add), reads=[rp[ppb], r_run], writes=[r_run])
            P.op("dve", tt(msl, slv, msk, ALU.mult), reads=rr, writes=rr)
            P.op("dve", lambda e, o=sf[:, 1:2], i=msl: e.tensor_reduce(out=o, in_=i, axis=AX.X, op=ALU.max),
                 reads=rr, writes=rr)
            P.op("dve", lambda e, o=tmp[:, 3:4], i=msl: e.tensor_reduce(out=o, in_=i, axis=AX.X, op=ALU.add),
                 reads=rr, writes=rr)
            P.op("dve", tt(sf[:, 0:1], tmp[:, 3:4], sf[:, 1:2], ALU.subtract), reads=rr, writes=rr)
            P.op("dve", ts(indb, msl, sf[:, 1:2], ALU.is_equal), reads=rr, writes=rr)
            P.op("dve", tt(indb, indb, cmb, ALU.mult), reads=rr, writes=rr)
            P.op("dve", lambda e, o=wts[:, gt, 1:2], i=indb: e.tensor_reduce(out=o, in_=i, axis=AX.X, op=ALU.add),
                 reads=rr, writes=[r_sl])
            P.op("dve", ts(wts[:, gt, 0:1], wts[:, gt, 1:2], -1.0, ALU.mult, 1.0, ALU.add), reads=[r_sl],
                 writes=[r_sl])
            P.op("dve", tcopy(slots_i[:, gt, :], sf[:, 0:2]), reads=rr, writes=[r_sl])
            for k in range(2):
                P.op("pool", lambda e, off=slots_i[:, gt, k:k + 1], src=h2.ap: e.indirect_dma_start(
                    out=HG_s[:, :], out_offset=bass.IndirectOffsetOnAxis(ap=off, axis=0), in_=src, in_offset=None,
                    bounds_check=bound_reg(e), oob_is_err=False), reads=[r_sl, h2.res], dma=True)
        zt = hf_ring.bufs[0]
        padf = rt_ring.bufs[0]
        P.op("sp", dma(padf.ap[:, 0:NE], iota_d), writes=[padf.res], dma=True)
        P.op("dve", memset(zt.ap.bitcast(BF16)[:, 0:D], 0.0), writes=[zt.res])
        P.op("dve", tt(padf.ap[:, 0:NE], padf.ap[:, 0:NE], runb, ALU.add), reads=[padf.res, r_run], writes=[padf.res])
        pad_i = padf.ap[:, 32:32 + NE].bitcast(I32)
        P.op("dve", tcopy(pad_i, padf.ap[:, 0:NE]), reads=[padf.res], writes=[padf.res])
        for e_ in range(NE):
            P.op("pool", lambda e, off=pad_i[:, e_:e_ + 1], src=zt.ap.bitcast(BF16)[:, 0:D]: e.indirect_dma_start(
                out=HG_s[:, :], out_offset=bass.IndirectOffsetOnAxis(ap=off, axis=0), in_=src, in_offset=None,
                bounds_check=bound_reg(e), oob_is_err=False), reads=[padf.res, zt.res], dma=True)
        P.op("dve", tt(runb, runb, ebase, ALU.subtract), reads=[r_run, r_ut, padf.res], writes=[r_run])
        P.op("dve", tcopy(cnt_i, runb), reads=[r_run], writes=[r_cnt])
        if debug and l == 0:
            P.op("sp", dma(DBG_s[:, 0:NG * 2], wts.rearrange("p a b -> p (a b)")), reads=[r_sl], dma=True)
            P.op("sp", dma(DBG_s[:, 256:256 + NE], runb), reads=[r_run], dma=True)
            P.op("dve", tcopy(xt_ring.bufs[0].ap[:, 0:NG * 2], slots_i.rearrange("p a b -> p (a b)")), reads=[r_sl],
                 writes=[xt_ring.bufs[0].res])
            P.op("sp", dma(DBG_s[:, 512:512 + NG * 2], xt_ring.bufs[0].ap[:, 0:NG * 2]),
                 reads=[xt_ring.bufs[0].res], dma=True)
        P.barrier()
        if stop_after == ("ROUTE", l):
            break
        for e_ in range(NE):
            P.vload("cnt%d_%d" % (l, e_), cnt_i[0:1, e_:e_ + 1], r_cnt, NB * TT)

        AR.reset(SUB)
        w_ring = Ring([Buf((AR.alloc([KC, 512], BF16), AR.alloc([KC, 512], BF16), AR.alloc([4, D], BF16)), P.R())
                       for _ in range(2)])
        hg_ring = Ring([Buf(AR.alloc([D], BF16), P.R()) for _ in range(3)])
        hgT_ring = Ring([Buf(AR.alloc([KC, 128], BF16), P.R()) for _ in range(2)])
        sg_ring = Ring([Buf(AR.alloc([512], F32), P.R()) for _ in range(2)])
        a_ring = Ring([Buf(AR.alloc([512], BF16), P.R()) for _ in range(2)])
        aT_ring = Ring([Buf(AR.alloc([4, 128], BF16), P.R()) for _ in range(2)])
        y_ring = Ring([Buf(AR.alloc([D], F32), P.R()) for _ in range(2)])
        gu_i = [0]

        def moe_GU(e_, jt, wbuf):
            wg, wu, wd = wbuf.ap
            hg, hgT = hg_ring.next(), hgT_ring.next()
            r0 = e_ * CAP + jt * 128
            P.op("sp", dma(hg.ap, HG_s[r0:r0 + 128, :]), writes=[hg.res], dma=True)
            psT = bank(5).bitcast(BF16).rearrange("p (a b) -> p a b", b=128)
            for kc in range(KC):
                P.op("pe", tr(psT[:, kc, :], hg.ap[:, kc * 128:(kc + 1) * 128], ident),
                     reads=[hg.res, r_const], writes=[rp[5]])
            P.op("act", tcopy_act(hgT.ap, psT), reads=[rp[5]], writes=[hgT.res])
            pb = (gu_i[0] % 2) * 2
            gu_i[0] += 1
            for which, wmat in ((0, wg), (1, wu)):
                for kc in range(KC):
                    P.op("pe", mm(bank(pb + which), hgT.ap[:, kc, :], wmat[:, kc, :], kc == 0, kc == KC - 1),
                         reads=[hgT.res, wbuf.res], writes=[rp[pb + which]])
            sg, ab_ = sg_ring.next(), a_ring.next()
            P.op("act", act(sg.ap, bank(pb), AF.Silu), reads=[rp[pb]], writes=[sg.res])
            P.op("dve", tt(ab_.ap, bank(pb + 1), sg.ap, ALU.mult), reads=[rp[pb + 1], sg.res], writes=[ab_.res])
            return ab_

        def moe_D(e_, jt, wbuf, ab_):
            wg, wu, wd = wbuf.ap
            par = jt % 2
            psT = bank(4)[:, par * 256:(par + 1) * 256].bitcast(BF16).rearrange("p (a b) -> p a b", b=128)
            aT, yb = aT_ring.next(), y_ring.next()
            for f in range(4):
                P.op("pe", tr(psT[:, f, :], ab_.ap[:, f * 128:(f + 1) * 128], ident), reads=[ab_.res, r_const],
                     writes=[rp[4]])
            P.op("dve", tcopy(aT.ap, psT), reads=[rp[4]], writes=[aT.res])
            for half in range(2):
                for f in range(4):
                    P.op("pe", mm(bank(6 + half), aT.ap[:, f, :], wd[:, f, half * 512:(half + 1) * 512],
                                  f == 0, f == 3), reads=[aT.res, wbuf.res], writes=[rp[6 + half]])
            P.op("act", tcopy_act(yb.ap, bank(6, 2)), reads=[rp[6], rp[7]], writes=[yb.res])
            r0 = e_ * CAP + jt * 128
            for hf_ in range(2):
                P.op("sp", dma(Y_s[hf_][r0:r0 + 128, :], yb.ap[:, hf_ * 512:(hf_ + 1) * 512]), reads=[yb.res],
                     dma=True)

        import os as _os
        _ne = int(_os.environ.get("MOE_NE", NE))
        _nt = int(_os.environ.get("MOE_NT", NG))
        _nocond = _os.environ.get("MOE_NOCOND") is not None

        class _NoCond:
            def __enter__(s_):
                return None

            def __exit__(s_, *a):
                return False
        for e_ in range(_ne):
            wbuf = w_ring.next()
            wg, wu, wd = wbuf.ap
            for hh in range(2):
                P.op("pool", dma(wg[:, hh * 4:(hh + 1) * 4, :],
                                 w_gate[l, e_, hh * 512:(hh + 1) * 512, :].rearrange("(kc p) f -> p kc f", p=128)),
                     writes=[wbuf.res], dma=True)
                P.op("pool", dma(wu[:, hh * 4:(hh + 1) * 4, :],
                                 w_up[l, e_, hh * 512:(hh + 1) * 512, :].rearrange("(kc p) f -> p kc f", p=128)),
                     writes=[wbuf.res], dma=True)
                P.op("pool", dma(wd[:, hh * 2:(hh + 1) * 2, :],
                                 w_down[l, e_, hh * 256:(hh + 1) * 256, :].rearrange("(kc p) f -> p kc f", p=128)),
                     writes=[wbuf.res], dma=True)
            key = "cnt%d_%d" % (l, e_)
            prevt = None
            for jt in range(_nt + 1):
                curt = None
                if jt < _nt:
                    with (_NoCond() if _nocond else P.cond((key, jt * 128))):
                        curt = (jt, moe_GU(e_, jt, wbuf))
                if prevt is not None:
                    with (_NoCond() if _nocond else P.cond((key, prevt[0] * 128))):
                        moe_D(e_, prevt[0], wbuf, prevt[1])
                prevt = curt
        P.barrier()

        if stop_after == ("EXP", l):
            if debug:
                for i_, jt_ in enumerate((0, 6, 7)):
                    P.op("sp", dma(DBG2_s[i_], Y_s[0][jt_ * 128:(jt_ + 1) * 128, :]), dma=True)
                P.barrier()
            break
        AR.reset(SUB)
        gbc = {j: AR.alloc([D], F32) for j in jl}
        lng = AR.alloc([D], F32)
        lnb = AR.alloc([D], F32)
        diag = AR.alloc([128], F32)
        r_g, r_ln, r_dg = P.R(), P.R(), P.R()
        xt_ring = Ring([Buf(AR.alloc([D], F32), P.R()) for _ in range(2)])
        z_ring = Ring([Buf(AR.alloc([D], F32), P.R()) for _ in range(2)])
        ya_ring = Ring([Buf(AR.alloc([D], F32), P.R()) for _ in range(2)])
        yb_ring = Ring([Buf(AR.alloc([D], F32), P.R()) for _ in range(2)])
        sm_ring = Ring([Buf(AR.alloc([24], F32), P.R()) for _ in range(2)])
        P.op("sp", dma(lng, ln_vecs[l, 2].partition_broadcast(128)), writes=[r_ln], dma=True)
        P.op("sp", dma(lnb, ln_vecs[l, 3].partition_broadcast(128)), writes=[r_ln], dma=True)
        for j in jl:
            make_bcast(gbc[j], r_g, g_f[:, :, j], diag, r_dg, 0)
        for gt, (b, t) in enumerate(tl):
            j = 2 if t < 2 else b
            xb_, zb, smb, ya, yb = xt_ring.next(), z_ring.next(), sm_ring.next(), ya_ring.next(), yb_ring.next()
            P.op("sp", dma(xb_.ap, X1_s[b, t * 128:(t + 1) * 128, :]), writes=[xb_.res], dma=True)
            for k, yy in ((0, ya), (1, yb)):
                for hf_ in range(2):
                    P.op("pool", lambda e, off=slots_i[:, gt, k:k + 1], dst=yy.ap[:, hf_ * 512:(hf_ + 1) * 512],
                         src=Y_s[hf_]: e.indirect_dma_start(
                        out=dst, out_offset=None, in_=src[:, :], in_offset=bass.IndirectOffsetOnAxis(ap=off, axis=0),
                        bounds_check=bound_reg(e), oob_is_err=False), reads=[r_sl], writes=[yy.res], dma=True)
            P.op("dve", ts(zb.ap, ya.ap, wts[:, gt, 0:1], ALU.mult), reads=[ya.res, r_sl], writes=[zb.res])
            P.op("dve", stt(zb.ap, yb.ap, wts[:, gt, 1:2], zb.ap, ALU.mult, ALU.add), reads=[yb.res, r_sl, zb.res],
                 writes=[zb.res])
            P.op("dve", tt(zb.ap, zb.ap, gbc[j], ALU.mult), reads=[zb.res, r_g], writes=[zb.res])
            P.op("dve", stt(zb.ap, xb_.ap, ALPHA, zb.ap, ALU.mult, ALU.add), reads=[xb_.res, zb.res],
                 writes=[zb.res])
            rstd, nmr = ln_stats(zb.ap, zb.res, smb.ap, smb.res)
            P.op("act", act(xb_.ap, zb.ap, AF.Identity, bias=nmr, scale=rstd), reads=[zb.res, smb.res],
                 writes=[xb_.res])
            P.op("dve", tt(xb_.ap, xb_.ap, lng, ALU.mult), reads=[xb_.res, r_ln], writes=[xb_.res])
            P.op("dve", tt(xb_.ap, xb_.ap, lnb, ALU.add), reads=[xb_.res, r_ln], writes=[xb_.res])
            dst = out_d[b, (t - 2) * 128:(t - 1) * 128, :] if last else X2_s[b, t * 128:(t + 1) * 128, :]
            P.op("sp", dma(dst, xb_.ap), reads=[xb_.res], dma=True)
        P.barrier()

    P.barrier()
    P.emit()
    st.close()
    return nc, P


def _consts():
    bf = ml_dtypes.bfloat16
    t = np.arange(S, dtype=np.float64)
    ang = 2.0 * np.pi * ((np.outer(t, t)) % S) / S
    dftc = (np.cos(ang) / math.sqrt(S)).astype(np.float32).astype(bf)
    dftns = (-np.sin(ang) / math.sqrt(S)).astype(np.float32).astype(bf)
    t2 = np.arange(LC, dtype=np.float64)
    a2 = 2.0 * np.pi * ((np.outer(t2, t2)) % LC) / LC
    dft256 = np.stack([np.cos(a2) / math.sqrt(LC), -np.sin(a2) / math.sqrt(LC)]).astype(np.float32).astype(bf)
    c = np.arange(64, dtype=np.float64)
    a3 = 2.0 * np.pi * ((np.outer(c, c)) % 64) / 64
    c64 = np.cos(a3) / 8.0
    s64 = np.sin(a3) / 8.0
    c64bd = np.zeros((2, 128, 128), np.float32)
    for i, m in enumerate((c64, s64)):
        c64bd[i, 0:64, 0:64] = m
        c64bd[i, 64:128, 64:128] = m
    freqs = (10000.0 ** (-np.arange(0, 32, 2, dtype=np.float32) / 32)).astype(np.float32)
    pos = np.arange(S)
    row = (pos // 64).astype(np.float32)
    col = (pos % 64).astype(np.float32)
    ang_row = row[:, None] * freqs
    ang_col = col[:, None] * freqs
    rope = np.zeros((2, 128, S), np.float32)
    for p in range(128):
        d = p % 64
        a = ang_row[:, d % 16] if d < 32 else ang_col[:, d % 16]
        rope[0, p] = np.cos(a)
        rope[1, p] = np.sin(a)
    R = np.zeros((128, 128), np.float32)
    for m in range(128):
        d = m % 64
        if (d % 32) < 16:
            R[m + 16, m] = -1.0
        else:
            R[m - 16, m] = 1.0
    return dict(dftc=dftc, dftns=dftns, dft256=dft256, c64bd=c64bd, rope_cs=rope,
                rmat=R.astype(bf), ident=np.eye(128, dtype=np.float32).astype(bf),
                uts=np.triu(np.ones((128, 128), np.float32), 1),
                ebase=(np.arange(NE, dtype=np.float32) * CAP).reshape(1, NE),
                iota_p=np.repeat(np.arange(128, dtype=np.float32)[:, None], NE, axis=1),
                identf=np.eye(128, dtype=np.float32))


def make_in_maps(inputs, cores=range(N_CORES)):
    f = lambda a: np.ascontiguousarray(np.asarray(a, dtype=np.float32))
    x, c, ctx, c_ctx = f(inputs["x"]), f(inputs["c"]), f(inputs["ctx"]), f(inputs["c_ctx"])
    shared = dict(
        w_mod=f(inputs["w_mod"]),
        b_mod_fm=np.ascontiguousarray(f(inputs["b_mod"]).reshape(DEPTH, 48, 128).transpose(0, 2, 1)),
        w_in=f(inputs["w_in"]),
        w_uT=np.ascontiguousarray(f(inputs["w_in"])[:, :, :256].transpose(0, 2, 1)),
        w_f=f(inputs["w_fourier"]).reshape(DEPTH, 256, 64),
        lam_qk=f(inputs["lam_qk"]).reshape(DEPTH, 1, 256),
        subln_g=f(inputs["subln_g"]).reshape(DEPTH, 1, 128),
        w_out=f(inputs["w_out"]),
        ln_vecs=np.ascontiguousarray(np.stack([f(inputs["ln_attn_g"]), f(inputs["ln_attn_b"]),
                                               f(inputs["ln_ffn_g"]), f(inputs["ln_ffn_b"])], axis=1)
                                     .reshape(DEPTH, 4, 1, D)),
        w_router=f(inputs["w_router"]),
        router_bias=f(inputs["router_bias"]).reshape(1, NE),
        w_gate=f(inputs["w_gate"]), w_up=f(inputs["w_up"]), w_down=f(inputs["w_down"]),
    )
    shared.update(_consts())
    maps = []
    for ci in cores:
        b0 = ci * NB
        cc = np.stack([c[b0], c[b0 + 1], c_ctx], axis=-1)
        c_fm = np.ascontiguousarray(cc.reshape(KC, 128, 3).transpose(1, 0, 2))
        m = dict(shared)
        m["x"] = np.ascontiguousarray(x[b0:b0 + NB])
        m["ctx"] = np.ascontiguousarray(ctx[b0:b0 + NB])
        m["c_fm"] = c_fm
        maps.append(m)
    return maps


_CACHE = {}


def kernel(**inputs):
    if "nc" not in _CACHE:
        _CACHE["nc"] = build_program()[0]
    nc = _CACHE["nc"]
    in_maps = make_in_maps(inputs)
    res = run_bass_kernel_spmd(nc, in_maps, core_ids=list(range(N_CORES)))
    out = np.concatenate([np.asarray(r["out"], dtype=np.float32) for r in res.results], axis=0)
    return out
```

```python
import math
from contextlib import ExitStack

import numpy as np
import ml_dtypes
import concourse.bass as bass
import concourse.mybir as mybir
from concourse.bass_utils import run_bass_kernel_spmd

F32 = mybir.dt.float32
BF16 = mybir.dt.bfloat16
ALU = mybir.AluOpType
AF = mybir.ActivationFunctionType
AX = mybir.AxisListType

N_CORES = 8
NB = 2
S = 2048
LC = 256
TT = S + LC
NT = TT // 128
D = 1024
KC = 8
DEPTH = 2
WCOLS = 2816
ALPHA = (2 * DEPTH) ** 0.25
LN_EPS = 1e-5
NE = 16
VST = 132
CAP = NB * TT + 128
NSLOT = NE * CAP
I32 = mybir.dt.int32

ENGS = ("pe", "act", "dve", "pool", "sp")
DMAQ = ("sp", "pool", "act")


class Res:
    __slots__ = ("name", "w", "r")

    def __init__(self, name=""):
        self.name = name
        self.w = None
        self.r = []


class Prog:
    def __init__(self, nc, nq=16):
        self.nc = nc
        self.NQ = nq
        self.ops = {e: [] for e in ENGS}
        self.waited = {e: {} for e in ENGS}
        self.dma_n = {q: 0 for q in DMAQ}
        self.res = []
        self.cur_cond = None
        self.vload_specs = {}

    def cond(self, key):
        prog = self

        class _C:
            def __enter__(s_):
                assert prog.cur_cond is None
                prog.cur_cond = key
                prog._saved_waited = {e: dict(prog.waited[e]) for e in ENGS}

            def __exit__(s_, *a):
                prog.cur_cond = None
                prog.waited = prog._saved_waited
                return False
        return _C()

    def vload(self, name, ap, res, max_val):
        self.vload_specs[name] = (ap, max_val)
        for e in ENGS:
            self.op(e, ("vload", name), reads=[res])

    def R(self, name=""):
        r = Res(name)
        self.res.append(r)
        return r

    def _add_wait(self, eng, o, d, raw):
        if d[0] == "c":
            _, e2, idx = d
            if e2 == eng and (eng == "pe" or not raw):
                return
            key = ("c", e2)
            if self.waited[eng].get(key, -1) >= idx:
                return
            self.waited[eng][key] = idx
            self.ops[e2][idx]["signal"] = True
            o["waits"].append(d)
        else:
            _, q, slot, cnt = d
            key = ("d", q, slot)
            if self.waited[eng].get(key, 0) >= cnt:
                return
            self.waited[eng][key] = cnt
            o["waits"].append(d)

    def op(self, eng, fn, reads=(), writes=(), dma=False):
        ops = self.ops[eng]
        idx = len(ops)
        o = dict(fn=fn, waits=[], signal=False, dma=None, cond=self.cur_cond)
        raw_deps = []
        oth_deps = []
        for r in reads:
            if r.w is not None:
                raw_deps.append(r.w)
        for r in writes:
            if r.w is not None:
                oth_deps.append(r.w)
            oth_deps.extend(r.r)
        if dma:
            n = self.dma_n[eng]
            slot = n % self.NQ
            cnt = 16 * (n // self.NQ + 1)
            self.dma_n[eng] += 1
            if n >= self.NQ:
                oth_deps.append(("d", eng, slot, cnt - 16))
            ev = ("d", eng, slot, cnt)
            o["dma"] = (slot, cnt)
        else:
            ev = ("c", eng, idx)
        best = {}
        for lst, raw in ((raw_deps, True), (oth_deps, False)):
            for d in lst:
                if d[0] == "c":
                    if d[1] == eng and (eng == "pe" or not raw):
                        continue
                    k = ("c", d[1])
                    if k not in best or best[k][2] < d[2]:
                        best[k] = d
                else:
                    k = ("d", d[1], d[2])
                    if k not in best or best[k][3] < d[3]:
                        best[k] = d
        for d in best.values():
            self._add_wait(eng, o, d, True)
        ops.append(o)
        for r in reads:
            r.r.append(ev)
        for r in writes:
            r.w = ev
            r.r = []
        return ev

    def barrier(self):
        evs = []
        for e in ENGS:
            for idx in range(len(self.ops[e]) - 1, -1, -1):
                o = self.ops[e][idx]
                if o["fn"] is not None and o["dma"] is None:
                    evs.append(("c", e, idx))
                    break
        for q in DMAQ:
            n = self.dma_n[q]
            for slot in range(min(n, self.NQ)):
                last_n = ((n - 1 - slot) // self.NQ) * self.NQ + slot
                evs.append(("d", q, slot, 16 * (last_n // self.NQ + 1)))
        for e in ENGS:
            assert self.cur_cond is None
            o = dict(fn=None, waits=[], signal=False, dma=None, cond=None)
            for d in evs:
                if d[0] == "c" and d[1] == e:
                    continue
                self._add_wait(e, o, d, True)
            self.ops[e].append(o)
        for r in self.res:
            r.w = None
            r.r = []

    def emit(self):
        nc = self.nc
        ranks = {}
        for e in ENGS:
            c = 0
            rk = []
            for o in self.ops[e]:
                if o["signal"]:
                    c += 1
                rk.append(c)
            ranks[e] = rk
        self.n_signal = {e: (ranks[e][-1] if ranks[e] else 0) for e in ENGS}
        self.n_ops = {e: len(self.ops[e]) for e in ENGS}
        with ExitStack() as st:
            csem = {e: st.enter_context(nc.semaphore("c_" + e)) for e in ENGS}
            dsem = {q: [st.enter_context(nc.semaphore("d_%s_%d" % (q, i))) for i in range(self.NQ)]
                    for q in DMAQ}
            block = st.enter_context(nc.Block())

            def run(e):
                def emit_waits(eng, o):
                    for d in o["waits"]:
                        if d[0] == "c":
                            eng.wait_ge(csem[d[1]], ranks[d[1]][d[2]])
                        else:
                            eng.wait_ge(dsem[d[1]][d[2]], d[3])

                def emit_op(eng, o, vals):
                    emit_waits(eng, o)
                    fn = o["fn"]
                    if fn is None:
                        return
                    if isinstance(fn, tuple) and fn[0] == "vload":
                        ap, mx = self.vload_specs[fn[1]]
                        vals[fn[1]] = eng.value_load(ap)
                        if o["signal"]:
                            eng.sem_inc(csem[e], 1)
                        return
                    ins = fn(eng)
                    if o["dma"] is not None:
                        ins.then_inc(dsem[e][o["dma"][0]], 16)
                    elif o["signal"]:
                        ins.then_inc(csem[e], 1)

                import os as _os2
                _cond_eng = _os2.environ.get("COND_ENG", "pe,act,dve,pool,sp").split(",")

                dma_before = []
                _cur = {}
                for o_ in self.ops[e]:
                    dma_before.append(dict(_cur))
                    if o_["dma"] is not None:
                        _cur[o_["dma"][0]] = o_["dma"][1]

                def body(eng):
                    vals = {}
                    ops = self.ops[e]
                    n = len(ops)
                    i = 0
                    while i < n:
                        o = ops[i]
                        if o["cond"] is None or e not in _cond_eng:
                            emit_op(eng, o, vals)
                            i += 1
                            continue
                        name = o["cond"][0]
                        groups = []
                        j = i
                        while j < n and ops[j]["cond"] is not None and ops[j]["cond"][0] == name:
                            key = ops[j]["cond"]
                            k = j
                            while k < n and ops[k]["cond"] == key:
                                k += 1
                            if groups:
                                assert key[1] > groups[-1][0][1]
                            groups.append((key, j, k))
                            j = k

                        def comp(gi):
                            i0 = groups[gi][1]
                            rank_before = ranks[e][i0 - 1] if i0 > 0 else 0
                            rest = ops[i0:groups[-1][2]]
                            nsig = sum(1 for g in rest if g["signal"])
                            dcnt = {}
                            for g in rest:
                                if g["dma"] is not None:
                                    dcnt[g["dma"][0]] = dcnt.get(g["dma"][0], 0) + 1
                            if nsig:
                                if rank_before > 0:
                                    eng.wait_ge(csem[e], rank_before)
                                eng.sem_inc(csem[e], nsig)
                            if dcnt:
                                for slot in range(self.NQ):
                                    c0 = dma_before[i0].get(slot, 0)
                                    if c0 > 0:
                                        eng.wait_ge(dsem[e][slot], c0)
                                for slot, m in dcnt.items():
                                    eng.sem_inc(dsem[e][slot], 16 * m)
                            return nsig or dcnt

                        def emit_chain(gi):
                            if gi == len(groups):
                                return
                            key, a, b = groups[gi]
                            rest = ops[a:groups[-1][2]]
                            need_else = any(g["signal"] or g["dma"] is not None for g in rest)
                            with eng.If(vals[key[0]] > key[1]):
                                for g in ops[a:b]:
                                    emit_op(eng, g, vals)
                                emit_chain(gi + 1)
                            if need_else:
                                with eng.Else():
                                    comp(gi)
                        emit_chain(0)
                        i = groups[-1][2]
                return body

            block.tensor(run("pe"))
            block.scalar(run("act"))
            block.vector(run("dve"))
            block.gpsimd(run("pool"))
            block.sync(run("sp"))


class Buf:
    __slots__ = ("ap", "res")

    def __init__(self, ap, res):
        self.ap = ap
        self.res = res


class Ring:
    def __init__(self, bufs):
        self.bufs = bufs
        self.i = 0

    def next(self):
        b = self.bufs[self.i % len(self.bufs)]
        self.i += 1
        return b


def mm(out, lhsT, rhs, start, stop):
    return lambda e: e.matmul(out, lhsT=lhsT, rhs=rhs, start=start, stop=stop)


def tr(out, in_, ident):
    return lambda e: e.transpose(out=out, in_=in_, identity=ident)


def dma(out, in_):
    return lambda e: e.dma_start(out=out, in_=in_)


def act(out, in_, func, bias=0.0, scale=1.0):
    return lambda e: e.activation(out=out, in_=in_, func=func, bias=bias, scale=scale)


def tcopy(out, in_):
    return lambda e: e.tensor_copy(out=out, in_=in_)


def tt(out, in0, in1, op):
    return lambda e: e.tensor_tensor(out=out, in0=in0, in1=in1, op=op)


def ts(out, in0, s1, op0, s2=None, op1=None):
    if op1 is None:
        return lambda e: e.tensor_scalar(out=out, in0=in0, scalar1=s1, scalar2=None, op0=op0)
    return lambda e: e.tensor_scalar(out=out, in0=in0, scalar1=s1, scalar2=s2, op0=op0, op1=op1)


def stt(out, in0, scalar, in1, op0, op1, accum_out=None):
    if accum_out is None:
        return lambda e: e.scalar_tensor_tensor(out=out, in0=in0, scalar=scalar, in1=in1, op0=op0, op1=op1)
    return lambda e: e.scalar_tensor_tensor(out=out, in0=in0, scalar=scalar, in1=in1, op0=op0, op1=op1,
                                            accum_out=accum_out)


def memset(ap, v):
    return lambda e: e.memset(ap, v)


ARENA_BYTES = 206 * 1024


class Arena:
    def __init__(self, ap_bf16):
        self.base = ap_bf16
        self.off = 0
        self.floor = 0

    def alloc(self, shape, dtype):
        n = 1
        for s in shape:
            n *= s
        nbytes = n * (2 if dtype == BF16 else 4)
        nbytes_al = (nbytes + 63) // 64 * 64
        assert self.off + nbytes_al <= ARENA_BYTES, ("SBUF arena overflow", self.off, nbytes_al)
        v = self.base[:, self.off // 2:(self.off + nbytes) // 2]
        self.off += nbytes_al
        if dtype != BF16:
            v = v.bitcast(dtype)
        if len(shape) == 2:
            v = v.rearrange("p (a b) -> p a b", b=shape[1])
        elif len(shape) == 3:
            v = v.rearrange("p (a b c) -> p a b c", b=shape[1], c=shape[2])
        return v

    def mark(self):
        return self.off

    def reset(self, to):
        self.off = to


def build_program(debug=False, stop_after=None):
    nc = bass.Bass("TRN2", target_bir_lowering=False)
    kin = "ExternalInput"
    dt_ = lambda name, shape, dtype, kind: nc.dram_tensor(name, shape, dtype, kind=kind).ap()
    x_in = dt_("x", [NB, S, D], F32, kin)
    ctx_in = dt_("ctx", [NB, LC, D], F32, kin)
    c_fm = dt_("c_fm", [128, KC, 3], F32, kin)
    w_mod = dt_("w_mod", [DEPTH, D, 6 * D], F32, kin)
    b_mod_fm = dt_("b_mod_fm", [DEPTH, 128, 48], F32, kin)
    w_in = dt_("w_in", [DEPTH, D, 2560], F32, kin)
    w_uT = dt_("w_uT", [DEPTH, 256, D], F32, kin)
    w_f = dt_("w_f", [DEPTH, 256, 64], F32, kin)
    lam_qk = dt_("lam_qk", [DEPTH, 1, 256], F32, kin)
    subln_g = dt_("subln_g", [DEPTH, 1, 128], F32, kin)
    w_out = dt_("w_out", [DEPTH, D, D], F32, kin)
    ln_vecs = dt_("ln_vecs", [DEPTH, 4, 1, D], F32, kin)
    w_router = dt_("w_router", [D, NE], F32, kin)
    router_bias = dt_("router_bias", [1, NE], F32, kin)
    w_gate = dt_("w_gate", [DEPTH, NE, D, 512], F32, kin)
    w_up = dt_("w_up", [DEPTH, NE, D, 512], F32, kin)
    w_down = dt_("w_down", [DEPTH, NE, 512, D], F32, kin)
    dftc = dt_("dftc", [S, S], BF16, kin)
    dftns = dt_("dftns", [S, S], BF16, kin)
    dft256 = dt_("dft256", [2, LC, LC], BF16, kin)
    c64bd = dt_("c64bd", [2, 128, 128], F32, kin)
    rope_cs = dt_("rope_cs", [2, 128, S], F32, kin)
    rmat_d = dt_("rmat", [128, 128], BF16, kin)
    ident_d = dt_("ident", [128, 128], BF16, kin)
    identf_d = dt_("identf", [128, 128], F32, kin)
    uts_d = dt_("uts", [128, 128], F32, kin)
    ebase_d = dt_("ebase", [1, NE], F32, kin)
    iota_d = dt_("iota_p", [128, NE], F32, kin)
    out_d = dt_("out", [NB, S, D], F32, "ExternalOutput")
    skind = "ExternalOutput" if debug else "Internal"
    QT_s = dt_("QT_s", [NB, 128, 6, TT], BF16, skind)
    KT_s = dt_("KT_s", [NB, 128, 6, TT], BF16, skind)
    VAB_s = dt_("VAB_s", [NB, TT, 1280], BF16, skind)
    X1_s = dt_("X1_s", [NB, TT, D], F32, skind)
    X2_s = dt_("X2_s", [NB, TT, D], F32, skind)
    HG_s = dt_("HG_s", [NSLOT, D], BF16, "Internal")
    Y_s = [dt_("Y_s%d" % i, [NSLOT, 512], F32, "Internal") for i in range(2)]
    MIX_s = dt_("MIX_s", [NB, 128, 8, TT], BF16, skind) if debug else None
    DBG_s = dt_("DBG_s", [128, 4096], F32, skind) if debug else None
    DBG2_s = dt_("DBG2_s", [3, 128, 512], F32, skind) if debug else None

    st = ExitStack()
    arena_t = st.enter_context(nc.sbuf_tensor("arena", [128, ARENA_BYTES // 2], BF16))
    psum_t = st.enter_context(nc.psum_tensor("psum", [128, 4096], F32))
    P = Prog(nc)
    AR = Arena(arena_t[:, :])

    def bank(b, n=1):
        return psum_t[:, b * 512:(b + n) * 512]

    rp = [P.R("bank%d" % i) for i in range(8)]

    ident = AR.alloc([128], BF16)
    identf = AR.alloc([128], F32)
    onesf = AR.alloc([128], F32)
    rmat = AR.alloc([128], BF16)
    modT = AR.alloc([48, 3], F32)
    s1p_a = AR.alloc([KC, 3], F32)
    s1p_f = AR.alloc([KC, 3], F32)
    nlam = AR.alloc([1], F32)
    gsub = AR.alloc([128], F32)
    rbias = AR.alloc([NE], F32)
    wr_sb = AR.alloc([KC, NE], BF16)
    r_const = P.R("const")
    r_mod = P.R("mod")
    P.op("sp", dma(ident, ident_d), writes=[r_const], dma=True)
    P.op("sp", dma(identf, identf_d), writes=[r_const], dma=True)
    P.op("sp", dma(rmat, rmat_d), writes=[r_const], dma=True)
    P.op("sp", dma(rbias, router_bias.partition_broadcast(128)), writes=[r_const], dma=True)
    P.op("pool", dma(wr_sb, w_router.rearrange("(kc p) e -> p kc e", p=128)), writes=[r_const], dma=True)
    P.op("dve", memset(onesf, 1.0), writes=[r_const])
    P.barrier()
    PERSIST = AR.mark()

    def ln_stats(xt_ap, r_x, sm, r_sm):
        P.op("dve", lambda e: e.bn_stats(out=sm[:, 0:6], in_=xt_ap[:, 0:512]), reads=[r_x], writes=[r_sm])
        P.op("dve", lambda e: e.bn_stats(out=sm[:, 6:12], in_=xt_ap[:, 512:1024]), reads=[r_x], writes=[r_sm])
        P.op("dve", lambda e: e.bn_aggr(out=sm[:, 12:14], in_=sm[:, 0:12].rearrange("p (a b) -> p a b", b=6)),
             reads=[r_sm], writes=[r_sm])
        P.op("act", act(sm[:, 14:15], sm[:, 13:14], AF.Ln, bias=LN_EPS, scale=1.0), reads=[r_sm], writes=[r_sm])
        P.op("act", act(sm[:, 15:16], sm[:, 14:15], AF.Exp, scale=-0.5), reads=[r_sm], writes=[r_sm])
        P.op("dve", stt(sm[:, 16:17], sm[:, 12:13], -1.0, sm[:, 15:16], ALU.mult, ALU.mult),
             reads=[r_sm], writes=[r_sm])
        return sm[:, 15:16], sm[:, 16:17]

    def make_bcast(dst, r_dst, src_fm, diag, r_diag, pbank):
        for c in range(KC):
            P.op("dve", ts(diag, identf, src_fm[:, c:c + 1], ALU.mult), reads=[r_const, r_mod], writes=[r_diag])
            half, cc = c // 4, c % 4
            P.op("pe", mm(bank(pbank + half)[:, cc * 128:(cc + 1) * 128], onesf, diag, True, True),
                 reads=[r_const, r_diag], writes=[rp[pbank + half]])
        P.op("dve", tcopy(dst, bank(pbank, 2)), reads=[rp[pbank], rp[pbank + 1]], writes=[r_dst])

    _breg = {}

    def bound_reg(eng):
        if "r" not in _breg:
            _breg["r"] = eng.alloc_register("slot_bound")
            eng.reg_mov(_breg["r"], NSLOT - 1)
        return _breg["r"]

    def tile_src(l, b, t):
        if l == 0:
            if t < 2:
                return ctx_in[b, t * 128:(t + 1) * 128, :]
            return x_in[b, (t - 2) * 128:(t - 1) * 128, :]
        return X2_s[b, t * 128:(t + 1) * 128, :]

    for l in range(DEPTH):
        last = (l == DEPTH - 1)
        lam_init = 0.8 - 0.6 * math.exp(-0.3 * l)
        tiles_q = list(range(2, NT)) if last else list(range(NT))
        AR.reset(PERSIST)
        cfm = AR.alloc([KC, 3], F32)
        silu_c = AR.alloc([KC, 3], F32)
        bmod = AR.alloc([48], F32)
        lqb = AR.alloc([256], F32)
        junk = AR.alloc([64], F32)
        s12 = AR.alloc([4], F32)
        wm_ring = Ring([Buf(AR.alloc([KC, 1024], F32), P.R()) for _ in range(2)])
        r_c, r_s, r_b, r_l, r_j = P.R(), P.R(), P.R(), P.R(), P.R()
        P.op("sp", dma(cfm, c_fm), writes=[r_c], dma=True)
        P.op("sp", dma(bmod, b_mod_fm[l]), writes=[r_b], dma=True)
        P.op("sp", dma(lqb, lam_qk[l].partition_broadcast(128)), writes=[r_l], dma=True)
        P.op("sp", dma(gsub, subln_g[l].partition_broadcast(128)), writes=[r_mod], dma=True)
        P.op("act", act(silu_c, cfm, AF.Silu), reads=[r_c], writes=[r_s])
        psm = bank(0)[:, 0:144].rearrange("p (a b) -> p a b", b=3)
        for cg in range(6):
            wb = wm_ring.next()
            for kc in range(KC):
                P.op("sp", dma(wb.ap[:, kc, :], w_mod[l, kc * 128:(kc + 1) * 128, cg * 1024:(cg + 1) * 1024]),
                     writes=[wb.res], dma=True)
            for j in range(8):
                for kc in range(KC):
                    P.op("pe", mm(psm[:, cg * 8 + j, :], wb.ap[:, kc, j * 128:(j + 1) * 128], silu_c[:, kc, :],
                                  kc == 0, kc == KC - 1), reads=[wb.res, r_s], writes=[rp[0]])
        for j in range(3):
            P.op("dve", tt(modT[:, :, j], psm[:, :, j], bmod, ALU.add), reads=[rp[0], r_b], writes=[r_mod])
        P.op("dve", ts(s1p_a, modT[:, 8:16, :], 1.0, ALU.add), reads=[r_mod], writes=[r_mod])
        P.op("dve", ts(s1p_f, modT[:, 32:40, :], 1.0, ALU.add), reads=[r_mod], writes=[r_mod])
        P.op("dve", memset(s12, 0.0), writes=[r_j])
        P.op("dve", stt(junk, lqb[:, 0:64], 1.0, lqb[:, 64:128], ALU.mult, ALU.mult, accum_out=s12[:, 0:1]),
             reads=[r_l], writes=[r_j])
        P.op("dve", stt(junk, lqb[:, 128:192], 1.0, lqb[:, 192:256], ALU.mult, ALU.mult, accum_out=s12[:, 1:2]),
             reads=[r_l], writes=[r_j])
        P.op("act", act(s12[:, 2:4], s12[:, 0:2], AF.Exp), reads=[r_j], writes=[r_j])
        P.op("dve", tt(nlam, s12[:, 3:4], s12[:, 2:3], ALU.subtract), reads=[r_j], writes=[r_mod])
        P.op("dve", ts(nlam, nlam, -lam_init, ALU.add), reads=[r_mod], writes=[r_mod])
        P.op("dve", ts(gsub, gsub, 1.0 - lam_init, ALU.mult), reads=[r_mod], writes=[r_mod])
        P.barrier()
        sh_a = modT[:, 0:8, :]
        g_a = modT[:, 16:24, :]
        sh_f = modT[:, 24:32, :]
        g_f = modT[:, 40:48, :]

        import os as _os
        _skipmix = _os.environ.get("SKIP_MIX") is not None
        AR.reset(PERSIST)
        w_sb = AR.alloc([KC, WCOLS], BF16)
        r_w = P.R("w_in")
        cs_sb = AR.alloc([2, S], F32)
        r_cs = P.R()
        xt_ring = Ring([Buf(AR.alloc([D], F32), P.R()) for _ in range(2)])
        xn_ring = Ring([Buf(AR.alloc([D], BF16), P.R()) for _ in range(2)])
        sm_ring = Ring([Buf(AR.alloc([24], F32), P.R()) for _ in range(2)])
        hT_ring = Ring([Buf(AR.alloc([KC, 512], BF16), P.R()) for _ in range(2)])
        pl_ring = Ring([Buf(AR.alloc([512], BF16), P.R()) for _ in range(2)])
        t1_ring = Ring([Buf(AR.alloc([512], F32), P.R()) for _ in range(2)])
        t2_ring = Ring([Buf(AR.alloc([512], F32), P.R()) for _ in range(2)])
        qk_ring = Ring([Buf(AR.alloc([12, 512], BF16), P.R()) for _ in range(2)])
        vab_ring = Ring([Buf(AR.alloc([1280], BF16), P.R()) for _ in range(2)])
        c64 = AR.alloc([2, 128], F32)
        wf_sb = AR.alloc([2, 64], F32)
        bd = AR.alloc([4, 128], BF16)
        wuT = AR.alloc([2, D], BF16)
        r_a, r_bd = P.R(), P.R()
        P.op("sp", dma(c64, c64bd.rearrange("a p n -> p a n")), writes=[r_a], dma=True)
        P.op("sp", dma(wf_sb, w_f[l].rearrange("(j p) d -> p j d", p=128)), writes=[r_a], dma=True)
        P.op("pool", dma(wuT, w_uT[l].rearrange("(j p) k -> p j k", p=128)), writes=[r_a], dma=True)
        P.op("sp", dma(cs_sb, rope_cs.rearrange("a p n -> p a n")), writes=[r_cs], dma=True)
        for kc in range(KC):
            P.op("pool", dma(w_sb[:, kc, 512:WCOLS], w_in[l, kc * 128:(kc + 1) * 128, 256:2560]),
                 writes=[r_w], dma=True)
        P.op("dve", memset(bd, 0.0), writes=[r_bd])
        for cs in range(2):
            for j in range(2):
                idx = cs * 2 + j
                P.op("pe", mm(bank(0)[:, idx * 64:(idx + 1) * 64], c64[:, cs, :], wf_sb[:, j, :], True, True),
                     reads=[r_a], writes=[rp[0]])
        for idx in range(4):
            P.op("dve", tcopy(bd[0:64, idx, 0:64], bank(0)[0:64, idx * 64:(idx + 1) * 64]),
                 reads=[rp[0]], writes=[r_bd])
            P.op("dve", tcopy(bd[64:128, idx, 64:128], bank(0)[64:128, idx * 64:(idx + 1) * 64]),
                 reads=[rp[0]], writes=[r_bd])
        for kc in range(KC):
            pb = 2 + (kc % 2)
            for cs in range(2):
                for j in range(2):
                    idx = cs * 2 + j
                    P.op("pe", mm(bank(pb)[:, idx * 128:(idx + 1) * 128], wuT[:, j, kc * 128:(kc + 1) * 128],
                                  bd[:, idx, :], True, True), reads=[r_a, r_bd], writes=[rp[pb]])
            P.op("dve", tcopy(w_sb[:, kc, 0:512], bank(pb)), reads=[rp[pb]], writes=[r_w])

        blocks = []
        for b in range(NB):
            blocks.append((b, 0, 2))
            for i in range(4):
                blocks.append((b, 2 + 4 * i, 4))
        psT_i = [0]
        fm_i = [0]
        tm_i = [0]
        rot_i = [0]

        def p1_A(blk):
            b, t0, ntl = blk
            hb = hT_ring.next()
            for ti in range(ntl):
                t = t0 + ti
                j = 2 if t < 2 else b
                xb_, xnb, smb = xt_ring.next(), xn_ring.next(), sm_ring.next()
                P.op("sp", dma(xb_.ap, tile_src(l, b, t)), writes=[xb_.res], dma=True)
                rstd, nmr = ln_stats(xb_.ap, xb_.res, smb.ap, smb.res)
                P.op("act", act(xnb.ap, xb_.ap, AF.Identity, bias=nmr, scale=rstd),
                     reads=[xb_.res, smb.res], writes=[xnb.res])
                pb = psT_i[0] % 2
                psT_i[0] += 1
                psT = bank(pb).bitcast(BF16).rearrange("p (a b) -> p a b", b=128)
                for kc in range(KC):
                    P.op("pe", tr(psT[:, kc, :], xnb.ap[:, kc * 128:(kc + 1) * 128], ident),
                         reads=[xnb.res, r_const], writes=[rp[pb]])
                for kc in range(KC):
                    P.op("dve", ts(hb.ap[:, kc, ti * 128:(ti + 1) * 128], psT[:, kc, :], s1p_a[:, kc, j:j + 1],
                                   ALU.mult, sh_a[:, kc, j:j + 1], ALU.add),
                         reads=[rp[pb], r_mod], writes=[hb.res])
            return hb

        def p1_B(blk, hb):
            b, t0, ntl = blk
            n = ntl * 128
            is_ctx = t0 < 2
            tok0 = t0 * 128
            qb = qk_ring.next()
            for c in range(12):
                pb = 2 + fm_i[0] % 2
                fm_i[0] += 1
                col = 512 + c * 128
                for kc in range(KC):
                    P.op("pe", mm(bank(pb)[:, 0:n], w_sb[:, kc, col:col + 128], hb.ap[:, kc, 0:n],
                                  kc == 0, kc == KC - 1), reads=[r_w, hb.res], writes=[rp[pb]])
                if is_ctx:
                    P.op("act", tcopy_act(qb.ap[:, c, 0:n], bank(pb)[:, 0:n]), reads=[rp[pb]], writes=[qb.res])
                else:
                    pl, t1, t2 = pl_ring.next(), t1_ring.next(), t2_ring.next()
                    P.op("act", tcopy_act(pl.ap[:, 0:n], bank(pb)[:, 0:n]), reads=[rp[pb]], writes=[pl.res])
                    prb = 4 + rot_i[0] % 2
                    rot_i[0] += 1
                    P.op("pe", mm(bank(prb)[:, 0:n], rmat, pl.ap[:, 0:n], True, True),
                         reads=[pl.res, r_const], writes=[rp[prb]])
                    s0 = tok0 - LC
                    P.op("dve", tt(t1.ap[:, 0:n], pl.ap[:, 0:n], cs_sb[:, 0, s0:s0 + n], ALU.mult),
                         reads=[pl.res, r_cs], writes=[t1.res])
                    P.op("dve", tt(t2.ap[:, 0:n], bank(prb)[:, 0:n], cs_sb[:, 1, s0:s0 + n], ALU.mult),
                         reads=[rp[prb], r_cs], writes=[t2.res])
                    P.op("dve", tt(qb.ap[:, c, 0:n], t1.ap[:, 0:n], t2.ap[:, 0:n], ALU.add),
                         reads=[t1.res, t2.res], writes=[qb.res])
            P.op("pool", dma(QT_s[b, :, :, tok0:tok0 + n], qb.ap[:, 0:6, 0:n]), reads=[qb.res], dma=True)
            P.op("pool", dma(KT_s[b, :, :, tok0:tok0 + n], qb.ap[:, 6:12, 0:n]), reads=[qb.res], dma=True)
            for ti in range(ntl):
                vb = vab_ring.next()
                for (c0, c1, wc0) in ((0, 512, 0), (512, 1024, 2048), (1024, 1280, 2560)):
                    pb = 6 + tm_i[0] % 2
                    tm_i[0] += 1
                    w_ = c1 - c0
                    for kc in range(KC):
                        P.op("pe", mm(bank(pb)[:, 0:w_], hb.ap[:, kc, ti * 128:(ti + 1) * 128],
                                      w_sb[:, kc, wc0:wc0 + w_], kc == 0, kc == KC - 1),
                             reads=[r_w, hb.res], writes=[rp[pb]])
                    P.op("act", tcopy_act(vb.ap[:, c0:c1], bank(pb)[:, 0:w_]), reads=[rp[pb]], writes=[vb.res])
                r0 = tok0 + ti * 128
                P.op("pool", dma(VAB_s[b, r0:r0 + 128, :], vb.ap), reads=[vb.res], dma=True)

        def tcopy_act(out, in_):
            return lambda e: e.copy(out=out, in_=in_)

        prev = None
        for i in range(0 if _skipmix else len(blocks) + 1):
            cur = None
            if i < len(blocks):
                cur = (blocks[i], p1_A(blocks[i]))
            if prev is not None:
                p1_B(*prev)
            prev = cur
        P.barrier()
        if stop_after == ("P1", l):
            break

        for b in range(0 if _skipmix else NB):
            AR.reset(PERSIST)
            fT = AR.alloc([2, TT], BF16)
            mixA = AR.alloc([6, TT], BF16)
            r_fT, r_mix = P.R(), P.R()
            SUB = AR.mark()
            ab_sb = AR.alloc([NT, 512], BF16)
            d256 = AR.alloc([2, 2, LC], BF16)
            tb_ring = Ring([Buf(AR.alloc([16, 512], BF16), P.R()) for _ in range(2)])
            r_ab, r_d = P.R(), P.R()
            P.op("sp", dma(ab_sb, VAB_s[b, :, 0:512].rearrange("(t p) n -> p t n", p=128)), writes=[r_ab], dma=True)
            if not last:
                for cs in range(2):
                    P.op("sp", dma(d256[:, :, cs, :], dft256[cs].rearrange("(tc p) n -> p tc n", p=128)),
                         writes=[r_d], dma=True)
                for j in range(2):
                    k = 0
                    for cs in range(2):
                        for tc in range(2):
                            P.op("pe", mm(bank(j)[:, 0:LC], ab_sb[:, tc, cs * 256 + j * 128: cs * 256 + (j + 1) * 128],
                                          d256[:, tc, cs, :], k == 0, k == 3), reads=[r_ab, r_d], writes=[rp[j]])
                            k += 1
                    P.op("act", tcopy_act(fT[:, j, 0:LC], bank(j)[:, 0:LC]), reads=[rp[j]], writes=[r_fT])
            for tb in range(4):
                for cs in range(2):
                    tbuf = tb_ring.next()
                    src = (dftc if cs == 0 else dftns)[:, tb * 512:(tb + 1) * 512].rearrange("(tc p) n -> p tc n", p=128)
                    for hh in range(2):
                        P.op("sp", dma(tbuf.ap[:, hh * 8:(hh + 1) * 8, :], src[:, hh * 8:(hh + 1) * 8, :]),
                             writes=[tbuf.res], dma=True)
                    for j in range(2):
                        pb = 2 + (tb % 2) * 2 + j
                        for tc in range(16):
                            P.op("pe", mm(bank(pb), ab_sb[:, 2 + tc, cs * 256 + j * 128: cs * 256 + (j + 1) * 128],
                                          tbuf.ap[:, tc, :], cs == 0 and tc == 0, cs == 1 and tc == 15),
                                 reads=[r_ab, tbuf.res], writes=[rp[pb]])
                for j in range(2):
                    pb = 2 + (tb % 2) * 2 + j
                    P.op("act", tcopy_act(fT[:, j, LC + tb * 512: LC + (tb + 1) * 512], bank(pb)),
                         reads=[rp[pb]], writes=[r_fT])
            if debug:
                P.op("sp", dma(MIX_s[b, :, 0:2, :], fT), reads=[r_fT], dma=True)
            P.barrier()

            AR.reset(SUB)
            qT = AR.alloc([6, TT], BF16)
            kT = AR.alloc([6, TT], BF16)
            vaug = AR.alloc([NT, 6, VST], BF16)
            r_q, r_k, r_v = P.R(), P.R(), P.R()
            PT_ring = Ring([Buf(AR.alloc([NT, 512], BF16), P.R()) for _ in range(2)])
            tq_ring = Ring([Buf(AR.alloc([4, 128], F32), P.R()) for _ in range(2)])
            o_ring = Ring([Buf(AR.alloc([4, 128], F32), P.R()) for _ in range(2)])
            on_ring = Ring([Buf(AR.alloc([4, 128], BF16), P.R()) for _ in range(2)])
            rs_ring = Ring([Buf(AR.alloc([16], F32), P.R()) for _ in range(4)])
            junk2 = AR.alloc([128], F32)
            r_j2 = P.R()
            P.op("sp", dma(qT, QT_s[b]), writes=[r_q], dma=True)
            P.op("sp", dma(kT, KT_s[b]), writes=[r_k], dma=True)
            P.op("dve", memset(vaug[:, :, :, 128:VST], 1.0), writes=[r_v])
            for h in range(6):
                P.op("sp", dma(vaug[:, :, h, 0:128],
                               VAB_s[b, :, 512 + h * 128: 512 + (h + 1) * 128].rearrange("(t p) d -> p t d", p=128)),
                     writes=[r_v], dma=True)
            units = []
            if not last:
                for h in range(6):
                    for sub in range(2):
                        units.append((0, 2, [0, 1], h, sub))
            for qb_ in range(4):
                for h in range(6):
                    for sub in range(2):
                        units.append((LC + qb_ * 512, 4, list(range(NT)), h, sub))
            sg_i = [0]

            def att_S(u, ui):
                q0, nqt, kts, h, sub = u
                n = nqt * 128
                pt = PT_ring.next()
                p0, p1 = sub * 64, (sub + 1) * 64
                for g in range(0, len(kts), 2):
                    pb = (sg_i[0] % 2) * 2
                    sg_i[0] += 1
                    grp = kts[g:g + 2]
                    for gi, kt in enumerate(grp):
                        P.op("pe", mm(bank(pb + gi)[:, 0:n], kT[p0:p1, h, kt * 128:(kt + 1) * 128],
                                      qT[p0:p1, h, q0:q0 + n], True, True),
                             reads=[r_q, r_k], writes=[rp[pb + gi]])
                    src = bank(pb, 2).rearrange("p (a b) -> p a b", b=512)[:, 0:len(grp), 0:n]
                    P.op("act", act(pt.ap[:, g:g + len(grp), 0:n], src, AF.Exp, scale=0.125),
                         reads=[rp[pb], rp[pb + 1]], writes=[pt.res])
                return pt

            def acc_ap(par, qt):
                if qt < 3:
                    return bank(4 + 2 * par)[:, qt * 160: qt * 160 + 129]
                return bank(5 + 2 * par)[:, 0:129]

            def att_AV(u, ui, pt, state):
                q0, nqt, kts, h, sub = u
                par = ui % 2
                for qt in range(nqt):
                    a = acc_ap(par, qt)
                    for ki, kt in enumerate(kts):
                        P.op("pe", mm(a, pt.ap[:, ki, qt * 128:(qt + 1) * 128], vaug[:, kt, h, 0:129],
                                      ki == 0, ki == len(kts) - 1),
                             reads=[pt.res, r_v], writes=[rp[4 + 2 * par], rp[5 + 2 * par]])
                accr = [rp[4 + 2 * par], rp[5 + 2 * par]]
                rs = rs_ring.next()
                if sub == 0:
                    tq = tq_ring.next()
                    state["tq"] = tq
                    for qt in range(nqt):
                        a = acc_ap(par, qt)
                        P.op("dve", lambda e, o=rs.ap[:, qt:qt + 1], i=a[:, 128:129]: e.reciprocal(out=o, in_=i),
                             reads=accr, writes=[rs.res])
                        P.op("dve", ts(tq.ap[:, qt, :], a[:, 0:128], rs.ap[:, qt:qt + 1], ALU.mult),
                             reads=accr + [rs.res], writes=[tq.res])
                else:
                    tq = state["tq"]
                    ob, onb = o_ring.next(), on_ring.next()
                    P.op("dve", memset(rs.ap[:, 8:12], 0.0), writes=[rs.res])
                    for qt in range(nqt):
                        a = acc_ap(par, qt)
                        P.op("dve", lambda e, o=rs.ap[:, qt:qt + 1], i=a[:, 128:129]: e.reciprocal(out=o, in_=i),
                             reads=accr, writes=[rs.res])
                        P.op("dve", ts(rs.ap[:, 4 + qt:5 + qt], rs.ap[:, qt:qt + 1], nlam[:, 0:1], ALU.mult),
                             reads=[rs.res, r_mod], writes=[rs.res])
                        P.op("dve", stt(ob.ap[:, qt, :], a[:, 0:128], rs.ap[:, 4 + qt:5 + qt], tq.ap[:, qt, :],
                                        ALU.mult, ALU.add), reads=accr + [rs.res, tq.res], writes=[ob.res])
                        P.op("dve", stt(junk2, ob.ap[:, qt, :], 1.0, ob.ap[:, qt, :], ALU.mult, ALU.mult,
                                        accum_out=rs.ap[:, 8 + qt:9 + qt]), reads=[ob.res], writes=[rs.res, r_j2])
                    P.op("act", act(rs.ap[:, 12:12 + nqt], rs.ap[:, 8:8 + nqt], AF.Ln, bias=LN_EPS, scale=1.0 / 128),
                         reads=[rs.res], writes=[rs.res])
                    P.op("act", act(rs.ap[:, 12:12 + nqt], rs.ap[:, 12:12 + nqt], AF.Exp, scale=-0.5),
                         reads=[rs.res], writes=[rs.res])
                    for qt in range(nqt):
                        P.op("dve", stt(onb.ap[:, qt, :], ob.ap[:, qt, :], rs.ap[:, 12 + qt:13 + qt], gsub,
                                        ALU.mult, ALU.mult), reads=[ob.res, rs.res, r_mod], writes=[onb.res])
                    psT = bank(5 + 2 * par)[:, 256:512].bitcast(BF16).rearrange("p (a b) -> p a b", b=128)
                    for qt in range(nqt):
                        P.op("pe", tr(psT[:, qt, :], onb.ap[:, qt, :], ident), reads=[onb.res, r_const],
                             writes=[rp[5 + 2 * par]])
                    P.op("dve", tcopy(mixA[:, h, q0:q0 + nqt * 128].rearrange("p (a b) -> p a b", b=128), psT[:, 0:nqt, :]),
                         reads=[rp[5 + 2 * par]], writes=[r_mix])

            state = {}
            prevu = None
            for ui in range(len(units) + 1):
                curu = None
                if ui < len(units):
                    curu = (units[ui], ui, att_S(units[ui], ui))
                if prevu is not None:
                    att_AV(prevu[0], prevu[1], prevu[2], state)
                prevu = curu
            if debug:
                P.op("sp", dma(MIX_s[b, :, 2:8, :], mixA), reads=[r_mix], dma=True)
            P.barrier()

            AR.reset(SUB)
            wo_sb = AR.alloc([KC, D], BF16)
            gbc = [AR.alloc([D], F32) for _ in range(2)]
            lng = AR.alloc([D], F32)
            lnb = AR.alloc([D], F32)
            diag = AR.alloc([128], F32)
            r_wo, r_g, r_ln, r_dg = P.R(), P.R(), P.R(), P.R()
            xt_ring = Ring([Buf(AR.alloc([D], F32), P.R()) for _ in range(2)])
            z_ring = Ring([Buf(AR.alloc([D], F32), P.R()) for _ in range(2)])
            sm_ring = Ring([Buf(AR.alloc([24], F32), P.R()) for _ in range(2)])
            for kc in range(KC):
                P.op("pool", dma(wo_sb[:, kc, :], w_out[l, kc * 128:(kc + 1) * 128, :]), writes=[r_wo], dma=True)
            P.op("sp", dma(lng, ln_vecs[l, 0].partition_broadcast(128)), writes=[r_ln], dma=True)
            P.op("sp", dma(lnb, ln_vecs[l, 1].partition_broadcast(128)), writes=[r_ln], dma=True)
            make_bcast(gbc[0], r_g, g_a[:, :, b], diag, r_dg, 0)
            if not last:
                make_bcast(gbc[1], r_g, g_a[:, :, 2], diag, r_dg, 0)
            for ti, t in enumerate(tiles_q):
                xb_, zb, smb = xt_ring.next(), z_ring.next(), sm_ring.next()
                P.op("sp", dma(xb_.ap, tile_src(l, b, t)), writes=[xb_.res], dma=True)
                pb = 2 + (ti % 3) * 2
                for half in range(2):
                    for kc in range(KC):
                        lhs = fT[:, kc, t * 128:(t + 1) * 128] if kc < 2 else mixA[:, kc - 2, t * 128:(t + 1) * 128]
                        P.op("pe", mm(bank(pb + half), lhs, wo_sb[:, kc, half * 512:(half + 1) * 512],
                                      kc == 0, kc == KC - 1), reads=[r_fT, r_mix, r_wo], writes=[rp[pb + half]])
                gsel = gbc[1] if t < 2 else gbc[0]
                P.op("dve", tt(zb.ap, bank(pb, 2), gsel, ALU.mult), reads=[rp[pb], rp[pb + 1], r_g], writes=[zb.res])
                P.op("dve", stt(zb.ap, xb_.ap, ALPHA, zb.ap, ALU.mult, ALU.add), reads=[xb_.res, zb.res],
                     writes=[zb.res])
                rstd, nmr = ln_stats(zb.ap, zb.res, smb.ap, smb.res)
                P.op("act", act(xb_.ap, zb.ap, AF.Identity, bias=nmr, scale=rstd), reads=[zb.res, smb.res],
                     writes=[xb_.res])
                P.op("dve", tt(xb_.ap, xb_.ap, lng, ALU.mult), reads=[xb_.res, r_ln], writes=[xb_.res])
                P.op("dve", tt(xb_.ap, xb_.ap, lnb, ALU.add), reads=[xb_.res, r_ln], writes=[xb_.res])
                P.op("pool", dma(X1_s[b, t * 128:(t + 1) * 128, :], xb_.ap), reads=[xb_.res], dma=True)
            P.barrier()
        if stop_after == ("MIX", l):
            break

        tl = [(b, t) for b in range(NB) for t in tiles_q]
        NG = len(tl)
        AR.reset(PERSIST)
        slots_i = AR.alloc([NG, 2], I32)
        wts = AR.alloc([NG, 2], F32)
        cnt_i = AR.alloc([NE], I32)
        r_sl, r_cnt = P.R(), P.R()
        SUB = AR.mark()
        jl = [0, 1] if last else [0, 1, 2]
        s_bc = {j: AR.alloc([D], F32) for j in jl}
        h_bc = {j: AR.alloc([D], F32) for j in jl}
        diag = AR.alloc([128], F32)
        uts = AR.alloc([128], F32)
        ebase = AR.alloc([NE], F32)
        runb = AR.alloc([NE], F32)
        r_bc, r_dg, r_ut, r_run = P.R(), P.R(), P.R(), P.R()
        xt_ring = Ring([Buf(AR.alloc([D], F32), P.R()) for _ in range(2)])
        xn_ring = Ring([Buf(AR.alloc([D], BF16), P.R()) for _ in range(2)])
        hf_ring = Ring([Buf(AR.alloc([D], F32), P.R()) for _ in range(2)])
        h2_ring = Ring([Buf(AR.alloc([D], BF16), P.R()) for _ in range(3)])
        h2T_ring = Ring([Buf(AR.alloc([KC, 128], BF16), P.R()) for _ in range(2)])
        sm_ring = Ring([Buf(AR.alloc([24], F32), P.R()) for _ in range(2)])
        rt_ring = Ring([Buf(AR.alloc([256], F32), P.R()) for _ in range(2)])
        P.op("sp", dma(uts, uts_d), writes=[r_ut], dma=True)
        P.op("sp", dma(ebase, ebase_d.partition_broadcast(128)), writes=[r_ut], dma=True)
        P.op("sp", dma(runb, ebase_d.partition_broadcast(128)), writes=[r_run], dma=True)
        for j in jl:
            make_bcast(s_bc[j], r_bc, s1p_f[:, :, j], diag, r_dg, 6)
            make_bcast(h_bc[j], r_bc, sh_f[:, :, j], diag, r_dg, 6)
        for gt, (b, t) in enumerate(tl):
            j = 2 if t < 2 else b
            xb_, xnb, smb, rt = xt_ring.next(), xn_ring.next(), sm_ring.next(), rt_ring.next()
            hf, h2, h2T = hf_ring.next(), h2_ring.next(), h2T_ring.next()
            P.op("sp", dma(xb_.ap, tile_src(0, b, t) if _skipmix else X1_s[b, t * 128:(t + 1) * 128, :]),
                 writes=[xb_.res], dma=True)
            rstd, nmr = ln_stats(xb_.ap, xb_.res, smb.ap, smb.res)
            P.op("act", act(xnb.ap, xb_.ap, AF.Identity, bias=nmr, scale=rstd),
                 reads=[xb_.res, smb.res], writes=[xnb.res])
            P.op("dve", tt(hf.ap, xnb.ap, s_bc[j], ALU.mult), reads=[xnb.res, r_bc], writes=[hf.res])
            P.op("dve", tt(h2.ap, hf.ap, h_bc[j], ALU.add), reads=[hf.res, r_bc], writes=[h2.res])
            pb = gt % 2
            psT = bank(pb).bitcast(BF16).rearrange("p (a b) -> p a b", b=128)
            for kc in range(KC):
                P.op("pe", tr(psT[:, kc, :], h2.ap[:, kc * 128:(kc + 1) * 128], ident),
                     reads=[h2.res, r_const], writes=[rp[pb]])
            P.op("act", tcopy_act(h2T.ap, psT), reads=[rp[pb]], writes=[h2T.res])
            prb = 2 + gt % 2
            for kc in range(KC):
                P.op("pe", mm(bank(prb)[:, 0:NE], h2T.ap[:, kc, :], wr_sb[:, kc, :], kc == 0, kc == KC - 1),
                     reads=[h2T.res, r_const], writes=[rp[prb]])
            A = rt.ap
            sc, sel, w4, gs, m2, gm, msk, ww, tmp = (A[:, 0:16], A[:, 16:32], A[:, 32:64], A[:, 64:68],
                                                       A[:, 68:72], A[:, 72:76], A[:, 80:96], A[:, 96:112],
                                                       A[:, 112:120])
            cmb, slv, msl, indb, sf = A[:, 128:144], A[:, 144:160], A[:, 160:176], A[:, 176:192], A[:, 192:196]
            P.op("act", act(sc, bank(prb)[:, 0:NE], AF.Exp, scale=-1.0), reads=[rp[prb]], writes=[rt.res])
            rr = [rt.res]
            P.op("dve", ts(sc, sc, 1.0, ALU.add), reads=rr, writes=rr)
            P.op("dve", lambda e, o=sc: e.reciprocal(out=o, in_=o), reads=rr, writes=rr)
            P.op("dve", tt(sel, sc, rbias, ALU.add), reads=rr + [r_const], writes=rr)
            sv = sel.rearrange("p (g e) -> p g e", e=4)
            hi01, lo01, hi23, lo23 = w4[:, 0:4], w4[:, 4:8], w4[:, 8:12], w4[:, 12:16]
            m1, mid, lom = w4[:, 16:20], w4[:, 20:24], w4[:, 24:28]
            P.op("dve", tt(hi01, sv[:, :, 0], sv[:, :, 1], ALU.max), reads=rr, writes=rr)
            P.op("dve", tt(lo01, sv[:, :, 0], sv[:, :, 1], ALU.min), reads=rr, writes=rr)
            P.op("dve", tt(hi23, sv[:, :, 2], sv[:, :, 3], ALU.max), reads=rr, writes=rr)
            P.op("dve", tt(lo23, sv[:, :, 2], sv[:, :, 3], ALU.min), reads=rr, writes=rr)
            P.op("dve", tt(m1, hi01, hi23, ALU.max), reads=rr, writes=rr)
            P.op("dve", tt(mid, hi01, hi23, ALU.min), reads=rr, writes=rr)
            P.op("dve", tt(lom, lo01, lo23, ALU.max), reads=rr, writes=rr)
            P.op("dve", tt(m2, mid, lom, ALU.max), reads=rr, writes=rr)
            P.op("dve", tt(gs, m1, m2, ALU.add), reads=rr, writes=rr)
            P.op("dve", lambda e, o=tmp[:, 0:1], i=gs: e.tensor_reduce(out=o, in_=i, axis=AX.X, op=ALU.max),
                 reads=rr, writes=rr)
            P.op("dve", ts(gm, gs, tmp[:, 0:1], ALU.is_ge), reads=rr, writes=rr)
            mv_ = msk.rearrange("p (g e) -> p g e", e=4)
            for ee in range(4):
                P.op("dve", tt(mv_[:, :, ee], sv[:, :, ee], m2, ALU.is_ge), reads=rr, writes=rr)
                P.op("dve", tt(mv_[:, :, ee], mv_[:, :, ee], gm, ALU.mult), reads=rr, writes=rr)
            P.op("dve", tt(ww, sc, msk, ALU.mult), reads=rr, writes=rr)
            P.op("dve", lambda e, o=tmp[:, 1:2], i=ww: e.tensor_reduce(out=o, in_=i, axis=AX.X, op=ALU.add),
                 reads=rr, writes=rr)
            P.op("dve", lambda e, o=tmp[:, 2:3], i=tmp[:, 1:2]: e.reciprocal(out=o, in_=i), reads=rr, writes=rr)
            P.op("dve", ts(cmb, ww, tmp[:, 2:3], ALU.mult), reads=rr, writes=rr)
            ppb = 4 + gt % 2
            P.op("pe", mm(bank(ppb)[:, 0:NE], uts, msk, True, True), reads=[r_ut, rt.res], writes=[rp[ppb]])
            P.op("pe", mm(bank(ppb)[:, 32:32 + NE], onesf, msk, True, True), reads=[r_const, rt.res],
                 writes=[rp[ppb]])
            P.op("dve", tt(slv, bank(ppb)[:, 0:NE], runb, ALU.add), reads=[rp[ppb], r_run] + rr, writes=rr)
            P.op("dve", tt(runb, runb, bank(ppb)[:, 32:32 + NE], ALU.add), reads=[rp[ppb], r_run], writes=[r_run])
            P.op("dve", tt(msl, slv, msk, ALU.mult), reads=rr, writes=rr)
            P.op("dve", lambda e, o=sf[:, 1:2], i=msl: e.tensor_reduce(out=o, in_=i, axis=AX.X, op=ALU.max),
                 reads=rr, writes=rr)
            P.op("dve", lambda e, o=tmp[:, 3:4], i=msl: e.tensor_reduce(out=o, in_=i, axis=AX.X, op=ALU.add),
                 reads=rr, writes=rr)
            P.op("dve", tt(sf[:, 0:1], tmp[:, 3:4], sf[:, 1:2], ALU.subtract), reads=rr, writes=rr)
            P.op("dve", ts(indb, msl, sf[:, 1:2], ALU.is_equal), reads=rr, writes=rr)
            P.op("dve", tt(indb, indb, cmb, ALU.mult), reads=rr, writes=rr)
            P.op("dve", lambda e, o=wts[:, gt, 1:2], i=indb: e.tensor_reduce(out=o, in_=i, axis=AX.X, op=ALU.add),
                 reads=rr, writes=[r_sl])
            P.op("dve", ts(wts[:, gt, 0:1], wts[:, gt, 1:2], -1.0, ALU.mult, 1.0, ALU.add), reads=[r_sl],
                 writes=[r_sl])
            P.op("dve", tcopy(slots_i[:, gt, :], sf[:, 0:2]), reads=rr, writes=[r_sl])
            for k in range(2):
                P.op("pool", lambda e, off=slots_i[:, gt, k:k + 1], src=h2.ap: e.indirect_dma_start(
                    out=HG_s[:, :], out_offset=bass.IndirectOffsetOnAxis(ap=off, axis=0), in_=src, in_offset=None,
                    bounds_check=bound_reg(e), oob_is_err=False), reads=[r_sl, h2.res], dma=True)
        zt = hf_ring.bufs[0]
        padf = rt_ring.bufs[0]
        P.op("sp", dma(padf.ap[:, 0:NE], iota_d), writes=[padf.res], dma=True)
        P.op("dve", memset(zt.ap.bitcast(BF16)[:, 0:D], 0.0), writes=[zt.res])
        P.op("dve", tt(padf.ap[:, 0:NE], padf.ap[:, 0:NE], runb, ALU.add), reads=[padf.res, r_run], writes=[padf.res])
        pad_i = padf.ap[:, 32:32 + NE].bitcast(I32)
        P.op("dve", tcopy(pad_i, padf.ap[:, 0:NE]), reads=[padf.res], writes=[padf.res])
        for e_ in range(NE):
            P.op("pool", lambda e, off=pad_i[:, e_:e_ + 1], src=zt.ap.bitcast(BF16)[:, 0:D]: e.indirect_dma_start(
                out=HG_s[:, :], out_offset=bass.IndirectOffsetOnAxis(ap=off, axis=0), in_=src, in_offset=None,
                bounds_check=bound_reg(e), oob_is_err=False), reads=[padf.res, zt.res], dma=True)
        P.op("dve", tt(runb, runb, ebase, ALU.subtract), reads=[r_run, r_ut, padf.res], writes=[r_run])
        P.op("dve", tcopy(cnt_i, runb), reads=[r_run], writes=[r_cnt])
        if debug and l == 0:
            P.op("sp", dma(DBG_s[:, 0:NG * 2], wts.rearrange("p a b -> p (a b)")), reads=[r_sl], dma=True)
            P.op("sp", dma(DBG_s[:, 256:256 + NE], runb), reads=[r_run], dma=True)
            P.op("dve", tcopy(xt_ring.bufs[0].ap[:, 0:NG * 2], slots_i.rearrange("p a b -> p (a b)")), reads=[r_sl],
                 writes=[xt_ring.bufs[0].res])
            P.op("sp", dma(DBG_s[:, 512:512 + NG * 2], xt_ring.bufs[0].ap[:, 0:NG * 2]),
                 reads=[xt_ring.bufs[0].res], dma=True)
        P.barrier()
        if stop_after == ("ROUTE", l):
            break
        for e_ in range(NE):
            P.vload("cnt%d_%d" % (l, e_), cnt_i[0:1, e_:e_ + 1], r_cnt, NB * TT)

        AR.reset(SUB)
        w_ring = Ring([Buf((AR.alloc([KC, 512], BF16), AR.alloc([KC, 512], BF16), AR.alloc([4, D], BF16)), P.R())
                       for _ in range(2)])
        hg_ring = Ring([Buf(AR.alloc([D], BF16), P.R()) for _ in range(3)])
        hgT_ring = Ring([Buf(AR.alloc([KC, 128], BF16), P.R()) for _ in range(2)])
        sg_ring = Ring([Buf(AR.alloc([512], F32), P.R()) for _ in range(2)])
        a_ring = Ring([Buf(AR.alloc([512], BF16), P.R()) for _ in range(2)])
        aT_ring = Ring([Buf(AR.alloc([4, 128], BF16), P.R()) for _ in range(2)])
        y_ring = Ring([Buf(AR.alloc([D], F32), P.R()) for _ in range(2)])
        gu_i = [0]

        def moe_GU(e_, jt, wbuf):
            wg, wu, wd = wbuf.ap
            hg, hgT = hg_ring.next(), hgT_ring.next()
            r0 = e_ * CAP + jt * 128
            P.op("sp", dma(hg.ap, HG_s[r0:r0 + 128, :]), writes=[hg.res], dma=True)
            psT = bank(5).bitcast(BF16).rearrange("p (a b) -> p a b", b=128)
            for kc in range(KC):
                P.op("pe", tr(psT[:, kc, :], hg.ap[:, kc * 128:(kc + 1) * 128], ident),
                     reads=[hg.res, r_const], writes=[rp[5]])
            P.op("act", tcopy_act(hgT.ap, psT), reads=[rp[5]], writes=[hgT.res])
            pb = (gu_i[0] % 2) * 2
            gu_i[0] += 1
            for which, wmat in ((0, wg), (1, wu)):
                for kc in range(KC):
                    P.op("pe", mm(bank(pb + which), hgT.ap[:, kc, :], wmat[:, kc, :], kc == 0, kc == KC - 1),
                         reads=[hgT.res, wbuf.res], writes=[rp[pb + which]])
            sg, ab_ = sg_ring.next(), a_ring.next()
            P.op("act", act(sg.ap, bank(pb), AF.Silu), reads=[rp[pb]], writes=[sg.res])
            P.op("dve", tt(ab_.ap, bank(pb + 1), sg.ap, ALU.mult), reads=[rp[pb + 1], sg.res], writes=[ab_.res])
            return ab_

        def moe_D(e_, jt, wbuf, ab_):
            wg, wu, wd = wbuf.ap
            par = jt % 2
            psT = bank(4)[:, par * 256:(par + 1) * 256].bitcast(BF16).rearrange("p (a b) -> p a b", b=128)
            aT, yb = aT_ring.next(), y_ring.next()
            for f in range(4):
                P.op("pe", tr(psT[:, f, :], ab_.ap[:, f * 128:(f + 1) * 128], ident), reads=[ab_.res, r_const],
                     writes=[rp[4]])
            P.op("dve", tcopy(aT.ap, psT), reads=[rp[4]], writes=[aT.res])
            for half in range(2):
                for f in range(4):
                    P.op("pe", mm(bank(6 + half), aT.ap[:, f, :], wd[:, f, half * 512:(half + 1) * 512],
                                  f == 0, f == 3), reads=[aT.res, wbuf.res], writes=[rp[6 + half]])
            P.op("act", tcopy_act(yb.ap, bank(6, 2)), reads=[rp[6], rp[7]], writes=[yb.res])
            r0 = e_ * CAP + jt * 128
            for hf_ in range(2):
                P.op("sp", dma(Y_s[hf_][r0:r0 + 128, :], yb.ap[:, hf_ * 512:(hf_ + 1) * 512]), reads=[yb.res],
                     dma=True)

        import os as _os
        _ne = int(_os.environ.get("MOE_NE", NE))
        _nt = int(_os.environ.get("MOE_NT", NG))
        _nocond = _os.environ.get("MOE_NOCOND") is not None

        class _NoCond:
            def __enter__(s_):
                return None

            def __exit__(s_, *a):
                return False
        for e_ in range(_ne):
            wbuf = w_ring.next()
            wg, wu, wd = wbuf.ap
            for hh in range(2):
                P.op("pool", dma(wg[:, hh * 4:(hh + 1) * 4, :],
                                 w_gate[l, e_, hh * 512:(hh + 1) * 512, :].rearrange("(kc p) f -> p kc f", p=128)),
                     writes=[wbuf.res], dma=True)
                P.op("pool", dma(wu[:, hh * 4:(hh + 1) * 4, :],
                                 w_up[l, e_, hh * 512:(hh + 1) * 512, :].rearrange("(kc p) f -> p kc f", p=128)),
                     writes=[wbuf.res], dma=True)
                P.op("pool", dma(wd[:, hh * 2:(hh + 1) * 2, :],
                                 w_down[l, e_, hh * 256:(hh + 1) * 256, :].rearrange("(kc p) f -> p kc f", p=128)),
                     writes=[wbuf.res], dma=True)
            key = "cnt%d_%d" % (l, e_)
            for jt in range(_nt):
                with (_NoCond() if _nocond else P.cond((key, jt * 128))):
                    ab_ = moe_GU(e_, jt, wbuf)
                    moe_D(e_, jt, wbuf, ab_)
        P.barrier()

        if stop_after == ("EXP", l):
            if debug:
                for i_, jt_ in enumerate((0, 6, 7)):
                    P.op("sp", dma(DBG2_s[i_], Y_s[0][jt_ * 128:(jt_ + 1) * 128, :]), dma=True)
                P.barrier()
            break
        AR.reset(SUB)
        gbc = {j: AR.alloc([D], F32) for j in jl}
        lng = AR.alloc([D], F32)
        lnb = AR.alloc([D], F32)
        diag = AR.alloc([128], F32)
        r_g, r_ln, r_dg = P.R(), P.R(), P.R()
        xt_ring = Ring([Buf(AR.alloc([D], F32), P.R()) for _ in range(2)])
        z_ring = Ring([Buf(AR.alloc([D], F32), P.R()) for _ in range(2)])
        ya_ring = Ring([Buf(AR.alloc([D], F32), P.R()) for _ in range(2)])
        yb_ring = Ring([Buf(AR.alloc([D], F32), P.R()) for _ in range(2)])
        sm_ring = Ring([Buf(AR.alloc([24], F32), P.R()) for _ in range(2)])
        P.op("sp", dma(lng, ln_vecs[l, 2].partition_broadcast(128)), writes=[r_ln], dma=True)
        P.op("sp", dma(lnb, ln_vecs[l, 3].partition_broadcast(128)), writes=[r_ln], dma=True)
        for j in jl:
            make_bcast(gbc[j], r_g, g_f[:, :, j], diag, r_dg, 0)
        for gt, (b, t) in enumerate(tl):
            j = 2 if t < 2 else b
            xb_, zb, smb, ya, yb = xt_ring.next(), z_ring.next(), sm_ring.next(), ya_ring.next(), yb_ring.next()
            P.op("sp", dma(xb_.ap, X1_s[b, t * 128:(t + 1) * 128, :]), writes=[xb_.res], dma=True)
            for k, yy in ((0, ya), (1, yb)):
                for hf_ in range(2):
                    P.op("pool", lambda e, off=slots_i[:, gt, k:k + 1], dst=yy.ap[:, hf_ * 512:(hf_ + 1) * 512],
                         src=Y_s[hf_]: e.indirect_dma_start(
                        out=dst, out_offset=None, in_=src[:, :], in_offset=bass.IndirectOffsetOnAxis(ap=off, axis=0),
                        bounds_check=bound_reg(e), oob_is_err=False), reads=[r_sl], writes=[yy.res], dma=True)
            P.op("dve", ts(zb.ap, ya.ap, wts[:, gt, 0:1], ALU.mult), reads=[ya.res, r_sl], writes=[zb.res])
            P.op("dve", stt(zb.ap, yb.ap, wts[:, gt, 1:2], zb.ap, ALU.mult, ALU.add), reads=[yb.res, r_sl, zb.res],
                 writes=[zb.res])
            P.op("dve", tt(zb.ap, zb.ap, gbc[j], ALU.mult), reads=[zb.res, r_g], writes=[zb.res])
            P.op("dve", stt(zb.ap, xb_.ap, ALPHA, zb.ap, ALU.mult, ALU.add), reads=[xb_.res, zb.res],
                 writes=[zb.res])
            rstd, nmr = ln_stats(zb.ap, zb.res, smb.ap, smb.res)
            P.op("act", act(xb_.ap, zb.ap, AF.Identity, bias=nmr, scale=rstd), reads=[zb.res, smb.res],
                 writes=[xb_.res])
            P.op("dve", tt(xb_.ap, xb_.ap, lng, ALU.mult), reads=[xb_.res, r_ln], writes=[xb_.res])
            P.op("dve", tt(xb_.ap, xb_.ap, lnb, ALU.add), reads=[xb_.res, r_ln], writes=[xb_.res])
            dst = out_d[b, (t - 2) * 128:(t - 1) * 128, :] if last else X2_s[b, t * 128:(t + 1) * 128, :]
            P.op("sp", dma(dst, xb_.ap), reads=[xb_.res], dma=True)
        P.barrier()

    P.barrier()
    P.emit()
    st.close()
    return nc, P


def _consts():
    bf = ml_dtypes.bfloat16
    t = np.arange(S, dtype=np.float64)
    ang = 2.0 * np.pi * ((np.outer(t, t)) % S) / S
    dftc = (np.cos(ang) / math.sqrt(S)).astype(np.float32).astype(bf)
    dftns = (-np.sin(ang) / math.sqrt(S)).astype(np.float32).astype(bf)
    t2 = np.arange(LC, dtype=np.float64)
    a2 = 2.0 * np.pi * ((np.outer(t2, t2)) % LC) / LC
    dft256 = np.stack([np.cos(a2) / math.sqrt(LC), -np.sin(a2) / math.sqrt(LC)]).astype(np.float32).astype(bf)
    c = np.arange(64, dtype=np.float64)
    a3 = 2.0 * np.pi * ((np.outer(c, c)) % 64) / 64
    c64 = np.cos(a3) / 8.0
    s64 = np.sin(a3) / 8.0
    c64bd = np.zeros((2, 128, 128), np.float32)
    for i, m in enumerate((c64, s64)):
        c64bd[i, 0:64, 0:64] = m
        c64bd[i, 64:128, 64:128] = m
    freqs = (10000.0 ** (-np.arange(0, 32, 2, dtype=np.float32) / 32)).astype(np.float32)
    pos = np.arange(S)
    row = (pos // 64).astype(np.float32)
    col = (pos % 64).astype(np.float32)
    ang_row = row[:, None] * freqs
    ang_col = col[:, None] * freqs
    rope = np.zeros((2, 128, S), np.float32)
    for p in range(128):
        d = p % 64
        a = ang_row[:, d % 16] if d < 32 else ang_col[:, d % 16]
        rope[0, p] = np.cos(a)
        rope[1, p] = np.sin(a)
    R = np.zeros((128, 128), np.float32)
    for m in range(128):
        d = m % 64
        if (d % 32) < 16:
            R[m + 16, m] = -1.0
        else:
            R[m - 16, m] = 1.0
    return dict(dftc=dftc, dftns=dftns, dft256=dft256, c64bd=c64bd, rope_cs=rope,
                rmat=R.astype(bf), ident=np.eye(128, dtype=np.float32).astype(bf),
                uts=np.triu(np.ones((128, 128), np.float32), 1),
                ebase=(np.arange(NE, dtype=np.float32) * CAP).reshape(1, NE),
                iota_p=np.repeat(np.arange(128, dtype=np.float32)[:, None], NE, axis=1),
                identf=np.eye(128, dtype=np.float32))


def make_in_maps(inputs, cores=range(N_CORES)):
    f = lambda a: np.ascontiguousarray(np.asarray(a, dtype=np.float32))
    x, c, ctx, c_ctx = f(inputs["x"]), f(inputs["c"]), f(inputs["ctx"]), f(inputs["c_ctx"])
    shared = dict(
        w_mod=f(inputs["w_mod"]),
        b_mod_fm=np.ascontiguousarray(f(inputs["b_mod"]).reshape(DEPTH, 48, 128).transpose(0, 2, 1)),
        w_in=f(inputs["w_in"]),
        w_uT=np.ascontiguousarray(f(inputs["w_in"])[:, :, :256].transpose(0, 2, 1)),
        w_f=f(inputs["w_fourier"]).reshape(DEPTH, 256, 64),
        lam_qk=f(inputs["lam_qk"]).reshape(DEPTH, 1, 256),
        subln_g=f(inputs["subln_g"]).reshape(DEPTH, 1, 128),
        w_out=f(inputs["w_out"]),
        ln_vecs=np.ascontiguousarray(np.stack([f(inputs["ln_attn_g"]), f(inputs["ln_attn_b"]),
                                               f(inputs["ln_ffn_g"]), f(inputs["ln_ffn_b"])], axis=1)
                                     .reshape(DEPTH, 4, 1, D)),
        w_router=f(inputs["w_router"]),
        router_bias=f(inputs["router_bias"]).reshape(1, NE),
        w_gate=f(inputs["w_gate"]), w_up=f(inputs["w_up"]), w_down=f(inputs["w_down"]),
    )
    shared.update(_consts())
    maps = []
    for ci in cores:
        b0 = ci * NB
        cc = np.stack([c[b0], c[b0 + 1], c_ctx], axis=-1)
        c_fm = np.ascontiguousarray(cc.reshape(KC, 128, 3).transpose(1, 0, 2))
        m = dict(shared)
        m["x"] = np.ascontiguousarray(x[b0:b0 + NB])
        m["ctx"] = np.ascontiguousarray(ctx[b0:b0 + NB])
        m["c_fm"] = c_fm
        maps.append(m)
    return maps


_CACHE = {}


def kernel(**inputs):
    if "nc" not in _CACHE:
        _CACHE["nc"] = build_program()[0]
    nc = _CACHE["nc"]
    in_maps = make_in_maps(inputs)
    res = run_bass_kernel_spmd(nc, in_maps, core_ids=list(range(N_CORES)))
    out = np.concatenate([np.asarray(r["out"], dtype=np.float32) for r in res.results], axis=0)
    return out
```

```python
import math
from contextlib import ExitStack

import numpy as np
import ml_dtypes
import concourse.bass as bass
import concourse.mybir as mybir
from concourse.bass_utils import run_bass_kernel_spmd

F32 = mybir.dt.float32
BF16 = mybir.dt.bfloat16
ALU = mybir.AluOpType
AF = mybir.ActivationFunctionType
AX = mybir.AxisListType

N_CORES = 8
NB = 2
S = 2048
LC = 256
TT = S + LC
NT = TT // 128
D = 1024
KC = 8
DEPTH = 2
WCOLS = 2816
ALPHA = (2 * DEPTH) ** 0.25
LN_EPS = 1e-5
NE = 16
VST = 132
CAP = NB * TT + 128
NSLOT = NE * CAP
I32 = mybir.dt.int32

ENGS = ("pe", "act", "dve", "pool", "sp")
DMAQ = ("sp", "pool", "act")


class Res:
    __slots__ = ("name", "w", "r")

    def __init__(self, name=""):
        self.name = name
        self.w = None
        self.r = []


class Prog:
    def __init__(self, nc, nq=16):
        self.nc = nc
        self.NQ = nq
        self.ops = {e: [] for e in ENGS}
        self.waited = {e: {} for e in ENGS}
        self.dma_n = {q: 0 for q in DMAQ}
        self.res = []
        self.cur_cond = None
        self.vload_specs = {}

    def cond(self, key):
        prog = self

        class _C:
            def __enter__(s_):
                assert prog.cur_cond is None
                prog.cur_cond = key
                prog._saved_waited = {e: dict(prog.waited[e]) for e in ENGS}

            def __exit__(s_, *a):
                prog.cur_cond = None
                prog.waited = prog._saved_waited
                return False
        return _C()

    def vload(self, name, ap, res, max_val):
        self.vload_specs[name] = (ap, max_val)
        for e in ENGS:
            self.op(e, ("vload", name), reads=[res])

    def R(self, name=""):
        r = Res(name)
        self.res.append(r)
        return r

    def _add_wait(self, eng, o, d, raw):
        if d[0] == "c":
            _, e2, idx = d
            if e2 == eng and (eng == "pe" or not raw):
                return
            key = ("c", e2)
            if self.waited[eng].get(key, -1) >= idx:
                return
            self.waited[eng][key] = idx
            self.ops[e2][idx]["signal"] = True
            o["waits"].append(d)
        else:
            _, q, slot, cnt = d
            key = ("d", q, slot)
            if self.waited[eng].get(key, 0) >= cnt:
                return
            self.waited[eng][key] = cnt
            o["waits"].append(d)

    def op(self, eng, fn, reads=(), writes=(), dma=False):
        ops = self.ops[eng]
        idx = len(ops)
        o = dict(fn=fn, waits=[], signal=False, dma=None, cond=self.cur_cond)
        raw_deps = []
        oth_deps = []
        for r in reads:
            if r.w is not None:
                raw_deps.append(r.w)
        for r in writes:
            if r.w is not None:
                oth_deps.append(r.w)
            oth_deps.extend(r.r)
        if dma:
            n = self.dma_n[eng]
            slot = n % self.NQ
            cnt = 16 * (n // self.NQ + 1)
            self.dma_n[eng] += 1
            if n >= self.NQ:
                oth_deps.append(("d", eng, slot, cnt - 16))
            ev = ("d", eng, slot, cnt)
            o["dma"] = (slot, cnt)
        else:
            ev = ("c", eng, idx)
        best = {}
        for lst, raw in ((raw_deps, True), (oth_deps, False)):
            for d in lst:
                if d[0] == "c":
                    if d[1] == eng and (eng == "pe" or not raw):
                        continue
                    k = ("c", d[1])
                    if k not in best or best[k][2] < d[2]:
                        best[k] = d
                else:
                    k = ("d", d[1], d[2])
                    if k not in best or best[k][3] < d[3]:
                        best[k] = d
        for d in best.values():
            self._add_wait(eng, o, d, True)
        ops.append(o)
        for r in reads:
            r.r.append(ev)
        for r in writes:
            r.w = ev
            r.r = []
        return ev

    def barrier(self):
        evs = []
        for e in ENGS:
            for idx in range(len(self.ops[e]) - 1, -1, -1):
                o = self.ops[e][idx]
                if o["fn"] is not None and o["dma"] is None:
                    evs.append(("c", e, idx))
                    break
        for q in DMAQ:
            n = self.dma_n[q]
            for slot in range(min(n, self.NQ)):
                last_n = ((n - 1 - slot) // self.NQ) * self.NQ + slot
                evs.append(("d", q, slot, 16 * (last_n // self.NQ + 1)))
        for e in ENGS:
            assert self.cur_cond is None
            o = dict(fn=None, waits=[], signal=False, dma=None, cond=None)
            for d in evs:
                if d[0] == "c" and d[1] == e:
                    continue
                self._add_wait(e, o, d, True)
            self.ops[e].append(o)
        for r in self.res:
            r.w = None
            r.r = []

    def emit(self):
        nc = self.nc
        ranks = {}
        for e in ENGS:
            c = 0
            rk = []
            for o in self.ops[e]:
                if o["signal"]:
                    c += 1
                rk.append(c)
            ranks[e] = rk
        self.n_signal = {e: (ranks[e][-1] if ranks[e] else 0) for e in ENGS}
        self.n_ops = {e: len(self.ops[e]) for e in ENGS}
        with ExitStack() as st:
            csem = {e: st.enter_context(nc.semaphore("c_" + e)) for e in ENGS}
            dsem = {q: [st.enter_context(nc.semaphore("d_%s_%d" % (q, i))) for i in range(self.NQ)]
                    for q in DMAQ}
            block = st.enter_context(nc.Block())

            def run(e):
                def emit_waits(eng, o):
                    for d in o["waits"]:
                        if d[0] == "c":
                            eng.wait_ge(csem[d[1]], ranks[d[1]][d[2]])
                        else:
                            eng.wait_ge(dsem[d[1]][d[2]], d[3])

                def emit_op(eng, o, vals):
                    emit_waits(eng, o)
                    fn = o["fn"]
                    if fn is None:
                        return
                    if isinstance(fn, tuple) and fn[0] == "vload":
                        ap, mx = self.vload_specs[fn[1]]
                        vals[fn[1]] = eng.value_load(ap)
                        if o["signal"]:
                            eng.sem_inc(csem[e], 1)
                        return
                    ins = fn(eng)
                    if o["dma"] is not None:
                        ins.then_inc(dsem[e][o["dma"][0]], 16)
                    elif o["signal"]:
                        ins.then_inc(csem[e], 1)

                import os as _os2
                _cond_eng = _os2.environ.get("COND_ENG", "pe,act,dve,pool,sp").split(",")

                dma_before = []
                _cur = {}
                for o_ in self.ops[e]:
                    dma_before.append(dict(_cur))
                    if o_["dma"] is not None:
                        _cur[o_["dma"][0]] = o_["dma"][1]

                def body(eng):
                    vals = {}
                    ops = self.ops[e]
                    n = len(ops)
                    i = 0
                    while i < n:
                        o = ops[i]
                        if o["cond"] is None or e not in _cond_eng:
                            emit_op(eng, o, vals)
                            i += 1
                            continue
                        name = o["cond"][0]
                        groups = []
                        j = i
                        while j < n and ops[j]["cond"] is not None and ops[j]["cond"][0] == name:
                            key = ops[j]["cond"]
                            k = j
                            while k < n and ops[k]["cond"] == key:
                                k += 1
                            if groups:
                                assert key[1] > groups[-1][0][1]
                            groups.append((key, j, k))
                            j = k

                        def comp(gi):
                            i0 = groups[gi][1]
                            rank_before = ranks[e][i0 - 1] if i0 > 0 else 0
                            rest = ops[i0:groups[-1][2]]
                            nsig = sum(1 for g in rest if g["signal"])
                            dcnt = {}
                            for g in rest:
                                if g["dma"] is not None:
                                    dcnt[g["dma"][0]] = dcnt.get(g["dma"][0], 0) + 1
                            if nsig:
                                if rank_before > 0:
                                    eng.wait_ge(csem[e], rank_before)
                                eng.sem_inc(csem[e], nsig)
                            if dcnt:
                                for slot in range(self.NQ):
                                    c0 = dma_before[i0].get(slot, 0)
                                    if c0 > 0:
                                        eng.wait_ge(dsem[e][slot], c0)
                                for slot, m in dcnt.items():
                                    eng.sem_inc(dsem[e][slot], 16 * m)
                            return nsig or dcnt

                        def emit_chain(gi):
                            if gi == len(groups):
                                return
                            key, a, b = groups[gi]
                            rest = ops[a:groups[-1][2]]
                            need_else = any(g["signal"] or g["dma"] is not None for g in rest)
                            with eng.If(vals[key[0]] > key[1]):
                                for g in ops[a:b]:
                                    emit_op(eng, g, vals)
                                emit_chain(gi + 1)
                            if need_else:
                                with eng.Else():
                                    comp(gi)
                        emit_chain(0)
                        i = groups[-1][2]
                return body

            block.tensor(run("pe"))
            block.scalar(run("act"))
            block.vector(run("dve"))
            block.gpsimd(run("pool"))
            block.sync(run("sp"))


class Buf:
    __slots__ = ("ap", "res")

    def __init__(self, ap, res):
        self.ap = ap
        self.res = res


class Ring:
    def __init__(self, bufs):
        self.bufs = bufs
        self.i = 0

    def next(self):
        b = self.bufs[self.i % len(self.bufs)]
        self.i += 1
        return b


def mm(out, lhsT, rhs, start, stop):
    return lambda e: e.matmul(out, lhsT=lhsT, rhs=rhs, start=start, stop=stop)


def tr(out, in_, ident):
    return lambda e: e.transpose(out=out, in_=in_, identity=ident)


def dma(out, in_):
    return lambda e: e.dma_start(out=out, in_=in_)


def act(out, in_, func, bias=0.0, scale=1.0):
    return lambda e: e.activation(out=out, in_=in_, func=func, bias=bias, scale=scale)


def tcopy(out, in_):
    return lambda e: e.tensor_copy(out=out, in_=in_)


def tt(out, in0, in1, op):
    return lambda e: e.tensor_tensor(out=out, in0=in0, in1=in1, op=op)


def ts(out, in0, s1, op0, s2=None, op1=None):
    if op1 is None:
        return lambda e: e.tensor_scalar(out=out, in0=in0, scalar1=s1, scalar2=None, op0=op0)
    return lambda e: e.tensor_scalar(out=out, in0=in0, scalar1=s1, scalar2=s2, op0=op0, op1=op1)


def stt(out, in0, scalar, in1, op0, op1, accum_out=None):
    if accum_out is None:
        return lambda e: e.scalar_tensor_tensor(out=out, in0=in0, scalar=scalar, in1=in1, op0=op0, op1=op1)
    return lambda e: e.scalar_tensor_tensor(out=out, in0=in0, scalar=scalar, in1=in1, op0=op0, op1=op1,
                                            accum_out=accum_out)


def memset(ap, v):
    return lambda e: e.memset(ap, v)


ARENA_BYTES = 206 * 1024


class Arena:
    def __init__(self, ap_bf16):
        self.base = ap_bf16
        self.off = 0
        self.floor = 0

    def alloc(self, shape, dtype):
        n = 1
        for s in shape:
            n *= s
        nbytes = n * (2 if dtype == BF16 else 4)
        nbytes_al = (nbytes + 63) // 64 * 64
        assert self.off + nbytes_al <= ARENA_BYTES, ("SBUF arena overflow", self.off, nbytes_al)
        v = self.base[:, self.off // 2:(self.off + nbytes) // 2]
        self.off += nbytes_al
        if dtype != BF16:
            v = v.bitcast(dtype)
        if len(shape) == 2:
            v = v.rearrange("p (a b) -> p a b", b=shape[1])
        elif len(shape) == 3:
            v = v.rearrange("p (a b c) -> p a b c", b=shape[1], c=shape[2])
        return v

    def mark(self):
        return self.off

    def reset(self, to):
        self.off = to


def build_program(debug=False, stop_after=None):
    nc = bass.Bass("TRN2", target_bir_lowering=False)
    kin = "ExternalInput"
    dt_ = lambda name, shape, dtype, kind: nc.dram_tensor(name, shape, dtype, kind=kind).ap()
    x_in = dt_("x", [NB, S, D], F32, kin)
    ctx_in = dt_("ctx", [NB, LC, D], F32, kin)
    c_fm = dt_("c_fm", [128, KC, 3], F32, kin)
    w_mod = dt_("w_mod", [DEPTH, D, 6 * D], F32, kin)
    b_mod_fm = dt_("b_mod_fm", [DEPTH, 128, 48], F32, kin)
    w_in = dt_("w_in", [DEPTH, D, 2560], F32, kin)
    w_uT = dt_("w_uT", [DEPTH, 256, D], F32, kin)
    w_f = dt_("w_f", [DEPTH, 256, 64], F32, kin)
    lam_qk = dt_("lam_qk", [DEPTH, 1, 256], F32, kin)
    subln_g = dt_("subln_g", [DEPTH, 1, 128], F32, kin)
    w_out = dt_("w_out", [DEPTH, D, D], F32, kin)
    ln_vecs = dt_("ln_vecs", [DEPTH, 4, 1, D], F32, kin)
    w_router = dt_("w_router", [D, NE], F32, kin)
    router_bias = dt_("router_bias", [1, NE], F32, kin)
    w_gate = dt_("w_gate", [DEPTH, NE, D, 512], F32, kin)
    w_up = dt_("w_up", [DEPTH, NE, D, 512], F32, kin)
    w_down = dt_("w_down", [DEPTH, NE, 512, D], F32, kin)
    dftc = dt_("dftc", [S, S], BF16, kin)
    dftns = dt_("dftns", [S, S], BF16, kin)
    dft256 = dt_("dft256", [2, LC, LC], BF16, kin)
    c64bd = dt_("c64bd", [2, 128, 128], F32, kin)
    rope_cs = dt_("rope_cs", [2, 128, S], F32, kin)
    rmat_d = dt_("rmat", [128, 128], BF16, kin)
    ident_d = dt_("ident", [128, 128], BF16, kin)
    identf_d = dt_("identf", [128, 128], F32, kin)
    uts_d = dt_("uts", [128, 128], F32, kin)
    ebase_d = dt_("ebase", [1, NE], F32, kin)
    iota_d = dt_("iota_p", [128, NE], F32, kin)
    out_d = dt_("out", [NB, S, D], F32, "ExternalOutput")
    skind = "ExternalOutput" if debug else "Internal"
    QT_s = dt_("QT_s", [NB, 128, 6, TT], BF16, skind)
    KT_s = dt_("KT_s", [NB, 128, 6, TT], BF16, skind)
    VAB_s = dt_("VAB_s", [NB, TT, 1280], BF16, skind)
    X1_s = dt_("X1_s", [NB, TT, D], F32, skind)
    X2_s = dt_("X2_s", [NB, TT, D], F32, skind)
    HG_s = dt_("HG_s", [NSLOT, D], BF16, "Internal")
    Y_s = [dt_("Y_s%d" % i, [NSLOT, 512], F32, "Internal") for i in range(2)]
    MIX_s = dt_("MIX_s", [NB, 128, 8, TT], BF16, skind) if debug else None
    DBG_s = dt_("DBG_s", [128, 4096], F32, skind) if debug else None
    DBG2_s = dt_("DBG2_s", [3, 128, 512], F32, skind) if debug else None

    st = ExitStack()
    arena_t = st.enter_context(nc.sbuf_tensor("arena", [128, ARENA_BYTES // 2], BF16))
    psum_t = st.enter_context(nc.psum_tensor("psum", [128, 4096], F32))
    P = Prog(nc)
    AR = Arena(arena_t[:, :])

    def bank(b, n=1):
        return psum_t[:, b * 512:(b + n) * 512]

    rp = [P.R("bank%d" % i) for i in range(8)]

    ident = AR.alloc([128], BF16)
    identf = AR.alloc([128], F32)
    onesf = AR.alloc([128], F32)
    rmat = AR.alloc([128], BF16)
    modT = AR.alloc([48, 3], F32)
    s1p_a = AR.alloc([KC, 3], F32)
    s1p_f = AR.alloc([KC, 3], F32)
    nlam = AR.alloc([1], F32)
    gsub = AR.alloc([128], F32)
    rbias = AR.alloc([NE], F32)
    wr_sb = AR.alloc([KC, NE], BF16)
    r_const = P.R("const")
    r_mod = P.R("mod")
    P.op("sp", dma(ident, ident_d), writes=[r_const], dma=True)
    P.op("sp", dma(identf, identf_d), writes=[r_const], dma=True)
    P.op("sp", dma(rmat, rmat_d), writes=[r_const], dma=True)
    P.op("sp", dma(rbias, router_bias.partition_broadcast(128)), writes=[r_const], dma=True)
    P.op("pool", dma(wr_sb, w_router.rearrange("(kc p) e -> p kc e", p=128)), writes=[r_const], dma=True)
    P.op("dve", memset(onesf, 1.0), writes=[r_const])
    P.barrier()
    PERSIST = AR.mark()

    def ln_stats(xt_ap, r_x, sm, r_sm):
        P.op("dve", lambda e: e.bn_stats(out=sm[:, 0:6], in_=xt_ap[:, 0:512]), reads=[r_x], writes=[r_sm])
        P.op("dve", lambda e: e.bn_stats(out=sm[:, 6:12], in_=xt_ap[:, 512:1024]), reads=[r_x], writes=[r_sm])
        P.op("dve", lambda e: e.bn_aggr(out=sm[:, 12:14], in_=sm[:, 0:12].rearrange("p (a b) -> p a b", b=6)),
             reads=[r_sm], writes=[r_sm])
        P.op("act", act(sm[:, 14:15], sm[:, 13:14], AF.Ln, bias=LN_EPS, scale=1.0), reads=[r_sm], writes=[r_sm])
        P.op("act", act(sm[:, 15:16], sm[:, 14:15], AF.Exp, scale=-0.5), reads=[r_sm], writes=[r_sm])
        P.op("dve", stt(sm[:, 16:17], sm[:, 12:13], -1.0, sm[:, 15:16], ALU.mult, ALU.mult),
             reads=[r_sm], writes=[r_sm])
        return sm[:, 15:16], sm[:, 16:17]

    def make_bcast(dst, r_dst, src_fm, diag, r_diag, pbank):
        for c in range(KC):
            P.op("dve", ts(diag, identf, src_fm[:, c:c + 1], ALU.mult), reads=[r_const, r_mod], writes=[r_diag])
            half, cc = c // 4, c % 4
            P.op("pe", mm(bank(pbank + half)[:, cc * 128:(cc + 1) * 128], onesf, diag, True, True),
                 reads=[r_const, r_diag], writes=[rp[pbank + half]])
        P.op("dve", tcopy(dst, bank(pbank, 2)), reads=[rp[pbank], rp[pbank + 1]], writes=[r_dst])

    _breg = {}

    def bound_reg(eng):
        if "r" not in _breg:
            _breg["r"] = eng.alloc_register("slot_bound")
            eng.reg_mov(_breg["r"], NSLOT - 1)
        return _breg["r"]

    def tile_src(l, b, t):
        if l == 0:
            if t < 2:
                return ctx_in[b, t * 128:(t + 1) * 128, :]
            return x_in[b, (t - 2) * 128:(t - 1) * 128, :]
        return X2_s[b, t * 128:(t + 1) * 128, :]

    for l in range(DEPTH):
        last = (l == DEPTH - 1)
        lam_init = 0.8 - 0.6 * math.exp(-0.3 * l)
        tiles_q = list(range(2, NT)) if last else list(range(NT))
        AR.reset(PERSIST)
        cfm = AR.alloc([KC, 3], F32)
        silu_c = AR.alloc([KC, 3], F32)
        bmod = AR.alloc([48], F32)
        lqb = AR.alloc([256], F32)
        junk = AR.alloc([64], F32)
        s12 = AR.alloc([4], F32)
        wm_ring = Ring([Buf(AR.alloc([KC, 1024], F32), P.R()) for _ in range(2)])
        r_c, r_s, r_b, r_l, r_j = P.R(), P.R(), P.R(), P.R(), P.R()
        P.op("sp", dma(cfm, c_fm), writes=[r_c], dma=True)
        P.op("sp", dma(bmod, b_mod_fm[l]), writes=[r_b], dma=True)
        P.op("sp", dma(lqb, lam_qk[l].partition_broadcast(128)), writes=[r_l], dma=True)
        P.op("sp", dma(gsub, subln_g[l].partition_broadcast(128)), writes=[r_mod], dma=True)
        P.op("act", act(silu_c, cfm, AF.Silu), reads=[r_c], writes=[r_s])
        psm = bank(0)[:, 0:144].rearrange("p (a b) -> p a b", b=3)
        for cg in range(6):
            wb = wm_ring.next()
            for kc in range(KC):
                P.op("sp", dma(wb.ap[:, kc, :], w_mod[l, kc * 128:(kc + 1) * 128, cg * 1024:(cg + 1) * 1024]),
                     writes=[wb.res], dma=True)
            for j in range(8):
                for kc in range(KC):
                    P.op("pe", mm(psm[:, cg * 8 + j, :], wb.ap[:, kc, j * 128:(j + 1) * 128], silu_c[:, kc, :],
                                  kc == 0, kc == KC - 1), reads=[wb.res, r_s], writes=[rp[0]])
        for j in range(3):
            P.op("dve", tt(modT[:, :, j], psm[:, :, j], bmod, ALU.add), reads=[rp[0], r_b], writes=[r_mod])
        P.op("dve", ts(s1p_a, modT[:, 8:16, :], 1.0, ALU.add), reads=[r_mod], writes=[r_mod])
        P.op("dve", ts(s1p_f, modT[:, 32:40, :], 1.0, ALU.add), reads=[r_mod], writes=[r_mod])
        P.op("dve", memset(s12, 0.0), writes=[r_j])
        P.op("dve", stt(junk, lqb[:, 0:64], 1.0, lqb[:, 64:128], ALU.mult, ALU.mult, accum_out=s12[:, 0:1]),
             reads=[r_l], writes=[r_j])
        P.op("dve", stt(junk, lqb[:, 128:192], 1.0, lqb[:, 192:256], ALU.mult, ALU.mult, accum_out=s12[:, 1:2]),
             reads=[r_l], writes=[r_j])
        P.op("act", act(s12[:, 2:4], s12[:, 0:2], AF.Exp), reads=[r_j], writes=[r_j])
        P.op("dve", tt(nlam, s12[:, 3:4], s12[:, 2:3], ALU.subtract), reads=[r_j], writes=[r_mod])
        P.op("dve", ts(nlam, nlam, -lam_init, ALU.add), reads=[r_mod], writes=[r_mod])
        P.op("dve", ts(gsub, gsub, 1.0 - lam_init, ALU.mult), reads=[r_mod], writes=[r_mod])
        P.barrier()
        sh_a = modT[:, 0:8, :]
        g_a = modT[:, 16:24, :]
        sh_f = modT[:, 24:32, :]
        g_f = modT[:, 40:48, :]

        import os as _os
        _skipmix = _os.environ.get("SKIP_MIX") is not None
        AR.reset(PERSIST)
        w_sb = AR.alloc([KC, WCOLS], BF16)
        r_w = P.R("w_in")
        cs_sb = AR.alloc([2, S], F32)
        r_cs = P.R()
        xt_ring = Ring([Buf(AR.alloc([D], F32), P.R()) for _ in range(2)])
        xn_ring = Ring([Buf(AR.alloc([D], BF16), P.R()) for _ in range(2)])
        sm_ring = Ring([Buf(AR.alloc([24], F32), P.R()) for _ in range(2)])
        hT_ring = Ring([Buf(AR.alloc([KC, 512], BF16), P.R()) for _ in range(2)])
        pl_ring = Ring([Buf(AR.alloc([512], BF16), P.R()) for _ in range(2)])
        t1_ring = Ring([Buf(AR.alloc([512], F32), P.R()) for _ in range(2)])
        t2_ring = Ring([Buf(AR.alloc([512], F32), P.R()) for _ in range(2)])
        qk_ring = Ring([Buf(AR.alloc([12, 512], BF16), P.R()) for _ in range(2)])
        vab_ring = Ring([Buf(AR.alloc([1280], BF16), P.R()) for _ in range(2)])
        c64 = AR.alloc([2, 128], F32)
        wf_sb = AR.alloc([2, 64], F32)
        bd = AR.alloc([4, 128], BF16)
        wuT = AR.alloc([2, D], BF16)
        r_a, r_bd = P.R(), P.R()
        P.op("sp", dma(c64, c64bd.rearrange("a p n -> p a n")), writes=[r_a], dma=True)
        P.op("sp", dma(wf_sb, w_f[l].rearrange("(j p) d -> p j d", p=128)), writes=[r_a], dma=True)
        P.op("pool", dma(wuT, w_uT[l].rearrange("(j p) k -> p j k", p=128)), writes=[r_a], dma=True)
        P.op("sp", dma(cs_sb, rope_cs.rearrange("a p n -> p a n")), writes=[r_cs], dma=True)
        for kc in range(KC):
            P.op("pool", dma(w_sb[:, kc, 512:WCOLS], w_in[l, kc * 128:(kc + 1) * 128, 256:2560]),
                 writes=[r_w], dma=True)
        P.op("dve", memset(bd, 0.0), writes=[r_bd])
        for cs in range(2):
            for j in range(2):
                idx = cs * 2 + j
                P.op("pe", mm(bank(0)[:, idx * 64:(idx + 1) * 64], c64[:, cs, :], wf_sb[:, j, :], True, True),
                     reads=[r_a], writes=[rp[0]])
        for idx in range(4):
            P.op("dve", tcopy(bd[0:64, idx, 0:64], bank(0)[0:64, idx * 64:(idx + 1) * 64]),
                 reads=[rp[0]], writes=[r_bd])
            P.op("dve", tcopy(bd[64:128, idx, 64:128], bank(0)[64:128, idx * 64:(idx + 1) * 64]),
                 reads=[rp[0]], writes=[r_bd])
        for kc in range(KC):
            pb = 2 + (kc % 2)
            for cs in range(2):
                for j in range(2):
                    idx = cs * 2 + j
                    P.op("pe", mm(bank(pb)[:, idx * 128:(idx + 1) * 128], wuT[:, j, kc * 128:(kc + 1) * 128],
                                  bd[:, idx, :], True, True), reads=[r_a, r_bd], writes=[rp[pb]])
            P.op("dve", tcopy(w_sb[:, kc, 0:512], bank(pb)), reads=[rp[pb]], writes=[r_w])

        blocks = []
        for b in range(NB):
            blocks.append((b, 0, 2))
            for i in range(4):
                blocks.append((b, 2 + 4 * i, 4))
        psT_i = [0]
        fm_i = [0]
        tm_i = [0]
        rot_i = [0]

        def p1_A(blk):
            b, t0, ntl = blk
            hb = hT_ring.next()
            for ti in range(ntl):
                t = t0 + ti
                j = 2 if t < 2 else b
                xb_, xnb, smb = xt_ring.next(), xn_ring.next(), sm_ring.next()
                P.op("sp", dma(xb_.ap, tile_src(l, b, t)), writes=[xb_.res], dma=True)
                rstd, nmr = ln_stats(xb_.ap, xb_.res, smb.ap, smb.res)
                P.op("act", act(xnb.ap, xb_.ap, AF.Identity, bias=nmr, scale=rstd),
                     reads=[xb_.res, smb.res], writes=[xnb.res])
                pb = psT_i[0] % 2
                psT_i[0] += 1
                psT = bank(pb).bitcast(BF16).rearrange("p (a b) -> p a b", b=128)
                for kc in range(KC):
                    P.op("pe", tr(psT[:, kc, :], xnb.ap[:, kc * 128:(kc + 1) * 128], ident),
                         reads=[xnb.res, r_const], writes=[rp[pb]])
                for kc in range(KC):
                    P.op("dve", ts(hb.ap[:, kc, ti * 128:(ti + 1) * 128], psT[:, kc, :], s1p_a[:, kc, j:j + 1],
                                   ALU.mult, sh_a[:, kc, j:j + 1], ALU.add),
                         reads=[rp[pb], r_mod], writes=[hb.res])
            return hb

        def p1_B(blk, hb):
            b, t0, ntl = blk
            n = ntl * 128
            is_ctx = t0 < 2
            tok0 = t0 * 128
            qb = qk_ring.next()
            for c in range(12):
                pb = 2 + fm_i[0] % 2
                fm_i[0] += 1
                col = 512 + c * 128
                for kc in range(KC):
                    P.op("pe", mm(bank(pb)[:, 0:n], w_sb[:, kc, col:col + 128], hb.ap[:, kc, 0:n],
                                  kc == 0, kc == KC - 1), reads=[r_w, hb.res], writes=[rp[pb]])
                if is_ctx:
                    P.op("act", tcopy_act(qb.ap[:, c, 0:n], bank(pb)[:, 0:n]), reads=[rp[pb]], writes=[qb.res])
                else:
                    pl, t1, t2 = pl_ring.next(), t1_ring.next(), t2_ring.next()
                    P.op("act", tcopy_act(pl.ap[:, 0:n], bank(pb)[:, 0:n]), reads=[rp[pb]], writes=[pl.res])
                    prb = 4 + rot_i[0] % 2
                    rot_i[0] += 1
                    P.op("pe", mm(bank(prb)[:, 0:n], rmat, pl.ap[:, 0:n], True, True),
                         reads=[pl.res, r_const], writes=[rp[prb]])
                    s0 = tok0 - LC
                    P.op("dve", tt(t1.ap[:, 0:n], pl.ap[:, 0:n], cs_sb[:, 0, s0:s0 + n], ALU.mult),
                         reads=[pl.res, r_cs], writes=[t1.res])
                    P.op("dve", tt(t2.ap[:, 0:n], bank(prb)[:, 0:n], cs_sb[:, 1, s0:s0 + n], ALU.mult),
                         reads=[rp[prb], r_cs], writes=[t2.res])
                    P.op("dve", tt(qb.ap[:, c, 0:n], t1.ap[:, 0:n], t2.ap[:, 0:n], ALU.add),
                         reads=[t1.res, t2.res], writes=[qb.res])
            P.op("pool", dma(QT_s[b, :, :, tok0:tok0 + n], qb.ap[:, 0:6, 0:n]), reads=[qb.res], dma=True)
            P.op("pool", dma(KT_s[b, :, :, tok0:tok0 + n], qb.ap[:, 6:12, 0:n]), reads=[qb.res], dma=True)
            for ti in range(ntl):
                vb = vab_ring.next()
                for (c0, c1, wc0) in ((0, 512, 0), (512, 1024, 2048), (1024, 1280, 2560)):
                    pb = 6 + tm_i[0] % 2
                    tm_i[0] += 1
                    w_ = c1 - c0
                    for kc in range(KC):
                        P.op("pe", mm(bank(pb)[:, 0:w_], hb.ap[:, kc, ti * 128:(ti + 1) * 128],
                                      w_sb[:, kc, wc0:wc0 + w_], kc == 0, kc == KC - 1),
                             reads=[r_w, hb.res], writes=[rp[pb]])
                    P.op("act", tcopy_act(vb.ap[:, c0:c1], bank(pb)[:, 0:w_]), reads=[rp[pb]], writes=[vb.res])
                r0 = tok0 + ti * 128
                P.op("pool", dma(VAB_s[b, r0:r0 + 128, :], vb.ap), reads=[vb.res], dma=True)

        def tcopy_act(out, in_):
            return lambda e: e.copy(out=out, in_=in_)

        prev = None
        for i in range(0 if _skipmix else len(blocks) + 1):
            cur = None
            if i < len(blocks):
                cur = (blocks[i], p1_A(blocks[i]))
            if prev is not None:
                p1_B(*prev)
            prev = cur
        P.barrier()
        if stop_after == ("P1", l):
            break

        for b in range(0 if _skipmix else NB):
            AR.reset(PERSIST)
            fT = AR.alloc([2, TT], BF16)
            mixA = AR.alloc([6, TT], BF16)
            r_fT, r_mix = P.R(), P.R()
            SUB = AR.mark()
            ab_sb = AR.alloc([NT, 512], BF16)
            d256 = AR.alloc([2, 2, LC], BF16)
            tb_ring = Ring([Buf(AR.alloc([16, 512], BF16), P.R()) for _ in range(2)])
            r_ab, r_d = P.R(), P.R()
            P.op("sp", dma(ab_sb, VAB_s[b, :, 0:512].rearrange("(t p) n -> p t n", p=128)), writes=[r_ab], dma=True)
            if not last:
                for cs in range(2):
                    P.op("sp", dma(d256[:, :, cs, :], dft256[cs].rearrange("(tc p) n -> p tc n", p=128)),
                         writes=[r_d], dma=True)
                for j in range(2):
                    k = 0
                    for cs in range(2):
                        for tc in range(2):
                            P.op("pe", mm(bank(j)[:, 0:LC], ab_sb[:, tc, cs * 256 + j * 128: cs * 256 + (j + 1) * 128],
                                          d256[:, tc, cs, :], k == 0, k == 3), reads=[r_ab, r_d], writes=[rp[j]])
                            k += 1
                    P.op("act", tcopy_act(fT[:, j, 0:LC], bank(j)[:, 0:LC]), reads=[rp[j]], writes=[r_fT])
            for tb in range(4):
                for cs in range(2):
                    tbuf = tb_ring.next()
                    src = (dftc if cs == 0 else dftns)[:, tb * 512:(tb + 1) * 512].rearrange("(tc p) n -> p tc n", p=128)
                    for hh in range(2):
                        P.op("sp", dma(tbuf.ap[:, hh * 8:(hh + 1) * 8, :], src[:, hh * 8:(hh + 1) * 8, :]),
                             writes=[tbuf.res], dma=True)
                    for j in range(2):
                        pb = 2 + (tb % 2) * 2 + j
                        for tc in range(16):
                            P.op("pe", mm(bank(pb), ab_sb[:, 2 + tc, cs * 256 + j * 128: cs * 256 + (j + 1) * 128],
                                          tbuf.ap[:, tc, :], cs == 0 and tc == 0, cs == 1 and tc == 15),
                                 reads=[r_ab, tbuf.res], writes=[rp[pb]])
                for j in range(2):
                    pb = 2 + (tb % 2) * 2 + j
                    P.op("act", tcopy_act(fT[:, j, LC + tb * 512: LC + (tb + 1) * 512], bank(pb)),
                         reads=[rp[pb]], writes=[r_fT])
            if debug:
                P.op("sp", dma(MIX_s[b, :, 0:2, :], fT), reads=[r_fT], dma=True)
            P.barrier()

            AR.reset(SUB)
            qT = AR.alloc([6, TT], BF16)
            kT = AR.alloc([6, TT], BF16)
            vaug = AR.alloc([NT, 6, VST], BF16)
            r_q, r_k, r_v = P.R(), P.R(), P.R()
            PT_ring = Ring([Buf(AR.alloc([NT, 512], BF16), P.R()) for _ in range(2)])
            tq_ring = Ring([Buf(AR.alloc([4, 128], F32), P.R()) for _ in range(2)])
            o_ring = Ring([Buf(AR.alloc([4, 128], F32), P.R()) for _ in range(2)])
            on_ring = Ring([Buf(AR.alloc([4, 128], BF16), P.R()) for _ in range(2)])
            rs_ring = Ring([Buf(AR.alloc([16], F32), P.R()) for _ in range(4)])
            junk2 = AR.alloc([128], F32)
            r_j2 = P.R()
            P.op("sp", dma(qT, QT_s[b]), writes=[r_q], dma=True)
            P.op("sp", dma(kT, KT_s[b]), writes=[r_k], dma=True)
            P.op("dve", memset(vaug[:, :, :, 128:VST], 1.0), writes=[r_v])
            for h in range(6):
                P.op("sp", dma(vaug[:, :, h, 0:128],
                               VAB_s[b, :, 512 + h * 128: 512 + (h + 1) * 128].rearrange("(t p) d -> p t d", p=128)),
                     writes=[r_v], dma=True)
            units = []
            if not last:
                for h in range(6):
                    for sub in range(2):
                        units.append((0, 2, [0, 1], h, sub))
            for qb_ in range(4):
                for h in range(6):
                    for sub in range(2):
                        units.append((LC + qb_ * 512, 4, list(range(NT)), h, sub))
            sg_i = [0]

            def att_S(u, ui):
                q0, nqt, kts, h, sub = u
                n = nqt * 128
                pt = PT_ring.next()
                p0, p1 = sub * 64, (sub + 1) * 64
                for g in range(0, len(kts), 2):
                    pb = (sg_i[0] % 2) * 2
                    sg_i[0] += 1
                    grp = kts[g:g + 2]
                    for gi, kt in enumerate(grp):
                        P.op("pe", mm(bank(pb + gi)[:, 0:n], kT[p0:p1, h, kt * 128:(kt + 1) * 128],
                                      qT[p0:p1, h, q0:q0 + n], True, True),
                             reads=[r_q, r_k], writes=[rp[pb + gi]])
                    src = bank(pb, 2).rearrange("p (a b) -> p a b", b=512)[:, 0:len(grp), 0:n]
                    P.op("act", act(pt.ap[:, g:g + len(grp), 0:n], src, AF.Exp, scale=0.125),
                         reads=[rp[pb], rp[pb + 1]], writes=[pt.res])
                return pt

            def acc_ap(par, qt):
                if qt < 3:
                    return bank(4 + 2 * par)[:, qt * 160: qt * 160 + 129]
                return bank(5 + 2 * par)[:, 0:129]

            def att_AV(u, ui, pt, state):
                q0, nqt, kts, h, sub = u
                par = ui % 2
                for qt in range(nqt):
                    a = acc_ap(par, qt)
                    for ki, kt in enumerate(kts):
                        P.op("pe", mm(a, pt.ap[:, ki, qt * 128:(qt + 1) * 128], vaug[:, kt, h, 0:129],
                                      ki == 0, ki == len(kts) - 1),
                             reads=[pt.res, r_v], writes=[rp[4 + 2 * par], rp[5 + 2 * par]])
                accr = [rp[4 + 2 * par], rp[5 + 2 * par]]
                rs = rs_ring.next()
                if sub == 0:
                    tq = tq_ring.next()
                    state["tq"] = tq
                    for qt in range(nqt):
                        a = acc_ap(par, qt)
                        P.op("dve", lambda e, o=rs.ap[:, qt:qt + 1], i=a[:, 128:129]: e.reciprocal(out=o, in_=i),
                             reads=accr, writes=[rs.res])
                        P.op("dve", ts(tq.ap[:, qt, :], a[:, 0:128], rs.ap[:, qt:qt + 1], ALU.mult),
                             reads=accr + [rs.res], writes=[tq.res])
                else:
                    tq = state["tq"]
                    ob, onb = o_ring.next(), on_ring.next()
                    P.op("dve", memset(rs.ap[:, 8:12], 0.0), writes=[rs.res])
                    for qt in range(nqt):
                        a = acc_ap(par, qt)
                        P.op("dve", lambda e, o=rs.ap[:, qt:qt + 1], i=a[:, 128:129]: e.reciprocal(out=o, in_=i),
                             reads=accr, writes=[rs.res])
                        P.op("dve", ts(rs.ap[:, 4 + qt:5 + qt], rs.ap[:, qt:qt + 1], nlam[:, 0:1], ALU.mult),
                             reads=[rs.res, r_mod], writes=[rs.res])
                        P.op("dve", stt(ob.ap[:, qt, :], a[:, 0:128], rs.ap[:, 4 + qt:5 + qt], tq.ap[:, qt, :],
                                        ALU.mult, ALU.add), reads=accr + [rs.res, tq.res], writes=[ob.res])
                        P.op("dve", stt(junk2, ob.ap[:, qt, :], 1.0, ob.ap[:, qt, :], ALU.mult, ALU.mult,
                                        accum_out=rs.ap[:, 8 + qt:9 + qt]), reads=[ob.res], writes=[rs.res, r_j2])
                    P.op("act", act(rs.ap[:, 12:12 + nqt], rs.ap[:, 8:8 + nqt], AF.Ln, bias=LN_EPS, scale=1.0 / 128),
                         reads=[rs.res], writes=[rs.res])
                    P.op("act", act(rs.ap[:, 12:12 + nqt], rs.ap[:, 12:12 + nqt], AF.Exp, scale=-0.5),
                         reads=[rs.res], writes=[rs.res])
                    for qt in range(nqt):
                        P.op("dve", stt(onb.ap[:, qt, :], ob.ap[:, qt, :], rs.ap[:, 12 + qt:13 + qt], gsub,
                                        ALU.mult, ALU.mult), reads=[ob.res, rs.res, r_mod], writes=[onb.res])
                    psT = bank(5 + 2 * par)[:, 256:512].bitcast(BF16).rearrange("p (a b) -> p a b", b=128)
                    for qt in range(nqt):
                        P.op("pe", tr(psT[:, qt, :], onb.ap[:, qt, :], ident), reads=[onb.res, r_const],
                             writes=[rp[5 + 2 * par]])
                    P.op("dve", tcopy(mixA[:, h, q0:q0 + nqt * 128].rearrange("p (a b) -> p a b", b=128), psT[:, 0:nqt, :]),
                         reads=[rp[5 + 2 * par]], writes=[r_mix])

            state = {}
            prevu = None
            for ui in range(len(units) + 1):
                curu = None
                if ui < len(units):
                    curu = (units[ui], ui, att_S(units[ui], ui))
                if prevu is not None:
                    att_AV(prevu[0], prevu[1], prevu[2], state)
                prevu = curu
            if debug:
                P.op("sp", dma(MIX_s[b, :, 2:8, :], mixA), reads=[r_mix], dma=True)
            P.barrier()

            AR.reset(SUB)
            wo_sb = AR.alloc([KC, D], BF16)
            gbc = [AR.alloc([D], F32) for _ in range(2)]
            lng = AR.alloc([D], F32)
            lnb = AR.alloc([D], F32)
            diag = AR.alloc([128], F32)
            r_wo, r_g, r_ln, r_dg = P.R(), P.R(), P.R(), P.R()
            xt_ring = Ring([Buf(AR.alloc([D], F32), P.R()) for _ in range(2)])
            z_ring = Ring([Buf(AR.alloc([D], F32), P.R()) for _ in range(2)])
            sm_ring = Ring([Buf(AR.alloc([24], F32), P.R()) for _ in range(2)])
            for kc in range(KC):
                P.op("pool", dma(wo_sb[:, kc, :], w_out[l, kc * 128:(kc + 1) * 128, :]), writes=[r_wo], dma=True)
            P.op("sp", dma(lng, ln_vecs[l, 0].partition_broadcast(128)), writes=[r_ln], dma=True)
            P.op("sp", dma(lnb, ln_vecs[l, 1].partition_broadcast(128)), writes=[r_ln], dma=True)
            make_bcast(gbc[0], r_g, g_a[:, :, b], diag, r_dg, 0)
            if not last:
                make_bcast(gbc[1], r_g, g_a[:, :, 2], diag, r_dg, 0)
            for ti, t in enumerate(tiles_q):
                xb_, zb, smb = xt_ring.next(), z_ring.next(), sm_ring.next()
                P.op("sp", dma(xb_.ap, tile_src(l, b, t)), writes=[xb_.res], dma=True)
                pb = 2 + (ti % 3) * 2
                for half in range(2):
                    for kc in range(KC):
                        lhs = fT[:, kc, t * 128:(t + 1) * 128] if kc < 2 else mixA[:, kc - 2, t * 128:(t + 1) * 128]
                        P.op("pe", mm(bank(pb + half), lhs, wo_sb[:, kc, half * 512:(half + 1) * 512],
                                      kc == 0, kc == KC - 1), reads=[r_fT, r_mix, r_wo], writes=[rp[pb + half]])
                gsel = gbc[1] if t < 2 else gbc[0]
                P.op("dve", tt(zb.ap, bank(pb, 2), gsel, ALU.mult), reads=[rp[pb], rp[pb + 1], r_g], writes=[zb.res])
                P.op("dve", stt(zb.ap, xb_.ap, ALPHA, zb.ap, ALU.mult, ALU.add), reads=[xb_.res, zb.res],
                     writes=[zb.res])
                rstd, nmr = ln_stats(zb.ap, zb.res, smb.ap, smb.res)
                P.op("act", act(xb_.ap, zb.ap, AF.Identity, bias=nmr, scale=rstd), reads=[zb.res, smb.res],
                     writes=[xb_.res])
                P.op("dve", tt(xb_.ap, xb_.ap, lng, ALU.mult), reads=[xb_.res, r_ln], writes=[xb_.res])
                P.op("dve", tt(xb_.ap, xb_.ap, lnb, ALU.add), reads=[xb_.res, r_ln], writes=[xb_.res])
                P.op("pool", dma(X1_s[b, t * 128:(t + 1) * 128, :], xb_.ap), reads=[xb_.res], dma=True)
            P.barrier()
        if stop_after == ("MIX", l):
            break

        tl = [(b, t) for b in range(NB) for t in tiles_q]
        NG = len(tl)
        AR.reset(PERSIST)
        slots_i = AR.alloc([NG, 2], I32)
        wts = AR.alloc([NG, 2], F32)
        cnt_i = AR.alloc([NE], I32)
        r_sl, r_cnt = P.R(), P.R()
        SUB = AR.mark()
        jl = [0, 1] if last else [0, 1, 2]
        s_bc = {j: AR.alloc([D], F32) for j in jl}
        h_bc = {j: AR.alloc([D], F32) for j in jl}
        diag = AR.alloc([128], F32)
        uts = AR.alloc([128], F32)
        ebase = AR.alloc([NE], F32)
        runb = AR.alloc([NE], F32)
        r_bc, r_dg, r_ut, r_run = P.R(), P.R(), P.R(), P.R()
        xt_ring = Ring([Buf(AR.alloc([D], F32), P.R()) for _ in range(2)])
        xn_ring = Ring([Buf(AR.alloc([D], BF16), P.R()) for _ in range(2)])
        hf_ring = Ring([Buf(AR.alloc([D], F32), P.R()) for _ in range(2)])
        h2_ring = Ring([Buf(AR.alloc([D], BF16), P.R()) for _ in range(3)])
        h2T_ring = Ring([Buf(AR.alloc([KC, 128], BF16), P.R()) for _ in range(2)])
        sm_ring = Ring([Buf(AR.alloc([24], F32), P.R()) for _ in range(2)])
        rt_ring = Ring([Buf(AR.alloc([256], F32), P.R()) for _ in range(2)])
        P.op("sp", dma(uts, uts_d), writes=[r_ut], dma=True)
        P.op("sp", dma(ebase, ebase_d.partition_broadcast(128)), writes=[r_ut], dma=True)
        P.op("sp", dma(runb, ebase_d.partition_broadcast(128)), writes=[r_run], dma=True)
        for j in jl:
            make_bcast(s_bc[j], r_bc, s1p_f[:, :, j], diag, r_dg, 6)
            make_bcast(h_bc[j], r_bc, sh_f[:, :, j], diag, r_dg, 6)
        for gt, (b, t) in enumerate(tl):
            j = 2 if t < 2 else b
            xb_, xnb, smb, rt = xt_ring.next(), xn_ring.next(), sm_ring.next(), rt_ring.next()
            hf, h2, h2T = hf_ring.next(), h2_ring.next(), h2T_ring.next()
            P.op("sp", dma(xb_.ap, tile_src(0, b, t) if _skipmix else X1_s[b, t * 128:(t + 1) * 128, :]),
                 writes=[xb_.res], dma=True)
            rstd, nmr = ln_stats(xb_.ap, xb_.res, smb.ap, smb.res)
            P.op("act", act(xnb.ap, xb_.ap, AF.Identity, bias=nmr, scale=rstd),
                 reads=[xb_.res, smb.res], writes=[xnb.res])
            P.op("dve", tt(hf.ap, xnb.ap, s_bc[j], ALU.mult), reads=[xnb.res, r_bc], writes=[hf.res])
            P.op("dve", tt(h2.ap, hf.ap, h_bc[j], ALU.add), reads=[hf.res, r_bc], writes=[h2.res])
            pb = gt % 2
            psT = bank(pb).bitcast(BF16).rearrange("p (a b) -> p a b", b=128)
            for kc in range(KC):
                P.op("pe", tr(psT[:, kc, :], h2.ap[:, kc * 128:(kc + 1) * 128], ident),
                     reads=[h2.res, r_const], writes=[rp[pb]])
            P.op("act", tcopy_act(h2T.ap, psT), reads=[rp[pb]], writes=[h2T.res])
            prb = 2 + gt % 2
            for kc in range(KC):
                P.op("pe", mm(bank(prb)[:, 0:NE], h2T.ap[:, kc, :], wr_sb[:, kc, :], kc == 0, kc == KC - 1),
                     reads=[h2T.res, r_const], writes=[rp[prb]])
            A = rt.ap
            sc, sel, w4, gs, m2, gm, msk, ww, tmp = (A[:, 0:16], A[:, 16:32], A[:, 32:64], A[:, 64:68],
                                                       A[:, 68:72], A[:, 72:76], A[:, 80:96], A[:, 96:112],
                                                       A[:, 112:120])
            cmb, slv, msl, indb, sf = A[:, 128:144], A[:, 144:160], A[:, 160:176], A[:, 176:192], A[:, 192:196]
            P.op("act", act(sc, bank(prb)[:, 0:NE], AF.Exp, scale=-1.0), reads=[rp[prb]], writes=[rt.res])
            rr = [rt.res]
            P.op("dve", ts(sc, sc, 1.0, ALU.add), reads=rr, writes=rr)
            P.op("dve", lambda e, o=sc: e.reciprocal(out=o, in_=o), reads=rr, writes=rr)
            P.op("dve", tt(sel, sc, rbias, ALU.add), reads=rr + [r_const], writes=rr)
            sv = sel.rearrange("p (g e) -> p g e", e=4)
            hi01, lo01, hi23, lo23 = w4[:, 0:4], w4[:, 4:8], w4[:, 8:12], w4[:, 12:16]
            m1, mid, lom = w4[:, 16:20], w4[:, 20:24], w4[:, 24:28]
            P.op("dve", tt(hi01, sv[:, :, 0], sv[:, :, 1], ALU.max), reads=rr, writes=rr)
            P.op("dve", tt(lo01, sv[:, :, 0], sv[:, :, 1], ALU.min), reads=rr, writes=rr)
            P.op("dve", tt(hi23, sv[:, :, 2], sv[:, :, 3], ALU.max), reads=rr, writes=rr)
            P.op("dve", tt(lo23, sv[:, :, 2], sv[:, :, 3], ALU.min), reads=rr, writes=rr)
            P.op("dve", tt(m1, hi01, hi23, ALU.max), reads=rr, writes=rr)
            P.op("dve", tt(mid, hi01, hi23, ALU.min), reads=rr, writes=rr)
            P.op("dve", tt(lom, lo01, lo23, ALU.max), reads=rr, writes=rr)
            P.op("dve", tt(m2, mid, lom, ALU.max), reads=rr, writes=rr)
            P.op("dve", tt(gs, m1, m2, ALU.add), reads=rr, writes=rr)
            P.op("dve", lambda e, o=tmp[:, 0:1], i=gs: e.tensor_reduce(out=o, in_=i, axis=AX.X, op=ALU.max),
                 reads=rr, writes=rr)
            P.op("dve", ts(gm, gs, tmp[:, 0:1], ALU.is_ge), reads=rr, writes=rr)
            mv_ = msk.rearrange("p (g e) -> p g e", e=4)
            for ee in range(4):
                P.op("dve", tt(mv_[:, :, ee], sv[:, :, ee], m2, ALU.is_ge), reads=rr, writes=rr)
                P.op("dve", tt(mv_[:, :, ee], mv_[:, :, ee], gm, ALU.mult), reads=rr, writes=rr)
            P.op("dve", tt(ww, sc, msk, ALU.mult), reads=rr, writes=rr)
            P.op("dve", lambda e, o=tmp[:, 1:2], i=ww: e.tensor_reduce(out=o, in_=i, axis=AX.X, op=ALU.add),
                 reads=rr, writes=rr)
            P.op("dve", lambda e, o=tmp[:, 2:3], i=tmp[:, 1:2]: e.reciprocal(out=o, in_=i), reads=rr, writes=rr)
            P.op("dve", ts(cmb, ww, tmp[:, 2:3], ALU.mult), reads=rr, writes=rr)
            ppb = 4 + gt % 2
            P.op("pe", mm(bank(ppb)[:, 0:NE], uts, msk, True, True), reads=[r_ut, rt.res], writes=[rp[ppb]])
            P.op("pe", mm(bank(ppb)[:, 32:32 + NE], onesf, msk, True, True), reads=[r_const, rt.res],
                 writes=[rp[ppb]])
            P.op("dve", tt(slv, bank(ppb)[:, 0:NE], runb, ALU.add), reads=[rp[ppb], r_run] + rr, writes=rr)
            P.op("dve", tt(runb, runb, bank(ppb)[:, 32:32 + NE], ALU.add), reads=[rp[ppb], r_run], writes=[r_run])
            P.op("dve", tt(msl, slv, msk, ALU.mult), reads=rr, writes=rr)
            P.op("dve", lambda e, o=sf[:, 1:2], i=msl: e.tensor_reduce(out=o, in_=i, axis=AX.X, op=ALU.max),
                 reads=rr, writes=rr)
            P.op("dve", lambda e, o=tmp[:, 3:4], i=msl: e.tensor_reduce(out=o, in_=i, axis=AX.X, op=ALU.add),
                 reads=rr, writes=rr)
            P.op("dve", tt(sf[:, 0:1], tmp[:, 3:4], sf[:, 1:2], ALU.subtract), reads=rr, writes=rr)
            P.op("dve", ts(indb, msl, sf[:, 1:2], ALU.is_equal), reads=rr, writes=rr)
            P.op("dve", tt(indb, indb, cmb, ALU.mult), reads=rr, writes=rr)
            P.op("dve", lambda e, o=wts[:, gt, 1:2], i=indb: e.tensor_reduce(out=o, in_=i, axis=AX.X, op=ALU.add),
                 reads=rr, writes=[r_sl])
            P.op("dve", ts(wts[:, gt, 0:1], wts[:, gt, 1:2], -1.0, ALU.mult, 1.0, ALU.add), reads=[r_sl],
                 writes=[r_sl])
            P.op("dve", tcopy(slots_i[:, gt, :], sf[:, 0:2]), reads=rr, writes=[r_sl])
            for k in range(2):
                P.op("pool", lambda e, off=slots_i[:, gt, k:k + 1], src=h2.ap: e.indirect_dma_start(
                    out=HG_s[:, :], out_offset=bass.IndirectOffsetOnAxis(ap=off, axis=0), in_=src, in_offset=None,
                    bounds_check=bound_reg(e), oob_is_err=False), reads=[r_sl, h2.res], dma=True)
        zt = hf_ring.bufs[0]
        padf = rt_ring.bufs[0]
        P.op("sp", dma(padf.ap[:, 0:NE], iota_d), writes=[padf.res], dma=True)
        P.op("dve", memset(zt.ap.bitcast(BF16)[:, 0:D], 0.0), writes=[zt.res])
        P.op("dve", tt(padf.ap[:, 0:NE], padf.ap[:, 0:NE], runb, ALU.add), reads=[padf.res, r_run], writes=[padf.res])
        pad_i = padf.ap[:, 32:32 + NE].bitcast(I32)
        P.op("dve", tcopy(pad_i, padf.ap[:, 0:NE]), reads=[padf.res], writes=[padf.res])
        for e_ in range(NE):
            P.op("pool", lambda e, off=pad_i[:, e_:e_ + 1], src=zt.ap.bitcast(BF16)[:, 0:D]: e.indirect_dma_start(
                out=HG_s[:, :], out_offset=bass.IndirectOffsetOnAxis(ap=off, axis=0), in_=src, in_offset=None,
                bounds_check=bound_reg(e), oob_is_err=False), reads=[padf.res, zt.res], dma=True)
        P.op("dve", tt(runb, runb, ebase, ALU.subtract), reads=[r_run, r_ut, padf.res], writes=[r_run])
        P.op("dve", tcopy(cnt_i, runb), reads=[r_run], writes=[r_cnt])
        if debug and l == 0:
            P.op("sp", dma(DBG_s[:, 0:NG * 2], wts.rearrange("p a b -> p (a b)")), reads=[r_sl], dma=True)
            P.op("sp", dma(DBG_s[:, 256:256 + NE], runb), reads=[r_run], dma=True)
            P.op("dve", tcopy(xt_ring.bufs[0].ap[:, 0:NG * 2], slots_i.rearrange("p a b -> p (a b)")), reads=[r_sl],
                 writes=[xt_ring.bufs[0].res])
            P.op("sp", dma(DBG_s[:, 512:512 + NG * 2], xt_ring.bufs[0].ap[:, 0:NG * 2]),
                 reads=[xt_ring.bufs[0].res], dma=True)
        P.barrier()
        if stop_after == ("ROUTE", l):
            break
        for e_ in range(NE):
            P.vload("cnt%d_%d" % (l, e_), cnt_i[0:1, e_:e_ + 1], r_cnt, NB * TT)

        AR.reset(SUB)
        w_ring = Ring([Buf((AR.alloc([KC, 512], BF16), AR.alloc([KC, 512], BF16), AR.alloc([4, D], BF16)), P.R())
                       for _ in range(2)])
        hg_ring = Ring([Buf(AR.alloc([D], BF16), P.R()) for _ in range(3)])
        hgT_ring = Ring([Buf(AR.alloc([KC, 128], BF16), P.R()) for _ in range(2)])
        sg_ring = Ring([Buf(AR.alloc([512], F32), P.R()) for _ in range(2)])
        a_ring = Ring([Buf(AR.alloc([512], BF16), P.R()) for _ in range(2)])
        aT_ring = Ring([Buf(AR.alloc([4, 128], BF16), P.R()) for _ in range(2)])
        y_ring = Ring([Buf(AR.alloc([D], F32), P.R()) for _ in range(2)])
        gu_i = [0]

        def moe_GU(e_, jt, wbuf):
            wg, wu, wd = wbuf.ap
            hg, hgT = hg_ring.next(), hgT_ring.next()
            r0 = e_ * CAP + jt * 128
            P.op("sp", dma(hg.ap, HG_s[r0:r0 + 128, :]), writes=[hg.res], dma=True)
            psT = bank(5).bitcast(BF16).rearrange("p (a b) -> p a b", b=128)
            for kc in range(KC):
                P.op("pe", tr(psT[:, kc, :], hg.ap[:, kc * 128:(kc + 1) * 128], ident),
                     reads=[hg.res, r_const], writes=[rp[5]])
            P.op("act", tcopy_act(hgT.ap, psT), reads=[rp[5]], writes=[hgT.res])
            pb = (gu_i[0] % 2) * 2
            gu_i[0] += 1
            for which, wmat in ((0, wg), (1, wu)):
                for kc in range(KC):
                    P.op("pe", mm(bank(pb + which), hgT.ap[:, kc, :], wmat[:, kc, :], kc == 0, kc == KC - 1),
                         reads=[hgT.res, wbuf.res], writes=[rp[pb + which]])
            sg, ab_ = sg_ring.next(), a_ring.next()
            P.op("act", act(sg.ap, bank(pb), AF.Silu), reads=[rp[pb]], writes=[sg.res])
            P.op("dve", tt(ab_.ap, bank(pb + 1), sg.ap, ALU.mult), reads=[rp[pb + 1], sg.res], writes=[ab_.res])
            return ab_

        def moe_D(e_, jt, wbuf, ab_):
            wg, wu, wd = wbuf.ap
            par = jt % 2
            psT = bank(4)[:, par * 256:(par + 1) * 256].bitcast(BF16).rearrange("p (a b) -> p a b", b=128)
            aT, yb = aT_ring.next(), y_ring.next()
            for f in range(4):
                P.op("pe", tr(psT[:, f, :], ab_.ap[:, f * 128:(f + 1) * 128], ident), reads=[ab_.res, r_const],
                     writes=[rp[4]])
            P.op("dve", tcopy(aT.ap, psT), reads=[rp[4]], writes=[aT.res])
            for half in range(2):
                for f in range(4):
                    P.op("pe", mm(bank(6 + half), aT.ap[:, f, :], wd[:, f, half * 512:(half + 1) * 512],
                                  f == 0, f == 3), reads=[aT.res, wbuf.res], writes=[rp[6 + half]])
            P.op("act", tcopy_act(yb.ap, bank(6, 2)), reads=[rp[6], rp[7]], writes=[yb.res])
            r0 = e_ * CAP + jt * 128
            for hf_ in range(2):
                P.op("act", dma(Y_s[hf_][r0:r0 + 128, :], yb.ap[:, hf_ * 512:(hf_ + 1) * 512]), reads=[yb.res],
                     dma=True)

        import os as _os
        _ne = int(_os.environ.get("MOE_NE", NE))
        _nt = int(_os.environ.get("MOE_NT", NG))
        _nocond = _os.environ.get("MOE_NOCOND") is not None

        class _NoCond:
            def __enter__(s_):
                return None

            def __exit__(s_, *a):
                return False
        for e_ in range(_ne):
            wbuf = w_ring.next()
            wg, wu, wd = wbuf.ap
            for hh in range(2):
                P.op("pool", dma(wg[:, hh * 4:(hh + 1) * 4, :],
                                 w_gate[l, e_, hh * 512:(hh + 1) * 512, :].rearrange("(kc p) f -> p kc f", p=128)),
                     writes=[wbuf.res], dma=True)
                P.op("pool", dma(wu[:, hh * 4:(hh + 1) * 4, :],
                                 w_up[l, e_, hh * 512:(hh + 1) * 512, :].rearrange("(kc p) f -> p kc f", p=128)),
                     writes=[wbuf.res], dma=True)
                P.op("pool", dma(wd[:, hh * 2:(hh + 1) * 2, :],
                                 w_down[l, e_, hh * 256:(hh + 1) * 256, :].rearrange("(kc p) f -> p kc f", p=128)),
                     writes=[wbuf.res], dma=True)
            key = "cnt%d_%d" % (l, e_)
            for jt in range(_nt):
                with (_NoCond() if _nocond else P.cond((key, jt * 128))):
                    ab_ = moe_GU(e_, jt, wbuf)
                    moe_D(e_, jt, wbuf, ab_)
        P.barrier()

        if stop_after == ("EXP", l):
            if debug:
                for i_, jt_ in enumerate((0, 6, 7)):
                    P.op("sp", dma(DBG2_s[i_], Y_s[0][jt_ * 128:(jt_ + 1) * 128, :]), dma=True)
                P.barrier()
            break
        AR.reset(SUB)
        gbc = {j: AR.alloc([D], F32) for j in jl}
        lng = AR.alloc([D], F32)
        lnb = AR.alloc([D], F32)
        diag = AR.alloc([128], F32)
        r_g, r_ln, r_dg = P.R(), P.R(), P.R()
        xt_ring = Ring([Buf(AR.alloc([D], F32), P.R()) for _ in range(2)])
        z_ring = Ring([Buf(AR.alloc([D], F32), P.R()) for _ in range(2)])
        ya_ring = Ring([Buf(AR.alloc([D], F32), P.R()) for _ in range(2)])
        yb_ring = Ring([Buf(AR.alloc([D], F32), P.R()) for _ in range(2)])
        sm_ring = Ring([Buf(AR.alloc([24], F32), P.R()) for _ in range(2)])
        P.op("sp", dma(lng, ln_vecs[l, 2].partition_broadcast(128)), writes=[r_ln], dma=True)
        P.op("sp", dma(lnb, ln_vecs[l, 3].partition_broadcast(128)), writes=[r_ln], dma=True)
        for j in jl:
            make_bcast(gbc[j], r_g, g_f[:, :, j], diag, r_dg, 0)
        for gt, (b, t) in enumerate(tl):
            j = 2 if t < 2 else b
            xb_, zb, smb, ya, yb = xt_ring.next(), z_ring.next(), sm_ring.next(), ya_ring.next(), yb_ring.next()
            P.op("sp", dma(xb_.ap, X1_s[b, t * 128:(t + 1) * 128, :]), writes=[xb_.res], dma=True)
            for k, yy in ((0, ya), (1, yb)):
                for hf_ in range(2):
                    P.op("pool", lambda e, off=slots_i[:, gt, k:k + 1], dst=yy.ap[:, hf_ * 512:(hf_ + 1) * 512],
                         src=Y_s[hf_]: e.indirect_dma_start(
                        out=dst, out_offset=None, in_=src[:, :], in_offset=bass.IndirectOffsetOnAxis(ap=off, axis=0),
                        bounds_check=bound_reg(e), oob_is_err=False), reads=[r_sl], writes=[yy.res], dma=True)
            P.op("dve", ts(zb.ap, ya.ap, wts[:, gt, 0:1], ALU.mult), reads=[ya.res, r_sl], writes=[zb.res])
            P.op("dve", stt(zb.ap, yb.ap, wts[:, gt, 1:2], zb.ap, ALU.mult, ALU.add), reads=[yb.res, r_sl, zb.res],
                 writes=[zb.res])
            P.op("dve", tt(zb.ap, zb.ap, gbc[j], ALU.mult), reads=[zb.res, r_g], writes=[zb.res])
            P.op("dve", stt(zb.ap, xb_.ap, ALPHA, zb.ap, ALU.mult, ALU.add), reads=[xb_.res, zb.res],
                 writes=[zb.res])
            rstd, nmr = ln_stats(zb.ap, zb.res, smb.ap, smb.res)
            P.op("act", act(xb_.ap, zb.ap, AF.Identity, bias=nmr, scale=rstd), reads=[zb.res, smb.res],
                 writes=[xb_.res])
            P.op("dve", tt(xb_.ap, xb_.ap, lng, ALU.mult), reads=[xb_.res, r_ln], writes=[xb_.res])
            P.op("dve", tt(xb_.ap, xb_.ap, lnb, ALU.add), reads=[xb_.res, r_ln], writes=[xb_.res])
            dst = out_d[b, (t - 2) * 128:(t - 1) * 128, :] if last else X2_s[b, t * 128:(t + 1) * 128, :]
            P.op("sp", dma(dst, xb_.ap), reads=[xb_.res], dma=True)
        P.barrier()

    P.barrier()
    P.emit()
    st.close()
    return nc, P


def _consts():
    bf = ml_dtypes.bfloat16
    t = np.arange(S, dtype=np.float64)
    ang = 2.0 * np.pi * ((np.outer(t, t)) % S) / S
    dftc = (np.cos(ang) / math.sqrt(S)).astype(np.float32).astype(bf)
    dftns = (-np.sin(ang) / math.sqrt(S)).astype(np.float32).astype(bf)
    t2 = np.arange(LC, dtype=np.float64)
    a2 = 2.0 * np.pi * ((np.outer(t2, t2)) % LC) / LC
    dft256 = np.stack([np.cos(a2) / math.sqrt(LC), -np.sin(a2) / math.sqrt(LC)]).astype(np.float32).astype(bf)
    c = np.arange(64, dtype=np.float64)
    a3 = 2.0 * np.pi * ((np.outer(c, c)) % 64) / 64
    c64 = np.cos(a3) / 8.0
    s64 = np.sin(a3) / 8.0
    c64bd = np.zeros((2, 128, 128), np.float32)
    for i, m in enumerate((c64, s64)):
        c64bd[i, 0:64, 0:64] = m
        c64bd[i, 64:128, 64:128] = m
    freqs = (10000.0 ** (-np.arange(0, 32, 2, dtype=np.float32) / 32)).astype(np.float32)
    pos = np.arange(S)
    row = (pos // 64).astype(np.float32)
    col = (pos % 64).astype(np.float32)
    ang_row = row[:, None] * freqs
    ang_col = col[:, None] * freqs
    rope = np.zeros((2, 128, S), np.float32)
    for p in range(128):
        d = p % 64
        a = ang_row[:, d % 16] if d < 32 else ang_col[:, d % 16]
        rope[0, p] = np.cos(a)
        rope[1, p] = np.sin(a)
    R = np.zeros((128, 128), np.float32)
    for m in range(128):
        d = m % 64
        if (d % 32) < 16:
            R[m + 16, m] = -1.0
        else:
            R[m - 16, m] = 1.0
    return dict(dftc=dftc, dftns=dftns, dft256=dft256, c64bd=c64bd, rope_cs=rope,
                rmat=R.astype(bf), ident=np.eye(128, dtype=np.float32).astype(bf),
                uts=np.triu(np.ones((128, 128), np.float32), 1),
                ebase=(np.arange(NE, dtype=np.float32) * CAP).reshape(1, NE),
                iota_p=np.repeat(np.arange(128, dtype=np.float32)[:, None], NE, axis=1),
                identf=np.eye(128, dtype=np.float32))


def make_in_maps(inputs, cores=range(N_CORES)):
    f = lambda a: np.ascontiguousarray(np.asarray(a, dtype=np.float32))
    x, c, ctx, c_ctx = f(inputs["x"]), f(inputs["c"]), f(inputs["ctx"]), f(inputs["c_ctx"])
    shared = dict(
        w_mod=f(inputs["w_mod"]),
        b_mod_fm=np.ascontiguousarray(f(inputs["b_mod"]).reshape(DEPTH, 48, 128).transpose(0, 2, 1)),
        w_in=f(inputs["w_in"]),
        w_uT=np.ascontiguousarray(f(inputs["w_in"])[:, :, :256].transpose(0, 2, 1)),
        w_f=f(inputs["w_fourier"]).reshape(DEPTH, 256, 64),
        lam_qk=f(inputs["lam_qk"]).reshape(DEPTH, 1, 256),
        subln_g=f(inputs["subln_g"]).reshape(DEPTH, 1, 128),
        w_out=f(inputs["w_out"]),
        ln_vecs=np.ascontiguousarray(np.stack([f(inputs["ln_attn_g"]), f(inputs["ln_attn_b"]),
                                               f(inputs["ln_ffn_g"]), f(inputs["ln_ffn_b"])], axis=1)
                                     .reshape(DEPTH, 4, 1, D)),
        w_router=f(inputs["w_router"]),
        router_bias=f(inputs["router_bias"]).reshape(1, NE),
        w_gate=f(inputs["w_gate"]), w_up=f(inputs["w_up"]), w_down=f(inputs["w_down"]),
    )
    shared.update(_consts())
    maps = []
    for ci in cores:
        b0 = ci * NB
        cc = np.stack([c[b0], c[b0 + 1], c_ctx], axis=-1)
        c_fm = np.ascontiguousarray(cc.reshape(KC, 128, 3).transpose(1, 0, 2))
        m = dict(shared)
        m["x"] = np.ascontiguousarray(x[b0:b0 + NB])
        m["ctx"] = np.ascontiguousarray(ctx[b0:b0 + NB])
        m["c_fm"] = c_fm
        maps.append(m)
    return maps


_CACHE = {}


def kernel(**inputs):
    if "nc" not in _CACHE:
        _CACHE["nc"] = build_program()[0]
    nc = _CACHE["nc"]
    in_maps = make_in_maps(inputs)
    res = run_bass_kernel_spmd(nc, in_maps, core_ids=list(range(N_CORES)))
    out = np.concatenate([np.asarray(r["out"], dtype=np.float32) for r in res.results], axis=0)
    return out
```

```python
import math
from contextlib import ExitStack

import numpy as np
import ml_dtypes
import concourse.bass as bass
import concourse.mybir as mybir
from concourse.bass_utils import run_bass_kernel_spmd

F32 = mybir.dt.float32
BF16 = mybir.dt.bfloat16
ALU = mybir.AluOpType
AF = mybir.ActivationFunctionType
AX = mybir.AxisListType

N_CORES = 8
NB = 2
S = 2048
LC = 256
TT = S + LC
NT = TT // 128
D = 1024
KC = 8
DEPTH = 2
WCOLS = 2816
ALPHA = (2 * DEPTH) ** 0.25
LN_EPS = 1e-5
NE = 16
VST = 132
CAP = NB * TT + 128
NSLOT = NE * CAP
I32 = mybir.dt.int32

ENGS = ("pe", "act", "dve", "pool", "sp")
DMAQ = ("sp", "pool", "act")


class Res:
    __slots__ = ("name", "w", "r")

    def __init__(self, name=""):
        self.name = name
        self.w = None
        self.r = []


class Prog:
    def __init__(self, nc, nq=16):
        self.nc = nc
        self.NQ = nq
        self.ops = {e: [] for e in ENGS}
        self.waited = {e: {} for e in ENGS}
        self.dma_n = {q: 0 for q in DMAQ}
        self.res = []
        self.cur_cond = None
        self.vload_specs = {}

    def cond(self, key):
        prog = self

        class _C:
            def __enter__(s_):
                assert prog.cur_cond is None
                prog.cur_cond = key
                prog._saved_waited = {e: dict(prog.waited[e]) for e in ENGS}

            def __exit__(s_, *a):
                prog.cur_cond = None
                prog.waited = prog._saved_waited
                return False
        return _C()

    def vload(self, name, ap, res, max_val):
        self.vload_specs[name] = (ap, max_val)
        for e in ENGS:
            self.op(e, ("vload", name), reads=[res])

    def R(self, name=""):
        r = Res(name)
        self.res.append(r)
        return r

    def _add_wait(self, eng, o, d, raw):
        if d[0] == "c":
            _, e2, idx = d
            if e2 == eng and (eng == "pe" or not raw):
                return
            key = ("c", e2)
            if self.waited[eng].get(key, -1) >= idx:
                return
            self.waited[eng][key] = idx
            self.ops[e2][idx]["signal"] = True
            o["waits"].append(d)
        else:
            _, q, slot, cnt = d
            key = ("d", q, slot)
            if self.waited[eng].get(key, 0) >= cnt:
                return
            self.waited[eng][key] = cnt
            o["waits"].append(d)

    def op(self, eng, fn, reads=(), writes=(), dma=False):
        ops = self.ops[eng]
        idx = len(ops)
        o = dict(fn=fn, waits=[], signal=False, dma=None, cond=self.cur_cond)
        raw_deps = []
        oth_deps = []
        for r in reads:
            if r.w is not None:
                raw_deps.append(r.w)
        for r in writes:
            if r.w is not None:
                oth_deps.append(r.w)
            oth_deps.extend(r.r)
        if dma:
            n = self.dma_n[eng]
            slot = n % self.NQ
            cnt = 16 * (n // self.NQ + 1)
            self.dma_n[eng] += 1
            if n >= self.NQ:
                oth_deps.append(("d", eng, slot, cnt - 16))
            ev = ("d", eng, slot, cnt)
            o["dma"] = (slot, cnt)
        else:
            ev = ("c", eng, idx)
        best = {}
        for lst, raw in ((raw_deps, True), (oth_deps, False)):
            for d in lst:
                if d[0] == "c":
                    if d[1] == eng and (eng == "pe" or not raw):
                        continue
                    k = ("c", d[1])
                    if k not in best or best[k][2] < d[2]:
                        best[k] = d
                else:
                    k = ("d", d[1], d[2])
                    if k not in best or best[k][3] < d[3]:
                        best[k] = d
        for d in best.values():
            self._add_wait(eng, o, d, True)
        ops.append(o)
        for r in reads:
            r.r.append(ev)
        for r in writes:
            r.w = ev
            r.r = []
        return ev

    def barrier(self):
        evs = []
        for e in ENGS:
            for idx in range(len(self.ops[e]) - 1, -1, -1):
                o = self.ops[e][idx]
                if o["fn"] is not None and o["dma"] is None:
                    evs.append(("c", e, idx))
                    break
        for q in DMAQ:
            n = self.dma_n[q]
            for slot in range(min(n, self.NQ)):
                last_n = ((n - 1 - slot) // self.NQ) * self.NQ + slot
                evs.append(("d", q, slot, 16 * (last_n // self.NQ + 1)))
        for e in ENGS:
            assert self.cur_cond is None
            o = dict(fn=None, waits=[], signal=False, dma=None, cond=None)
            for d in evs:
                if d[0] == "c" and d[1] == e:
                    continue
                self._add_wait(e, o, d, True)
            self.ops[e].append(o)
        for r in self.res:
            r.w = None
            r.r = []

    def emit(self):
        nc = self.nc
        ranks = {}
        for e in ENGS:
            c = 0
            rk = []
            for o in self.ops[e]:
                if o["signal"]:
                    c += 1
                rk.append(c)
            ranks[e] = rk
        self.n_signal = {e: (ranks[e][-1] if ranks[e] else 0) for e in ENGS}
        self.n_ops = {e: len(self.ops[e]) for e in ENGS}
        with ExitStack() as st:
            csem = {e: st.enter_context(nc.semaphore("c_" + e)) for e in ENGS}
            dsem = {q: [st.enter_context(nc.semaphore("d_%s_%d" % (q, i))) for i in range(self.NQ)]
                    for q in DMAQ}
            block = st.enter_context(nc.Block())

            def run(e):
                def emit_waits(eng, o):
                    for d in o["waits"]:
                        if d[0] == "c":
                            eng.wait_ge(csem[d[1]], ranks[d[1]][d[2]])
                        else:
                            eng.wait_ge(dsem[d[1]][d[2]], d[3])

                def emit_op(eng, o, vals):
                    emit_waits(eng, o)
                    fn = o["fn"]
                    if fn is None:
                        return
                    if isinstance(fn, tuple) and fn[0] == "vload":
                        ap, mx = self.vload_specs[fn[1]]
                        vals[fn[1]] = eng.value_load(ap)
                        if o["signal"]:
                            eng.sem_inc(csem[e], 1)
                        return
                    ins = fn(eng)
                    if o["dma"] is not None:
                        ins.then_inc(dsem[e][o["dma"][0]], 16)
                    elif o["signal"]:
                        ins.then_inc(csem[e], 1)

                import os as _os2
                _cond_eng = _os2.environ.get("COND_ENG", "pe,act,dve,pool,sp").split(",")

                dma_before = []
                _cur = {}
                for o_ in self.ops[e]:
                    dma_before.append(dict(_cur))
                    if o_["dma"] is not None:
                        _cur[o_["dma"][0]] = o_["dma"][1]

                def body(eng):
                    vals = {}
                    ops = self.ops[e]
                    n = len(ops)
                    i = 0
                    while i < n:
                        o = ops[i]
                        if o["cond"] is None or e not in _cond_eng:
                            emit_op(eng, o, vals)
                            i += 1
                            continue
                        name = o["cond"][0]
                        groups = []
                        j = i
                        while j < n and ops[j]["cond"] is not None and ops[j]["cond"][0] == name:
                            key = ops[j]["cond"]
                            k = j
                            while k < n and ops[k]["cond"] == key:
                                k += 1
                            if groups:
                                assert key[1] > groups[-1][0][1]
                            groups.append((key, j, k))
                            j = k

                        def comp(gi):
                            i0 = groups[gi][1]
                            rank_before = ranks[e][i0 - 1] if i0 > 0 else 0
                            rest = ops[i0:groups[-1][2]]
                            nsig = sum(1 for g in rest if g["signal"])
                            dcnt = {}
                            for g in rest:
                                if g["dma"] is not None:
                                    dcnt[g["dma"][0]] = dcnt.get(g["dma"][0], 0) + 1
                            if nsig:
                                if rank_before > 0:
                                    eng.wait_ge(csem[e], rank_before)
                                eng.sem_inc(csem[e], nsig)
                            if dcnt:
                                for slot in range(self.NQ):
                                    c0 = dma_before[i0].get(slot, 0)
                                    if c0 > 0:
                                        eng.wait_ge(dsem[e][slot], c0)
                                for slot, m in dcnt.items():
                                    eng.sem_inc(dsem[e][slot], 16 * m)
                            return nsig or dcnt

                        def emit_chain(gi):
                            if gi == len(groups):
                                return
                            key, a, b = groups[gi]
                            rest = ops[a:groups[-1][2]]
                            need_else = any(g["signal"] or g["dma"] is not None for g in rest)
                            with eng.If(vals[key[0]] > key[1]):
                                for g in ops[a:b]:
                                    emit_op(eng, g, vals)
                                emit_chain(gi + 1)
                            if need_else:
                                with eng.Else():
                                    comp(gi)
                        emit_chain(0)
                        i = groups[-1][2]
                return body

            block.tensor(run("pe"))
            block.scalar(run("act"))
            block.vector(run("dve"))
            block.gpsimd(run("pool"))
            block.sync(run("sp"))


class Buf:
    __slots__ = ("ap", "res")

    def __init__(self, ap, res):
        self.ap = ap
        self.res = res


class Ring:
    def __init__(self, bufs):
        self.bufs = bufs
        self.i = 0

    def next(self):
        b = self.bufs[self.i % len(self.bufs)]
        self.i += 1
        return b


def mm(out, lhsT, rhs, start, stop):
    return lambda e: e.matmul(out, lhsT=lhsT, rhs=rhs, start=start, stop=stop)


def tr(out, in_, ident):
    return lambda e: e.transpose(out=out, in_=in_, identity=ident)


def dma(out, in_):
    return lambda e: e.dma_start(out=out, in_=in_)


def act(out, in_, func, bias=0.0, scale=1.0):
    return lambda e: e.activation(out=out, in_=in_, func=func, bias=bias, scale=scale)


def tcopy(out, in_):
    return lambda e: e.tensor_copy(out=out, in_=in_)


def tt(out, in0, in1, op):
    return lambda e: e.tensor_tensor(out=out, in0=in0, in1=in1, op=op)


def ts(out, in0, s1, op0, s2=None, op1=None):
    if op1 is None:
        return lambda e: e.tensor_scalar(out=out, in0=in0, scalar1=s1, scalar2=None, op0=op0)
    return lambda e: e.tensor_scalar(out=out, in0=in0, scalar1=s1, scalar2=s2, op0=op0, op1=op1)


def stt(out, in0, scalar, in1, op0, op1, accum_out=None):
    if accum_out is None:
        return lambda e: e.scalar_tensor_tensor(out=out, in0=in0, scalar=scalar, in1=in1, op0=op0, op1=op1)
    return lambda e: e.scalar_tensor_tensor(out=out, in0=in0, scalar=scalar, in1=in1, op0=op0, op1=op1,
                                            accum_out=accum_out)


def memset(ap, v):
    return lambda e: e.memset(ap, v)


ARENA_BYTES = 206 * 1024


class Arena:
    def __init__(self, ap_bf16):
        self.base = ap_bf16
        self.off = 0
        self.floor = 0

    def alloc(self, shape, dtype):
        n = 1
        for s in shape:
            n *= s
        nbytes = n * (2 if dtype == BF16 else 4)
        nbytes_al = (nbytes + 63) // 64 * 64
        assert self.off + nbytes_al <= ARENA_BYTES, ("SBUF arena overflow", self.off, nbytes_al)
        v = self.base[:, self.off // 2:(self.off + nbytes) // 2]
        self.off += nbytes_al
        if dtype != BF16:
            v = v.bitcast(dtype)
        if len(shape) == 2:
            v = v.rearrange("p (a b) -> p a b", b=shape[1])
        elif len(shape) == 3:
            v = v.rearrange("p (a b c) -> p a b c", b=shape[1], c=shape[2])
        return v

    def mark(self):
        return self.off

    def reset(self, to):
        self.off = to


def build_program(debug=False, stop_after=None):
    nc = bass.Bass("TRN2", target_bir_lowering=False)
    kin = "ExternalInput"
    dt_ = lambda name, shape, dtype, kind: nc.dram_tensor(name, shape, dtype, kind=kind).ap()
    x_in = dt_("x", [NB, S, D], F32, kin)
    ctx_in = dt_("ctx", [NB, LC, D], F32, kin)
    c_fm = dt_("c_fm", [128, KC, 3], F32, kin)
    w_mod = dt_("w_mod", [DEPTH, D, 6 * D], F32, kin)
    b_mod_fm = dt_("b_mod_fm", [DEPTH, 128, 48], F32, kin)
    w_in = dt_("w_in", [DEPTH, D, 2560], F32, kin)
    w_uT = dt_("w_uT", [DEPTH, 256, D], F32, kin)
    w_f = dt_("w_f", [DEPTH, 256, 64], F32, kin)
    lam_qk = dt_("lam_qk", [DEPTH, 1, 256], F32, kin)
    subln_g = dt_("subln_g", [DEPTH, 1, 128], F32, kin)
    w_out = dt_("w_out", [DEPTH, D, D], F32, kin)
    ln_vecs = dt_("ln_vecs", [DEPTH, 4, 1, D], F32, kin)
    w_router = dt_("w_router", [D, NE], F32, kin)
    router_bias = dt_("router_bias", [1, NE], F32, kin)
    w_gate = dt_("w_gate", [DEPTH, NE, D, 512], F32, kin)
    w_up = dt_("w_up", [DEPTH, NE, D, 512], F32, kin)
    w_down = dt_("w_down", [DEPTH, NE, 512, D], F32, kin)
    dftc = dt_("dftc", [S, S], BF16, kin)
    dftns = dt_("dftns", [S, S], BF16, kin)
    dft256 = dt_("dft256", [2, LC, LC], BF16, kin)
    c64bd = dt_("c64bd", [2, 128, 128], F32, kin)
    rope_cs = dt_("rope_cs", [2, 128, S], F32, kin)
    rmat_d = dt_("rmat", [128, 128], BF16, kin)
    ident_d = dt_("ident", [128, 128], BF16, kin)
    identf_d = dt_("identf", [128, 128], F32, kin)
    uts_d = dt_("uts", [128, 128], F32, kin)
    ebase_d = dt_("ebase", [1, NE], F32, kin)
    iota_d = dt_("iota_p", [128, NE], F32, kin)
    rbias_rep_d = dt_("rbias_rep", [1, NB * NT * NE], F32, kin)
    out_d = dt_("out", [NB, S, D], F32, "ExternalOutput")
    skind = "ExternalOutput" if debug else "Internal"
    QT_s = dt_("QT_s", [NB, 128, 6, TT], BF16, skind)
    KT_s = dt_("KT_s", [NB, 128, 6, TT], BF16, skind)
    VAB_s = dt_("VAB_s", [NB, TT, 1280], BF16, skind)
    X1_s = dt_("X1_s", [NB, TT, D], F32, skind)
    X2_s = dt_("X2_s", [NB, TT, D], F32, skind)
    HG_s = dt_("HG_s", [NSLOT, D], BF16, "Internal")
    Y_s = [dt_("Y_s%d" % i, [NSLOT, 512], F32, "Internal") for i in range(2)]
    MIX_s = dt_("MIX_s", [NB, 128, 8, TT], BF16, skind) if debug else None
    DBG_s = dt_("DBG_s", [128, 4096], F32, skind) if debug else None
    DBG2_s = dt_("DBG2_s", [3, 128, 512], F32, skind) if debug else None

    st = ExitStack()
    arena_t = st.enter_context(nc.sbuf_tensor("arena", [128, ARENA_BYTES // 2], BF16))
    psum_t = st.enter_context(nc.psum_tensor("psum", [128, 4096], F32))
    P = Prog(nc)
    AR = Arena(arena_t[:, :])

    def bank(b, n=1):
        return psum_t[:, b * 512:(b + n) * 512]

    rp = [P.R("bank%d" % i) for i in range(8)]

    ident = AR.alloc([128], BF16)
    identf = AR.alloc([128], F32)
    onesf = AR.alloc([128], F32)
    rmat = AR.alloc([128], BF16)
    modT = AR.alloc([48, 3], F32)
    s1p_a = AR.alloc([KC, 3], F32)
    s1p_f = AR.alloc([KC, 3], F32)
    nlam = AR.alloc([1], F32)
    gsub = AR.alloc([128], F32)
    rbias = AR.alloc([NE], F32)
    wr_sb = AR.alloc([KC, NE], BF16)
    r_const = P.R("const")
    r_mod = P.R("mod")
    P.op("sp", dma(ident, ident_d), writes=[r_const], dma=True)
    P.op("sp", dma(identf, identf_d), writes=[r_const], dma=True)
    P.op("sp", dma(rmat, rmat_d), writes=[r_const], dma=True)
    P.op("sp", dma(rbias, router_bias.partition_broadcast(128)), writes=[r_const], dma=True)
    P.op("pool", dma(wr_sb, w_router.rearrange("(kc p) e -> p kc e", p=128)), writes=[r_const], dma=True)
    P.op("dve", memset(onesf, 1.0), writes=[r_const])
    P.barrier()
    PERSIST = AR.mark()

    def ln_stats(xt_ap, r_x, sm, r_sm):
        P.op("dve", lambda e: e.bn_stats(out=sm[:, 0:6], in_=xt_ap[:, 0:512]), reads=[r_x], writes=[r_sm])
        P.op("dve", lambda e: e.bn_stats(out=sm[:, 6:12], in_=xt_ap[:, 512:1024]), reads=[r_x], writes=[r_sm])
        P.op("dve", lambda e: e.bn_aggr(out=sm[:, 12:14], in_=sm[:, 0:12].rearrange("p (a b) -> p a b", b=6)),
             reads=[r_sm], writes=[r_sm])
        P.op("act", act(sm[:, 14:15], sm[:, 13:14], AF.Ln, bias=LN_EPS, scale=1.0), reads=[r_sm], writes=[r_sm])
        P.op("act", act(sm[:, 15:16], sm[:, 14:15], AF.Exp, scale=-0.5), reads=[r_sm], writes=[r_sm])
        P.op("dve", stt(sm[:, 16:17], sm[:, 12:13], -1.0, sm[:, 15:16], ALU.mult, ALU.mult),
             reads=[r_sm], writes=[r_sm])
        return sm[:, 15:16], sm[:, 16:17]

    def make_bcast(dst, r_dst, src_fm, diag, r_diag, pbank):
        for c in range(KC):
            P.op("dve", ts(diag, identf, src_fm[:, c:c + 1], ALU.mult), reads=[r_const, r_mod], writes=[r_diag])
            half, cc = c // 4, c % 4
            P.op("pe", mm(bank(pbank + half)[:, cc * 128:(cc + 1) * 128], onesf, diag, True, True),
                 reads=[r_const, r_diag], writes=[rp[pbank + half]])
        P.op("dve", tcopy(dst, bank(pbank, 2)), reads=[rp[pbank], rp[pbank + 1]], writes=[r_dst])

    _breg = {}

    def bound_reg(eng):
        if "r" not in _breg:
            _breg["r"] = eng.alloc_register("slot_bound")
            eng.reg_mov(_breg["r"], NSLOT - 1)
        return _breg["r"]

    def tile_src(l, b, t):
        if l == 0:
            if t < 2:
                return ctx_in[b, t * 128:(t + 1) * 128, :]
            return x_in[b, (t - 2) * 128:(t - 1) * 128, :]
        return X2_s[b, t * 128:(t + 1) * 128, :]

    for l in range(DEPTH):
        last = (l == DEPTH - 1)
        lam_init = 0.8 - 0.6 * math.exp(-0.3 * l)
        tiles_q = list(range(2, NT)) if last else list(range(NT))
        AR.reset(PERSIST)
        cfm = AR.alloc([KC, 3], F32)
        silu_c = AR.alloc([KC, 3], F32)
        bmod = AR.alloc([48], F32)
        lqb = AR.alloc([256], F32)
        junk = AR.alloc([64], F32)
        s12 = AR.alloc([4], F32)
        wm_ring = Ring([Buf(AR.alloc([KC, 1024], F32), P.R()) for _ in range(2)])
        r_c, r_s, r_b, r_l, r_j = P.R(), P.R(), P.R(), P.R(), P.R()
        P.op("sp", dma(cfm, c_fm), writes=[r_c], dma=True)
        P.op("sp", dma(bmod, b_mod_fm[l]), writes=[r_b], dma=True)
        P.op("sp", dma(lqb, lam_qk[l].partition_broadcast(128)), writes=[r_l], dma=True)
        P.op("sp", dma(gsub, subln_g[l].partition_broadcast(128)), writes=[r_mod], dma=True)
        P.op("act", act(silu_c, cfm, AF.Silu), reads=[r_c], writes=[r_s])
        psm = bank(0)[:, 0:144].rearrange("p (a b) -> p a b", b=3)
        for cg in range(6):
            wb = wm_ring.next()
            for kc in range(KC):
                P.op("sp", dma(wb.ap[:, kc, :], w_mod[l, kc * 128:(kc + 1) * 128, cg * 1024:(cg + 1) * 1024]),
                     writes=[wb.res], dma=True)
            for j in range(8):
                for kc in range(KC):
                    P.op("pe", mm(psm[:, cg * 8 + j, :], wb.ap[:, kc, j * 128:(j + 1) * 128], silu_c[:, kc, :],
                                  kc == 0, kc == KC - 1), reads=[wb.res, r_s], writes=[rp[0]])
        for j in range(3):
            P.op("dve", tt(modT[:, :, j], psm[:, :, j], bmod, ALU.add), reads=[rp[0], r_b], writes=[r_mod])
        P.op("dve", ts(s1p_a, modT[:, 8:16, :], 1.0, ALU.add), reads=[r_mod], writes=[r_mod])
        P.op("dve", ts(s1p_f, modT[:, 32:40, :], 1.0, ALU.add), reads=[r_mod], writes=[r_mod])
        P.op("dve", memset(s12, 0.0), writes=[r_j])
        P.op("dve", stt(junk, lqb[:, 0:64], 1.0, lqb[:, 64:128], ALU.mult, ALU.mult, accum_out=s12[:, 0:1]),
             reads=[r_l], writes=[r_j])
        P.op("dve", stt(junk, lqb[:, 128:192], 1.0, lqb[:, 192:256], ALU.mult, ALU.mult, accum_out=s12[:, 1:2]),
             reads=[r_l], writes=[r_j])
        P.op("act", act(s12[:, 2:4], s12[:, 0:2], AF.Exp), reads=[r_j], writes=[r_j])
        P.op("dve", tt(nlam, s12[:, 3:4], s12[:, 2:3], ALU.subtract), reads=[r_j], writes=[r_mod])
        P.op("dve", ts(nlam, nlam, -lam_init, ALU.add), reads=[r_mod], writes=[r_mod])
        P.op("dve", ts(gsub, gsub, 1.0 - lam_init, ALU.mult), reads=[r_mod], writes=[r_mod])
        P.barrier()
        sh_a = modT[:, 0:8, :]
        g_a = modT[:, 16:24, :]
        sh_f = modT[:, 24:32, :]
        g_f = modT[:, 40:48, :]

        import os as _os
        _skipmix = _os.environ.get("SKIP_MIX") is not None
        AR.reset(PERSIST)
        w_sb = AR.alloc([KC, WCOLS], BF16)
        r_w = P.R("w_in")
        cs_sb = AR.alloc([2, S], F32)
        r_cs = P.R()
        xt_ring = Ring([Buf(AR.alloc([D], F32), P.R()) for _ in range(2)])
        xn_ring = Ring([Buf(AR.alloc([D], BF16), P.R()) for _ in range(2)])
        sm_ring = Ring([Buf(AR.alloc([24], F32), P.R()) for _ in range(2)])
        hT_ring = Ring([Buf(AR.alloc([KC, 512], BF16), P.R()) for _ in range(2)])
        pl_ring = Ring([Buf(AR.alloc([512], BF16), P.R()) for _ in range(2)])
        t1_ring = Ring([Buf(AR.alloc([512], F32), P.R()) for _ in range(2)])
        t2_ring = Ring([Buf(AR.alloc([512], F32), P.R()) for _ in range(2)])
        qk_ring = Ring([Buf(AR.alloc([12, 512], BF16), P.R()) for _ in range(2)])
        vab_ring = Ring([Buf(AR.alloc([1280], BF16), P.R()) for _ in range(2)])
        c64 = AR.alloc([2, 128], F32)
        wf_sb = AR.alloc([2, 64], F32)
        bd = AR.alloc([4, 128], BF16)
        wuT = AR.alloc([2, D], BF16)
        r_a, r_bd = P.R(), P.R()
        P.op("sp", dma(c64, c64bd.rearrange("a p n -> p a n")), writes=[r_a], dma=True)
        P.op("sp", dma(wf_sb, w_f[l].rearrange("(j p) d -> p j d", p=128)), writes=[r_a], dma=True)
        P.op("pool", dma(wuT, w_uT[l].rearrange("(j p) k -> p j k", p=128)), writes=[r_a], dma=True)
        P.op("sp", dma(cs_sb, rope_cs.rearrange("a p n -> p a n")), writes=[r_cs], dma=True)
        for kc in range(KC):
            P.op("pool", dma(w_sb[:, kc, 512:WCOLS], w_in[l, kc * 128:(kc + 1) * 128, 256:2560]),
                 writes=[r_w], dma=True)
        P.op("dve", memset(bd, 0.0), writes=[r_bd])
        for cs in range(2):
            for j in range(2):
                idx = cs * 2 + j
                P.op("pe", mm(bank(0)[:, idx * 64:(idx + 1) * 64], c64[:, cs, :], wf_sb[:, j, :], True, True),
                     reads=[r_a], writes=[rp[0]])
        for idx in range(4):
            P.op("dve", tcopy(bd[0:64, idx, 0:64], bank(0)[0:64, idx * 64:(idx + 1) * 64]),
                 reads=[rp[0]], writes=[r_bd])
            P.op("dve", tcopy(bd[64:128, idx, 64:128], bank(0)[64:128, idx * 64:(idx + 1) * 64]),
                 reads=[rp[0]], writes=[r_bd])
        for kc in range(KC):
            pb = 2 + (kc % 2)
            for cs in range(2):
                for j in range(2):
                    idx = cs * 2 + j
                    P.op("pe", mm(bank(pb)[:, idx * 128:(idx + 1) * 128], wuT[:, j, kc * 128:(kc + 1) * 128],
                                  bd[:, idx, :], True, True), reads=[r_a, r_bd], writes=[rp[pb]])
            P.op("dve", tcopy(w_sb[:, kc, 0:512], bank(pb)), reads=[rp[pb]], writes=[r_w])

        blocks = []
        for b in range(NB):
            blocks.append((b, 0, 2))
            for i in range(4):
                blocks.append((b, 2 + 4 * i, 4))
        psT_i = [0]
        fm_i = [0]
        tm_i = [0]
        rot_i = [0]

        def p1_A(blk):
            b, t0, ntl = blk
            hb = hT_ring.next()
            for ti in range(ntl):
                t = t0 + ti
                j = 2 if t < 2 else b
                xb_, xnb, smb = xt_ring.next(), xn_ring.next(), sm_ring.next()
                P.op("sp", dma(xb_.ap, tile_src(l, b, t)), writes=[xb_.res], dma=True)
                rstd, nmr = ln_stats(xb_.ap, xb_.res, smb.ap, smb.res)
                P.op("act", act(xnb.ap, xb_.ap, AF.Identity, bias=nmr, scale=rstd),
                     reads=[xb_.res, smb.res], writes=[xnb.res])
                pb = psT_i[0] % 2
                psT_i[0] += 1
                psT = bank(pb).bitcast(BF16).rearrange("p (a b) -> p a b", b=128)
                for kc in range(KC):
                    P.op("pe", tr(psT[:, kc, :], xnb.ap[:, kc * 128:(kc + 1) * 128], ident),
                         reads=[xnb.res, r_const], writes=[rp[pb]])
                for kc in range(KC):
                    P.op("dve", ts(hb.ap[:, kc, ti * 128:(ti + 1) * 128], psT[:, kc, :], s1p_a[:, kc, j:j + 1],
                                   ALU.mult, sh_a[:, kc, j:j + 1], ALU.add),
                         reads=[rp[pb], r_mod], writes=[hb.res])
            return hb

        def p1_B(blk, hb):
            b, t0, ntl = blk
            n = ntl * 128
            is_ctx = t0 < 2
            tok0 = t0 * 128
            qb = qk_ring.next()
            for c in range(12):
                pb = 2 + fm_i[0] % 2
                fm_i[0] += 1
                col = 512 + c * 128
                for kc in range(KC):
                    P.op("pe", mm(bank(pb)[:, 0:n], w_sb[:, kc, col:col + 128], hb.ap[:, kc, 0:n],
                                  kc == 0, kc == KC - 1), reads=[r_w, hb.res], writes=[rp[pb]])
                if is_ctx:
                    P.op("act", tcopy_act(qb.ap[:, c, 0:n], bank(pb)[:, 0:n]), reads=[rp[pb]], writes=[qb.res])
                else:
                    pl, t1, t2 = pl_ring.next(), t1_ring.next(), t2_ring.next()
                    P.op("act", tcopy_act(pl.ap[:, 0:n], bank(pb)[:, 0:n]), reads=[rp[pb]], writes=[pl.res])
                    prb = 4 + rot_i[0] % 2
                    rot_i[0] += 1
                    P.op("pe", mm(bank(prb)[:, 0:n], rmat, pl.ap[:, 0:n], True, True),
                         reads=[pl.res, r_const], writes=[rp[prb]])
                    s0 = tok0 - LC
                    P.op("dve", tt(t1.ap[:, 0:n], pl.ap[:, 0:n], cs_sb[:, 0, s0:s0 + n], ALU.mult),
                         reads=[pl.res, r_cs], writes=[t1.res])
                    P.op("dve", tt(t2.ap[:, 0:n], bank(prb)[:, 0:n], cs_sb[:, 1, s0:s0 + n], ALU.mult),
                         reads=[rp[prb], r_cs], writes=[t2.res])
                    P.op("dve", tt(qb.ap[:, c, 0:n], t1.ap[:, 0:n], t2.ap[:, 0:n], ALU.add),
                         reads=[t1.res, t2.res], writes=[qb.res])
            P.op("pool", dma(QT_s[b, :, :, tok0:tok0 + n], qb.ap[:, 0:6, 0:n]), reads=[qb.res], dma=True)
            P.op("pool", dma(KT_s[b, :, :, tok0:tok0 + n], qb.ap[:, 6:12, 0:n]), reads=[qb.res], dma=True)
            for ti in range(ntl):
                vb = vab_ring.next()
                for (c0, c1, wc0) in ((0, 512, 0), (512, 1024, 2048), (1024, 1280, 2560)):
                    pb = 6 + tm_i[0] % 2
                    tm_i[0] += 1
                    w_ = c1 - c0
                    for kc in range(KC):
                        P.op("pe", mm(bank(pb)[:, 0:w_], hb.ap[:, kc, ti * 128:(ti + 1) * 128],
                                      w_sb[:, kc, wc0:wc0 + w_], kc == 0, kc == KC - 1),
                             reads=[r_w, hb.res], writes=[rp[pb]])
                    P.op("act", tcopy_act(vb.ap[:, c0:c1], bank(pb)[:, 0:w_]), reads=[rp[pb]], writes=[vb.res])
                r0 = tok0 + ti * 128
                P.op("pool", dma(VAB_s[b, r0:r0 + 128, :], vb.ap), reads=[vb.res], dma=True)

        def tcopy_act(out, in_):
            return lambda e: e.copy(out=out, in_=in_)

        prev = None
        for i in range(0 if _skipmix else len(blocks) + 1):
            cur = None
            if i < len(blocks):
                cur = (blocks[i], p1_A(blocks[i]))
            if prev is not None:
                p1_B(*prev)
            prev = cur
        P.barrier()
        if stop_after == ("P1", l):
            break

        for b in range(0 if _skipmix else NB):
            AR.reset(PERSIST)
            fT = AR.alloc([2, TT], BF16)
            mixA = AR.alloc([6, TT], BF16)
            r_fT, r_mix = P.R(), P.R()
            SUB = AR.mark()
            ab_sb = AR.alloc([NT, 512], BF16)
            d256 = AR.alloc([2, 2, LC], BF16)
            tb_ring = Ring([Buf(AR.alloc([16, 512], BF16), P.R()) for _ in range(2)])
            r_ab, r_d = P.R(), P.R()
            P.op("sp", dma(ab_sb, VAB_s[b, :, 0:512].rearrange("(t p) n -> p t n", p=128)), writes=[r_ab], dma=True)
            if not last:
                for cs in range(2):
                    P.op("sp", dma(d256[:, :, cs, :], dft256[cs].rearrange("(tc p) n -> p tc n", p=128)),
                         writes=[r_d], dma=True)
                for j in range(2):
                    k = 0
                    for cs in range(2):
                        for tc in range(2):
                            P.op("pe", mm(bank(j)[:, 0:LC], ab_sb[:, tc, cs * 256 + j * 128: cs * 256 + (j + 1) * 128],
                                          d256[:, tc, cs, :], k == 0, k == 3), reads=[r_ab, r_d], writes=[rp[j]])
                            k += 1
                    P.op("act", tcopy_act(fT[:, j, 0:LC], bank(j)[:, 0:LC]), reads=[rp[j]], writes=[r_fT])
            for tb in range(4):
                for cs in range(2):
                    tbuf = tb_ring.next()
                    src = (dftc if cs == 0 else dftns)[:, tb * 512:(tb + 1) * 512].rearrange("(tc p) n -> p tc n", p=128)
                    for hh in range(2):
                        P.op("sp", dma(tbuf.ap[:, hh * 8:(hh + 1) * 8, :], src[:, hh * 8:(hh + 1) * 8, :]),
                             writes=[tbuf.res], dma=True)
                    for j in range(2):
                        pb = 2 + (tb % 2) * 2 + j
                        for tc in range(16):
                            P.op("pe", mm(bank(pb), ab_sb[:, 2 + tc, cs * 256 + j * 128: cs * 256 + (j + 1) * 128],
                                          tbuf.ap[:, tc, :], cs == 0 and tc == 0, cs == 1 and tc == 15),
                                 reads=[r_ab, tbuf.res], writes=[rp[pb]])
                for j in range(2):
                    pb = 2 + (tb % 2) * 2 + j
                    P.op("act", tcopy_act(fT[:, j, LC + tb * 512: LC + (tb + 1) * 512], bank(pb)),
                         reads=[rp[pb]], writes=[r_fT])
            if debug:
                P.op("sp", dma(MIX_s[b, :, 0:2, :], fT), reads=[r_fT], dma=True)
            P.barrier()

            AR.reset(SUB)
            qT = AR.alloc([6, TT], BF16)
            kT = AR.alloc([6, TT], BF16)
            vaug = AR.alloc([NT, 6, VST], BF16)
            r_q, r_k, r_v = P.R(), P.R(), P.R()
            PT_ring = Ring([Buf(AR.alloc([NT, 512], BF16), P.R()) for _ in range(2)])
            tq_ring = Ring([Buf(AR.alloc([4, 128], F32), P.R()) for _ in range(2)])
            o_ring = Ring([Buf(AR.alloc([4, 128], F32), P.R()) for _ in range(2)])
            on_ring = Ring([Buf(AR.alloc([4, 128], BF16), P.R()) for _ in range(2)])
            rs_ring = Ring([Buf(AR.alloc([16], F32), P.R()) for _ in range(4)])
            junk2 = AR.alloc([128], F32)
            r_j2 = P.R()
            P.op("sp", dma(qT, QT_s[b]), writes=[r_q], dma=True)
            P.op("sp", dma(kT, KT_s[b]), writes=[r_k], dma=True)
            P.op("dve", memset(vaug[:, :, :, 128:VST], 1.0), writes=[r_v])
            for h in range(6):
                P.op("sp", dma(vaug[:, :, h, 0:128],
                               VAB_s[b, :, 512 + h * 128: 512 + (h + 1) * 128].rearrange("(t p) d -> p t d", p=128)),
                     writes=[r_v], dma=True)
            units = []
            if not last:
                for h in range(6):
                    for sub in range(2):
                        units.append((0, 2, [0, 1], h, sub))
            for qb_ in range(4):
                for h in range(6):
                    for sub in range(2):
                        units.append((LC + qb_ * 512, 4, list(range(NT)), h, sub))
            sg_i = [0]

            def att_S(u, ui):
                q0, nqt, kts, h, sub = u
                n = nqt * 128
                pt = PT_ring.next()
                p0, p1 = sub * 64, (sub + 1) * 64
                for g in range(0, len(kts), 2):
                    pb = (sg_i[0] % 2) * 2
                    sg_i[0] += 1
                    grp = kts[g:g + 2]
                    for gi, kt in enumerate(grp):
                        P.op("pe", mm(bank(pb + gi)[:, 0:n], kT[p0:p1, h, kt * 128:(kt + 1) * 128],
                                      qT[p0:p1, h, q0:q0 + n], True, True),
                             reads=[r_q, r_k], writes=[rp[pb + gi]])
                    src = bank(pb, 2).rearrange("p (a b) -> p a b", b=512)[:, 0:len(grp), 0:n]
                    P.op("act", act(pt.ap[:, g:g + len(grp), 0:n], src, AF.Exp, scale=0.125),
                         reads=[rp[pb], rp[pb + 1]], writes=[pt.res])
                return pt

            def acc_ap(par, qt):
                if qt < 3:
                    return bank(4 + 2 * par)[:, qt * 160: qt * 160 + 129]
                return bank(5 + 2 * par)[:, 0:129]

            def att_AV(u, ui, pt, state):
                q0, nqt, kts, h, sub = u
                par = ui % 2
                for qt in range(nqt):
                    a = acc_ap(par, qt)
                    for ki, kt in enumerate(kts):
                        P.op("pe", mm(a, pt.ap[:, ki, qt * 128:(qt + 1) * 128], vaug[:, kt, h, 0:129],
                                      ki == 0, ki == len(kts) - 1),
                             reads=[pt.res, r_v], writes=[rp[4 + 2 * par], rp[5 + 2 * par]])
                accr = [rp[4 + 2 * par], rp[5 + 2 * par]]
                rs = rs_ring.next()
                if sub == 0:
                    tq = tq_ring.next()
                    state["tq"] = tq
                    for qt in range(nqt):
                        a = acc_ap(par, qt)
                        P.op("dve", lambda e, o=rs.ap[:, qt:qt + 1], i=a[:, 128:129]: e.reciprocal(out=o, in_=i),
                             reads=accr, writes=[rs.res])
                        P.op("dve", ts(tq.ap[:, qt, :], a[:, 0:128], rs.ap[:, qt:qt + 1], ALU.mult),
                             reads=accr + [rs.res], writes=[tq.res])
                else:
                    tq = state["tq"]
                    ob, onb = o_ring.next(), on_ring.next()
                    P.op("dve", memset(rs.ap[:, 8:12], 0.0), writes=[rs.res])
                    for qt in range(nqt):
                        a = acc_ap(par, qt)
                        P.op("dve", lambda e, o=rs.ap[:, qt:qt + 1], i=a[:, 128:129]: e.reciprocal(out=o, in_=i),
                             reads=accr, writes=[rs.res])
                        P.op("dve", ts(rs.ap[:, 4 + qt:5 + qt], rs.ap[:, qt:qt + 1], nlam[:, 0:1], ALU.mult),
                             reads=[rs.res, r_mod], writes=[rs.res])
                        P.op("dve", stt(ob.ap[:, qt, :], a[:, 0:128], rs.ap[:, 4 + qt:5 + qt], tq.ap[:, qt, :],
                                        ALU.mult, ALU.add), reads=accr + [rs.res, tq.res], writes=[ob.res])
                        P.op("dve", stt(junk2, ob.ap[:, qt, :], 1.0, ob.ap[:, qt, :], ALU.mult, ALU.mult,
                                        accum_out=rs.ap[:, 8 + qt:9 + qt]), reads=[ob.res], writes=[rs.res, r_j2])
                    P.op("act", act(rs.ap[:, 12:12 + nqt], rs.ap[:, 8:8 + nqt], AF.Ln, bias=LN_EPS, scale=1.0 / 128),
                         reads=[rs.res], writes=[rs.res])
                    P.op("act", act(rs.ap[:, 12:12 + nqt], rs.ap[:, 12:12 + nqt], AF.Exp, scale=-0.5),
                         reads=[rs.res], writes=[rs.res])
                    for qt in range(nqt):
                        P.op("dve", stt(onb.ap[:, qt, :], ob.ap[:, qt, :], rs.ap[:, 12 + qt:13 + qt], gsub,
                                        ALU.mult, ALU.mult), reads=[ob.res, rs.res, r_mod], writes=[onb.res])
                    psT = bank(5 + 2 * par)[:, 256:512].bitcast(BF16).rearrange("p (a b) -> p a b", b=128)
                    for qt in range(nqt):
                        P.op("pe", tr(psT[:, qt, :], onb.ap[:, qt, :], ident), reads=[onb.res, r_const],
                             writes=[rp[5 + 2 * par]])
                    P.op("dve", tcopy(mixA[:, h, q0:q0 + nqt * 128].rearrange("p (a b) -> p a b", b=128), psT[:, 0:nqt, :]),
                         reads=[rp[5 + 2 * par]], writes=[r_mix])

            state = {}
            prevu = None
            for ui in range(len(units) + 1):
                curu = None
                if ui < len(units):
                    curu = (units[ui], ui, att_S(units[ui], ui))
                if prevu is not None:
                    att_AV(prevu[0], prevu[1], prevu[2], state)
                prevu = curu
            if debug:
                P.op("sp", dma(MIX_s[b, :, 2:8, :], mixA), reads=[r_mix], dma=True)
            P.barrier()

            AR.reset(SUB)
            wo_sb = AR.alloc([KC, D], BF16)
            gbc = [AR.alloc([D], F32) for _ in range(2)]
            lng = AR.alloc([D], F32)
            lnb = AR.alloc([D], F32)
            diag = AR.alloc([128], F32)
            r_wo, r_g, r_ln, r_dg = P.R(), P.R(), P.R(), P.R()
            xt_ring = Ring([Buf(AR.alloc([D], F32), P.R()) for _ in range(2)])
            z_ring = Ring([Buf(AR.alloc([D], F32), P.R()) for _ in range(2)])
            sm_ring = Ring([Buf(AR.alloc([24], F32), P.R()) for _ in range(2)])
            for kc in range(KC):
                P.op("pool", dma(wo_sb[:, kc, :], w_out[l, kc * 128:(kc + 1) * 128, :]), writes=[r_wo], dma=True)
            P.op("sp", dma(lng, ln_vecs[l, 0].partition_broadcast(128)), writes=[r_ln], dma=True)
            P.op("sp", dma(lnb, ln_vecs[l, 1].partition_broadcast(128)), writes=[r_ln], dma=True)
            make_bcast(gbc[0], r_g, g_a[:, :, b], diag, r_dg, 0)
            if not last:
                make_bcast(gbc[1], r_g, g_a[:, :, 2], diag, r_dg, 0)
            for ti, t in enumerate(tiles_q):
                xb_, zb, smb = xt_ring.next(), z_ring.next(), sm_ring.next()
                P.op("sp", dma(xb_.ap, tile_src(l, b, t)), writes=[xb_.res], dma=True)
                pb = 2 + (ti % 3) * 2
                for half in range(2):
                    for kc in range(KC):
                        lhs = fT[:, kc, t * 128:(t + 1) * 128] if kc < 2 else mixA[:, kc - 2, t * 128:(t + 1) * 128]
                        P.op("pe", mm(bank(pb + half), lhs, wo_sb[:, kc, half * 512:(half + 1) * 512],
                                      kc == 0, kc == KC - 1), reads=[r_fT, r_mix, r_wo], writes=[rp[pb + half]])
                gsel = gbc[1] if t < 2 else gbc[0]
                P.op("dve", tt(zb.ap, bank(pb, 2), gsel, ALU.mult), reads=[rp[pb], rp[pb + 1], r_g], writes=[zb.res])
                P.op("dve", stt(zb.ap, xb_.ap, ALPHA, zb.ap, ALU.mult, ALU.add), reads=[xb_.res, zb.res],
                     writes=[zb.res])
                rstd, nmr = ln_stats(zb.ap, zb.res, smb.ap, smb.res)
                P.op("act", act(xb_.ap, zb.ap, AF.Identity, bias=nmr, scale=rstd), reads=[zb.res, smb.res],
                     writes=[xb_.res])
                P.op("dve", tt(xb_.ap, xb_.ap, lng, ALU.mult), reads=[xb_.res, r_ln], writes=[xb_.res])
                P.op("dve", tt(xb_.ap, xb_.ap, lnb, ALU.add), reads=[xb_.res, r_ln], writes=[xb_.res])
                P.op("pool", dma(X1_s[b, t * 128:(t + 1) * 128, :], xb_.ap), reads=[xb_.res], dma=True)
            P.barrier()
        if stop_after == ("MIX", l):
            break

        tl = [(b, t) for b in range(NB) for t in tiles_q]
        NG = len(tl)
        AR.reset(PERSIST)
        slots_i = AR.alloc([NG, 2], I32)
        wts = AR.alloc([NG, 2], F32)
        cnt_i = AR.alloc([NE], I32)
        r_sl, r_cnt = P.R(), P.R()
        SUB = AR.mark()
        jl = [0, 1] if last else [0, 1, 2]
        s_bc = {j: AR.alloc([D], F32) for j in jl}
        h_bc = {j: AR.alloc([D], F32) for j in jl}
        diag = AR.alloc([128], F32)
        uts = AR.alloc([128], F32)
        ebase = AR.alloc([NE], F32)
        r_bc, r_dg, r_ut, r_run = P.R(), P.R(), P.R(), P.R()
        xt_ring = Ring([Buf(AR.alloc([D], F32), P.R()) for _ in range(2)])
        xn_ring = Ring([Buf(AR.alloc([D], BF16), P.R()) for _ in range(2)])
        hf_ring = Ring([Buf(AR.alloc([D], F32), P.R()) for _ in range(2)])
        h2_all = AR.alloc([NG, D], BF16)
        h2T_ring = Ring([Buf(AR.alloc([KC, 128], BF16), P.R()) for _ in range(2)])
        sm_ring = Ring([Buf(AR.alloc([24], F32), P.R()) for _ in range(2)])
        W_ = NG * NE
        sc_a, sel_a, msk_a, ww_a, cmb_a, slv_a, msl_a, rb_rep = [AR.alloc([W_], F32) for _ in range(8)]
        w4 = AR.alloc([7, NG * 4], F32)
        gs_a, m2_a, gm_a = [AR.alloc([NG * 4], F32) for _ in range(3)]
        red = AR.alloc([6, NG], F32)
        runb_all = AR.alloc([NG + 1, NE], F32)
        padf = Buf(AR.alloc([64], F32), P.R())
        r_rt = P.R()
        P.op("sp", dma(uts, uts_d), writes=[r_ut], dma=True)
        P.op("sp", dma(ebase, ebase_d.partition_broadcast(128)), writes=[r_ut], dma=True)
        P.op("sp", dma(rb_rep, rbias_rep_d[:, 0:W_].partition_broadcast(128)), writes=[r_ut], dma=True)
        P.op("sp", dma(runb_all[:, 0, :], ebase_d.partition_broadcast(128)), writes=[r_run], dma=True)
        for j in jl:
            make_bcast(s_bc[j], r_bc, s1p_f[:, :, j], diag, r_dg, 6)
            make_bcast(h_bc[j], r_bc, sh_f[:, :, j], diag, r_dg, 6)
        r_h2t = [P.R() for _ in range(NG)]
        for gt, (b, t) in enumerate(tl):
            j = 2 if t < 2 else b
            xb_, xnb, smb = xt_ring.next(), xn_ring.next(), sm_ring.next()
            hf, h2T = hf_ring.next(), h2T_ring.next()
            h2ap = h2_all[:, gt, :]
            P.op("sp", dma(xb_.ap, tile_src(0, b, t) if _skipmix else X1_s[b, t * 128:(t + 1) * 128, :]),
                 writes=[xb_.res], dma=True)
            rstd, nmr = ln_stats(xb_.ap, xb_.res, smb.ap, smb.res)
            P.op("act", act(xnb.ap, xb_.ap, AF.Identity, bias=nmr, scale=rstd),
                 reads=[xb_.res, smb.res], writes=[xnb.res])
            P.op("dve", tt(hf.ap, xnb.ap, s_bc[j], ALU.mult), reads=[xnb.res, r_bc], writes=[hf.res])
            P.op("dve", tt(h2ap, hf.ap, h_bc[j], ALU.add), reads=[hf.res, r_bc], writes=[r_h2t[gt]])
            pb = gt % 2
            psT = bank(pb).bitcast(BF16).rearrange("p (a b) -> p a b", b=128)
            for kc in range(KC):
                P.op("pe", tr(psT[:, kc, :], h2ap[:, kc * 128:(kc + 1) * 128], ident),
                     reads=[r_h2t[gt], r_const], writes=[rp[pb]])
            P.op("act", tcopy_act(h2T.ap, psT), reads=[rp[pb]], writes=[h2T.res])
            prb = 2 + gt % 2
            for kc in range(KC):
                P.op("pe", mm(bank(prb)[:, 0:NE], h2T.ap[:, kc, :], wr_sb[:, kc, :], kc == 0, kc == KC - 1),
                     reads=[h2T.res, r_const], writes=[rp[prb]])
            P.op("act", act(sc_a[:, gt * NE:(gt + 1) * NE], bank(prb)[:, 0:NE], AF.Exp, scale=-1.0),
                 reads=[rp[prb]], writes=[r_rt])
        rr = [r_rt]

        def dv(fn, extra=()):
            P.op("dve", fn, reads=rr + list(extra), writes=rr)

        def red_(out, in_, op):
            return lambda e: e.tensor_reduce(out=out, in_=in_, axis=AX.X, op=op)

        def rcp(out, in_):
            return lambda e: e.reciprocal(out=out, in_=in_)
        dv(ts(sc_a, sc_a, 1.0, ALU.add))
        dv(rcp(sc_a, sc_a))
        dv(tt(sel_a, sc_a, rb_rep, ALU.add), [r_ut])
        sv = sel_a.rearrange("p (g e) -> p g e", e=4)
        hi01, lo01, hi23, lo23, m1, mid, lom = [w4[:, i_, :] for i_ in range(7)]
        dv(tt(hi01, sv[:, :, 0], sv[:, :, 1], ALU.max))
        dv(tt(lo01, sv[:, :, 0], sv[:, :, 1], ALU.min))
        dv(tt(hi23, sv[:, :, 2], sv[:, :, 3], ALU.max))
        dv(tt(lo23, sv[:, :, 2], sv[:, :, 3], ALU.min))
        dv(tt(m1, hi01, hi23, ALU.max))
        dv(tt(mid, hi01, hi23, ALU.min))
        dv(tt(lom, lo01, lo23, ALU.max))
        dv(tt(m2_a, mid, lom, ALU.max))
        dv(tt(gs_a, m1, m2_a, ALU.add))
        gs3 = gs_a.rearrange("p (t g) -> p t g", g=4)
        gm3 = gm_a.rearrange("p (t g) -> p t g", g=4)
        dv(red_(red[:, 0, :], gs3, ALU.max))
        for g_ in range(4):
            dv(tt(gm3[:, :, g_], gs3[:, :, g_], red[:, 0, :], ALU.is_ge))
        mv_ = msk_a.rearrange("p (g e) -> p g e", e=4)
        for ee in range(4):
            dv(tt(mv_[:, :, ee], sv[:, :, ee], m2_a, ALU.is_ge))
            dv(tt(mv_[:, :, ee], mv_[:, :, ee], gm_a, ALU.mult))
        dv(tt(ww_a, sc_a, msk_a, ALU.mult))
        ww3 = ww_a.rearrange("p (t e) -> p t e", e=NE)
        cmb3 = cmb_a.rearrange("p (t e) -> p t e", e=NE)
        msk3 = msk_a.rearrange("p (t e) -> p t e", e=NE)
        slv3 = slv_a.rearrange("p (t e) -> p t e", e=NE)
        msl3 = msl_a.rearrange("p (t e) -> p t e", e=NE)
        dv(red_(red[:, 1, :], ww3, ALU.add))
        dv(rcp(red[:, 2, :], red[:, 1, :]))
        for e_ in range(NE):
            dv(tt(cmb3[:, :, e_], ww3[:, :, e_], red[:, 2, :], ALU.mult))
        for gt in range(NG):
            pbk, c0 = 4 + gt // 16, (gt % 16) * 32
            P.op("pe", mm(bank(pbk)[:, c0:c0 + NE], uts, msk3[:, gt, :], True, True), reads=[r_ut, r_rt],
                 writes=[rp[pbk]])
            P.op("pe", mm(bank(pbk)[:, c0 + 16:c0 + 16 + NE], onesf, msk3[:, gt, :], True, True),
                 reads=[r_const, r_rt], writes=[rp[pbk]])
        nbk = (NG + 15) // 16
        pbanks = [rp[4 + k_] for k_ in range(nbk)]
        for gt in range(NG):
            pbk, c0 = 4 + gt // 16, (gt % 16) * 32
            P.op("dve", tt(runb_all[:, gt + 1, :], runb_all[:, gt, :], bank(pbk)[:, c0 + 16:c0 + 16 + NE], ALU.add),
                 reads=pbanks + [r_run], writes=[r_run])
        for k_ in range(nbk):
            nt_ = min(16, NG - k_ * 16)
            pos3 = bank(4 + k_).rearrange("p (t c) -> p t c", c=32)[:, 0:nt_, 0:NE]
            dv(tt(slv3[:, k_ * 16:k_ * 16 + nt_, :], pos3, runb_all[:, k_ * 16:k_ * 16 + nt_, :], ALU.add),
               pbanks + [r_run])
        dv(tt(msl_a, slv_a, msk_a, ALU.mult))
        dv(red_(red[:, 3, :], msl3, ALU.max))
        dv(red_(red[:, 4, :], msl3, ALU.add))
        dv(tt(red[:, 5, :], red[:, 4, :], red[:, 3, :], ALU.subtract))
        for e_ in range(NE):
            dv(tt(slv3[:, :, e_], msl3[:, :, e_], red[:, 3, :], ALU.is_equal))
        dv(tt(slv_a, slv_a, cmb_a, ALU.mult))
        P.op("dve", red_(wts[:, :, 1], slv3, ALU.add), reads=rr, writes=[r_sl])
        P.op("dve", ts(wts[:, :, 0], wts[:, :, 1], -1.0, ALU.mult, 1.0, ALU.add), reads=[r_sl], writes=[r_sl])
        P.op("dve", tcopy(slots_i[:, :, 0], red[:, 5, :]), reads=rr, writes=[r_sl])
        P.op("dve", tcopy(slots_i[:, :, 1], red[:, 3, :]), reads=rr, writes=[r_sl])
        for gt in range(NG):
            for k in range(2):
                P.op("pool", lambda e, off=slots_i[:, gt, k:k + 1], src=h2_all[:, gt, :]: e.indirect_dma_start(
                    out=HG_s[:, :], out_offset=bass.IndirectOffsetOnAxis(ap=off, axis=0), in_=src, in_offset=None,
                    bounds_check=bound_reg(e), oob_is_err=False), reads=[r_sl, r_h2t[gt]], dma=True)
        runb = runb_all[:, NG, :]
        zt = hf_ring.bufs[0]
        P.op("sp", dma(padf.ap[:, 0:NE], iota_d), writes=[padf.res], dma=True)
        P.op("dve", memset(zt.ap.bitcast(BF16)[:, 0:D], 0.0), writes=[zt.res])
        P.op("dve", tt(padf.ap[:, 0:NE], padf.ap[:, 0:NE], runb, ALU.add), reads=[padf.res, r_run], writes=[padf.res])
        pad_i = padf.ap[:, 32:32 + NE].bitcast(I32)
        P.op("dve", tcopy(pad_i, padf.ap[:, 0:NE]), reads=[padf.res], writes=[padf.res])
        for e_ in range(NE):
            P.op("pool", lambda e, off=pad_i[:, e_:e_ + 1], src=zt.ap.bitcast(BF16)[:, 0:D]: e.indirect_dma_start(
                out=HG_s[:, :], out_offset=bass.IndirectOffsetOnAxis(ap=off, axis=0), in_=src, in_offset=None,
                bounds_check=bound_reg(e), oob_is_err=False), reads=[padf.res, zt.res], dma=True)
        P.op("dve", tt(padf.ap[:, 48:48 + NE], runb, ebase, ALU.subtract), reads=[r_run, r_ut, padf.res],
             writes=[padf.res])
        P.op("dve", tcopy(cnt_i, padf.ap[:, 48:48 + NE]), reads=[padf.res], writes=[r_cnt])
        if debug and l == 0:
            P.op("sp", dma(DBG_s[:, 0:NG * 2], wts.rearrange("p a b -> p (a b)")), reads=[r_sl], dma=True)
            P.op("sp", dma(DBG_s[:, 256:256 + NE], padf.ap[:, 48:48 + NE]), reads=[padf.res], dma=True)
            P.op("dve", tcopy(xt_ring.bufs[0].ap[:, 0:NG * 2], slots_i.rearrange("p a b -> p (a b)")), reads=[r_sl],
                 writes=[xt_ring.bufs[0].res])
            P.op("sp", dma(DBG_s[:, 512:512 + NG * 2], xt_ring.bufs[0].ap[:, 0:NG * 2]),
                 reads=[xt_ring.bufs[0].res], dma=True)
        P.barrier()
        if stop_after == ("ROUTE", l):
            break
        for e_ in range(NE):
            P.vload("cnt%d_%d" % (l, e_), cnt_i[0:1, e_:e_ + 1], r_cnt, NB * TT)

        AR.reset(SUB)
        w_ring = Ring([Buf((AR.alloc([KC, 512], BF16), AR.alloc([KC, 512], BF16), AR.alloc([4, D], BF16)), P.R())
                       for _ in range(2)])
        hg_ring = Ring([Buf(AR.alloc([D], BF16), P.R()) for _ in range(3)])
        hgT_ring = Ring([Buf(AR.alloc([KC, 128], BF16), P.R()) for _ in range(2)])
        sg_ring = Ring([Buf(AR.alloc([512], F32), P.R()) for _ in range(2)])
        a_ring = Ring([Buf(AR.alloc([512], BF16), P.R()) for _ in range(2)])
        aT_ring = Ring([Buf(AR.alloc([4, 128], BF16), P.R()) for _ in range(2)])
        y_ring = Ring([Buf(AR.alloc([D], F32), P.R()) for _ in range(2)])
        gu_i = [0]

        def moe_GU(e_, jt, wbuf):
            wg, wu, wd = wbuf.ap
            hg, hgT = hg_ring.next(), hgT_ring.next()
            r0 = e_ * CAP + jt * 128
            P.op("sp", dma(hg.ap, HG_s[r0:r0 + 128, :]), writes=[hg.res], dma=True)
            psT = bank(5).bitcast(BF16).rearrange("p (a b) -> p a b", b=128)
            for kc in range(KC):
                P.op("pe", tr(psT[:, kc, :], hg.ap[:, kc * 128:(kc + 1) * 128], ident),
                     reads=[hg.res, r_const], writes=[rp[5]])
            P.op("act", tcopy_act(hgT.ap, psT), reads=[rp[5]], writes=[hgT.res])
            pb = (gu_i[0] % 2) * 2
            gu_i[0] += 1
            for which, wmat in ((0, wg), (1, wu)):
                for kc in range(KC):
                    P.op("pe", mm(bank(pb + which), hgT.ap[:, kc, :], wmat[:, kc, :], kc == 0, kc == KC - 1),
                         reads=[hgT.res, wbuf.res], writes=[rp[pb + which]])
            sg, ab_ = sg_ring.next(), a_ring.next()
            P.op("act", act(sg.ap, bank(pb), AF.Silu), reads=[rp[pb]], writes=[sg.res])
            P.op("dve", tt(ab_.ap, bank(pb + 1), sg.ap, ALU.mult), reads=[rp[pb + 1], sg.res], writes=[ab_.res])
            return ab_

        def moe_D(e_, jt, wbuf, ab_):
            wg, wu, wd = wbuf.ap
            par = jt % 2
            psT = bank(4)[:, par * 256:(par + 1) * 256].bitcast(BF16).rearrange("p (a b) -> p a b", b=128)
            aT, yb = aT_ring.next(), y_ring.next()
            for f in range(4):
                P.op("pe", tr(psT[:, f, :], ab_.ap[:, f * 128:(f + 1) * 128], ident), reads=[ab_.res, r_const],
                     writes=[rp[4]])
            P.op("dve", tcopy(aT.ap, psT), reads=[rp[4]], writes=[aT.res])
            for half in range(2):
                for f in range(4):
                    P.op("pe", mm(bank(6 + half), aT.ap[:, f, :], wd[:, f, half * 512:(half + 1) * 512],
                                  f == 0, f == 3), reads=[aT.res, wbuf.res], writes=[rp[6 + half]])
            P.op("act", tcopy_act(yb.ap, bank(6, 2)), reads=[rp[6], rp[7]], writes=[yb.res])
            r0 = e_ * CAP + jt * 128
            for hf_ in range(2):
                P.op("act", dma(Y_s[hf_][r0:r0 + 128, :], yb.ap[:, hf_ * 512:(hf_ + 1) * 512]), reads=[yb.res],
                     dma=True)

        import os as _os
        _ne = int(_os.environ.get("MOE_NE", NE))
        _nt = int(_os.environ.get("MOE_NT", NG))
        _nocond = _os.environ.get("MOE_NOCOND") is not None

        class _NoCond:
            def __enter__(s_):
                return None

            def __exit__(s_, *a):
                return False
        for e_ in range(_ne):
            wbuf = w_ring.next()
            wg, wu, wd = wbuf.ap
            for hh in range(2):
                P.op("pool", dma(wg[:, hh * 4:(hh + 1) * 4, :],
                                 w_gate[l, e_, hh * 512:(hh + 1) * 512, :].rearrange("(kc p) f -> p kc f", p=128)),
                     writes=[wbuf.res], dma=True)
                P.op("pool", dma(wu[:, hh * 4:(hh + 1) * 4, :],
                                 w_up[l, e_, hh * 512:(hh + 1) * 512, :].rearrange("(kc p) f -> p kc f", p=128)),
                     writes=[wbuf.res], dma=True)
                P.op("pool", dma(wd[:, hh * 2:(hh + 1) * 2, :],
                                 w_down[l, e_, hh * 256:(hh + 1) * 256, :].rearrange("(kc p) f -> p kc f", p=128)),
                     writes=[wbuf.res], dma=True)
            key = "cnt%d_%d" % (l, e_)
            for jt in range(_nt):
                with (_NoCond() if _nocond else P.cond((key, jt * 128))):
                    ab_ = moe_GU(e_, jt, wbuf)
                    moe_D(e_, jt, wbuf, ab_)
        P.barrier()

        if stop_after == ("EXP", l):
            if debug:
                for i_, jt_ in enumerate((0, 6, 7)):
                    P.op("sp", dma(DBG2_s[i_], Y_s[0][jt_ * 128:(jt_ + 1) * 128, :]), dma=True)
                P.barrier()
            break
        AR.reset(SUB)
        gbc = {j: AR.alloc([D], F32) for j in jl}
        lng = AR.alloc([D], F32)
        lnb = AR.alloc([D], F32)
        diag = AR.alloc([128], F32)
        r_g, r_ln, r_dg = P.R(), P.R(), P.R()
        xt_ring = Ring([Buf(AR.alloc([D], F32), P.R()) for _ in range(2)])
        z_ring = Ring([Buf(AR.alloc([D], F32), P.R()) for _ in range(2)])
        ya_ring = Ring([Buf(AR.alloc([D], F32), P.R()) for _ in range(2)])
        yb_ring = Ring([Buf(AR.alloc([D], F32), P.R()) for _ in range(2)])
        sm_ring = Ring([Buf(AR.alloc([24], F32), P.R()) for _ in range(2)])
        P.op("sp", dma(lng, ln_vecs[l, 2].partition_broadcast(128)), writes=[r_ln], dma=True)
        P.op("sp", dma(lnb, ln_vecs[l, 3].partition_broadcast(128)), writes=[r_ln], dma=True)
        for j in jl:
            make_bcast(gbc[j], r_g, g_f[:, :, j], diag, r_dg, 0)
        for gt, (b, t) in enumerate(tl):
            j = 2 if t < 2 else b
            xb_, zb, smb, ya, yb = xt_ring.next(), z_ring.next(), sm_ring.next(), ya_ring.next(), yb_ring.next()
            P.op("sp", dma(xb_.ap, X1_s[b, t * 128:(t + 1) * 128, :]), writes=[xb_.res], dma=True)
            for k, yy in ((0, ya), (1, yb)):
                for hf_ in range(2):
                    P.op("pool", lambda e, off=slots_i[:, gt, k:k + 1], dst=yy.ap[:, hf_ * 512:(hf_ + 1) * 512],
                         src=Y_s[hf_]: e.indirect_dma_start(
                        out=dst, out_offset=None, in_=src[:, :], in_offset=bass.IndirectOffsetOnAxis(ap=off, axis=0),
                        bounds_check=bound_reg(e), oob_is_err=False), reads=[r_sl], writes=[yy.res], dma=True)
            P.op("dve", ts(zb.ap, ya.ap, wts[:, gt, 0:1], ALU.mult), reads=[ya.res, r_sl], writes=[zb.res])
            P.op("dve", stt(zb.ap, yb.ap, wts[:, gt, 1:2], zb.ap, ALU.mult, ALU.add), reads=[yb.res, r_sl, zb.res],
                 writes=[zb.res])
            P.op("dve", tt(zb.ap, zb.ap, gbc[j], ALU.mult), reads=[zb.res, r_g], writes=[zb.res])
            P.op("dve", stt(zb.ap, xb_.ap, ALPHA, zb.ap, ALU.mult, ALU.add), reads=[xb_.res, zb.res],
                 writes=[zb.res])
            rstd, nmr = ln_stats(zb.ap, zb.res, smb.ap, smb.res)
            P.op("act", act(xb_.ap, zb.ap, AF.Identity, bias=nmr, scale=rstd), reads=[zb.res, smb.res],
                 writes=[xb_.res])
            P.op("dve", tt(xb_.ap, xb_.ap, lng, ALU.mult), reads=[xb_.res, r_ln], writes=[xb_.res])
            P.op("dve", tt(xb_.ap, xb_.ap, lnb, ALU.add), reads=[xb_.res, r_ln], writes=[xb_.res])
            dst = out_d[b, (t - 2) * 128:(t - 1) * 128, :] if last else X2_s[b, t * 128:(t + 1) * 128, :]
            P.op("sp", dma(dst, xb_.ap), reads=[xb_.res], dma=True)
        P.barrier()

    P.barrier()
    P.emit()
    st.close()
    return nc, P


def _consts():
    bf = ml_dtypes.bfloat16
    t = np.arange(S, dtype=np.float64)
    ang = 2.0 * np.pi * ((np.outer(t, t)) % S) / S
    dftc = (np.cos(ang) / math.sqrt(S)).astype(np.float32).astype(bf)
    dftns = (-np.sin(ang) / math.sqrt(S)).astype(np.float32).astype(bf)
    t2 = np.arange(LC, dtype=np.float64)
    a2 = 2.0 * np.pi * ((np.outer(t2, t2)) % LC) / LC
    dft256 = np.stack([np.cos(a2) / math.sqrt(LC), -np.sin(a2) / math.sqrt(LC)]).astype(np.float32).astype(bf)
    c = np.arange(64, dtype=np.float64)
    a3 = 2.0 * np.pi * ((np.outer(c, c)) % 64) / 64
    c64 = np.cos(a3) / 8.0
    s64 = np.sin(a3) / 8.0
    c64bd = np.zeros((2, 128, 128), np.float32)
    for i, m in enumerate((c64, s64)):
        c64bd[i, 0:64, 0:64] = m
        c64bd[i, 64:128, 64:128] = m
    freqs = (10000.0 ** (-np.arange(0, 32, 2, dtype=np.float32) / 32)).astype(np.float32)
    pos = np.arange(S)
    row = (pos // 64).astype(np.float32)
    col = (pos % 64).astype(np.float32)
    ang_row = row[:, None] * freqs
    ang_col = col[:, None] * freqs
    rope = np.zeros((2, 128, S), np.float32)
    for p in range(128):
        d = p % 64
        a = ang_row[:, d % 16] if d < 32 else ang_col[:, d % 16]
        rope[0, p] = np.cos(a)
        rope[1, p] = np.sin(a)
    R = np.zeros((128, 128), np.float32)
    for m in range(128):
        d = m % 64
        if (d % 32) < 16:
            R[m + 16, m] = -1.0
        else:
            R[m - 16, m] = 1.0
    return dict(dftc=dftc, dftns=dftns, dft256=dft256, c64bd=c64bd, rope_cs=rope,
                rmat=R.astype(bf), ident=np.eye(128, dtype=np.float32).astype(bf),
                uts=np.triu(np.ones((128, 128), np.float32), 1),
                ebase=(np.arange(NE, dtype=np.float32) * CAP).reshape(1, NE),
                iota_p=np.repeat(np.arange(128, dtype=np.float32)[:, None], NE, axis=1),
                identf=np.eye(128, dtype=np.float32))


def make_in_maps(inputs, cores=range(N_CORES)):
    f = lambda a: np.ascontiguousarray(np.asarray(a, dtype=np.float32))
    x, c, ctx, c_ctx = f(inputs["x"]), f(inputs["c"]), f(inputs["ctx"]), f(inputs["c_ctx"])
    shared = dict(
        w_mod=f(inputs["w_mod"]),
        b_mod_fm=np.ascontiguousarray(f(inputs["b_mod"]).reshape(DEPTH, 48, 128).transpose(0, 2, 1)),
        w_in=f(inputs["w_in"]),
        w_uT=np.ascontiguousarray(f(inputs["w_in"])[:, :, :256].transpose(0, 2, 1)),
        w_f=f(inputs["w_fourier"]).reshape(DEPTH, 256, 64),
        lam_qk=f(inputs["lam_qk"]).reshape(DEPTH, 1, 256),
        subln_g=f(inputs["subln_g"]).reshape(DEPTH, 1, 128),
        w_out=f(inputs["w_out"]),
        ln_vecs=np.ascontiguousarray(np.stack([f(inputs["ln_attn_g"]), f(inputs["ln_attn_b"]),
                                               f(inputs["ln_ffn_g"]), f(inputs["ln_ffn_b"])], axis=1)
                                     .reshape(DEPTH, 4, 1, D)),
        w_router=f(inputs["w_router"]),
        router_bias=f(inputs["router_bias"]).reshape(1, NE),
        rbias_rep=np.ascontiguousarray(np.tile(f(inputs["router_bias"]).reshape(1, NE), (1, NB * NT))),
        w_gate=f(inputs["w_gate"]), w_up=f(inputs["w_up"]), w_down=f(inputs["w_down"]),
    )
    shared.update(_consts())
    maps = []
    for ci in cores:
        b0 = ci * NB
        cc = np.stack([c[b0], c[b0 + 1], c_ctx], axis=-1)
        c_fm = np.ascontiguousarray(cc.reshape(KC, 128, 3).transpose(1, 0, 2))
        m = dict(shared)
        m["x"] = np.ascontiguousarray(x[b0:b0 + NB])
        m["ctx"] = np.ascontiguousarray(ctx[b0:b0 + NB])
        m["c_fm"] = c_fm
        maps.append(m)
    return maps


_CACHE = {}


def kernel(**inputs):
    if "nc" not in _CACHE:
        _CACHE["nc"] = build_program()[0]
    nc = _CACHE["nc"]
    in_maps = make_in_maps(inputs)
    res = run_bass_kernel_spmd(nc, in_maps, core_ids=list(range(N_CORES)))
    out = np.concatenate([np.asarray(r["out"], dtype=np.float32) for r in res.results], axis=0)
    return out
```
